# Optimizing a Trainium2 kernel written in Bass

```python
import jax, jax.numpy as jnp
from jax import lax
import numpy as np

D_MODEL = 2048
BATCH = 2
SEQ = 4096
DEPTH = 2

MEM_LEN = 256
ATTN_HEADS = 8
ATTN_HD = 128
ATTN_W = ATTN_HEADS * ATTN_HD
MOBA_BLOCK = 256
MOBA_TOPK = 3
Q_CHUNK = 64
SSD_HD = 64
SSD_HEADS = 48
D_SSD = SSD_HEADS * SSD_HD
SSD_GROUPS = 8
SSD_HPG = SSD_HEADS // SSD_GROUPS
SSD_STATE = 128
SSD_CONV = 4
SSD_CHUNK = 128
CONV_DIM = D_SSD + 2 * SSD_GROUPS * SSD_STATE
MIX_W = ATTN_W + D_SSD
N_IN = 3 * ATTN_W + D_SSD + CONV_DIM + SSD_HEADS
MEM_HEADS = 4
MEM_HD = 128
MEM_W = MEM_HEADS * MEM_HD
D_FF = 5632
EPS = 1e-6

kernel_name = "hymba_moba_ssd_macaron_alibi"


def rmsnorm(x, w):
    xf = x.astype(jnp.float32)
    y = xf * lax.rsqrt(jnp.mean(xf * xf, axis=-1, keepdims=True) + EPS)
    return (y * w.astype(jnp.float32)).astype(x.dtype)


def swiglu(h, w_gu, w_down):
    g, u = jnp.split(h @ w_gu, 2, axis=-1)
    return (jax.nn.silu(g) * u) @ w_down


def alibi_slopes(n):
    return jnp.exp2(-8.0 * jnp.arange(1, n + 1, dtype=jnp.float32) / n)


def moba_attention(q, k, v, slopes):
    B_, H, S, hd = q.shape
    nb = max(-(-S // MOBA_BLOCK), MOBA_TOPK)
    pad = nb * MOBA_BLOCK - S
    kp = jnp.pad(k, ((0, 0), (0, 0), (0, pad), (0, 0)))
    vp = jnp.pad(v, ((0, 0), (0, 0), (0, pad), (0, 0)))
    kb = kp.reshape(B_, H, nb, MOBA_BLOCK, hd)
    vb = vp.reshape(B_, H, nb, MOBA_BLOCK, hd)
    kmean = jnp.mean(kb.astype(jnp.float32), axis=3)
    n_chunks = S // Q_CHUNK
    qc = jnp.moveaxis(q.reshape(B_, H, n_chunks, Q_CHUNK, hd), 2, 0)
    scale = hd ** -0.5
    b_ix = jnp.arange(B_)[:, None, None, None]
    h_ix = jnp.arange(H)[None, :, None, None]
    blk_ar = jnp.arange(MOBA_BLOCK)

    def one_chunk(args):
        q_blk, c = args
        t = c * Q_CHUNK + jnp.arange(Q_CHUNK)
        cur = (c * Q_CHUNK) // MOBA_BLOCK
        gate = jnp.einsum('bhqd,bhnd->bhqn', q_blk.astype(jnp.float32), kmean)
        gate = jnp.where(jnp.arange(nb) < cur, gate, -jnp.inf)
        _, idx = lax.top_k(gate, MOBA_TOPK)
        slot_ok = jnp.arange(MOBA_TOPK) < cur
        kg = kb[b_ix, h_ix, idx]
        vg = vb[b_ix, h_ix, idx]
        s_pos = idx[..., None] * MOBA_BLOCK + blk_ar
        dist_sel = (t[:, None, None] - s_pos).astype(jnp.float32)
        logit_sel = (jnp.einsum('bhqd,bhqnkd->bhqnk', q_blk, kg).astype(jnp.float32) * scale
                     - slopes[:, None, None, None] * dist_sel)
        logit_sel = jnp.where(slot_ok[:, None], logit_sel, -jnp.inf)
        own_start = cur * MOBA_BLOCK
        k_own = lax.dynamic_slice_in_dim(kp, own_start, MOBA_BLOCK, axis=2)
        v_own = lax.dynamic_slice_in_dim(vp, own_start, MOBA_BLOCK, axis=2)
        dist_own = t[:, None] - (own_start + blk_ar)[None, :]
        logit_own = (jnp.einsum('bhqd,bhkd->bhqk', q_blk, k_own).astype(jnp.float32) * scale
                     - slopes[:, None, None] * dist_own.astype(jnp.float32))
        logit_own = jnp.where(dist_own >= 0, logit_own, -jnp.inf)
        n_sel = MOBA_TOPK * MOBA_BLOCK
        logits = jnp.concatenate(
            [logit_sel.reshape(B_, H, Q_CHUNK, n_sel), logit_own], axis=-1)
        p = jax.nn.softmax(logits, axis=-1).astype(v.dtype)
        p_sel = p[..., :n_sel].reshape(B_, H, Q_CHUNK, MOBA_TOPK, MOBA_BLOCK)
        p_own = p[..., n_sel:]
        return (jnp.einsum('bhqnk,bhqnkd->bhqd', p_sel, vg)
                + jnp.einsum('bhqk,bhkd->bhqd', p_own, v_own))

    out = lax.map(one_chunk, (qc, jnp.arange(n_chunks)))
    return jnp.moveaxis(out, 0, 2).reshape(B_, H, S, hd)


def causal_dwconv(u, w, b):
    out = lax.conv_general_dilated(
        u, w[:, None, :], window_strides=(1,), padding=[(w.shape[0] - 1, 0)],
        dimension_numbers=('NWC', 'WIO', 'NWC'), feature_group_count=u.shape[-1])
    return out + b


def ssd_scan(xs, dt, a, bm, cm):
    B_, S, G, J, P = xs.shape
    N = bm.shape[-1]
    nc, L = S // SSD_CHUNK, SSD_CHUNK
    x = (xs.astype(jnp.float32) * dt[..., None]).reshape(B_, nc, L, G, J, P)
    da = (dt * a).reshape(B_, nc, L, G, J)
    acs = jnp.cumsum(da, axis=2)
    bc = bm.astype(jnp.float32).reshape(B_, nc, L, G, N)
    cc = cm.astype(jnp.float32).reshape(B_, nc, L, G, N)
    acs_t = jnp.transpose(acs, (0, 1, 3, 4, 2))
    seg = acs_t[..., :, None] - acs_t[..., None, :]
    causal = jnp.tril(jnp.ones((L, L), dtype=bool))
    decay_in = jnp.exp(jnp.where(causal, seg, -jnp.inf))
    cb = jnp.einsum('bclgn,bcsgn->bcgls', cc, bc)
    scores = cb[:, :, :, None] * decay_in
    y_diag = jnp.einsum('bcgjls,bcsgjp->bclgjp', scores, x)
    decay_st = jnp.exp(acs[:, :, -1:] - acs)
    states = jnp.einsum('bclgn,bclgjp->cbgjpn', bc, x * decay_st[..., None])
    chunk_decay = jnp.transpose(jnp.exp(acs[:, :, -1]), (1, 0, 2, 3))

    def step(h, inp):
        st, dec = inp
        return h * dec[..., None, None] + st, h

    h0 = jnp.zeros((B_, G, J, P, N), jnp.float32)
    _, h_prev = lax.scan(step, h0, (states, chunk_decay))
    y_off = jnp.einsum('bclgn,cbgjpn->bclgjp', cc, h_prev) * jnp.exp(acs)[..., None]
    return (y_diag + y_off).reshape(B_, S, G, J, P)


def ssd_mixer(z, xbc, dt_raw, conv_w, conv_b, dt_bias, a_log, d_skip, norm_w):
    B_, S, _ = xbc.shape
    xbc = jax.nn.silu(causal_dwconv(xbc, conv_w, conv_b))
    GN = SSD_GROUPS * SSD_STATE
    xs, bm, cm = jnp.split(xbc, [D_SSD, D_SSD + GN], axis=-1)
    xs = xs.reshape(B_, S, SSD_GROUPS, SSD_HPG, SSD_HD)
    bm = bm.reshape(B_, S, SSD_GROUPS, SSD_STATE)
    cm = cm.reshape(B_, S, SSD_GROUPS, SSD_STATE)
    dt = jax.nn.softplus(dt_raw.astype(jnp.float32) + dt_bias.astype(jnp.float32))
    dt = dt.reshape(B_, S, SSD_GROUPS, SSD_HPG)
    a = -jnp.exp(a_log.astype(jnp.float32)).reshape(SSD_GROUPS, SSD_HPG)
    y = ssd_scan(xs, dt, a, bm, cm)
    y = y + d_skip.astype(jnp.float32).reshape(SSD_GROUPS, SSD_HPG, 1) * xs.astype(jnp.float32)
    g = y.reshape(B_, S, D_SSD) * jax.nn.silu(z.astype(jnp.float32))
    gg = g.reshape(B_, S, SSD_GROUPS, D_SSD // SSD_GROUPS)
    gg = gg * lax.rsqrt(jnp.mean(gg * gg, axis=-1, keepdims=True) + EPS)
    return (gg.reshape(B_, S, D_SSD) * norm_w.astype(jnp.float32)).astype(z.dtype)


def hybrid_mixer(h, w_in, q_norm, k_norm, conv_w, conv_b, dt_bias, a_log, d_skip,
                 ssd_norm, w_out, slopes):
    B_, S, _ = h.shape
    proj = h @ w_in
    o = [ATTN_W, 2 * ATTN_W, 3 * ATTN_W, 3 * ATTN_W + D_SSD, 3 * ATTN_W + D_SSD + CONV_DIM]
    q, k, v, z, xbc, dt_raw = jnp.split(proj, o, axis=-1)
    heads = lambda t: jnp.transpose(t.reshape(B_, S, ATTN_HEADS, ATTN_HD), (0, 2, 1, 3))
    q = heads(rmsnorm(q.reshape(B_, S, ATTN_HEADS, ATTN_HD), q_norm).reshape(B_, S, ATTN_W))
    k = heads(rmsnorm(k.reshape(B_, S, ATTN_HEADS, ATTN_HD), k_norm).reshape(B_, S, ATTN_W))
    v = heads(v)
    y_attn = moba_attention(q, k, v, slopes)
    y_attn = jnp.transpose(y_attn, (0, 2, 1, 3)).reshape(B_, S, ATTN_W).astype(h.dtype)
    y_ssd = ssd_mixer(z, xbc, dt_raw, conv_w, conv_b, dt_bias, a_log, d_skip, ssd_norm)
    return jnp.concatenate([y_attn, y_ssd], axis=-1) @ w_out


def memory_cross_attention(h, m, wq, wk, wv, qn, kn, wo):
    B_, S, _ = h.shape
    M = m.shape[1]
    q = rmsnorm((h @ wq).reshape(B_, S, MEM_HEADS, MEM_HD), qn)
    k = rmsnorm((m @ wk).reshape(B_, M, MEM_HEADS, MEM_HD), kn)
    v = (m @ wv).reshape(B_, M, MEM_HEADS, MEM_HD)
    s = jnp.einsum('bqhd,bkhd->bhqk', q, k).astype(jnp.float32) * MEM_HD ** -0.5
    p = jax.nn.softmax(s, axis=-1).astype(v.dtype)
    o = jnp.einsum('bhqk,bkhd->bqhd', p, v).reshape(B_, S, MEM_W)
    return o @ wo


def setup_inputs(seed: int = 0) -> dict:
    key = jax.random.key(seed)
    ks = jax.random.split(key, 32)
    L = DEPTH
    nrm = lambda k, shape, fan: jax.random.normal(k, shape, jnp.float32) * fan ** -0.5
    gain = lambda k, shape: 1.0 + 0.02 * jax.random.normal(k, shape, jnp.float32)
    dt0 = jnp.exp(jax.random.uniform(ks[12], (L, SSD_HEADS), jnp.float32)
                  * (np.log(0.1) - np.log(0.001)) + np.log(0.001))
    dt_bias = dt0 + jnp.log(-jnp.expm1(-dt0))
    a_log = jnp.log(jax.random.uniform(ks[13], (L, SSD_HEADS), jnp.float32, 1.0, 16.0))
    return {
        "x": jax.random.normal(ks[0], (BATCH, SEQ, D_MODEL), jnp.float32),
        "mem": jax.random.normal(ks[1], (BATCH, MEM_LEN, D_MODEL), jnp.float32),
        "ff1_norm": gain(ks[2], (L, D_MODEL)),
        "ff1_w_gu": nrm(ks[3], (L, D_MODEL, 2 * D_FF), D_MODEL),
        "ff1_w_down": nrm(ks[4], (L, D_FF, D_MODEL), D_FF),
        "mix_norm": gain(ks[5], (L, D_MODEL)),
        "w_in": nrm(ks[6], (L, D_MODEL, N_IN), D_MODEL),
        "q_norm": gain(ks[7], (L, ATTN_HD)),
        "k_norm": gain(ks[8], (L, ATTN_HD)),
        "conv_w": nrm(ks[9], (L, SSD_CONV, CONV_DIM), SSD_CONV),
        "conv_b": 0.02 * jax.random.normal(ks[10], (L, CONV_DIM), jnp.float32),
        "dt_bias": dt_bias,
        "a_log": a_log,
        "d_skip": gain(ks[14], (L, SSD_HEADS)),
        "ssd_norm": gain(ks[15], (L, D_SSD)),
        "w_out": nrm(ks[16], (L, MIX_W, D_MODEL), MIX_W),
        "xmem_norm": gain(ks[17], (L, D_MODEL)),
        "mem_norm": gain(ks[18], (L, D_MODEL)),
        "mem_wq": nrm(ks[19], (L, D_MODEL, MEM_W), D_MODEL),
        "mem_wk": nrm(ks[20], (L, D_MODEL, MEM_W), D_MODEL),
        "mem_wv": nrm(ks[21], (L, D_MODEL, MEM_W), D_MODEL),
        "mem_q_norm": gain(ks[22], (L, MEM_HD)),
        "mem_k_norm": gain(ks[23], (L, MEM_HD)),
        "mem_wo": nrm(ks[24], (L, MEM_W, D_MODEL), MEM_W),
        "ff2_norm": gain(ks[25], (L, D_MODEL)),
        "ff2_w_gu": nrm(ks[26], (L, D_MODEL, 2 * D_FF), D_MODEL),
        "ff2_w_down": nrm(ks[27], (L, D_FF, D_MODEL), D_FF),
    }


def reference(x, mem, ff1_norm, ff1_w_gu, ff1_w_down, mix_norm, w_in, q_norm, k_norm,
              conv_w, conv_b, dt_bias, a_log, d_skip, ssd_norm, w_out, xmem_norm, mem_norm,
              mem_wq, mem_wk, mem_wv, mem_q_norm, mem_k_norm, mem_wo, ff2_norm, ff2_w_gu,
              ff2_w_down):
    slopes = alibi_slopes(ATTN_HEADS)
    for l in range(DEPTH):
        x = x + 0.5 * swiglu(rmsnorm(x, ff1_norm[l]), ff1_w_gu[l], ff1_w_down[l])
        x = x + hybrid_mixer(rmsnorm(x, mix_norm[l]), w_in[l], q_norm[l], k_norm[l],
                             conv_w[l], conv_b[l], dt_bias[l], a_log[l], d_skip[l],
                             ssd_norm[l], w_out[l], slopes)
        x = x + memory_cross_attention(rmsnorm(x, xmem_norm[l]), rmsnorm(mem, mem_norm[l]),
                                       mem_wq[l], mem_wk[l], mem_wv[l], mem_q_norm[l],
                                       mem_k_norm[l], mem_wo[l])
        x = x + 0.5 * swiglu(rmsnorm(x, ff2_norm[l]), ff2_w_gu[l], ff2_w_down[l])
    return x
```

```python
import numpy as np
import contextlib
import concourse.bass as bass
import concourse.mybir as mybir
from concourse.bass_utils import run_bass_kernel_spmd

F32 = mybir.dt.float32
BF16 = mybir.dt.bfloat16
AF = mybir.ActivationFunctionType
ALU = mybir.AluOpType
AX = mybir.AxisListType

D = 2048
DFF = 5632
NTOK = 1024
NCORES = 8
EPS = 1e-6


class Buf:
    def __init__(self, t, name=""):
        self.t = t
        self.name = name
        self.last_w = None
        self.readers = []

    def __getitem__(self, idx):
        return self.t[idx]


class Op:
    __slots__ = ("eng", "emit", "deps", "marked", "count", "dma_sem", "dma_val", "is_dma", "is_cc")

    def __init__(self, eng, emit, is_dma=False):
        self.eng = eng
        self.emit = emit
        self.deps = []
        self.marked = False
        self.count = None
        self.is_dma = is_dma
        self.dma_sem = None
        self.dma_val = None
        self.is_cc = False


class Prog:
    ENGS = ("pe", "dve", "act", "pool", "sp")
    NS = 8

    def __init__(self, nc, stack):
        self.nc = nc
        self.stack = stack
        self.ops = {e: [] for e in self.ENGS}
        self.sem = {e: stack.enter_context(nc.semaphore("prog_" + e)) for e in ("pe", "dve", "act", "pool")}
        self.dsem = {q: [stack.enter_context(nc.semaphore("dma_%s_%d" % (q, i))) for i in range(self.NS)]
                     for q in ("sp", "act", "pool")}
        self.ndma = {q: 0 for q in ("sp", "act", "pool")}
        self.nbuf = 0
        self.cc_sem = stack.enter_context(nc.semaphore("cc_sem"))
        self.ncc = 0

    def sbuf(self, shape, dtype, name=None):
        self.nbuf += 1
        name = "s_%s_%d" % (name or "sb", self.nbuf)
        t = self.stack.enter_context(self.nc.sbuf_tensor(name, list(shape), dtype))
        return Buf(t, name)

    def psum(self, shape, dtype=F32, name=None):
        self.nbuf += 1
        name = "%s_%d" % (name or "ps", self.nbuf)
        t = self.stack.enter_context(self.nc.psum_tensor(name, list(shape), dtype))
        return Buf(t, name)

    def add(self, eng, emit, reads=(), writes=(), is_dma=False):
        op = Op(eng, emit, is_dma)
        deps = []
        for b in reads:
            if b.last_w is not None:
                deps.append(b.last_w)
        for b in writes:
            if b.last_w is not None:
                deps.append(b.last_w)
            deps.extend(b.readers)
        seen = set()
        for d in deps:
            if d is op or id(d) in seen:
                continue
            seen.add(id(d))
            if d.eng == "pe" and eng == "pe" and not d.is_dma:
                continue
            op.deps.append(d)
            if not d.is_dma:
                d.marked = True
        for b in reads:
            b.readers.append(op)
        for b in writes:
            b.last_w = op
            b.readers = []
        if is_dma:
            q = eng
            i = self.ndma[q]
            self.ndma[q] += 1
            op.dma_sem = self.dsem[q][i % self.NS]
            op.dma_val = 16 * (i // self.NS + 1)
        self.ops[eng].append(op)
        return op

    def op(self, eng, method, *args, reads=(), writes=(), **kw):
        return self.add(eng, lambda e: getattr(e, method)(*args, **kw), reads, writes)

    def collective(self, kind, groups, src_ap, dst_ap, reads=(), writes=()):
        op = self.add("pool", lambda e: e.collective_compute(kind, ALU.bypass, replica_groups=groups, ins=[src_ap], outs=[dst_ap]),
                      reads, writes, is_dma=True)
        self.ndma["pool"] -= 1
        self.ncc += 1
        op.dma_sem = self.cc_sem
        op.dma_val = self.ncc
        op.is_cc = True
        return op

    def dma(self, q, out, in_, reads=(), writes=()):
        return self.add(q, lambda e: e.dma_start(out=out, in_=in_), reads, writes, is_dma=True)

    def begin_phases(self):
        self.cnt = {e: 0 for e in ("pe", "dve", "act", "pool")}
        self.waited = {e: {} for e in self.ENGS}
        self.barrier = []
        self.phase_id = 0
        self.last_dma = {}

    @contextlib.contextmanager
    def phase(self, final=False):
        outer = self.stack
        with contextlib.ExitStack() as st:
            self.stack = st
            try:
                yield
                self.emit_phase(final)
            finally:
                self.stack = outer

    def emit_phase(self, final=False):
        nc = self.nc
        prog = self
        for e in ("pe", "dve", "act", "pool"):
            for op in reversed(self.ops[e]):
                if not op.is_dma:
                    op.marked = True
                    break
        for e in ("pe", "dve", "act", "pool"):
            for op in self.ops[e]:
                if op.is_dma:
                    continue
                if op.marked:
                    self.cnt[e] += 1
                    op.count = self.cnt[e]
        barrier = list(self.barrier)

        def run(engname, engine):
            waited = prog.waited[engname]

            def wait(s, v):
                if waited.get(id(s), 0) >= v:
                    return
                engine.wait_ge(s, v)
                waited[id(s)] = v
            for (s, v) in barrier:
                wait(s, v)
            for op in prog.ops[engname]:
                if op.is_dma and not op.is_cc:
                    prev = op.dma_val - 16
                    if prev > 0:
                        wait(op.dma_sem, prev)
                for d in op.deps:
                    if d.is_dma:
                        wait(d.dma_sem, d.dma_val)
                    elif d.count is not None:
                        wait(prog.sem[d.eng], d.count)
                ins = op.emit(engine)
                if op.is_cc:
                    ins.then_inc(op.dma_sem, 1)
                elif op.is_dma:
                    ins.then_inc(op.dma_sem, 16)
                elif op.marked:
                    ins.then_inc(prog.sem[engname], 1)
            if final:
                for (s, v) in final_pairs:
                    wait(s, v)

        for e in self.ENGS:
            for op in self.ops[e]:
                if op.is_dma and not op.is_cc:
                    self.last_dma[id(op.dma_sem)] = (op.dma_sem, op.dma_val)
        nb = [(self.sem[e], self.cnt[e]) for e in ("pe", "dve", "act", "pool") if self.cnt[e] > 0]
        nb += list(self.last_dma.values())
        final_pairs = nb
        with nc.Block() as block:
            @block.tensor
            def _(eng):
                run("pe", eng)

            @block.vector
            def _(eng):
                run("dve", eng)

            @block.scalar
            def _(eng):
                run("act", eng)

            @block.gpsimd
            def _(eng):
                run("pool", eng)

            @block.sync
            def _(eng):
                run("sp", eng)
        self.barrier = nb
        self.phase_id += 1
        for e in self.ENGS:
            for op in self.ops[e]:
                op.emit = None
                if not op.is_dma and op.count is None:
                    op.count = -1
            self.ops[e] = []

    def emit(self, final_wait_ops=()):
        nc = self.nc
        for e in ("pe", "dve", "act", "pool"):
            c = 0
            for op in self.ops[e]:
                if op.is_dma:
                    continue
                if op.marked:
                    c += 1
                    op.count = c
        prog = self

        def run(engname, engine):
            waited = {}
            for op in prog.ops[engname]:
                if op.is_dma and not op.is_cc:
                    prev = op.dma_val - 16
                    if prev > 0 and waited.get(id(op.dma_sem), 0) < prev:
                        engine.wait_ge(op.dma_sem, prev)
                        waited[id(op.dma_sem)] = prev
                for d in op.deps:
                    if d.is_dma:
                        s, v = d.dma_sem, d.dma_val
                    else:
                        s, v = prog.sem[d.eng], d.count
                    if waited.get(id(s), 0) >= v:
                        continue
                    engine.wait_ge(s, v)
                    waited[id(s)] = v
                ins = op.emit(engine)
                if op.is_cc:
                    ins.then_inc(op.dma_sem, 1)
                elif op.is_dma:
                    ins.then_inc(op.dma_sem, 16)
                elif op.marked:
                    ins.then_inc(prog.sem[engname], 1)
            if engname == "sp":
                for d in final_wait_ops:
                    s, v = (d.dma_sem, d.dma_val) if d.is_dma else (prog.sem[d.eng], d.count)
                    engine.wait_ge(s, v)

        with nc.Block() as block:
            @block.tensor
            def _(eng):
                run("pe", eng)

            @block.vector
            def _(eng):
                run("dve", eng)

            @block.scalar
            def _(eng):
                run("act", eng)

            @block.gpsimd
            def _(eng):
                run("pool", eng)

            @block.sync
            def _(eng):
                run("sp", eng)


class PsumPool:
    def __init__(self, P, n=8, pfx="psb"):
        self.bufs = [P.psum([128, 512], F32, name="%s%d" % (pfx, i)) for i in range(n)]
        self.i = 0

    def get(self):
        b = self.bufs[self.i % len(self.bufs)]
        self.i += 1
        return b


class Ring:
    def __init__(self, bufs):
        self.bufs = bufs
        self.i = 0

    def get(self):
        b = self.bufs[self.i % len(self.bufs)]
        self.i += 1
        return b


def rmsnorm_fm(P, pp, xT, gain, hT, ones_bf, sq_ring, rstd, nchunk, T, dim):
    for h0 in range(0, T, 512):
        w = min(512, T - h0)
        ps = pp.get()
        for c in range(nchunk):
            sq = sq_ring.get()
            P.op("act", "activation", out=sq[:, 0:w], in_=xT[c][:, h0:h0 + w], func=AF.Square,
                 reads=[xT[c]], writes=[sq])
            P.op("pe", "matmul", ps[:, 0:w], ones_bf[:, :], sq[:, 0:w], start=(c == 0), stop=(c == nchunk - 1),
                 reads=[sq, ones_bf], writes=[ps])
        P.op("dve", "tensor_scalar", rstd[:, h0:h0 + w], ps[:, 0:w], 1.0 / dim, EPS, ALU.mult, ALU.add,
             reads=[ps], writes=[rstd])
        P.op("act", "activation", out=rstd[:, h0:h0 + w], in_=rstd[:, h0:h0 + w], func=AF.Sqrt,
             reads=[rstd], writes=[rstd])
        P.op("dve", "reciprocal", rstd[:, h0:h0 + w], rstd[:, h0:h0 + w], reads=[rstd], writes=[rstd])
        for c in range(nchunk):
            P.op("dve", "scalar_tensor_tensor", out=hT[c][:, h0:h0 + w], in0=xT[c][:, h0:h0 + w],
                 scalar=gain[:, c:c + 1], in1=rstd[:, h0:h0 + w], op0=ALU.mult, op1=ALU.mult,
                 reads=[xT[c], gain, rstd], writes=[hT[c]])


def ffn_fm(P, pp, xT, hT, w_gu, w_down, aT, wg_ring, wu_ring, wd_ring, sg_ring, T):
    NKC = D // 128
    groups = [(0, 6), (6, 12), (12, 17), (17, 22)]
    wgu_v = w_gu.rearrange("(kc p) f -> p kc f", p=128)
    wd_v = w_down.rearrange("(fc p) d -> p fc d", p=128)
    halves = [(h0, min(512, T - h0)) for h0 in range(0, T, 512)]
    for (p0, p1) in groups:
        nfc = 2 * (p1 - p0)
        for pr in range(p0, p1):
            wg = wg_ring.get()
            wu = wu_ring.get()
            P.dma("pool", wg[:, :, :], wgu_v[:, :, pr * 256:(pr + 1) * 256], writes=[wg])
            P.dma("pool", wu[:, :, :], wgu_v[:, :, DFF + pr * 256:DFF + (pr + 1) * 256], writes=[wu])
            for j in range(2):
                fl = 2 * (pr - p0) + j
                for (h0, w) in halves:
                    pg = pp.get()
                    pu = pp.get()
                    for kc in range(NKC):
                        P.op("pe", "matmul", pg[:, 0:w], wg[:, kc, j * 128:(j + 1) * 128], hT[kc][:, h0:h0 + w],
                             start=(kc == 0), stop=(kc == NKC - 1), reads=[wg, hT[kc]], writes=[pg])
                    for kc in range(NKC):
                        P.op("pe", "matmul", pu[:, 0:w], wu[:, kc, j * 128:(j + 1) * 128], hT[kc][:, h0:h0 + w],
                             start=(kc == 0), stop=(kc == NKC - 1), reads=[wu, hT[kc]], writes=[pu])
                    sg = sg_ring.get()
                    P.op("act", "activation", out=sg[:, 0:w], in_=pg[:, 0:w], func=AF.Silu, reads=[pg], writes=[sg])
                    P.op("dve", "tensor_tensor", out=aT[fl][:, h0:h0 + w], in0=pu[:, 0:w], in1=sg[:, 0:w], op=ALU.mult,
                         reads=[pu, sg], writes=[aT[fl]])
        for dq in range(D // 512):
            wd = wd_ring.get()
            P.dma("pool", wd[:, 0:nfc, :], wd_v[:, 2 * p0:2 * p1, dq * 512:(dq + 1) * 512], writes=[wd])
            for dj in range(4):
                dc = dq * 4 + dj
                for (h0, w) in halves:
                    po = pp.get()
                    for fl in range(nfc):
                        P.op("pe", "matmul", po[:, 0:w], wd[:, fl, dj * 128:(dj + 1) * 128], aT[fl][:, h0:h0 + w],
                             start=(fl == 0), stop=(fl == nfc - 1), reads=[wd, aT[fl]], writes=[po])
                    P.op("dve", "scalar_tensor_tensor", out=xT[dc][:, h0:h0 + w], in0=po[:, 0:w], scalar=0.5,
                         in1=xT[dc][:, h0:h0 + w], op0=ALU.mult, op1=ALU.add, reads=[po, xT[dc]], writes=[xT[dc]])


def build_ffn_prog(T=NTOK):
    nc = bass.Bass("TRN2", target_bir_lowering=False)
    xin = nc.dram_tensor("xT", [D, T], F32, kind="ExternalInput").ap()
    gin = nc.dram_tensor("gain", [128, D // 128], F32, kind="ExternalInput").ap()
    wgu = nc.dram_tensor("w_gu", [D, 2 * DFF], F32, kind="ExternalInput").ap()
    wdn = nc.dram_tensor("w_down", [DFF, D], F32, kind="ExternalInput").ap()
    yout = nc.dram_tensor("yT", [D, T], F32, kind="ExternalOutput").ap()
    NKC = D // 128
    with contextlib.ExitStack() as stack:
        P = Prog(nc, stack)
        pp = PsumPool(P, 8)
        xT = [P.sbuf([128, T], F32, "xT%d" % c) for c in range(NKC)]
        hT = [P.sbuf([128, T], BF16, "hT%d" % c) for c in range(NKC)]
        aT = [P.sbuf([128, T], BF16, "aT%d" % c) for c in range(12)]
        gain = P.sbuf([128, NKC], F32, "gain")
        ones_bf = P.sbuf([128, 128], BF16, "ones")
        rstd = P.sbuf([128, T], F32, "rstd")
        sq_ring = Ring([P.sbuf([128, 512], BF16, "sq%d" % i) for i in range(3)])
        sg_ring = Ring([P.sbuf([128, 512], F32, "sg%d" % i) for i in range(3)])
        wg_ring = Ring([P.sbuf([128, NKC, 256], BF16, "wg%d" % i) for i in range(2)])
        wu_ring = Ring([P.sbuf([128, NKC, 256], BF16, "wu%d" % i) for i in range(2)])
        wd_ring = Ring([P.sbuf([128, 12, 512], BF16, "wd%d" % i) for i in range(2)])

        xv = xin.rearrange("(c p) t -> c p t", p=128)
        yv = yout.rearrange("(c p) t -> c p t", p=128)
        P.op("pool", "memset", ones_bf[:, :], 1.0, writes=[ones_bf])
        P.dma("sp", gain[:, :], gin[:, :], writes=[gain])
        for c in range(NKC):
            P.dma("sp", xT[c][:, :], xv[c], writes=[xT[c]])
        rmsnorm_fm(P, pp, xT, gain, hT, ones_bf, sq_ring, rstd, NKC, T, D)
        ffn_fm(P, pp, xT, hT, wgu, wdn, aT, wg_ring, wu_ring, wd_ring, sg_ring, T)
        outs = []
        for c in range(NKC):
            outs.append(P.dma("sp", yv[c], xT[c][:, :], reads=[xT[c]]))
        P.emit(final_wait_ops=outs)
    return nc


def lin_fm(P, pp, hT, w_ap, ncols, w_ring, T, consume, nkc=D // 128):
    wv = w_ap.rearrange("(kc p) f -> p kc f", p=128)
    halves = [(h0, min(512, T - h0)) for h0 in range(0, T, 512)]
    for c0 in range(0, ncols, 256):
        cw = min(256, ncols - c0)
        wt = w_ring.get()
        P.dma("pool", wt[:, 0:nkc, 0:cw], wv[:, :, c0:c0 + cw], writes=[wt])
        for j in range(cw // 128):
            for (h0, w) in halves:
                ps = pp.get()
                for kc in range(nkc):
                    P.op("pe", "matmul", ps[:, 0:w], wt[:, kc, j * 128:(j + 1) * 128], hT[kc][:, h0:h0 + w],
                         start=(kc == 0), stop=(kc == nkc - 1), reads=[wt, hT[kc]], writes=[ps])
                consume(c0 // 128 + j, h0, w, ps)


def lin_tm(P, pp, hT, w_ap, ncols, w_ring, T, consume, nkc=D // 128):
    wv = w_ap.rearrange("(kc p) f -> p kc f", p=128)
    for c0 in range(0, ncols, 256):
        cw = min(256, ncols - c0)
        wt = w_ring.get()
        P.dma("pool", wt[:, 0:nkc, 0:cw], wv[:, :, c0:c0 + cw], writes=[wt])
        for tt in range(T // 128):
            ps = pp.get()
            for kc in range(nkc):
                P.op("pe", "matmul", ps[:, 0:cw], hT[kc][:, tt * 128:(tt + 1) * 128], wt[:, kc, 0:cw],
                     start=(kc == 0), stop=(kc == nkc - 1), reads=[wt, hT[kc]], writes=[ps])
            consume(tt, c0, cw, ps)


NQ, NK, NV, NZ, NXBC, NDT = 1024, 1024, 1024, 3072, 5120, 48
OQ, OK_, OV, OZ, OXBC, ODT = 0, 1024, 2048, 3072, 6144, 11264
N_IN = 11312


def headnorm_consume(P, pp, ones_bf, gain_hd, sq_ring, rs_ring, dst_of):
    def consume(fc, h0, w, ps):
        sq = sq_ring.get()
        P.op("act", "activation", out=sq[:, 0:w], in_=ps[:, 0:w], func=AF.Square, reads=[ps], writes=[sq])
        ps2 = pp.get()
        P.op("pe", "matmul", ps2[:, 0:w], ones_bf[:, :], sq[:, 0:w], start=True, stop=True,
             reads=[sq, ones_bf], writes=[ps2])
        rs = rs_ring.get()
        P.op("dve", "tensor_scalar", rs[:, 0:w], ps2[:, 0:w], 1.0 / 128, EPS, ALU.mult, ALU.add, reads=[ps2], writes=[rs])
        P.op("act", "activation", out=rs[:, 0:w], in_=rs[:, 0:w], func=AF.Sqrt, reads=[rs], writes=[rs])
        P.op("dve", "reciprocal", rs[:, 0:w], rs[:, 0:w], reads=[rs], writes=[rs])
        ap, buf = dst_of(fc, h0, w)
        P.op("dve", "scalar_tensor_tensor", out=ap, in0=ps[:, 0:w], scalar=gain_hd[:, 0:1], in1=rs[:, 0:w],
             op0=ALU.mult, op1=ALU.mult, reads=[ps, gain_hd, rs], writes=[buf])
    return consume


def build_progA(T=NTOK, do_ffn=True):
    nc = bass.Bass("TRN2", target_bir_lowering=False)
    di = lambda n, s: nc.dram_tensor(n, s, F32, kind="ExternalInput").ap()
    do = lambda n, s: nc.dram_tensor(n, s, F32, kind="ExternalOutput").ap()
    xin = di("xT", [D, T]); ffg = di("ffg", [128, 16]); mixg = di("mixg", [128, 16])
    wgu = di("w_gu", [D, 2 * DFF]); wdn = di("w_down", [DFF, D]); win = di("w_in", [D, N_IN])
    qg_in = di("qg", [128, 1]); kg_in = di("kg", [128, 1])
    x1o = do("x1T", [D, T]); qo = do("qT", [NQ, T]); ko = do("kT", [NK, T]); vo = do("v", [T, NV])
    zo = do("z", [T, NZ]); xbco = do("xbcT", [NXBC, T]); dto = do("dt", [T, NDT])
    NKC = D // 128
    with contextlib.ExitStack() as stack:
        P = Prog(nc, stack)
        pp = PsumPool(P, 8)
        xT = [P.sbuf([128, T], F32, "xT%d" % c) for c in range(NKC)]
        hT = [P.sbuf([128, T], BF16, "hT%d" % c) for c in range(NKC)]
        aT = [P.sbuf([128, T], BF16, "aT%d" % c) for c in range(12)]
        g1 = P.sbuf([128, NKC], F32, "g1"); g2 = P.sbuf([128, NKC], F32, "g2")
        qg = P.sbuf([128, 1], F32, "qg"); kg = P.sbuf([128, 1], F32, "kg")
        ones_bf = P.sbuf([128, 128], BF16, "ones")
        rstd = P.sbuf([128, T], F32, "rstd")
        sq_ring = Ring([P.sbuf([128, 512], BF16, "sq%d" % i) for i in range(3)])
        sg_ring = Ring([P.sbuf([128, 512], F32, "sg%d" % i) for i in range(3)])
        wg_ring = Ring([P.sbuf([128, NKC, 256], BF16, "wg%d" % i) for i in range(2)])
        wu_ring = Ring([P.sbuf([128, NKC, 256], BF16, "wu%d" % i) for i in range(2)])
        wd_ring = Ring([P.sbuf([128, 12, 512], BF16, "wd%d" % i) for i in range(2)])
        w_ring = Ring(wg_ring.bufs + wu_ring.bufs)
        st_ring = Ring([P.sbuf([128, 512], F32, "st%d" % i) for i in range(4)])

        xv = xin.rearrange("(c p) t -> c p t", p=128)
        P.op("pool", "memset", ones_bf[:, :], 1.0, writes=[ones_bf])
        P.dma("sp", g1[:, :], ffg[:, :], writes=[g1]); P.dma("sp", g2[:, :], mixg[:, :], writes=[g2])
        P.dma("sp", qg[:, :], qg_in[:, :], writes=[qg]); P.dma("sp", kg[:, :], kg_in[:, :], writes=[kg])
        for c in range(NKC):
            P.dma("sp", xT[c][:, :], xv[c], writes=[xT[c]])
        outs = []
        if do_ffn:
            rmsnorm_fm(P, pp, xT, g1, hT, ones_bf, sq_ring, rstd, NKC, T, D)
            ffn_fm(P, pp, xT, hT, wgu, wdn, aT, wg_ring, wu_ring, wd_ring, sg_ring, T)
        x1v = x1o.rearrange("(c p) t -> c p t", p=128)
        for c in range(NKC):
            outs.append(P.dma("sp", x1v[c], xT[c][:, :], reads=[xT[c]]))
        rmsnorm_fm(P, pp, xT, g2, hT, ones_bf, sq_ring, rstd, NKC, T, D)

        for (o_ap, col0, gbuf) in ((qo, OQ, qg), (ko, OK_, kg)):
            ov = o_ap.rearrange("(c p) t -> c p t", p=128)

            def dst_of(fc, h0, w, ov=ov):
                st = st_ring.get()
                dst_of.last = (st, fc, h0, w)
                return st[:, 0:w], st
            cons0 = headnorm_consume(P, pp, ones_bf, gbuf, sq_ring, sg_ring, dst_of)

            def cons(fc, h0, w, ps, cons0=cons0, ov=ov):
                cons0(fc, h0, w, ps)
                st, fc, h0, w = dst_of.last
                outs.append(P.dma("sp", ov[fc][:, h0:h0 + w], st[:, 0:w], reads=[st]))
            lin_fm(P, pp, hT, win[:, col0:col0 + 1024], 1024, w_ring, T, cons)

        xbv = xbco.rearrange("(c p) t -> c p t", p=128)

        def cons_xbc(fc, h0, w, ps):
            st = st_ring.get()
            P.op("act", "activation", out=st[:, 0:w], in_=ps[:, 0:w], func=AF.Copy, reads=[ps], writes=[st])
            outs.append(P.dma("sp", xbv[fc][:, h0:h0 + w], st[:, 0:w], reads=[st]))
        lin_fm(P, pp, hT, win[:, OXBC:OXBC + NXBC], NXBC, w_ring, T, cons_xbc)

        for (o_ap, col0, n) in ((vo, OV, NV), (zo, OZ, NZ), (dto, ODT, NDT)):
            def cons_tm(tt, c0, cw, ps, o_ap=o_ap):
                st = st_ring.get()
                P.op("dve", "tensor_copy", st[:, 0:cw], ps[:, 0:cw], reads=[ps], writes=[st])
                outs.append(P.dma("sp", o_ap[tt * 128:(tt + 1) * 128, c0:c0 + cw], st[:, 0:cw], reads=[st]))
            lin_tm(P, pp, hT, win[:, col0:col0 + n], n, w_ring, T, cons_tm)
        P.emit(final_wait_ops=outs)
    return nc


SEQ = 4096
NCH = SEQ // 128


def build_progB():
    nc = bass.Bass("TRN2", target_bir_lowering=False)
    di = lambda n, sh: nc.dram_tensor(n, sh, F32, kind="ExternalInput").ap()
    xbc = di("xbcT", [1280, SEQ + 3]); convw_i = di("convw", [128, 10, 4]); convb_i = di("convb", [128, 10])
    dtraw_i = di("dtraw", [SEQ, 12]); dtb_i = di("dtb", [128, 384]); alog_i = di("alog", [128, 384])
    dsk_i = di("dsk", [128, 768]); normw_i = di("normw", [128, 768]); z_i = di("z", [SEQ, 768])
    tri_i = di("tri", [128, 128]); strict_i = di("strict", [128, 128]); onesf_i = di("onesf", [128, 128])
    ident_i = di("ident", [128, 128])
    yo = nc.dram_tensor("y", [SEQ, 768], F32, kind="ExternalOutput").ap()
    with contextlib.ExitStack() as stack:
        P = Prog(nc, stack)
        pp = PsumPool(P, 8)
        cw = P.sbuf([128, 10, 4], F32, "cw"); cb = P.sbuf([128, 10], F32, "cb")
        dt_all = P.sbuf([128, 384], F32, "dt_all"); da_all = P.sbuf([128, 384], F32, "da_all")
        tmpa = P.sbuf([128, 384], F32, "tmpa"); tmpb = P.sbuf([128, 384], F32, "tmpb")
        dsk = P.sbuf([128, 12, 64], F32, "dsk"); normw = P.sbuf([128, 768], F32, "normw")
        tri = P.sbuf([128, 128], F32, "tri"); strict = P.sbuf([128, 128], F32, "strict")
        onesf = P.sbuf([128, 128], F32, "onesf"); ident = P.sbuf([128, 128], F32, "ident")
        for (b, a) in ((cw, convw_i), (cb, convb_i), (tmpa, dtb_i), (tmpb, alog_i),
                       (normw, normw_i), (tri, tri_i), (strict, strict_i), (onesf, onesf_i), (ident, ident_i)):
            P.dma("sp", b[:], a, writes=[b])
        P.dma("sp", dsk[:], dsk_i.rearrange("p (j d) -> p j d", d=64), writes=[dsk])
        P.dma("sp", dt_all[:].rearrange("p (c j) -> p c j", j=12), dtraw_i.rearrange("(c l) j -> l c j", l=128),
              writes=[dt_all])
        P.op("dve", "tensor_tensor", out=dt_all[:], in0=dt_all[:], in1=tmpa[:], op=ALU.add, reads=[dt_all, tmpa], writes=[dt_all])
        P.op("act", "activation", out=dt_all[:], in_=dt_all[:], func=AF.Exp, reads=[dt_all], writes=[dt_all])
        P.op("dve", "tensor_scalar", dt_all[:], dt_all[:], 1.0, None, ALU.add, reads=[dt_all], writes=[dt_all])
        P.op("act", "activation", out=dt_all[:], in_=dt_all[:], func=AF.Ln, reads=[dt_all], writes=[dt_all])
        P.op("act", "activation", out=tmpb[:], in_=tmpb[:], func=AF.Exp, reads=[tmpb], writes=[tmpb])
        P.op("dve", "scalar_tensor_tensor", out=da_all[:], in0=dt_all[:], scalar=-1.0, in1=tmpb[:], op0=ALU.mult,
             op1=ALU.mult, reads=[dt_all, tmpb], writes=[da_all])

        h = [P.sbuf([128, 6, 64], F32, "h%d" % g) for g in range(2)]
        hb = [P.sbuf([128, 6, 64], BF16, "hb%d" % g) for g in range(2)]
        for g in range(2):
            P.op("pool", "memset", h[g][:], 0.0, writes=[h[g]])
            P.op("pool", "memset", hb[g][:], 0.0, writes=[hb[g]])

        xr_ring = Ring([P.sbuf([128, 515], F32, "xr%d" % i) for i in range(4)])
        acc_ring = Ring([P.sbuf([128, 512], F32, "acc%d" % i) for i in range(2)])
        xc_sets = [[P.sbuf([128, 512], F32, "xc%d_%d" % (k, i)) for i in range(8)] for k in range(2)]
        bc_sets = [[P.sbuf([128, 512], BF16, "bc%d_%d" % (k, i)) for i in range(4)] for k in range(2)]
        xt_ring = Ring([P.sbuf([128, 12, 64], F32, "xt%d" % i) for i in range(2)])
        bt_ring = Ring([P.sbuf([128, 256], BF16, "bt%d" % i) for i in range(2)])
        E_ring = Ring([P.sbuf([128, 36], F32, "E%d" % i) for i in range(2)])
        s2_ring = Ring([P.sbuf([128, 12], F32, "s2%d" % i) for i in range(2)])
        xdt_ring = Ring([P.sbuf([128, 12, 64], BF16, "xdt%d" % i) for i in range(2)])
        xw_ring = Ring([P.sbuf([128, 12, 64], BF16, "xw%d" % i) for i in range(2)])
        xd_ring = Ring([P.sbuf([128, 12, 64], F32, "xd%d" % i) for i in range(2)])
        cbm_ring = Ring([P.sbuf([128, 128], F32, "cbm%d" % i) for i in range(2)])
        aj_ring = Ring([P.sbuf([128, 128], F32, "aj%d" % i) for i in range(3)])
        dec_ring = Ring([P.sbuf([128, 128], F32, "dec%d" % i) for i in range(3)])
        sc_ring = Ring([P.sbuf([128, 128], BF16, "sc%d" % i) for i in range(3)])
        y_ring = Ring([P.sbuf([128, 12, 64], F32, "y%d" % i) for i in range(2)])
        t1_ring = Ring([P.sbuf([128, 6, 64], F32, "t1%d" % i) for i in range(2)])
        z_ring = Ring([P.sbuf([128, 768], F32, "z%d" % i) for i in range(2)])
        sq_ring = Ring([P.sbuf([128, 768], F32, "sqq%d" % i) for i in range(2)])
        ss_ring = Ring([P.sbuf([128, 2], F32, "ss%d" % i) for i in range(2)])
        o_ring = Ring([P.sbuf([128, 768], F32, "o%d" % i) for i in range(2)])
        outs = []
        for tb in range(SEQ // 512):
            xc = xc_sets[tb % 2]
            bc = bc_sets[tb % 2]
            for ch in range(10):
                xr = xr_ring.get()
                P.dma("sp", xr[:, :], xbc[ch * 128:(ch + 1) * 128, tb * 512:tb * 512 + 515], writes=[xr])
                acc = acc_ring.get()
                P.op("dve", "tensor_scalar", acc[:, :], xr[:, 0:512], cw[:, ch, 0:1], cb[:, ch:ch + 1], ALU.mult, ALU.add,
                     reads=[xr, cw, cb], writes=[acc])
                for k in range(1, 4):
                    P.op("dve", "scalar_tensor_tensor", out=acc[:, :], in0=xr[:, k:k + 512], scalar=cw[:, ch, k:k + 1],
                         in1=acc[:, :], op0=ALU.mult, op1=ALU.add, reads=[xr, cw, acc], writes=[acc])
                if ch < 8:
                    P.op("act", "activation", out=xc[ch][:, :], in_=acc[:, :], func=AF.Silu, reads=[acc], writes=[xc[ch]])
                    if ch >= 6:
                        P.op("dve", "tensor_copy", bc[ch - 6][:, :], xc[ch][:, :], reads=[xc[ch]], writes=[bc[ch - 6]])
                else:
                    P.op("act", "activation", out=bc[ch - 6][:, :], in_=acc[:, :], func=AF.Silu, reads=[acc], writes=[bc[ch - 6]])
            for sc_i in range(4):
                c = tb * 4 + sc_i
                ts = slice(sc_i * 128, (sc_i + 1) * 128)
                xt = xt_ring.get(); bt = bt_ring.get()
                xtf = xt[:].rearrange("p j d -> p (j d)")
                for (lo, n) in ((0, 4), (4, 2)):
                    ps = pp.get()
                    for i in range(n):
                        P.op("pe", "transpose", ps[:, i * 128:(i + 1) * 128], xc[lo + i][:, ts], ident[:, :],
                             reads=[xc[lo + i], ident], writes=[ps])
                    P.op("dve", "tensor_copy", xtf[:, lo * 128:(lo + n) * 128], ps[:, 0:n * 128], reads=[ps], writes=[xt])
                ps = pp.get()
                for i in range(2):
                    P.op("pe", "transpose", ps[:, i * 128:(i + 1) * 128], xc[6 + i][:, ts], ident[:, :],
                         reads=[xc[6 + i], ident], writes=[ps])
                P.op("act", "activation", out=bt[:, :], in_=ps[:, 0:256], func=AF.Copy, reads=[ps], writes=[bt])
                dac = da_all[:, c * 12:(c + 1) * 12]
                dtc = dt_all[:, c * 12:(c + 1) * 12]
                ps = pp.get()
                P.op("pe", "matmul", ps[:, 0:12], tri[:, :], dac, start=True, stop=True, reads=[tri, da_all], writes=[ps])
                P.op("pe", "matmul", ps[:, 12:24], strict[:, :], dac, start=True, stop=True, reads=[strict, da_all], writes=[ps])
                P.op("pe", "matmul", ps[:, 24:36], onesf[:, :], dac, start=True, stop=True, reads=[onesf, da_all], writes=[ps])
                E = E_ring.get()
                P.op("act", "activation", out=E[:, :], in_=ps[:, 0:36], func=AF.Exp, reads=[ps], writes=[E])
                s2 = s2_ring.get()
                P.op("dve", "tensor_tensor", out=s2[:, :], in0=dtc, in1=E[:, 12:24], op=ALU.mult, reads=[dt_all, E], writes=[s2])
                xdt = xdt_ring.get(); xw = xw_ring.get(); xd = xd_ring.get()
                P.op("dve", "tensor_tensor", out=xdt[:], in0=xt[:], in1=dtc.unsqueeze(2).broadcast_to([128, 12, 64]),
                     op=ALU.mult, reads=[xt, dt_all], writes=[xdt])
                P.op("dve", "tensor_tensor", out=xw[:], in0=xt[:], in1=s2[:, :].unsqueeze(2).broadcast_to([128, 12, 64]),
                     op=ALU.mult, reads=[xt, s2], writes=[xw])
                P.op("pool", "tensor_tensor", out=xd[:], in0=xt[:], in1=dsk[:], op=ALU.mult, reads=[xt, dsk], writes=[xd])
                y = y_ring.get()
                for gi in range(2):
                    BT = bc[gi]; CT = bc[2 + gi]
                    ps_cb = pp.get()
                    P.op("pe", "matmul", ps_cb[:, 0:128], BT[:, ts], CT[:, ts], start=True, stop=True, reads=[BT, CT], writes=[ps_cb])
                    cbm = cbm_ring.get()
                    P.op("dve", "tensor_tensor", out=cbm[:, :], in0=ps_cb[:, 0:128], in1=tri[:, :], op=ALU.mult,
                         reads=[ps_cb, tri], writes=[cbm])
                    ps_yo = pp.get()
                    P.op("pe", "matmul", ps_yo[:, 0:384], CT[:, ts], hb[gi][:].rearrange("p j d -> p (j d)"),
                         start=True, stop=True, reads=[CT, hb[gi]], writes=[ps_yo])
                    ps_yd = pp.get()
                    for jj in range(6):
                        j = gi * 6 + jj
                        aj = aj_ring.get()
                        P.op("pool", "tensor_scalar", aj[:, :], strict[:, :], da_all[:, c * 12 + j:c * 12 + j + 1], None, ALU.mult,
                             reads=[strict, da_all], writes=[aj])
                        ps_seg = pp.get()
                        P.op("pe", "matmul", ps_seg[:, 0:128], aj[:, :], tri[:, :], start=True, stop=True, reads=[aj, tri], writes=[ps_seg])
                        dec = dec_ring.get()
                        P.op("act", "activation", out=dec[:, :], in_=ps_seg[:, 0:128], func=AF.Exp, reads=[ps_seg], writes=[dec])
                        scb = sc_ring.get()
                        P.op("dve", "tensor_tensor", out=scb[:, :], in0=dec[:, :], in1=cbm[:, :], op=ALU.mult,
                             reads=[dec, cbm], writes=[scb])
                        P.op("pe", "matmul", ps_yd[:, jj * 64:(jj + 1) * 64], scb[:, :], xdt[:, j, :], start=True, stop=True,
                             reads=[scb, xdt], writes=[ps_yd])
                    t1 = t1_ring.get()
                    P.op("dve", "tensor_tensor", out=t1[:], in0=ps_yo[:, 0:384].rearrange("p (j d) -> p j d", d=64),
                         in1=E[:, gi * 6:gi * 6 + 6].unsqueeze(2).broadcast_to([128, 6, 64]), op=ALU.mult,
                         reads=[ps_yo, E], writes=[t1])
                    P.op("dve", "tensor_tensor", out=t1[:], in0=ps_yd[:, 0:384].rearrange("p (j d) -> p j d", d=64),
                         in1=t1[:], op=ALU.add, reads=[ps_yd, t1], writes=[t1])
                    P.op("dve", "tensor_tensor", out=y[:, gi * 6:(gi + 1) * 6, :], in0=xd[:, gi * 6:(gi + 1) * 6, :], in1=t1[:],
                         op=ALU.add, reads=[xd, t1], writes=[y])
                    ps_st = pp.get()
                    P.op("pe", "matmul", ps_st[:, 0:384], bt[:, gi * 128:(gi + 1) * 128],
                         xw[:, gi * 6:(gi + 1) * 6, :].rearrange("p j d -> p (j d)"), start=True, stop=True,
                         reads=[bt, xw], writes=[ps_st])
                    P.op("dve", "tensor_tensor", out=h[gi][:], in0=h[gi][:],
                         in1=E[:, 24 + gi * 6:24 + gi * 6 + 6].unsqueeze(2).broadcast_to([128, 6, 64]), op=ALU.mult,
                         reads=[h[gi], E], writes=[h[gi]])
                    P.op("dve", "tensor_tensor", out=h[gi][:], in0=ps_st[:, 0:384].rearrange("p (j d) -> p j d", d=64),
                         in1=h[gi][:], op=ALU.add, reads=[ps_st, h[gi]], writes=[h[gi]])
                    P.op("act", "activation", out=hb[gi][:], in_=h[gi][:], func=AF.Copy, reads=[h[gi]], writes=[hb[gi]])
                zt = z_ring.get()
                P.dma("sp", zt[:, :], z_i[c * 128:(c + 1) * 128, :], writes=[zt])
                P.op("act", "activation", out=zt[:, :], in_=zt[:, :], func=AF.Silu, reads=[zt], writes=[zt])
                yf = y[:].rearrange("p j d -> p (j d)")
                P.op("dve", "tensor_tensor", out=yf, in0=yf, in1=zt[:, :], op=ALU.mult, reads=[y, zt], writes=[y])
                sq = sq_ring.get()
                P.op("act", "activation", out=sq[:, :], in_=yf, func=AF.Square, reads=[y], writes=[sq])
                ss = ss_ring.get()
                P.op("dve", "tensor_reduce", out=ss[:, :], in_=sq[:, :].rearrange("p (g f) -> p g f", g=2), axis=AX.X, op=ALU.add,
                     reads=[sq], writes=[ss])
                P.op("dve", "tensor_scalar", ss[:, :], ss[:, :], 1.0 / 384, EPS, ALU.mult, ALU.add, reads=[ss], writes=[ss])
                P.op("act", "activation", out=ss[:, :], in_=ss[:, :], func=AF.Sqrt, reads=[ss], writes=[ss])
                P.op("dve", "reciprocal", ss[:, :], ss[:, :], reads=[ss], writes=[ss])
                ot = o_ring.get()
                for gi in range(2):
                    P.op("dve", "scalar_tensor_tensor", out=ot[:, gi * 384:(gi + 1) * 384], in0=yf[:, gi * 384:(gi + 1) * 384],
                         scalar=ss[:, gi:gi + 1], in1=normw[:, gi * 384:(gi + 1) * 384], op0=ALU.mult, op1=ALU.mult,
                         reads=[y, ss, normw], writes=[ot])
                outs.append(P.dma("sp", yo[c * 128:(c + 1) * 128, :], ot[:, :], reads=[ot]))
        P.emit(final_wait_ops=outs)
    return nc


def _rep(v, n=128):
    return np.ascontiguousarray(np.broadcast_to(np.asarray(v, np.float32).reshape(1, -1), (n, np.asarray(v).size)))


def ssd_consts():
    k = np.arange(128)
    tri = (k[:, None] <= k[None, :]).astype(np.float32)
    strict = (k[:, None] > k[None, :]).astype(np.float32)
    return {"tri": tri, "strict": strict, "onesf": np.ones((128, 128), np.float32), "ident": np.eye(128, dtype=np.float32)}


def progB_inputs(xbcT_b, dt_b, z_b, Pm):
    maps = []
    cst = ssd_consts()
    for c in range(NCORES):
        b, gp = c // 4, c % 4
        g0 = 2 * gp
        chs = np.concatenate([np.arange(g0 * 384, (g0 + 2) * 384), 3072 + np.arange(g0 * 128, (g0 + 2) * 128),
                              4096 + np.arange(g0 * 128, (g0 + 2) * 128)])
        hs = np.arange(g0 * 6, g0 * 6 + 12)
        xp = np.zeros((1280, SEQ + 3), np.float32)
        xp[:, 3:] = xbcT_b[b][chs]
        m = {"xbcT": xp,
             "convw": np.ascontiguousarray(Pm["conv_w"][:, chs].T.reshape(10, 128, 4).transpose(1, 0, 2)),
             "convb": np.ascontiguousarray(Pm["conv_b"][chs].reshape(10, 128).T),
             "dtraw": np.ascontiguousarray(dt_b[b][:, hs]),
             "dtb": _rep(np.tile(Pm["dt_bias"][hs], NCH)), "alog": _rep(np.tile(Pm["a_log"][hs], NCH)),
             "dsk": _rep(np.repeat(Pm["d_skip"][hs], 64)), "normw": _rep(Pm["ssd_norm"][g0 * 384:(g0 + 2) * 384]),
             "z": np.ascontiguousarray(z_b[b][:, g0 * 384:(g0 + 2) * 384])}
        m.update(cst)
        maps.append(m)
    return maps


def progB_gather(ys):
    out = np.zeros((2, SEQ, 3072), np.float32)
    for c in range(NCORES):
        b, gp = c // 4, c % 4
        out[b][:, gp * 768:(gp + 1) * 768] = ys[c]
    return out


NBLK = SEQ // 256
BIGNEG = -30000.0


def build_progC1():
    nc = bass.Bass("TRN2", target_bir_lowering=False)
    di = lambda n, sh: nc.dram_tensor(n, sh, F32, kind="ExternalInput").ap()
    q_i = di("qT", [1024, NTOK]); k_i = di("kT_all", [1024, SEQ]); v_i = di("v_all", [SEQ, 1024])
    ko_i = di("kT_own", [1024, NTOK]); vo_i = di("v_own", [NTOK, 1024])
    tna_i = di("Tna", [128, 8 * 384]); tca_i = di("Tca", [128, 8 * 384]); ab_i = di("abias", [128, 512])
    gb_i = di("gbias", [128, 64]); pm_i = di("pastmask", [128, 64]); oh_i = di("onehot", [16, 16 * 128])
    id_i = di("ident", [128, 128])
    yo = nc.dram_tensor("yT", [1024, NTOK], F32, kind="ExternalOutput").ap()
    scale = 128.0 ** -0.5
    with contextlib.ExitStack() as stack:
        P = Prog(nc, stack)
        pp = PsumPool(P, 4)
        pacc = PsumPool(P, 4, pfx="pacc")
        tna = P.sbuf([128, 8, 384], F32, "tna"); tca = P.sbuf([128, 8, 384], F32, "tca")
        abias = P.sbuf([128, 512], F32, "abias"); gbias = P.sbuf([128, 64], F32, "gbias")
        pmask = P.sbuf([128, 64], F32, "pmask"); onehot = P.sbuf([16, 16, 128], BF16, "onehot")
        ident = P.sbuf([128, 128], F32, "ident"); ones_bf = P.sbuf([128, 128], BF16, "ones")
        P.dma("sp", tna[:], tna_i.rearrange("p (h u) -> p h u", h=8), writes=[tna])
        P.dma("sp", tca[:], tca_i.rearrange("p (h u) -> p h u", h=8), writes=[tca])
        for (b, a) in ((abias, ab_i), (gbias, gb_i), (pmask, pm_i), (ident, id_i)):
            P.dma("sp", b[:], a, writes=[b])
        P.dma("pool", onehot[:], oh_i.rearrange("k (n m) -> k n m", n=16), writes=[onehot])
        P.op("pool", "memset", ones_bf[:, :], 1.0, writes=[ones_bf])
        kf_ring = Ring([P.sbuf([128, SEQ], F32, "kf%d" % i) for i in range(2)])
        kb_ring = Ring([P.sbuf([128, SEQ], BF16, "kb%d" % i) for i in range(2)])
        qf_ring = Ring([P.sbuf([128, NTOK], F32, "qf%d" % i) for i in range(2)])
        qb_ring = Ring([P.sbuf([128, NTOK], BF16, "qb%d" % i) for i in range(2)])
        ko_ring = Ring([P.sbuf([128, NTOK], BF16, "ko%d" % i) for i in range(2)])
        vb_ring = Ring([P.sbuf([128, 32, 128], BF16, "vb%d" % i) for i in range(2)])
        vo_ring = Ring([P.sbuf([128, 8, 128], BF16, "vo%d" % i) for i in range(2)])
        km_ring = Ring([P.sbuf([128, 16], F32, "km%d" % i) for i in range(2)])
        gm_ring = Ring([P.sbuf([128, 16], F32, "gm%d" % i) for i in range(2)])
        t8_ring = Ring([P.sbuf([128, 8], F32, "t8%d" % i) for i in range(2)])
        sel_ring = Ring([P.sbuf([128, 16], F32, "sel%d" % i) for i in range(2)])
        ns_ring = Ring([P.sbuf([16, 256], BF16, "ns%d" % i) for i in range(2)])
        lg_ring = Ring([P.sbuf([128, 256], F32, "lg%d" % i) for i in range(3)])
        pT_ring = Ring([P.sbuf([128, 256], BF16, "pT%d" % i) for i in range(3)])
        rd_ring = Ring([P.sbuf([128, 256], F32, "rd%d" % i) for i in range(2)])
        o_ring = Ring([P.sbuf([128, 256], F32, "oo%d" % i) for i in range(2)])
        outs = []
        for h in range(8):
            hr = slice(h * 128, (h + 1) * 128)
            kf = kf_ring.get(); kb = kb_ring.get(); qf = qf_ring.get(); qb = qb_ring.get()
            ko = ko_ring.get(); vb = vb_ring.get(); vo = vo_ring.get(); km = km_ring.get()
            P.dma("sp", kf[:, :], k_i[hr, :], writes=[kf])
            P.dma("sp", qf[:, :], q_i[hr, :], writes=[qf])
            P.dma("pool", ko[:, :], ko_i[hr, :], writes=[ko])
            P.dma("pool", vb[:], v_i[:, hr].rearrange("(t p) d -> p t d", p=128), writes=[vb])
            P.dma("pool", vo[:], vo_i[:, hr].rearrange("(t p) d -> p t d", p=128), writes=[vo])
            P.op("act", "activation", out=kb[:, :], in_=kf[:, :], func=AF.Copy, reads=[kf], writes=[kb])
            P.op("act", "activation", out=qb[:, :], in_=qf[:, :], func=AF.Copy, reads=[qf], writes=[qb])
            P.op("dve", "tensor_reduce", out=km[:, :], in_=kf[:, :].rearrange("p (n s) -> p n s", s=256), axis=AX.X, op=ALU.add,
                 reads=[kf], writes=[km])
            P.op("dve", "tensor_scalar", km[:, :], km[:, :], 1.0 / 256, None, ALU.mult, reads=[km], writes=[km])
            for qi in range(4):
                qs_ = slice(qi * 256, (qi + 1) * 256)
                ns = ns_ring.get()
                for qs in range(2):
                    ps_g = pp.get()
                    P.op("pe", "matmul", ps_g[:, 0:16], qf[:, qi * 256 + qs * 128:qi * 256 + (qs + 1) * 128], km[:, :],
                         start=True, stop=True, reads=[qf, km], writes=[ps_g])
                    gm = gm_ring.get(); t8 = t8_ring.get(); sel = sel_ring.get()
                    P.op("dve", "tensor_tensor", out=gm[:, :], in0=ps_g[:, 0:16], in1=gbias[:, qi * 16:(qi + 1) * 16], op=ALU.add,
                         reads=[ps_g, gbias], writes=[gm])
                    P.op("dve", "max", out=t8[:, :], in_=gm[:, :], reads=[gm], writes=[t8])
                    P.op("dve", "tensor_scalar", sel[:, :], gm[:, :], t8[:, 2:3], None, ALU.is_ge, reads=[gm, t8], writes=[sel])
                    P.op("dve", "tensor_tensor", out=sel[:, :], in0=sel[:, :], in1=pmask[:, qi * 16:(qi + 1) * 16], op=ALU.mult,
                         reads=[sel, pmask], writes=[sel])
                    P.op("dve", "tensor_scalar", sel[:, :], sel[:, :], -1.0, -BIGNEG, ALU.add, ALU.mult, reads=[sel], writes=[sel])
                    ps_t = pp.get()
                    P.op("pe", "transpose", ps_t[0:16, 0:128], sel[:, :], ident[:, :], reads=[sel, ident], writes=[ps_t])
                    P.op("act", "activation", out=ns[:, qs * 128:(qs + 1) * 128], in_=ps_t[0:16, 0:128], func=AF.Copy,
                         reads=[ps_t], writes=[ns])
                ps_o = pacc.get(); ps_d = pacc.get()
                tiles = [(n, kt) for n in range(NBLK) for kt in range(2)] + [(-1, 0), (-1, 1)]
                for ti, (n, kt) in enumerate(tiles):
                    first, last = (ti == 0), (ti == len(tiles) - 1)
                    ps_s = pp.get()
                    lg = lg_ring.get(); pT = pT_ring.get()
                    tsl = slice(128, 384) if kt == 0 else slice(0, 256)
                    if n >= 0:
                        P.op("pe", "matmul", ps_s[:, 0:256], kb[:, n * 256 + kt * 128:n * 256 + (kt + 1) * 128], qb[:, qs_],
                             start=True, stop=False, reads=[kb, qb], writes=[ps_s])
                        P.op("pe", "matmul", ps_s[:, 0:256], onehot[:, n, :], ns[:, :], start=False, stop=True,
                             reads=[onehot, ns], writes=[ps_s])
                        P.op("dve", "scalar_tensor_tensor", out=lg[:, :], in0=ps_s[:, 0:256], scalar=scale, in1=tna[:, h, tsl],
                             op0=ALU.mult, op1=ALU.add, reads=[ps_s, tna], writes=[lg])
                        bi = (h * 4 + qi) * 16 + n
                        P.op("act", "activation", out=pT[:, :], in_=lg[:, :], func=AF.Exp, bias=abias[:, bi:bi + 1],
                             reads=[lg, abias], writes=[pT])
                        vl = vb[:, n * 2 + kt, :]
                        vbuf = vb
                    else:
                        P.op("pe", "matmul", ps_s[:, 0:256], ko[:, qi * 256 + kt * 128:qi * 256 + (kt + 1) * 128], qb[:, qs_],
                             start=True, stop=True, reads=[ko, qb], writes=[ps_s])
                        P.op("dve", "scalar_tensor_tensor", out=lg[:, :], in0=ps_s[:, 0:256], scalar=scale, in1=tca[:, h, tsl],
                             op0=ALU.mult, op1=ALU.add, reads=[ps_s, tca], writes=[lg])
                        P.op("act", "activation", out=pT[:, :], in_=lg[:, :], func=AF.Exp, reads=[lg], writes=[pT])
                        vl = vo[:, qi * 2 + kt, :]
                        vbuf = vo
                    P.op("pe", "matmul", ps_o[:, 0:256], vl, pT[:, :], start=first, stop=last, reads=[vbuf, pT], writes=[ps_o])
                    P.op("pe", "matmul", ps_d[:, 0:256], ones_bf[:, :], pT[:, :], start=first, stop=last,
                         reads=[ones_bf, pT], writes=[ps_d])
                rd = rd_ring.get(); ot = o_ring.get()
                P.op("dve", "reciprocal", rd[:, :], ps_d[:, 0:256], reads=[ps_d], writes=[rd])
                P.op("dve", "tensor_tensor", out=ot[:, :], in0=ps_o[:, 0:256], in1=rd[:, :], op=ALU.mult, reads=[ps_o, rd], writes=[ot])
                outs.append(P.dma("sp", yo[hr, qs_], ot[:, :], reads=[ot]))
        P.emit(final_wait_ops=outs)
    return nc


def attn_consts(seg):
    sl = np.arange(128)[:, None].astype(np.float64)
    u = np.arange(384)[None, :].astype(np.float64)
    dist = u - 128 - sl
    slopes = 2.0 ** (-(np.arange(8) + 1.0))
    tna = np.zeros((128, 8, 384), np.float32); tca = np.zeros((128, 8, 384), np.float32)
    for h in range(8):
        tna[:, h] = -slopes[h] * dist
        tca[:, h] = np.where(dist >= 0, -slopes[h] * dist, BIGNEG)
    abias = np.zeros((8, 4, 16), np.float32); gb = np.zeros((4, 16), np.float32); pm = np.zeros((4, 16), np.float32)
    for qi in range(4):
        G = 4 * seg + qi
        for n in range(16):
            if n < G:
                pm[qi, n] = 1.0
                abias[:, qi, n] = -slopes * 256.0 * (G - n)
            else:
                gb[qi, n] = -1e30
    oh = np.zeros((16, 16, 128), np.float32)
    for n in range(16):
        oh[n, n, :] = 1.0
    return {"Tna": tna.reshape(128, -1), "Tca": tca.reshape(128, -1), "abias": _rep(abias.reshape(-1)),
            "gbias": _rep(gb.reshape(-1)), "pastmask": _rep(pm.reshape(-1)), "onehot": oh.reshape(16, -1),
            "ident": np.eye(128, dtype=np.float32)}


def progC1_inputs(qT_c, kT_c, v_c):
    maps = []
    for c in range(NCORES):
        b, seg = c // 4, c % 4
        m = {"qT": qT_c[c], "kT_own": kT_c[c], "v_own": v_c[c],
             "kT_all": np.ascontiguousarray(np.concatenate([kT_c[b * 4 + s] for s in range(4)], axis=1)),
             "v_all": np.ascontiguousarray(np.concatenate([v_c[b * 4 + s] for s in range(4)], axis=0))}
        m.update(attn_consts(seg))
        maps.append(m)
    return maps


MEMLEN = 256


def build_progC2(T=NTOK):
    nc = bass.Bass("TRN2", target_bir_lowering=False)
    di = lambda n, sh: nc.dram_tensor(n, sh, F32, kind="ExternalInput").ap()
    xin = di("xT", [D, T]); yin = di("yT", [4096, T]); wout = di("w_out", [4096, D]); mem_i = di("memT", [D, MEMLEN])
    xmg_i = di("xmg", [128, 16]); mmg_i = di("mmg", [128, 16]); ffg_i = di("ffg", [128, 16])
    mqg_i = di("mqg", [128, 1]); mkg_i = di("mkg", [128, 1])
    wq = di("wq", [D, 512]); wk = di("wk", [D, 512]); wv = di("wv", [D, 512]); wo = di("wo", [512, D])
    wgu = di("w_gu", [D, 2 * DFF]); wdn = di("w_down", [DFF, D])
    xo = nc.dram_tensor("xoT", [D, T], F32, kind="ExternalOutput").ap()
    NKC = D // 128
    scale = 128.0 ** -0.5
    with contextlib.ExitStack() as stack:
        P = Prog(nc, stack)
        pp = PsumPool(P, 8)
        xT = [P.sbuf([128, T], F32, "xT%d" % c) for c in range(NKC)]
        hT = [P.sbuf([128, T], BF16, "hT%d" % c) for c in range(NKC)]
        aT = [P.sbuf([128, T], BF16, "aT%d" % c) for c in range(12)]
        g_xm = P.sbuf([128, NKC], F32, "g_xm"); g_mm = P.sbuf([128, NKC], F32, "g_mm"); g_ff = P.sbuf([128, NKC], F32, "g_ff")
        mqg = P.sbuf([128, 1], F32, "mqg"); mkg = P.sbuf([128, 1], F32, "mkg")
        ones_bf = P.sbuf([128, 128], BF16, "ones")
        rstd = P.sbuf([128, T], F32, "rstd")
        sq_ring = Ring([P.sbuf([128, 512], BF16, "sq%d" % i) for i in range(3)])
        sg_ring = Ring([P.sbuf([128, 512], F32, "sg%d" % i) for i in range(3)])
        wg_ring = Ring([P.sbuf([128, NKC, 256], BF16, "wg%d" % i) for i in range(2)])
        wu_ring = Ring([P.sbuf([128, NKC, 256], BF16, "wu%d" % i) for i in range(2)])
        wd_ring = Ring([P.sbuf([128, 12, 512], BF16, "wd%d" % i) for i in range(2)])
        w_ring = Ring(wg_ring.bufs + wu_ring.bufs)
        mr_ring = Ring([P.sbuf([128, MEMLEN], F32, "mr%d" % i) for i in range(3)])
        mhT = [P.sbuf([128, MEMLEN], BF16, "mhT%d" % c) for c in range(NKC)]
        kmT = [P.sbuf([128, MEMLEN], BF16, "kmT%d" % c) for c in range(4)]
        vm = [P.sbuf([128, 512], BF16, "vm%d" % c) for c in range(2)]
        pT_ring = sq_ring
        rd_ring = sg_ring

        xv = xin.rearrange("(c p) t -> c p t", p=128)
        yv = yin.rearrange("(c p) t -> c p t", p=128)
        mv = mem_i.rearrange("(c p) t -> c p t", p=128)
        P.op("pool", "memset", ones_bf[:, :], 1.0, writes=[ones_bf])
        for (b, a) in ((g_xm, xmg_i), (g_mm, mmg_i), (g_ff, ffg_i), (mqg, mqg_i), (mkg, mkg_i)):
            P.dma("sp", b[:], a, writes=[b])
        for c in range(NKC):
            P.dma("sp", xT[c][:, :], xv[c], writes=[xT[c]])

        def cons_add(fc, h0, w, ps):
            P.op("dve", "tensor_tensor", out=xT[fc][:, h0:h0 + w], in0=ps[:, 0:w], in1=xT[fc][:, h0:h0 + w], op=ALU.add,
                 reads=[ps, xT[fc]], writes=[xT[fc]])
        for kh in range(2):
            for c in range(NKC):
                P.dma("pool", hT[c][:, :], yv[kh * NKC + c], writes=[hT[c]])
            lin_fm(P, pp, hT, wout[kh * D:(kh + 1) * D, :], D, w_ring, T, cons_add)
        ps_m = pp.get()
        for c in range(NKC):
            mt = mr_ring.get()
            P.dma("sp", mt[:, :], mv[c], writes=[mt])
            sq = sq_ring.get()
            P.op("act", "activation", out=sq[:, 0:MEMLEN], in_=mt[:, :], func=AF.Square, reads=[mt], writes=[sq])
            P.op("pe", "matmul", ps_m[:, 0:MEMLEN], ones_bf[:, :], sq[:, 0:MEMLEN], start=(c == 0), stop=(c == NKC - 1),
                 reads=[sq, ones_bf], writes=[ps_m])
        P.op("dve", "tensor_scalar", rstd[:, 0:MEMLEN], ps_m[:, 0:MEMLEN], 1.0 / D, EPS, ALU.mult, ALU.add, reads=[ps_m], writes=[rstd])
        P.op("act", "activation", out=rstd[:, 0:MEMLEN], in_=rstd[:, 0:MEMLEN], func=AF.Sqrt, reads=[rstd], writes=[rstd])
        P.op("dve", "reciprocal", rstd[:, 0:MEMLEN], rstd[:, 0:MEMLEN], reads=[rstd], writes=[rstd])
        for c in range(NKC):
            mt = mr_ring.get()
            P.dma("sp", mt[:, :], mv[c], writes=[mt])
            P.op("dve", "scalar_tensor_tensor", out=mhT[c][:, :], in0=mt[:, :], scalar=g_mm[:, c:c + 1], in1=rstd[:, 0:MEMLEN],
                 op0=ALU.mult, op1=ALU.mult, reads=[mt, g_mm, rstd], writes=[mhT[c]])

        def dst_k(fc, h0, w):
            return kmT[fc][:, h0:h0 + w], kmT[fc]
        lin_fm(P, pp, mhT, wk, 512, w_ring, MEMLEN, headnorm_consume(P, pp, ones_bf, mkg, sq_ring, sg_ring, dst_k))

        def cons_v(tt, c0, cw, ps):
            P.op("act", "activation", out=vm[tt][:, c0:c0 + cw], in_=ps[:, 0:cw], func=AF.Copy, reads=[ps], writes=[vm[tt]])
        lin_tm(P, pp, mhT, wv, 512, w_ring, MEMLEN, cons_v)
        rmsnorm_fm(P, pp, xT, g_xm, hT, ones_bf, sq_ring, rstd, NKC, T, D)
        qmT = aT[0:4]
        oT = aT[4:8]

        def dst_q(fc, h0, w):
            return qmT[fc][:, h0:h0 + w], qmT[fc]
        lin_fm(P, pp, hT, wq, 512, w_ring, T, headnorm_consume(P, pp, ones_bf, mqg, sq_ring, sg_ring, dst_q))
        for mh in range(4):
            for h0 in range(0, T, 512):
                w = min(512, T - h0)
                ps_o = pp.get(); ps_d = pp.get()
                for kt in range(2):
                    ps_s = pp.get()
                    P.op("pe", "matmul", ps_s[:, 0:w], kmT[mh][:, kt * 128:(kt + 1) * 128], qmT[mh][:, h0:h0 + w],
                         start=True, stop=True, reads=[kmT[mh], qmT[mh]], writes=[ps_s])
                    pT = pT_ring.get()
                    P.op("act", "activation", out=pT[:, 0:w], in_=ps_s[:, 0:w], func=AF.Exp, scale=scale, reads=[ps_s], writes=[pT])
                    P.op("pe", "matmul", ps_o[:, 0:w], vm[kt][:, mh * 128:(mh + 1) * 128], pT[:, 0:w], start=(kt == 0),
                         stop=(kt == 1), reads=[vm[kt], pT], writes=[ps_o])
                    P.op("pe", "matmul", ps_d[:, 0:w], ones_bf[:, :], pT[:, 0:w], start=(kt == 0), stop=(kt == 1),
                         reads=[ones_bf, pT], writes=[ps_d])
                rd = rd_ring.get()
                P.op("dve", "reciprocal", rd[:, 0:w], ps_d[:, 0:w], reads=[ps_d], writes=[rd])
                P.op("dve", "tensor_tensor", out=oT[mh][:, h0:h0 + w], in0=ps_o[:, 0:w], in1=rd[:, 0:w], op=ALU.mult,
                     reads=[ps_o, rd], writes=[oT[mh]])
        lin_fm(P, pp, oT, wo, D, w_ring, T, cons_add, nkc=4)
        rmsnorm_fm(P, pp, xT, g_ff, hT, ones_bf, sq_ring, rstd, NKC, T, D)
        ffn_fm(P, pp, xT, hT, wgu, wdn, aT, wg_ring, wu_ring, wd_ring, sg_ring, T)
        xov = xo.rearrange("(c p) t -> c p t", p=128)
        outs = [P.dma("sp", xov[c], xT[c][:, :], reads=[xT[c]]) for c in range(NKC)]
        P.emit(final_wait_ops=outs)
    return nc


U32 = mybir.dt.uint32


def _xbc_row0(fc):
    if fc < 24:
        j, loc = fc // 6, fc % 6
    elif fc < 32:
        j, loc = (fc - 24) // 2, 6 + (fc - 24) % 2
    else:
        j, loc = (fc - 32) // 2, 8 + (fc - 32) % 2
    return j * 1280 + loc * 128


def build_fused(stop=None, skip_cc=()):
    T = NTOK
    NKC = D // 128
    nc = bass.Bass("TRN2", target_bir_lowering=False)
    di = lambda n, sh, dt=F32: nc.dram_tensor(n, sh, dt, kind="ExternalInput").ap()
    x_in = di("xT", [D, T]); mem_i = di("memT", [D, MEMLEN])
    W = {n: di(n, sh) for n, sh in (("w_gu1", [2, D, 2 * DFF]), ("w_down1", [2, DFF, D]), ("w_in", [2, D, N_IN]),
                                   ("w_out", [2, 4096, D]), ("wq", [2, D, 512]), ("wk", [2, D, 512]), ("wv", [2, D, 512]),
                                   ("wo", [2, 512, D]), ("w_gu2", [2, D, 2 * DFF]), ("w_down2", [2, DFF, D]))}
    G = {n: di(n, [2, 128, 16]) for n in ("ffg1", "mixg", "xmg", "mmg", "ffg2")}
    G.update({n: di(n, [2, 128, 1]) for n in ("qg", "kg", "mqg", "mkg")})
    S = {n: di(n, sh) for n, sh in (("convw", [2, 128, 10, 4]), ("convb", [2, 128, 10]), ("dtb", [2, 128, 384]),
                                   ("alog", [2, 128, 384]), ("dsk", [2, 128, 768]), ("normw", [2, 128, 768]),
                                   ("tri", [128, 128]), ("strict", [128, 128]), ("onesf", [128, 128]), ("ident", [128, 128]),
                                   ("Tna", [128, 8 * 384]), ("Tca", [128, 8 * 384]), ("abias", [128, 512]),
                                   ("gbias", [128, 64]), ("pastmask", [128, 64]), ("onehot", [16, 16 * 128]))}
    IDX = {n: di(n, [128, 1], U32) for n in ("idx_x", "idx_t", "idx_d", "idx_y")}
    out_ap = nc.dram_tensor("xoT", [D, T], F32, kind="ExternalOutput").ap()
    dr = lambda n, sh: Buf(nc.dram_tensor(n, sh, F32).ap(), n)
    xres = dr("xres", [D, T]); xres1 = dr("xres1", [D, T]); q_s = dr("q_s", [1024, T]); ya_s = dr("ya_s", [1024, T])
    kv_send = dr("kv_send", [2048, T]); kv_g = dr("kv_g", [4 * 2048, T])
    x_send = dr("x_send", [5120, T]); x_g = dr("x_g", [4 * 5120, T])
    z_send = dr("z_send", [4 * T, 768]); z_g = dr("z_g", [16 * T, 768])
    dt_send = dr("dt_send", [4 * T, 12]); dt_g = dr("dt_g", [16 * T, 12])
    y_send = dr("y_send", [3072, T]); y_g = dr("y_g", [4 * 3072, T])
    RG = [[0, 1, 2, 3], [4, 5, 6, 7]]
    scale = 128.0 ** -0.5

    with contextlib.ExitStack() as stack:
        P = Prog(nc, stack)
        P.begin_phases()

        def gather(out_ap_, out_buf, src, idx_t, row0, reads=()):
            width = src.t.shape[1]
            return P.add("pool", lambda e: e.indirect_dma_start(
                out=out_ap_, out_offset=None, in_=src.t, in_offset=bass.IndirectOffsetOnAxis(ap=idx_t[:, 0:1], axis=0),
                element_offset=row0 * width), reads=[src, idx_t] + list(reads), writes=[out_buf], is_dma=True)

        def finish():
            with P.phase(final=True):
                tb_ = [P.sbuf([128, T], F32, "fin%d" % i) for i in range(2)]
                sv = xres1.t.rearrange("(c p) t -> c p t", p=128)
                dv_ = out_ap.rearrange("(c p) t -> c p t", p=128)
                for c in range(NKC):
                    P.dma("sp", tb_[c % 2][:, :], sv[c], reads=[xres1], writes=[tb_[c % 2]])
                    P.dma("sp", dv_[c], tb_[c % 2][:, :], reads=[tb_[c % 2]])

        _coll = P.collective

        def coll(kind, groups, a, b, reads=(), writes=(), tag=None):
            if tag in skip_cc:
                return None
            rows = a.shape[0]
            if tag == "dt":
                return _coll(kind, groups, a, b, reads=reads, writes=writes)
            for k in range(rows // 256):
                _coll(kind, groups, a[k * 256:(k + 1) * 256, :], b[k * 1024:(k + 1) * 1024, :], reads=reads, writes=writes)

        for l in range(2):
            with P.phase():
                pp = PsumPool(P, 8)
                xT = [P.sbuf([128, T], F32, "xT%d" % c) for c in range(NKC)]
                hT = [P.sbuf([128, T], BF16, "hT%d" % c) for c in range(NKC)]
                aT = [P.sbuf([128, T], BF16, "aT%d" % c) for c in range(12)]
                g1 = P.sbuf([128, NKC], F32, "g1"); g2 = P.sbuf([128, NKC], F32, "g2")
                qg = P.sbuf([128, 1], F32, "qg"); kg = P.sbuf([128, 1], F32, "kg")
                ones_bf = P.sbuf([128, 128], BF16, "ones")
                rstd = P.sbuf([128, T], F32, "rstd")
                sq_ring = Ring([P.sbuf([128, 512], BF16, "sq%d" % i) for i in range(3)])
                sg_ring = Ring([P.sbuf([128, 512], F32, "sg%d" % i) for i in range(3)])
                wg_ring = Ring([P.sbuf([128, NKC, 256], BF16, "wg%d" % i) for i in range(2)])
                wu_ring = Ring([P.sbuf([128, NKC, 256], BF16, "wu%d" % i) for i in range(2)])
                wd_ring = Ring([P.sbuf([128, 12, 512], BF16, "wd%d" % i) for i in range(2)])
                w_ring = Ring(wg_ring.bufs + wu_ring.bufs)
                st_ring = Ring([P.sbuf([128, 512], F32, "st%d" % i) for i in range(4)])
                P.op("dve", "memset", ones_bf[:, :], 1.0, writes=[ones_bf])
                P.dma("sp", g1[:, :], G["ffg1"][l], writes=[g1]); P.dma("sp", g2[:, :], G["mixg"][l], writes=[g2])
                P.dma("sp", qg[:, :], G["qg"][l], writes=[qg]); P.dma("sp", kg[:, :], G["kg"][l], writes=[kg])
                src = x_in if l == 0 else xres.t
                xv = src.rearrange("(c p) t -> c p t", p=128)
                for c in range(NKC):
                    P.dma("sp", xT[c][:, :], xv[c], reads=([] if l == 0 else [xres]), writes=[xT[c]])
                rmsnorm_fm(P, pp, xT, g1, hT, ones_bf, sq_ring, rstd, NKC, T, D)
                ffn_fm(P, pp, xT, hT, W["w_gu1"][l], W["w_down1"][l], aT, wg_ring, wu_ring, wd_ring, sg_ring, T)
                x1v = xres1.t.rearrange("(c p) t -> c p t", p=128)
                for c in range(NKC):
                    P.dma("sp", x1v[c], xT[c][:, :], reads=[xT[c]], writes=[xres1])
                rmsnorm_fm(P, pp, xT, g2, hT, ones_bf, sq_ring, rstd, NKC, T, D)
                win = W["w_in"][l]
                for (dstb, rbase, col0, gbuf) in ((q_s, 0, OQ, qg), (kv_send, 0, OK_, kg)):
                    holder = {}

                    def dst_of(fc, h0, w, holder=holder):
                        st = st_ring.get()
                        holder["st"] = st
                        return st[:, 0:w], st
                    cons0 = headnorm_consume(P, pp, ones_bf, gbuf, sq_ring, sg_ring, dst_of)

                    def cons(fc, h0, w, ps, cons0=cons0, holder=holder, dstb=dstb, rbase=rbase):
                        cons0(fc, h0, w, ps)
                        st = holder["st"]
                        P.dma("sp", dstb.t[rbase + fc * 128:rbase + (fc + 1) * 128, h0:h0 + w], st[:, 0:w], reads=[st], writes=[dstb])
                    lin_fm(P, pp, hT, win[:, col0:col0 + 1024], 1024, w_ring, T, cons)

                def cons_xbc(fc, h0, w, ps):
                    st = st_ring.get()
                    P.op("act", "activation", out=st[:, 0:w], in_=ps[:, 0:w], func=AF.Copy, reads=[ps], writes=[st])
                    r0 = _xbc_row0(fc)
                    P.dma("sp", x_send.t[r0:r0 + 128, h0:h0 + w], st[:, 0:w], reads=[st], writes=[x_send])
                lin_fm(P, pp, hT, win[:, OXBC:OXBC + NXBC], NXBC, w_ring, T, cons_xbc)

                def cons_v(tt, c0, cw, ps):
                    st = st_ring.get()
                    P.op("dve", "tensor_copy", st[:, 0:cw], ps[:, 0:cw], reads=[ps], writes=[st])
                    P.dma("sp", kv_send.t[1024 + tt * 128:1024 + (tt + 1) * 128, c0:c0 + cw], st[:, 0:cw], reads=[st], writes=[kv_send])
                lin_tm(P, pp, hT, win[:, OV:OV + NV], NV, w_ring, T, cons_v)

                def cons_z(tt, c0, cw, ps):
                    st = st_ring.get()
                    P.op("dve", "tensor_copy", st[:, 0:cw], ps[:, 0:cw], reads=[ps], writes=[st])
                    j, lc0 = c0 // 768, c0 % 768
                    P.dma("sp", z_send.t[j * T + tt * 128:j * T + (tt + 1) * 128, lc0:lc0 + cw], st[:, 0:cw], reads=[st], writes=[z_send])
                lin_tm(P, pp, hT, win[:, OZ:OZ + NZ], NZ, w_ring, T, cons_z)

                def cons_dt(tt, c0, cw, ps):
                    st = st_ring.get()
                    P.op("dve", "tensor_copy", st[:, 0:cw], ps[:, 0:cw], reads=[ps], writes=[st])
                    for j in range(4):
                        P.dma("sp", dt_send.t[j * T + tt * 128:j * T + (tt + 1) * 128, :], st[:, 12 * j:12 * j + 12], reads=[st], writes=[dt_send])
                lin_tm(P, pp, hT, win[:, ODT:ODT + NDT], NDT, w_ring, T, cons_dt)
                coll("AllGather", RG, kv_send.t, kv_g.t, reads=[kv_send], writes=[kv_g], tag="kv")
                coll("AllGather", RG, x_send.t, x_g.t, reads=[x_send], writes=[x_g], tag="x")
                coll("AllGather", RG, z_send.t, z_g.t, reads=[z_send], writes=[z_g], tag="z")
                coll("AllGather", RG, dt_send.t, dt_g.t, reads=[dt_send], writes=[dt_g], tag="dt")

            if stop == (l, "A"):
                finish()
                return nc
            with P.phase():
                pp = PsumPool(P, 4)
                pacc = PsumPool(P, 4, pfx="pacc")
                tna = P.sbuf([128, 8, 384], F32, "tna"); tca = P.sbuf([128, 8, 384], F32, "tca")
                abias = P.sbuf([128, 512], F32, "abias"); gbias = P.sbuf([128, 64], F32, "gbias")
                pmask = P.sbuf([128, 64], F32, "pmask"); onehot = P.sbuf([16, 16, 128], BF16, "onehot")
                ident = P.sbuf([128, 128], F32, "ident"); ones_bf = P.sbuf([128, 128], BF16, "ones")
                P.dma("sp", tna[:], S["Tna"].rearrange("p (h u) -> p h u", h=8), writes=[tna])
                P.dma("sp", tca[:], S["Tca"].rearrange("p (h u) -> p h u", h=8), writes=[tca])
                for (b, a) in ((abias, S["abias"]), (gbias, S["gbias"]), (pmask, S["pastmask"]), (ident, S["ident"])):
                    P.dma("sp", b[:], a, writes=[b])
                ohs = P.sbuf([16, 16, 128], F32, "ohs")
                P.dma("sp", ohs[:], S["onehot"].rearrange("k (n m) -> k n m", n=16), writes=[ohs])
                P.op("dve", "tensor_copy", onehot[:], ohs[:], reads=[ohs], writes=[onehot])
                P.op("dve", "memset", ones_bf[:, :], 1.0, writes=[ones_bf])
                kf_ring = Ring([P.sbuf([128, SEQ], F32, "kf%d" % i) for i in range(2)])
                kb_ring = Ring([P.sbuf([128, SEQ], BF16, "kb%d" % i) for i in range(2)])
                qf_ring = Ring([P.sbuf([128, NTOK], F32, "qf%d" % i) for i in range(2)])
                qb_ring = Ring([P.sbuf([128, NTOK], BF16, "qb%d" % i) for i in range(2)])
                ko_ring = Ring([P.sbuf([128, NTOK], BF16, "ko%d" % i) for i in range(2)])
                vb_ring = Ring([P.sbuf([128, 32, 128], BF16, "vb%d" % i) for i in range(2)])
                vo_ring = Ring([P.sbuf([128, 8, 128], BF16, "vo%d" % i) for i in range(2)])
                kos_ring = Ring([P.sbuf([128, NTOK], F32, "kos%d" % i) for i in range(2)])
                vbs_ring = Ring([P.sbuf([128, 32, 128], F32, "vbs%d" % i) for i in range(2)])
                vos_ring = Ring([P.sbuf([128, 8, 128], F32, "vos%d" % i) for i in range(2)])
                km_ring = Ring([P.sbuf([128, 16], F32, "km%d" % i) for i in range(2)])
                gm_ring = Ring([P.sbuf([128, 16], F32, "gm%d" % i) for i in range(2)])
                t8_ring = Ring([P.sbuf([128, 8], F32, "t8%d" % i) for i in range(2)])
                sel_ring = Ring([P.sbuf([128, 16], F32, "sel%d" % i) for i in range(2)])
                ns_ring = Ring([P.sbuf([16, 256], BF16, "ns%d" % i) for i in range(2)])
                lg_ring = Ring([P.sbuf([128, 256], F32, "lg%d" % i) for i in range(4)])
                pT_ring = Ring([P.sbuf([128, 256], BF16, "pT%d" % i) for i in range(8)])
                rd_ring = Ring([P.sbuf([128, 256], F32, "rd%d" % i) for i in range(2)])
                o_ring = Ring([P.sbuf([128, 256], F32, "oo%d" % i) for i in range(2)])
                for h in range(8):
                    hr = slice(h * 128, (h + 1) * 128)
                    kf = kf_ring.get(); kb = kb_ring.get(); qf = qf_ring.get(); qb = qb_ring.get()
                    ko = ko_ring.get(); vb = vb_ring.get(); vo = vo_ring.get(); km = km_ring.get()
                    kos = kos_ring.get(); vbs = vbs_ring.get(); vos = vos_ring.get()
                    for sgm in range(4):
                        kr0 = (h // 2) * 1024 + sgm * 256 + (h % 2) * 128
                        P.dma("sp", kf[:, sgm * T:(sgm + 1) * T], kv_g.t[kr0:kr0 + 128, :], reads=[kv_g], writes=[kf])
                        for a_ in range(4):
                            vr0 = (4 + a_) * 1024 + sgm * 256
                            P.dma("sp", vbs[:, sgm * 8 + 2 * a_:sgm * 8 + 2 * a_ + 2, :],
                                  kv_g.t[vr0:vr0 + 256, hr].rearrange("(t p) d -> p t d", p=128), reads=[kv_g], writes=[vbs])
                    P.dma("sp", qf[:, :], q_s.t[hr, :], reads=[q_s], writes=[qf])
                    P.dma("sp", kos[:, :], kv_send.t[hr, :], reads=[kv_send], writes=[kos])
                    P.dma("sp", vos[:], kv_send.t[1024:2048, hr].rearrange("(t p) d -> p t d", p=128), reads=[kv_send], writes=[vos])
                    P.op("act", "activation", out=ko[:, :], in_=kos[:, :], func=AF.Copy, reads=[kos], writes=[ko])
                    P.op("dve", "tensor_copy", vb[:], vbs[:], reads=[vbs], writes=[vb])
                    P.op("dve", "tensor_copy", vo[:], vos[:], reads=[vos], writes=[vo])
                    P.op("act", "activation", out=kb[:, :], in_=kf[:, :], func=AF.Copy, reads=[kf], writes=[kb])
                    P.op("act", "activation", out=qb[:, :], in_=qf[:, :], func=AF.Copy, reads=[qf], writes=[qb])
                    P.op("dve", "tensor_reduce", out=km[:, :], in_=kf[:, :].rearrange("p (n s) -> p n s", s=256), axis=AX.X,
                         op=ALU.add, reads=[kf], writes=[km])
                    P.op("dve", "tensor_scalar", km[:, :], km[:, :], 1.0 / 256, None, ALU.mult, reads=[km], writes=[km])
                    for qi in range(4):
                        qs_ = slice(qi * 256, (qi + 1) * 256)
                        ns = ns_ring.get()
                        for qs in range(2):
                            ps_g = pp.get()
                            P.op("pe", "matmul", ps_g[:, 0:16], qf[:, qi * 256 + qs * 128:qi * 256 + (qs + 1) * 128], km[:, :],
                                 start=True, stop=True, reads=[qf, km], writes=[ps_g])
                            gm = gm_ring.get(); t8 = t8_ring.get(); sel = sel_ring.get()
                            P.op("dve", "tensor_tensor", out=gm[:, :], in0=ps_g[:, 0:16], in1=gbias[:, qi * 16:(qi + 1) * 16],
                                 op=ALU.add, reads=[ps_g, gbias], writes=[gm])
                            P.op("dve", "max", out=t8[:, :], in_=gm[:, :], reads=[gm], writes=[t8])
                            P.op("dve", "tensor_scalar", sel[:, :], gm[:, :], t8[:, 2:3], None, ALU.is_ge, reads=[gm, t8], writes=[sel])
                            P.op("dve", "tensor_tensor", out=sel[:, :], in0=sel[:, :], in1=pmask[:, qi * 16:(qi + 1) * 16],
                                 op=ALU.mult, reads=[sel, pmask], writes=[sel])
                            P.op("dve", "tensor_scalar", sel[:, :], sel[:, :], -1.0, -BIGNEG, ALU.add, ALU.mult, reads=[sel], writes=[sel])
                            ps_t = pp.get()
                            P.op("pe", "transpose", ps_t[0:16, 0:128], sel[:, :], ident[:, :], reads=[sel, ident], writes=[ps_t])
                            P.op("act", "activation", out=ns[:, qs * 128:(qs + 1) * 128], in_=ps_t[0:16, 0:128], func=AF.Copy,
                                 reads=[ps_t], writes=[ns])
                        ps_o = pacc.get(); ps_d = pacc.get()
                        tiles = [(n, kt) for n in range(NBLK) for kt in range(2)] + [(-1, 0), (-1, 1)]
                        LA = 3
                        pend = {}

                        def stage1(ti):
                            n, kt = tiles[ti]
                            ps_s = pp.get()
                            lg = lg_ring.get(); pT = pT_ring.get()
                            tsl = slice(128, 384) if kt == 0 else slice(0, 256)
                            if n >= 0:
                                P.op("pe", "matmul", ps_s[:, 0:256], kb[:, n * 256 + kt * 128:n * 256 + (kt + 1) * 128], qb[:, qs_],
                                     start=True, stop=False, reads=[kb, qb], writes=[ps_s])
                                P.op("pe", "matmul", ps_s[:, 0:256], onehot[:, n, :], ns[:, :], start=False, stop=True,
                                     reads=[onehot, ns], writes=[ps_s])
                                P.op("dve", "scalar_tensor_tensor", out=lg[:, :], in0=ps_s[:, 0:256], scalar=scale, in1=tna[:, h, tsl],
                                     op0=ALU.mult, op1=ALU.add, reads=[ps_s, tna], writes=[lg])
                                bi = (h * 4 + qi) * 16 + n
                                P.op("act", "activation", out=pT[:, :], in_=lg[:, :], func=AF.Exp, bias=abias[:, bi:bi + 1],
                                     reads=[lg, abias], writes=[pT])
                                pend[ti] = (vb[:, n * 2 + kt, :], vb, pT)
                            else:
                                P.op("pe", "matmul", ps_s[:, 0:256], ko[:, qi * 256 + kt * 128:qi * 256 + (kt + 1) * 128], qb[:, qs_],
                                     start=True, stop=True, reads=[ko, qb], writes=[ps_s])
                                P.op("dve", "scalar_tensor_tensor", out=lg[:, :], in0=ps_s[:, 0:256], scalar=scale, in1=tca[:, h, tsl],
                                     op0=ALU.mult, op1=ALU.add, reads=[ps_s, tca], writes=[lg])
                                P.op("act", "activation", out=pT[:, :], in_=lg[:, :], func=AF.Exp, reads=[lg], writes=[pT])
                                pend[ti] = (vo[:, qi * 2 + kt, :], vo, pT)

                        def stage2(ti):
                            vl, vbuf, pT = pend.pop(ti)
                            first, last = (ti == 0), (ti == len(tiles) - 1)
                            P.op("pe", "matmul", ps_o[:, 0:256], vl, pT[:, :], start=first, stop=last, reads=[vbuf, pT], writes=[ps_o])
                            P.op("pe", "matmul", ps_d[:, 0:256], ones_bf[:, :], pT[:, :], start=first, stop=last,
                                 reads=[ones_bf, pT], writes=[ps_d])
                        for ti in range(len(tiles) + LA):
                            if ti < len(tiles):
                                stage1(ti)
                            if ti - LA >= 0:
                                stage2(ti - LA)
                        rd = rd_ring.get(); ot = o_ring.get()
                        P.op("dve", "reciprocal", rd[:, :], ps_d[:, 0:256], reads=[ps_d], writes=[rd])
                        P.op("dve", "tensor_tensor", out=ot[:, :], in0=ps_o[:, 0:256], in1=rd[:, :], op=ALU.mult,
                             reads=[ps_o, rd], writes=[ot])
                        P.dma("sp", ya_s.t[hr, qs_], ot[:, :], reads=[ot], writes=[ya_s])

            if stop == (l, "C1"):
                finish()
                return nc
            with P.phase():
                pp = PsumPool(P, 8)
                cw = P.sbuf([128, 10, 4], F32, "cw"); cb = P.sbuf([128, 10], F32, "cb")
                dt_all = P.sbuf([128, 384], F32, "dt_all"); da_all = P.sbuf([128, 384], F32, "da_all")
                tmpa = P.sbuf([128, 384], F32, "tmpa"); tmpb = P.sbuf([128, 384], F32, "tmpb")
                dsk = P.sbuf([128, 12, 64], F32, "dsk"); normw = P.sbuf([128, 768], F32, "normw")
                tri = P.sbuf([128, 128], F32, "tri"); strict = P.sbuf([128, 128], F32, "strict")
                onesf = P.sbuf([128, 128], F32, "onesf"); ident = P.sbuf([128, 128], F32, "ident")
                idx_x = P.sbuf([128, 1], U32, "idx_x"); idx_t = P.sbuf([128, 1], U32, "idx_t"); idx_d = P.sbuf([128, 1], U32, "idx_d")
                for (b, a) in ((cw, S["convw"][l]), (cb, S["convb"][l]), (tmpa, S["dtb"][l]), (tmpb, S["alog"][l]),
                               (normw, S["normw"][l]), (tri, S["tri"]), (strict, S["strict"]), (onesf, S["onesf"]),
                               (ident, S["ident"]), (idx_x, IDX["idx_x"]), (idx_t, IDX["idx_t"]), (idx_d, IDX["idx_d"])):
                    P.dma("sp", b[:], a, writes=[b])
                P.dma("sp", dsk[:], S["dsk"][l].rearrange("p (j d) -> p j d", d=64), writes=[dsk])
                for c in range(NCH):
                    sgm, lc = c // 8, c % 8
                    gather(dt_all[:, c * 12:(c + 1) * 12], dt_all, dt_g, idx_d, sgm * 4 * T + lc * 128)
                P.op("dve", "tensor_tensor", out=dt_all[:], in0=dt_all[:], in1=tmpa[:], op=ALU.add, reads=[dt_all, tmpa], writes=[dt_all])
                P.op("act", "activation", out=dt_all[:], in_=dt_all[:], func=AF.Exp, reads=[dt_all], writes=[dt_all])
                P.op("dve", "tensor_scalar", dt_all[:], dt_all[:], 1.0, None, ALU.add, reads=[dt_all], writes=[dt_all])
                P.op("act", "activation", out=dt_all[:], in_=dt_all[:], func=AF.Ln, reads=[dt_all], writes=[dt_all])
                P.op("act", "activation", out=tmpb[:], in_=tmpb[:], func=AF.Exp, reads=[tmpb], writes=[tmpb])
                P.op("dve", "scalar_tensor_tensor", out=da_all[:], in0=dt_all[:], scalar=-1.0, in1=tmpb[:], op0=ALU.mult,
                     op1=ALU.mult, reads=[dt_all, tmpb], writes=[da_all])
                h = [P.sbuf([128, 6, 64], F32, "h%d" % g) for g in range(2)]
                hb = [P.sbuf([128, 6, 64], BF16, "hb%d" % g) for g in range(2)]
                for g in range(2):
                    P.op("pool", "memset", h[g][:], 0.0, writes=[h[g]])
                    P.op("pool", "memset", hb[g][:], 0.0, writes=[hb[g]])
                halo = [P.sbuf([128, 4], F32, "halo%d" % i) for i in range(10)]
                xr_ring = Ring([P.sbuf([128, T + 3], F32, "xr%d" % i) for i in range(3)])
                acc_ring = Ring([P.sbuf([128, 512], F32, "acc%d" % i) for i in range(2)])
                xc_sets = [[P.sbuf([128, T], F32, "xc%d_%d" % (k, i)) for i in range(8)] for k in range(2)]
                bc_sets = [[P.sbuf([128, T], BF16, "bc%d_%d" % (k, i)) for i in range(4)] for k in range(2)]
                xt_ring = Ring([P.sbuf([128, 12, 64], F32, "xt%d" % i) for i in range(2)])
                bt_ring = Ring([P.sbuf([128, 256], BF16, "bt%d" % i) for i in range(2)])
                E_ring = Ring([P.sbuf([128, 36], F32, "E%d" % i) for i in range(2)])
                s2_ring = Ring([P.sbuf([128, 12], F32, "s2%d" % i) for i in range(2)])
                xdt_ring = Ring([P.sbuf([128, 12, 64], BF16, "xdt%d" % i) for i in range(2)])
                xw_ring = Ring([P.sbuf([128, 12, 64], BF16, "xw%d" % i) for i in range(2)])
                xd_ring = Ring([P.sbuf([128, 12, 64], F32, "xd%d" % i) for i in range(2)])
                cbm_ring = Ring([P.sbuf([128, 128], F32, "cbm%d" % i) for i in range(2)])
                ajall_ring = Ring([P.sbuf([128, 12, 128], F32, "ajall%d" % i) for i in range(2)])
                scall_ring = Ring([P.sbuf([128, 12, 128], BF16, "scall%d" % i) for i in range(2)])
                dec_ring = Ring([P.sbuf([128, 512], F32, "dec%d" % i) for i in range(3)])
                y_ring = Ring([P.sbuf([128, 12, 64], F32, "y%d" % i) for i in range(2)])
                t1_ring = Ring([P.sbuf([128, 6, 64], F32, "t1%d" % i) for i in range(2)])
                z_ring = Ring([P.sbuf([128, 768], F32, "z%d" % i) for i in range(2)])
                sq_ring = Ring([P.sbuf([128, 768], F32, "sqq%d" % i) for i in range(2)])
                ss_ring = Ring([P.sbuf([128, 2], F32, "ss%d" % i) for i in range(2)])
                o_ring = Ring([P.sbuf([128, 768], F32, "o%d" % i) for i in range(2)])
                yT_ring = Ring([P.sbuf([128, 6, 128], F32, "yTt%d" % i) for i in range(2)])
                ysv = y_send.t.rearrange("(s f p) t -> s p f t", s=4, p=128)
                y_seg = [Buf(None, "y_seg%d" % i) for i in range(4)]
                for sgm in range(4):
                    xc = xc_sets[sgm % 2]
                    bc = bc_sets[sgm % 2]
                    for ch in range(10):
                        xr = xr_ring.get()
                        gather(xr[:, 3:T + 3], xr, x_g, idx_x, (ch // 2) * 1024 + (ch % 2) * 128 + sgm * 256)
                        if sgm == 0:
                            P.op("pool", "memset", xr[:, 0:3], 0.0, writes=[xr])
                        else:
                            P.op("act", "activation", out=xr[:, 0:3], in_=halo[ch][:, 0:3], func=AF.Copy, reads=[halo[ch]], writes=[xr])
                        P.op("act", "activation", out=halo[ch][:, 0:3], in_=xr[:, T:T + 3], func=AF.Copy, reads=[xr], writes=[halo[ch]])
                        for h0 in (0, 512):
                            acc = acc_ring.get()
                            P.op("dve", "tensor_scalar", acc[:, :], xr[:, h0:h0 + 512], cw[:, ch, 0:1], cb[:, ch:ch + 1], ALU.mult, ALU.add,
                                 reads=[xr, cw, cb], writes=[acc])
                            for k in range(1, 4):
                                P.op("dve", "scalar_tensor_tensor", out=acc[:, :], in0=xr[:, h0 + k:h0 + k + 512], scalar=cw[:, ch, k:k + 1],
                                     in1=acc[:, :], op0=ALU.mult, op1=ALU.add, reads=[xr, cw, acc], writes=[acc])
                            if ch < 8:
                                P.op("act", "activation", out=xc[ch][:, h0:h0 + 512], in_=acc[:, :], func=AF.Silu, reads=[acc], writes=[xc[ch]])
                                if ch >= 6:
                                    P.op("dve", "tensor_copy", bc[ch - 6][:, h0:h0 + 512], xc[ch][:, h0:h0 + 512], reads=[xc[ch]], writes=[bc[ch - 6]])
                            else:
                                P.op("act", "activation", out=bc[ch - 6][:, h0:h0 + 512], in_=acc[:, :], func=AF.Silu, reads=[acc], writes=[bc[ch - 6]])
                    for lc in range(8):
                        c = sgm * 8 + lc
                        ts = slice(lc * 128, (lc + 1) * 128)
                        xt = xt_ring.get(); bt = bt_ring.get()
                        xtf = xt[:].rearrange("p j d -> p (j d)")
                        for (lo, n) in ((0, 4), (4, 2)):
                            ps = pp.get()
                            for i in range(n):
                                P.op("pe", "transpose", ps[:, i * 128:(i + 1) * 128], xc[lo + i][:, ts], ident[:, :],
                                     reads=[xc[lo + i], ident], writes=[ps])
                            P.op("dve", "tensor_copy", xtf[:, lo * 128:(lo + n) * 128], ps[:, 0:n * 128], reads=[ps], writes=[xt])
                        ps = pp.get()
                        for i in range(2):
                            P.op("pe", "transpose", ps[:, i * 128:(i + 1) * 128], xc[6 + i][:, ts], ident[:, :],
                                 reads=[xc[6 + i], ident], writes=[ps])
                        P.op("act", "activation", out=bt[:, :], in_=ps[:, 0:256], func=AF.Copy, reads=[ps], writes=[bt])
                        dac = da_all[:, c * 12:(c + 1) * 12]
                        dtc = dt_all[:, c * 12:(c + 1) * 12]
                        ps = pp.get()
                        P.op("pe", "matmul", ps[:, 0:12], tri[:, :], dac, start=True, stop=True, reads=[tri, da_all], writes=[ps])
                        P.op("pe", "matmul", ps[:, 12:24], strict[:, :], dac, start=True, stop=True, reads=[strict, da_all], writes=[ps])
                        P.op("pe", "matmul", ps[:, 24:36], onesf[:, :], dac, start=True, stop=True, reads=[onesf, da_all], writes=[ps])
                        E = E_ring.get()
                        P.op("act", "activation", out=E[:, :], in_=ps[:, 0:36], func=AF.Exp, reads=[ps], writes=[E])
                        s2 = s2_ring.get()
                        P.op("dve", "tensor_tensor", out=s2[:, :], in0=dtc, in1=E[:, 12:24], op=ALU.mult, reads=[dt_all, E], writes=[s2])
                        xdt = xdt_ring.get(); xw = xw_ring.get(); xd = xd_ring.get()
                        P.op("dve", "tensor_tensor", out=xdt[:], in0=xt[:], in1=dtc.unsqueeze(2).broadcast_to([128, 12, 64]),
                             op=ALU.mult, reads=[xt, dt_all], writes=[xdt])
                        P.op("dve", "tensor_tensor", out=xw[:], in0=xt[:], in1=s2[:, :].unsqueeze(2).broadcast_to([128, 12, 64]),
                             op=ALU.mult, reads=[xt, s2], writes=[xw])
                        P.op("dve", "tensor_tensor", out=xd[:], in0=xt[:], in1=dsk[:], op=ALU.mult, reads=[xt, dsk], writes=[xd])
                        y = y_ring.get()
                        ajall = ajall_ring.get()
                        P.op("dve", "tensor_tensor", out=ajall[:], in0=strict[:, :].unsqueeze(1).broadcast_to([128, 12, 128]),
                             in1=dac.unsqueeze(2).broadcast_to([128, 12, 128]), op=ALU.mult, reads=[strict, da_all], writes=[ajall])
                        cbms = []; yos = []
                        for gi in range(2):
                            BT = bc[gi]; CT = bc[2 + gi]
                            ps_cb = pp.get()
                            P.op("pe", "matmul", ps_cb[:, 0:128], BT[:, ts], CT[:, ts], start=True, stop=True, reads=[BT, CT], writes=[ps_cb])
                            cbm = cbm_ring.get()
                            P.op("dve", "tensor_tensor", out=cbm[:, :], in0=ps_cb[:, 0:128], in1=tri[:, :], op=ALU.mult,
                                 reads=[ps_cb, tri], writes=[cbm])
                            ps_yo = pp.get()
                            P.op("pe", "matmul", ps_yo[:, 0:384], CT[:, ts], hb[gi][:].rearrange("p j d -> p (j d)"),
                                 start=True, stop=True, reads=[CT, hb[gi]], writes=[ps_yo])
                            cbms.append(cbm); yos.append(ps_yo)
                        scall = scall_ring.get()
                        for gi in range(2):
                            for (j0, nh) in ((0, 4), (4, 2)):
                                ps_seg = pp.get()
                                for i in range(nh):
                                    j = gi * 6 + j0 + i
                                    P.op("pe", "matmul", ps_seg[:, i * 128:(i + 1) * 128], ajall[:, j, :], tri[:, :], start=True, stop=True,
                                         reads=[ajall, tri], writes=[ps_seg])
                                dec = dec_ring.get()
                                P.op("act", "activation", out=dec[:, 0:nh * 128], in_=ps_seg[:, 0:nh * 128], func=AF.Exp, reads=[ps_seg], writes=[dec])
                                P.op("dve", "tensor_tensor", out=scall[:, gi * 6 + j0:gi * 6 + j0 + nh, :],
                                     in0=dec[:, 0:nh * 128].rearrange("p (j s) -> p j s", s=128),
                                     in1=cbms[gi][:, :].unsqueeze(1).broadcast_to([128, nh, 128]), op=ALU.mult,
                                     reads=[dec, cbms[gi]], writes=[scall])
                        for gi in range(2):
                            ps_yd = pp.get()
                            ps_yo = yos[gi]
                            for jj in range(6):
                                j = gi * 6 + jj
                                P.op("pe", "matmul", ps_yd[:, jj * 64:(jj + 1) * 64], scall[:, j, :], xdt[:, j, :], start=True, stop=True,
                                     reads=[scall, xdt], writes=[ps_yd])
                            t1 = t1_ring.get()
                            P.op("dve", "tensor_tensor", out=t1[:], in0=ps_yo[:, 0:384].rearrange("p (j d) -> p j d", d=64),
                                 in1=E[:, gi * 6:gi * 6 + 6].unsqueeze(2).broadcast_to([128, 6, 64]), op=ALU.mult,
                                 reads=[ps_yo, E], writes=[t1])
                            P.op("dve", "tensor_tensor", out=t1[:], in0=ps_yd[:, 0:384].rearrange("p (j d) -> p j d", d=64),
                                 in1=t1[:], op=ALU.add, reads=[ps_yd, t1], writes=[t1])
                            P.op("dve", "tensor_tensor", out=y[:, gi * 6:(gi + 1) * 6, :], in0=xd[:, gi * 6:(gi + 1) * 6, :], in1=t1[:],
                                 op=ALU.add, reads=[xd, t1], writes=[y])
                            ps_st = pp.get()
                            P.op("pe", "matmul", ps_st[:, 0:384], bt[:, gi * 128:(gi + 1) * 128],
                                 xw[:, gi * 6:(gi + 1) * 6, :].rearrange("p j d -> p (j d)"), start=True, stop=True,
                                 reads=[bt, xw], writes=[ps_st])
                            P.op("dve", "tensor_tensor", out=h[gi][:], in0=h[gi][:],
                                 in1=E[:, 24 + gi * 6:24 + gi * 6 + 6].unsqueeze(2).broadcast_to([128, 6, 64]), op=ALU.mult,
                                 reads=[h[gi], E], writes=[h[gi]])
                            P.op("dve", "tensor_tensor", out=h[gi][:], in0=ps_st[:, 0:384].rearrange("p (j d) -> p j d", d=64),
                                 in1=h[gi][:], op=ALU.add, reads=[ps_st, h[gi]], writes=[h[gi]])
                            P.op("act", "activation", out=hb[gi][:], in_=h[gi][:], func=AF.Copy, reads=[h[gi]], writes=[hb[gi]])
                        zt = z_ring.get()
                        gather(zt[:, :], zt, z_g, idx_t, (lc // 2) * 1024 + sgm * 256 + (lc % 2) * 128)
                        P.op("act", "activation", out=zt[:, :], in_=zt[:, :], func=AF.Silu, reads=[zt], writes=[zt])
                        yf = y[:].rearrange("p j d -> p (j d)")
                        P.op("dve", "tensor_tensor", out=yf, in0=yf, in1=zt[:, :], op=ALU.mult, reads=[y, zt], writes=[y])
                        sq = sq_ring.get()
                        P.op("act", "activation", out=sq[:, :], in_=yf, func=AF.Square, reads=[y], writes=[sq])
                        ss = ss_ring.get()
                        P.op("dve", "tensor_reduce", out=ss[:, :], in_=sq[:, :].rearrange("p (g f) -> p g f", g=2), axis=AX.X, op=ALU.add,
                             reads=[sq], writes=[ss])
                        P.op("dve", "tensor_scalar", ss[:, :], ss[:, :], 1.0 / 384, EPS, ALU.mult, ALU.add, reads=[ss], writes=[ss])
                        P.op("act", "activation", out=ss[:, :], in_=ss[:, :], func=AF.Sqrt, reads=[ss], writes=[ss])
                        P.op("dve", "reciprocal", ss[:, :], ss[:, :], reads=[ss], writes=[ss])
                        ot = o_ring.get()
                        for gi in range(2):
                            P.op("dve", "scalar_tensor_tensor", out=ot[:, gi * 384:(gi + 1) * 384], in0=yf[:, gi * 384:(gi + 1) * 384],
                                 scalar=ss[:, gi:gi + 1], in1=normw[:, gi * 384:(gi + 1) * 384], op0=ALU.mult, op1=ALU.mult,
                                 reads=[y, ss, normw], writes=[ot])
                        yTt = yT_ring.get()
                        yTf = yTt[:].rearrange("p f t -> p (f t)")
                        for (lo, n) in ((0, 4), (4, 2)):
                            ps = pp.get()
                            for i in range(n):
                                P.op("pe", "transpose", ps[:, i * 128:(i + 1) * 128], ot[:, (lo + i) * 128:(lo + i + 1) * 128], ident[:, :],
                                     reads=[ot, ident], writes=[ps])
                            P.op("act", "activation", out=yTf[:, lo * 128:(lo + n) * 128], in_=ps[:, 0:n * 128], func=AF.Copy,
                                 reads=[ps], writes=[yTt])
                        P.dma("sp", ysv[sgm][:, :, ts], yTt[:], reads=[yTt], writes=[y_seg[sgm]])
                    if "y" not in skip_cc:
                        for k in range(3 * sgm, 3 * sgm + 3):
                            _coll("AllGather", RG, y_send.t[k * 256:(k + 1) * 256, :], y_g.t[k * 1024:(k + 1) * 1024, :],
                                  reads=[y_seg[sgm]], writes=[y_g])

            if stop == (l, "B"):
                finish()
                return nc
            with P.phase(final=(l == 1)):
                pp = PsumPool(P, 8)
                xT = [P.sbuf([128, T], F32, "xT%d" % c) for c in range(NKC)]
                hT = [P.sbuf([128, T], BF16, "hT%d" % c) for c in range(NKC)]
                aT = [P.sbuf([128, T], BF16, "aT%d" % c) for c in range(12)]
                g_xm = P.sbuf([128, NKC], F32, "g_xm"); g_mm = P.sbuf([128, NKC], F32, "g_mm"); g_ff = P.sbuf([128, NKC], F32, "g_ff")
                mqg = P.sbuf([128, 1], F32, "mqg"); mkg = P.sbuf([128, 1], F32, "mkg")
                idx_y = P.sbuf([128, 1], U32, "idx_y")
                ones_bf = P.sbuf([128, 128], BF16, "ones")
                rstd = P.sbuf([128, T], F32, "rstd")
                sq_ring = Ring([P.sbuf([128, 512], BF16, "sq%d" % i) for i in range(3)])
                sg_ring = Ring([P.sbuf([128, 512], F32, "sg%d" % i) for i in range(3)])
                wg_ring = Ring([P.sbuf([128, NKC, 256], BF16, "wg%d" % i) for i in range(2)])
                wu_ring = Ring([P.sbuf([128, NKC, 256], BF16, "wu%d" % i) for i in range(2)])
                wd_ring = Ring([P.sbuf([128, 12, 512], BF16, "wd%d" % i) for i in range(2)])
                w_ring = Ring(wg_ring.bufs + wu_ring.bufs)
                mr_ring = Ring([P.sbuf([128, MEMLEN], F32, "mr%d" % i) for i in range(3)])
                mhT = [P.sbuf([128, MEMLEN], BF16, "mhT%d" % c) for c in range(NKC)]
                kmT = [P.sbuf([128, MEMLEN], BF16, "kmT%d" % c) for c in range(4)]
                vm = [P.sbuf([128, 512], BF16, "vm%d" % c) for c in range(2)]
                pT_ring = sq_ring
                rd_ring = sg_ring
                ystg = Ring([rstd])
                xv = xres1.t.rearrange("(c p) t -> c p t", p=128)
                yav = ya_s.t.rearrange("(c p) t -> c p t", p=128)
                mv = mem_i.rearrange("(c p) t -> c p t", p=128)
                P.op("dve", "memset", ones_bf[:, :], 1.0, writes=[ones_bf])
                for (b, a) in ((g_xm, G["xmg"][l]), (g_mm, G["mmg"][l]), (g_ff, G["ffg2"][l]), (mqg, G["mqg"][l]),
                               (mkg, G["mkg"][l]), (idx_y, IDX["idx_y"])):
                    P.dma("sp", b[:], a, writes=[b])
                for c in range(NKC):
                    P.dma("sp", xT[c][:, :], xv[c], reads=[xres1], writes=[xT[c]])

                def cons_add(fc, h0, w, ps):
                    P.op("dve", "tensor_tensor", out=xT[fc][:, h0:h0 + w], in0=ps[:, 0:w], in1=xT[fc][:, h0:h0 + w], op=ALU.add,
                         reads=[ps, xT[fc]], writes=[xT[fc]])
                for kh in range(2):
                    for c in range(NKC):
                        m = kh * NKC + c
                        if m < 8:
                            P.dma("pool", hT[c][:, :], yav[m], reads=[ya_s], writes=[hT[c]])
                        else:
                            ms = m - 8
                            r, fcx = ms // 6, ms % 6
                            stg = ystg.get()
                            gather(stg[:, :], stg, y_g, idx_y, (fcx // 2) * 1024 + r * 256 + (fcx % 2) * 128)
                            P.op("act", "activation", out=hT[c][:, :], in_=stg[:, :], func=AF.Copy, reads=[stg], writes=[hT[c]])
                    lin_fm(P, pp, hT, W["w_out"][l][kh * D:(kh + 1) * D, :], D, w_ring, T, cons_add)
                ps_m = pp.get()
                for c in range(NKC):
                    mt = mr_ring.get()
                    P.dma("sp", mt[:, :], mv[c], writes=[mt])
                    sq = sq_ring.get()
                    P.op("act", "activation", out=sq[:, 0:MEMLEN], in_=mt[:, :], func=AF.Square, reads=[mt], writes=[sq])
                    P.op("pe", "matmul", ps_m[:, 0:MEMLEN], ones_bf[:, :], sq[:, 0:MEMLEN], start=(c == 0), stop=(c == NKC - 1),
                         reads=[sq, ones_bf], writes=[ps_m])
                P.op("dve", "tensor_scalar", rstd[:, 0:MEMLEN], ps_m[:, 0:MEMLEN], 1.0 / D, EPS, ALU.mult, ALU.add, reads=[ps_m], writes=[rstd])
                P.op("act", "activation", out=rstd[:, 0:MEMLEN], in_=rstd[:, 0:MEMLEN], func=AF.Sqrt, reads=[rstd], writes=[rstd])
                P.op("dve", "reciprocal", rstd[:, 0:MEMLEN], rstd[:, 0:MEMLEN], reads=[rstd], writes=[rstd])
                for c in range(NKC):
                    mt = mr_ring.get()
                    P.dma("sp", mt[:, :], mv[c], writes=[mt])
                    P.op("dve", "scalar_tensor_tensor", out=mhT[c][:, :], in0=mt[:, :], scalar=g_mm[:, c:c + 1], in1=rstd[:, 0:MEMLEN],
                         op0=ALU.mult, op1=ALU.mult, reads=[mt, g_mm, rstd], writes=[mhT[c]])

                def dst_k(fc, h0, w):
                    return kmT[fc][:, h0:h0 + w], kmT[fc]
                lin_fm(P, pp, mhT, W["wk"][l], 512, w_ring, MEMLEN, headnorm_consume(P, pp, ones_bf, mkg, sq_ring, sg_ring, dst_k))

                def cons_vm(tt, c0, cw_, ps):
                    P.op("act", "activation", out=vm[tt][:, c0:c0 + cw_], in_=ps[:, 0:cw_], func=AF.Copy, reads=[ps], writes=[vm[tt]])
                lin_tm(P, pp, mhT, W["wv"][l], 512, w_ring, MEMLEN, cons_vm)
                rmsnorm_fm(P, pp, xT, g_xm, hT, ones_bf, sq_ring, rstd, NKC, T, D)
                qmT = aT[0:4]
                oT = aT[4:8]

                def dst_q(fc, h0, w):
                    return qmT[fc][:, h0:h0 + w], qmT[fc]
                lin_fm(P, pp, hT, W["wq"][l], 512, w_ring, T, headnorm_consume(P, pp, ones_bf, mqg, sq_ring, sg_ring, dst_q))
                for mh in range(4):
                    for h0 in range(0, T, 512):
                        w = min(512, T - h0)
                        ps_o = pp.get(); ps_d = pp.get()
                        for kt in range(2):
                            ps_s = pp.get()
                            P.op("pe", "matmul", ps_s[:, 0:w], kmT[mh][:, kt * 128:(kt + 1) * 128], qmT[mh][:, h0:h0 + w],
                                 start=True, stop=True, reads=[kmT[mh], qmT[mh]], writes=[ps_s])
                            pT = pT_ring.get()
                            P.op("act", "activation", out=pT[:, 0:w], in_=ps_s[:, 0:w], func=AF.Exp, scale=scale, reads=[ps_s], writes=[pT])
                            P.op("pe", "matmul", ps_o[:, 0:w], vm[kt][:, mh * 128:(mh + 1) * 128], pT[:, 0:w], start=(kt == 0),
                                 stop=(kt == 1), reads=[vm[kt], pT], writes=[ps_o])
                            P.op("pe", "matmul", ps_d[:, 0:w], ones_bf[:, :], pT[:, 0:w], start=(kt == 0), stop=(kt == 1),
                                 reads=[ones_bf, pT], writes=[ps_d])
                        rd = rd_ring.get()
                        P.op("dve", "reciprocal", rd[:, 0:w], ps_d[:, 0:w], reads=[ps_d], writes=[rd])
                        P.op("dve", "tensor_tensor", out=oT[mh][:, h0:h0 + w], in0=ps_o[:, 0:w], in1=rd[:, 0:w], op=ALU.mult,
                             reads=[ps_o, rd], writes=[oT[mh]])
                lin_fm(P, pp, oT, W["wo"][l], D, w_ring, T, cons_add, nkc=4)
                rmsnorm_fm(P, pp, xT, g_ff, hT, ones_bf, sq_ring, rstd, NKC, T, D)
                ffn_fm(P, pp, xT, hT, W["w_gu2"][l], W["w_down2"][l], aT, wg_ring, wu_ring, wd_ring, sg_ring, T)
                dst = xres if l == 0 else None
                dv = (xres.t if l == 0 else out_ap).rearrange("(c p) t -> c p t", p=128)
                for c in range(NKC):
                    P.dma("sp", dv[c], xT[c][:, :], reads=[xT[c]], writes=([xres] if l == 0 else []))
    return nc


def fused_inputs(I):
    T = NTOK
    xs = I["x"].astype(np.float32).reshape(NCORES, T, D)
    st2 = lambda k: np.ascontiguousarray(np.stack([_pg(I[k][l]) for l in range(2)]))
    shared = {"w_gu1": I["ff1_w_gu"], "w_down1": I["ff1_w_down"], "w_in": I["w_in"], "w_out": I["w_out"],
              "wq": I["mem_wq"], "wk": I["mem_wk"], "wv": I["mem_wv"], "wo": I["mem_wo"],
              "w_gu2": I["ff2_w_gu"], "w_down2": I["ff2_w_down"],
              "ffg1": st2("ff1_norm"), "mixg": st2("mix_norm"), "xmg": st2("xmem_norm"), "mmg": st2("mem_norm"),
              "ffg2": st2("ff2_norm"), "qg": st2("q_norm"), "kg": st2("k_norm"), "mqg": st2("mem_q_norm"), "mkg": st2("mem_k_norm")}
    shared = {k: np.ascontiguousarray(np.asarray(v, np.float32)) for k, v in shared.items()}
    shared.update(ssd_consts())
    p = np.arange(128, dtype=np.uint32).reshape(128, 1)
    maps = []
    for c in range(NCORES):
        b, seg = c // 4, c % 4
        gp = seg
        g0 = 2 * gp
        chs = np.concatenate([np.arange(g0 * 384, (g0 + 2) * 384), 3072 + np.arange(g0 * 128, (g0 + 2) * 128),
                              4096 + np.arange(g0 * 128, (g0 + 2) * 128)])
        hs = np.arange(g0 * 6, g0 * 6 + 12)
        m = dict(shared)
        m["xT"] = np.ascontiguousarray(xs[c].T)
        m["memT"] = np.ascontiguousarray(I["mem"][b].astype(np.float32).T)
        m["convw"] = np.ascontiguousarray(np.stack([I["conv_w"][l][:, chs].T.reshape(10, 128, 4).transpose(1, 0, 2) for l in range(2)])).astype(np.float32)
        m["convb"] = np.ascontiguousarray(np.stack([I["conv_b"][l][chs].reshape(10, 128).T for l in range(2)])).astype(np.float32)
        m["dtb"] = np.stack([_rep(np.tile(I["dt_bias"][l][hs], NCH)) for l in range(2)])
        m["alog"] = np.stack([_rep(np.tile(I["a_log"][l][hs], NCH)) for l in range(2)])
        m["dsk"] = np.stack([_rep(np.repeat(I["d_skip"][l][hs], 64)) for l in range(2)])
        m["normw"] = np.stack([_rep(I["ssd_norm"][l][g0 * 384:(g0 + 2) * 384]) for l in range(2)])
        ac = attn_consts(seg)
        m.update(ac)
        m["idx_x"] = (gp * 5120 + p).astype(np.uint32)
        m["idx_t"] = (gp * 4096 + p).astype(np.uint32)
        m["idx_d"] = (gp * T + p).astype(np.uint32)
        m["idx_y"] = (seg * 3072 + p).astype(np.uint32)
        maps.append(m)
    return maps


def kernel_fused(**inputs):
    I = {k: np.asarray(v) for k, v in inputs.items()}
    nc = _prog("fused", build_fused)
    res = _run(nc, fused_inputs(I))
    out = np.stack([res[c]["xoT"].T for c in range(NCORES)], axis=0).reshape(2, SEQ, D)
    return np.ascontiguousarray(out.astype(np.float32))


_PROGS = {}


def _prog(name, fn):
    if name not in _PROGS:
        _PROGS[name] = fn()
    return _PROGS[name]


def _pg(g):
    return np.ascontiguousarray(np.asarray(g, np.float32).reshape(-1, 128).T)


def _run(nc, in_maps):
    res = run_bass_kernel_spmd(nc, in_maps, core_ids=list(range(NCORES)))
    return res.results


def kernel_unfused(**inputs):
    I = {k: np.asarray(v) for k, v in inputs.items()}
    T = NTOK
    x = I["x"].astype(np.float32)
    xs = x.reshape(NCORES, T, D)
    xT = [np.ascontiguousarray(xs[c].T) for c in range(NCORES)]
    memT = [np.ascontiguousarray(I["mem"][b].T) for b in range(2)]
    ncA = _prog("A", build_progA)
    ncB = _prog("B", build_progB)
    ncC1 = _prog("C1", build_progC1)
    ncC2 = _prog("C2", build_progC2)
    for l in range(2):
        mapsA = [{"xT": xT[c], "ffg": _pg(I["ff1_norm"][l]), "mixg": _pg(I["mix_norm"][l]),
                  "w_gu": I["ff1_w_gu"][l], "w_down": I["ff1_w_down"][l], "w_in": I["w_in"][l],
                  "qg": _pg(I["q_norm"][l]), "kg": _pg(I["k_norm"][l])} for c in range(NCORES)]
        rA = _run(ncA, mapsA)
        xbcT_b = [np.concatenate([rA[b * 4 + s]["xbcT"] for s in range(4)], axis=1) for b in range(2)]
        dt_b = [np.concatenate([rA[b * 4 + s]["dt"] for s in range(4)], axis=0) for b in range(2)]
        z_b = [np.concatenate([rA[b * 4 + s]["z"] for s in range(4)], axis=0) for b in range(2)]
        Pm = {k: I[k][l] for k in ("conv_w", "conv_b", "dt_bias", "a_log", "d_skip", "ssd_norm")}
        rB = _run(ncB, progB_inputs(xbcT_b, dt_b, z_b, Pm))
        y_ssd = progB_gather([r["y"] for r in rB])
        rC1 = _run(ncC1, progC1_inputs([rA[c]["qT"] for c in range(NCORES)], [rA[c]["kT"] for c in range(NCORES)],
                                       [rA[c]["v"] for c in range(NCORES)]))
        mapsC2 = []
        for c in range(NCORES):
            b, seg = c // 4, c % 4
            yT = np.ascontiguousarray(np.concatenate([rC1[c]["yT"], y_ssd[b, seg * T:(seg + 1) * T].T], axis=0))
            mapsC2.append({"xT": rA[c]["x1T"], "yT": yT, "w_out": I["w_out"][l], "memT": memT[b],
                           "xmg": _pg(I["xmem_norm"][l]), "mmg": _pg(I["mem_norm"][l]), "ffg": _pg(I["ff2_norm"][l]),
                           "mqg": _pg(I["mem_q_norm"][l]), "mkg": _pg(I["mem_k_norm"][l]),
                           "wq": I["mem_wq"][l], "wk": I["mem_wk"][l], "wv": I["mem_wv"][l], "wo": I["mem_wo"][l],
                           "w_gu": I["ff2_w_gu"][l], "w_down": I["ff2_w_down"][l]})
        rC2 = _run(ncC2, mapsC2)
        xT = [rC2[c]["xoT"] for c in range(NCORES)]
    out = np.stack([xT[c].T for c in range(NCORES)], axis=0).reshape(2, SEQ, D)
    return np.ascontiguousarray(out.astype(np.float32))


def kernel(**inputs):
    return kernel_fused(**inputs)
```

```python
import numpy as np
import contextlib
import concourse.bass as bass
import concourse.mybir as mybir
from concourse.bass_utils import run_bass_kernel_spmd

F32 = mybir.dt.float32
BF16 = mybir.dt.bfloat16
AF = mybir.ActivationFunctionType
ALU = mybir.AluOpType
AX = mybir.AxisListType

D = 2048
DFF = 5632
NTOK = 1024
NCORES = 8
EPS = 1e-6


class Buf:
    def __init__(self, t, name=""):
        self.t = t
        self.name = name
        self.last_w = None
        self.readers = []

    def __getitem__(self, idx):
        return self.t[idx]


class Op:
    __slots__ = ("eng", "emit", "deps", "marked", "count", "dma_sem", "dma_val", "is_dma", "is_cc")

    def __init__(self, eng, emit, is_dma=False):
        self.eng = eng
        self.emit = emit
        self.deps = []
        self.marked = False
        self.count = None
        self.is_dma = is_dma
        self.dma_sem = None
        self.dma_val = None
        self.is_cc = False


class Prog:
    ENGS = ("pe", "dve", "act", "pool", "sp")
    NS = 8

    def __init__(self, nc, stack):
        self.nc = nc
        self.stack = stack
        self.ops = {e: [] for e in self.ENGS}
        self.sem = {e: stack.enter_context(nc.semaphore("prog_" + e)) for e in ("pe", "dve", "act", "pool")}
        self.dsem = {q: [stack.enter_context(nc.semaphore("dma_%s_%d" % (q, i))) for i in range(self.NS)]
                     for q in ("sp", "act", "pool")}
        self.ndma = {q: 0 for q in ("sp", "act", "pool")}
        self.nbuf = 0
        self.cc_sem = stack.enter_context(nc.semaphore("cc_sem"))
        self.ncc = 0

    def sbuf(self, shape, dtype, name=None):
        self.nbuf += 1
        name = "s_%s_%d" % (name or "sb", self.nbuf)
        t = self.stack.enter_context(self.nc.sbuf_tensor(name, list(shape), dtype))
        return Buf(t, name)

    def psum(self, shape, dtype=F32, name=None):
        self.nbuf += 1
        name = "%s_%d" % (name or "ps", self.nbuf)
        t = self.stack.enter_context(self.nc.psum_tensor(name, list(shape), dtype))
        return Buf(t, name)

    def add(self, eng, emit, reads=(), writes=(), is_dma=False):
        op = Op(eng, emit, is_dma)
        deps = []
        for b in reads:
            if b.last_w is not None:
                deps.append(b.last_w)
        for b in writes:
            if b.last_w is not None:
                deps.append(b.last_w)
            deps.extend(b.readers)
        seen = set()
        for d in deps:
            if d is op or id(d) in seen:
                continue
            seen.add(id(d))
            if d.eng == "pe" and eng == "pe" and not d.is_dma:
                continue
            op.deps.append(d)
            if not d.is_dma:
                d.marked = True
        for b in reads:
            b.readers.append(op)
        for b in writes:
            b.last_w = op
            b.readers = []
        if is_dma:
            q = eng
            i = self.ndma[q]
            self.ndma[q] += 1
            op.dma_sem = self.dsem[q][i % self.NS]
            op.dma_val = 16 * (i // self.NS + 1)
        self.ops[eng].append(op)
        return op

    def op(self, eng, method, *args, reads=(), writes=(), **kw):
        return self.add(eng, lambda e: getattr(e, method)(*args, **kw), reads, writes)

    def collective(self, kind, groups, src_ap, dst_ap, reads=(), writes=()):
        op = self.add("pool", lambda e: e.collective_compute(kind, ALU.bypass, replica_groups=groups, ins=[src_ap], outs=[dst_ap]),
                      reads, writes, is_dma=True)
        self.ndma["pool"] -= 1
        self.ncc += 1
        op.dma_sem = self.cc_sem
        op.dma_val = self.ncc
        op.is_cc = True
        return op

    def dma(self, q, out, in_, reads=(), writes=()):
        return self.add(q, lambda e: e.dma_start(out=out, in_=in_), reads, writes, is_dma=True)

    def begin_phases(self):
        self.cnt = {e: 0 for e in ("pe", "dve", "act", "pool")}
        self.waited = {e: {} for e in self.ENGS}
        self.barrier = []
        self.phase_id = 0
        self.last_dma = {}

    @contextlib.contextmanager
    def phase(self, final=False):
        outer = self.stack
        with contextlib.ExitStack() as st:
            self.stack = st
            try:
                yield
                self.emit_phase(final)
            finally:
                self.stack = outer

    def emit_phase(self, final=False):
        nc = self.nc
        prog = self
        for e in ("pe", "dve", "act", "pool"):
            for op in reversed(self.ops[e]):
                if not op.is_dma:
                    op.marked = True
                    break
        for e in ("pe", "dve", "act", "pool"):
            for op in self.ops[e]:
                if op.is_dma:
                    continue
                if op.marked:
                    self.cnt[e] += 1
                    op.count = self.cnt[e]
        barrier = list(self.barrier)

        def run(engname, engine):
            waited = prog.waited[engname]

            def wait(s, v):
                if waited.get(id(s), 0) >= v:
                    return
                engine.wait_ge(s, v)
                waited[id(s)] = v
            for (s, v) in barrier:
                wait(s, v)
            for op in prog.ops[engname]:
                if op.is_dma and not op.is_cc:
                    prev = op.dma_val - 16
                    if prev > 0:
                        wait(op.dma_sem, prev)
                for d in op.deps:
                    if d.is_dma:
                        wait(d.dma_sem, d.dma_val)
                    elif d.count is not None:
                        wait(prog.sem[d.eng], d.count)
                ins = op.emit(engine)
                if op.is_cc:
                    ins.then_inc(op.dma_sem, 1)
                elif op.is_dma:
                    ins.then_inc(op.dma_sem, 16)
                elif op.marked:
                    ins.then_inc(prog.sem[engname], 1)
            if final:
                for (s, v) in final_pairs:
                    wait(s, v)

        for e in self.ENGS:
            for op in self.ops[e]:
                if op.is_dma and not op.is_cc:
                    self.last_dma[id(op.dma_sem)] = (op.dma_sem, op.dma_val)
        nb = [(self.sem[e], self.cnt[e]) for e in ("pe", "dve", "act", "pool") if self.cnt[e] > 0]
        nb += list(self.last_dma.values())
        final_pairs = nb
        with nc.Block() as block:
            @block.tensor
            def _(eng):
                run("pe", eng)

            @block.vector
            def _(eng):
                run("dve", eng)

            @block.scalar
            def _(eng):
                run("act", eng)

            @block.gpsimd
            def _(eng):
                run("pool", eng)

            @block.sync
            def _(eng):
                run("sp", eng)
        self.barrier = nb
        self.phase_id += 1
        for e in self.ENGS:
            for op in self.ops[e]:
                op.emit = None
                if not op.is_dma and op.count is None:
                    op.count = -1
            self.ops[e] = []

    def emit(self, final_wait_ops=()):
        nc = self.nc
        for e in ("pe", "dve", "act", "pool"):
            c = 0
            for op in self.ops[e]:
                if op.is_dma:
                    continue
                if op.marked:
                    c += 1
                    op.count = c
        prog = self

        def run(engname, engine):
            waited = {}
            for op in prog.ops[engname]:
                if op.is_dma and not op.is_cc:
                    prev = op.dma_val - 16
                    if prev > 0 and waited.get(id(op.dma_sem), 0) < prev:
                        engine.wait_ge(op.dma_sem, prev)
                        waited[id(op.dma_sem)] = prev
                for d in op.deps:
                    if d.is_dma:
                        s, v = d.dma_sem, d.dma_val
                    else:
                        s, v = prog.sem[d.eng], d.count
                    if waited.get(id(s), 0) >= v:
                        continue
                    engine.wait_ge(s, v)
                    waited[id(s)] = v
                ins = op.emit(engine)
                if op.is_cc:
                    ins.then_inc(op.dma_sem, 1)
                elif op.is_dma:
                    ins.then_inc(op.dma_sem, 16)
                elif op.marked:
                    ins.then_inc(prog.sem[engname], 1)
            if engname == "sp":
                for d in final_wait_ops:
                    s, v = (d.dma_sem, d.dma_val) if d.is_dma else (prog.sem[d.eng], d.count)
                    engine.wait_ge(s, v)

        with nc.Block() as block:
            @block.tensor
            def _(eng):
                run("pe", eng)

            @block.vector
            def _(eng):
                run("dve", eng)

            @block.scalar
            def _(eng):
                run("act", eng)

            @block.gpsimd
            def _(eng):
                run("pool", eng)

            @block.sync
            def _(eng):
                run("sp", eng)


class PsumPool:
    def __init__(self, P, n=8, pfx="psb"):
        self.bufs = [P.psum([128, 512], F32, name="%s%d" % (pfx, i)) for i in range(n)]
        self.i = 0

    def get(self):
        b = self.bufs[self.i % len(self.bufs)]
        self.i += 1
        return b


class Ring:
    def __init__(self, bufs):
        self.bufs = bufs
        self.i = 0

    def get(self):
        b = self.bufs[self.i % len(self.bufs)]
        self.i += 1
        return b


def rmsnorm_fm(P, pp, xT, gain, hT, ones_bf, sq_ring, rstd, nchunk, T, dim):
    for h0 in range(0, T, 512):
        w = min(512, T - h0)
        ps = pp.get()
        for c in range(nchunk):
            sq = sq_ring.get()
            P.op("act", "activation", out=sq[:, 0:w], in_=xT[c][:, h0:h0 + w], func=AF.Square,
                 reads=[xT[c]], writes=[sq])
            P.op("pe", "matmul", ps[:, 0:w], ones_bf[:, :], sq[:, 0:w], start=(c == 0), stop=(c == nchunk - 1),
                 reads=[sq, ones_bf], writes=[ps])
        P.op("dve", "tensor_scalar", rstd[:, h0:h0 + w], ps[:, 0:w], 1.0 / dim, EPS, ALU.mult, ALU.add,
             reads=[ps], writes=[rstd])
        P.op("act", "activation", out=rstd[:, h0:h0 + w], in_=rstd[:, h0:h0 + w], func=AF.Sqrt,
             reads=[rstd], writes=[rstd])
        P.op("dve", "reciprocal", rstd[:, h0:h0 + w], rstd[:, h0:h0 + w], reads=[rstd], writes=[rstd])
        for c in range(nchunk):
            P.op("dve", "scalar_tensor_tensor", out=hT[c][:, h0:h0 + w], in0=xT[c][:, h0:h0 + w],
                 scalar=gain[:, c:c + 1], in1=rstd[:, h0:h0 + w], op0=ALU.mult, op1=ALU.mult,
                 reads=[xT[c], gain, rstd], writes=[hT[c]])


def ffn_fm(P, pp, xT, hT, w_gu, w_down, aT, wg_ring, wu_ring, wd_ring, sg_ring, T):
    NKC = D // 128
    groups = [(0, 6), (6, 12), (12, 17), (17, 22)]
    wgu_v = w_gu.rearrange("(kc p) f -> p kc f", p=128)
    wd_v = w_down.rearrange("(fc p) d -> p fc d", p=128)
    halves = [(h0, min(512, T - h0)) for h0 in range(0, T, 512)]
    for (p0, p1) in groups:
        nfc = 2 * (p1 - p0)
        for pr in range(p0, p1):
            wg = wg_ring.get()
            wu = wu_ring.get()
            P.dma("pool", wg[:, :, :], wgu_v[:, :, pr * 256:(pr + 1) * 256], writes=[wg])
            P.dma("pool", wu[:, :, :], wgu_v[:, :, DFF + pr * 256:DFF + (pr + 1) * 256], writes=[wu])
            for j in range(2):
                fl = 2 * (pr - p0) + j
                for (h0, w) in halves:
                    pg = pp.get()
                    pu = pp.get()
                    for kc in range(NKC):
                        P.op("pe", "matmul", pg[:, 0:w], wg[:, kc, j * 128:(j + 1) * 128], hT[kc][:, h0:h0 + w],
                             start=(kc == 0), stop=(kc == NKC - 1), reads=[wg, hT[kc]], writes=[pg])
                    for kc in range(NKC):
                        P.op("pe", "matmul", pu[:, 0:w], wu[:, kc, j * 128:(j + 1) * 128], hT[kc][:, h0:h0 + w],
                             start=(kc == 0), stop=(kc == NKC - 1), reads=[wu, hT[kc]], writes=[pu])
                    sg = sg_ring.get()
                    P.op("act", "activation", out=sg[:, 0:w], in_=pg[:, 0:w], func=AF.Silu, reads=[pg], writes=[sg])
                    P.op("dve", "tensor_tensor", out=aT[fl][:, h0:h0 + w], in0=pu[:, 0:w], in1=sg[:, 0:w], op=ALU.mult,
                         reads=[pu, sg], writes=[aT[fl]])
        for dq in range(D // 512):
            wd = wd_ring.get()
            P.dma("pool", wd[:, 0:nfc, :], wd_v[:, 2 * p0:2 * p1, dq * 512:(dq + 1) * 512], writes=[wd])
            for dj in range(4):
                dc = dq * 4 + dj
                for (h0, w) in halves:
                    po = pp.get()
                    for fl in range(nfc):
                        P.op("pe", "matmul", po[:, 0:w], wd[:, fl, dj * 128:(dj + 1) * 128], aT[fl][:, h0:h0 + w],
                             start=(fl == 0), stop=(fl == nfc - 1), reads=[wd, aT[fl]], writes=[po])
                    P.op("dve", "scalar_tensor_tensor", out=xT[dc][:, h0:h0 + w], in0=po[:, 0:w], scalar=0.5,
                         in1=xT[dc][:, h0:h0 + w], op0=ALU.mult, op1=ALU.add, reads=[po, xT[dc]], writes=[xT[dc]])


def build_ffn_prog(T=NTOK):
    nc = bass.Bass("TRN2", target_bir_lowering=False)
    xin = nc.dram_tensor("xT", [D, T], F32, kind="ExternalInput").ap()
    gin = nc.dram_tensor("gain", [128, D // 128], F32, kind="ExternalInput").ap()
    wgu = nc.dram_tensor("w_gu", [D, 2 * DFF], F32, kind="ExternalInput").ap()
    wdn = nc.dram_tensor("w_down", [DFF, D], F32, kind="ExternalInput").ap()
    yout = nc.dram_tensor("yT", [D, T], F32, kind="ExternalOutput").ap()
    NKC = D // 128
    with contextlib.ExitStack() as stack:
        P = Prog(nc, stack)
        pp = PsumPool(P, 8)
        xT = [P.sbuf([128, T], F32, "xT%d" % c) for c in range(NKC)]
        hT = [P.sbuf([128, T], BF16, "hT%d" % c) for c in range(NKC)]
        aT = [P.sbuf([128, T], BF16, "aT%d" % c) for c in range(12)]
        gain = P.sbuf([128, NKC], F32, "gain")
        ones_bf = P.sbuf([128, 128], BF16, "ones")
        rstd = P.sbuf([128, T], F32, "rstd")
        sq_ring = Ring([P.sbuf([128, 512], BF16, "sq%d" % i) for i in range(3)])
        sg_ring = Ring([P.sbuf([128, 512], F32, "sg%d" % i) for i in range(3)])
        wg_ring = Ring([P.sbuf([128, NKC, 256], BF16, "wg%d" % i) for i in range(2)])
        wu_ring = Ring([P.sbuf([128, NKC, 256], BF16, "wu%d" % i) for i in range(2)])
        wd_ring = Ring([P.sbuf([128, 12, 512], BF16, "wd%d" % i) for i in range(2)])

        xv = xin.rearrange("(c p) t -> c p t", p=128)
        yv = yout.rearrange("(c p) t -> c p t", p=128)
        P.op("pool", "memset", ones_bf[:, :], 1.0, writes=[ones_bf])
        P.dma("sp", gain[:, :], gin[:, :], writes=[gain])
        for c in range(NKC):
            P.dma("sp", xT[c][:, :], xv[c], writes=[xT[c]])
        rmsnorm_fm(P, pp, xT, gain, hT, ones_bf, sq_ring, rstd, NKC, T, D)
        ffn_fm(P, pp, xT, hT, wgu, wdn, aT, wg_ring, wu_ring, wd_ring, sg_ring, T)
        outs = []
        for c in range(NKC):
            outs.append(P.dma("sp", yv[c], xT[c][:, :], reads=[xT[c]]))
        P.emit(final_wait_ops=outs)
    return nc


def lin_fm(P, pp, hT, w_ap, ncols, w_ring, T, consume, nkc=D // 128):
    wv = w_ap.rearrange("(kc p) f -> p kc f", p=128)
    halves = [(h0, min(512, T - h0)) for h0 in range(0, T, 512)]
    for c0 in range(0, ncols, 256):
        cw = min(256, ncols - c0)
        wt = w_ring.get()
        P.dma("pool", wt[:, 0:nkc, 0:cw], wv[:, :, c0:c0 + cw], writes=[wt])
        for j in range(cw // 128):
            for (h0, w) in halves:
                ps = pp.get()
                for kc in range(nkc):
                    P.op("pe", "matmul", ps[:, 0:w], wt[:, kc, j * 128:(j + 1) * 128], hT[kc][:, h0:h0 + w],
                         start=(kc == 0), stop=(kc == nkc - 1), reads=[wt, hT[kc]], writes=[ps])
                consume(c0 // 128 + j, h0, w, ps)


def lin_tm(P, pp, hT, w_ap, ncols, w_ring, T, consume, nkc=D // 128):
    wv = w_ap.rearrange("(kc p) f -> p kc f", p=128)
    for c0 in range(0, ncols, 256):
        cw = min(256, ncols - c0)
        wt = w_ring.get()
        P.dma("pool", wt[:, 0:nkc, 0:cw], wv[:, :, c0:c0 + cw], writes=[wt])
        for tt in range(T // 128):
            ps = pp.get()
            for kc in range(nkc):
                P.op("pe", "matmul", ps[:, 0:cw], hT[kc][:, tt * 128:(tt + 1) * 128], wt[:, kc, 0:cw],
                     start=(kc == 0), stop=(kc == nkc - 1), reads=[wt, hT[kc]], writes=[ps])
            consume(tt, c0, cw, ps)


NQ, NK, NV, NZ, NXBC, NDT = 1024, 1024, 1024, 3072, 5120, 48
OQ, OK_, OV, OZ, OXBC, ODT = 0, 1024, 2048, 3072, 6144, 11264
N_IN = 11312


def headnorm_consume(P, pp, ones_bf, gain_hd, sq_ring, rs_ring, dst_of):
    def consume(fc, h0, w, ps):
        sq = sq_ring.get()
        P.op("act", "activation", out=sq[:, 0:w], in_=ps[:, 0:w], func=AF.Square, reads=[ps], writes=[sq])
        ps2 = pp.get()
        P.op("pe", "matmul", ps2[:, 0:w], ones_bf[:, :], sq[:, 0:w], start=True, stop=True,
             reads=[sq, ones_bf], writes=[ps2])
        rs = rs_ring.get()
        P.op("dve", "tensor_scalar", rs[:, 0:w], ps2[:, 0:w], 1.0 / 128, EPS, ALU.mult, ALU.add, reads=[ps2], writes=[rs])
        P.op("act", "activation", out=rs[:, 0:w], in_=rs[:, 0:w], func=AF.Sqrt, reads=[rs], writes=[rs])
        P.op("dve", "reciprocal", rs[:, 0:w], rs[:, 0:w], reads=[rs], writes=[rs])
        ap, buf = dst_of(fc, h0, w)
        P.op("dve", "scalar_tensor_tensor", out=ap, in0=ps[:, 0:w], scalar=gain_hd[:, 0:1], in1=rs[:, 0:w],
             op0=ALU.mult, op1=ALU.mult, reads=[ps, gain_hd, rs], writes=[buf])
    return consume


def build_progA(T=NTOK, do_ffn=True):
    nc = bass.Bass("TRN2", target_bir_lowering=False)
    di = lambda n, s: nc.dram_tensor(n, s, F32, kind="ExternalInput").ap()
    do = lambda n, s: nc.dram_tensor(n, s, F32, kind="ExternalOutput").ap()
    xin = di("xT", [D, T]); ffg = di("ffg", [128, 16]); mixg = di("mixg", [128, 16])
    wgu = di("w_gu", [D, 2 * DFF]); wdn = di("w_down", [DFF, D]); win = di("w_in", [D, N_IN])
    qg_in = di("qg", [128, 1]); kg_in = di("kg", [128, 1])
    x1o = do("x1T", [D, T]); qo = do("qT", [NQ, T]); ko = do("kT", [NK, T]); vo = do("v", [T, NV])
    zo = do("z", [T, NZ]); xbco = do("xbcT", [NXBC, T]); dto = do("dt", [T, NDT])
    NKC = D // 128
    with contextlib.ExitStack() as stack:
        P = Prog(nc, stack)
        pp = PsumPool(P, 8)
        xT = [P.sbuf([128, T], F32, "xT%d" % c) for c in range(NKC)]
        hT = [P.sbuf([128, T], BF16, "hT%d" % c) for c in range(NKC)]
        aT = [P.sbuf([128, T], BF16, "aT%d" % c) for c in range(12)]
        g1 = P.sbuf([128, NKC], F32, "g1"); g2 = P.sbuf([128, NKC], F32, "g2")
        qg = P.sbuf([128, 1], F32, "qg"); kg = P.sbuf([128, 1], F32, "kg")
        ones_bf = P.sbuf([128, 128], BF16, "ones")
        rstd = P.sbuf([128, T], F32, "rstd")
        sq_ring = Ring([P.sbuf([128, 512], BF16, "sq%d" % i) for i in range(3)])
        sg_ring = Ring([P.sbuf([128, 512], F32, "sg%d" % i) for i in range(3)])
        wg_ring = Ring([P.sbuf([128, NKC, 256], BF16, "wg%d" % i) for i in range(2)])
        wu_ring = Ring([P.sbuf([128, NKC, 256], BF16, "wu%d" % i) for i in range(2)])
        wd_ring = Ring([P.sbuf([128, 12, 512], BF16, "wd%d" % i) for i in range(2)])
        w_ring = Ring(wg_ring.bufs + wu_ring.bufs)
        st_ring = Ring([P.sbuf([128, 512], F32, "st%d" % i) for i in range(4)])

        xv = xin.rearrange("(c p) t -> c p t", p=128)
        P.op("pool", "memset", ones_bf[:, :], 1.0, writes=[ones_bf])
        P.dma("sp", g1[:, :], ffg[:, :], writes=[g1]); P.dma("sp", g2[:, :], mixg[:, :], writes=[g2])
        P.dma("sp", qg[:, :], qg_in[:, :], writes=[qg]); P.dma("sp", kg[:, :], kg_in[:, :], writes=[kg])
        for c in range(NKC):
            P.dma("sp", xT[c][:, :], xv[c], writes=[xT[c]])
        outs = []
        if do_ffn:
            rmsnorm_fm(P, pp, xT, g1, hT, ones_bf, sq_ring, rstd, NKC, T, D)
            ffn_fm(P, pp, xT, hT, wgu, wdn, aT, wg_ring, wu_ring, wd_ring, sg_ring, T)
        x1v = x1o.rearrange("(c p) t -> c p t", p=128)
        for c in range(NKC):
            outs.append(P.dma("sp", x1v[c], xT[c][:, :], reads=[xT[c]]))
        rmsnorm_fm(P, pp, xT, g2, hT, ones_bf, sq_ring, rstd, NKC, T, D)

        for (o_ap, col0, gbuf) in ((qo, OQ, qg), (ko, OK_, kg)):
            ov = o_ap.rearrange("(c p) t -> c p t", p=128)

            def dst_of(fc, h0, w, ov=ov):
                st = st_ring.get()
                dst_of.last = (st, fc, h0, w)
                return st[:, 0:w], st
            cons0 = headnorm_consume(P, pp, ones_bf, gbuf, sq_ring, sg_ring, dst_of)

            def cons(fc, h0, w, ps, cons0=cons0, ov=ov):
                cons0(fc, h0, w, ps)
                st, fc, h0, w = dst_of.last
                outs.append(P.dma("sp", ov[fc][:, h0:h0 + w], st[:, 0:w], reads=[st]))
            lin_fm(P, pp, hT, win[:, col0:col0 + 1024], 1024, w_ring, T, cons)

        xbv = xbco.rearrange("(c p) t -> c p t", p=128)

        def cons_xbc(fc, h0, w, ps):
            st = st_ring.get()
            P.op("act", "activation", out=st[:, 0:w], in_=ps[:, 0:w], func=AF.Copy, reads=[ps], writes=[st])
            outs.append(P.dma("sp", xbv[fc][:, h0:h0 + w], st[:, 0:w], reads=[st]))
        lin_fm(P, pp, hT, win[:, OXBC:OXBC + NXBC], NXBC, w_ring, T, cons_xbc)

        for (o_ap, col0, n) in ((vo, OV, NV), (zo, OZ, NZ), (dto, ODT, NDT)):
            def cons_tm(tt, c0, cw, ps, o_ap=o_ap):
                st = st_ring.get()
                P.op("dve", "tensor_copy", st[:, 0:cw], ps[:, 0:cw], reads=[ps], writes=[st])
                outs.append(P.dma("sp", o_ap[tt * 128:(tt + 1) * 128, c0:c0 + cw], st[:, 0:cw], reads=[st]))
            lin_tm(P, pp, hT, win[:, col0:col0 + n], n, w_ring, T, cons_tm)
        P.emit(final_wait_ops=outs)
    return nc


SEQ = 4096
NCH = SEQ // 128


def build_progB():
    nc = bass.Bass("TRN2", target_bir_lowering=False)
    di = lambda n, sh: nc.dram_tensor(n, sh, F32, kind="ExternalInput").ap()
    xbc = di("xbcT", [1280, SEQ + 3]); convw_i = di("convw", [128, 10, 4]); convb_i = di("convb", [128, 10])
    dtraw_i = di("dtraw", [SEQ, 12]); dtb_i = di("dtb", [128, 384]); alog_i = di("alog", [128, 384])
    dsk_i = di("dsk", [128, 768]); normw_i = di("normw", [128, 768]); z_i = di("z", [SEQ, 768])
    tri_i = di("tri", [128, 128]); strict_i = di("strict", [128, 128]); onesf_i = di("onesf", [128, 128])
    ident_i = di("ident", [128, 128])
    yo = nc.dram_tensor("y", [SEQ, 768], F32, kind="ExternalOutput").ap()
    with contextlib.ExitStack() as stack:
        P = Prog(nc, stack)
        pp = PsumPool(P, 8)
        cw = P.sbuf([128, 10, 4], F32, "cw"); cb = P.sbuf([128, 10], F32, "cb")
        dt_all = P.sbuf([128, 384], F32, "dt_all"); da_all = P.sbuf([128, 384], F32, "da_all")
        tmpa = P.sbuf([128, 384], F32, "tmpa"); tmpb = P.sbuf([128, 384], F32, "tmpb")
        dsk = P.sbuf([128, 12, 64], F32, "dsk"); normw = P.sbuf([128, 768], F32, "normw")
        tri = P.sbuf([128, 128], F32, "tri"); strict = P.sbuf([128, 128], F32, "strict")
        onesf = P.sbuf([128, 128], F32, "onesf"); ident = P.sbuf([128, 128], F32, "ident")
        for (b, a) in ((cw, convw_i), (cb, convb_i), (tmpa, dtb_i), (tmpb, alog_i),
                       (normw, normw_i), (tri, tri_i), (strict, strict_i), (onesf, onesf_i), (ident, ident_i)):
            P.dma("sp", b[:], a, writes=[b])
        P.dma("sp", dsk[:], dsk_i.rearrange("p (j d) -> p j d", d=64), writes=[dsk])
        P.dma("sp", dt_all[:].rearrange("p (c j) -> p c j", j=12), dtraw_i.rearrange("(c l) j -> l c j", l=128),
              writes=[dt_all])
        P.op("dve", "tensor_tensor", out=dt_all[:], in0=dt_all[:], in1=tmpa[:], op=ALU.add, reads=[dt_all, tmpa], writes=[dt_all])
        P.op("act", "activation", out=dt_all[:], in_=dt_all[:], func=AF.Exp, reads=[dt_all], writes=[dt_all])
        P.op("dve", "tensor_scalar", dt_all[:], dt_all[:], 1.0, None, ALU.add, reads=[dt_all], writes=[dt_all])
        P.op("act", "activation", out=dt_all[:], in_=dt_all[:], func=AF.Ln, reads=[dt_all], writes=[dt_all])
        P.op("act", "activation", out=tmpb[:], in_=tmpb[:], func=AF.Exp, reads=[tmpb], writes=[tmpb])
        P.op("dve", "scalar_tensor_tensor", out=da_all[:], in0=dt_all[:], scalar=-1.0, in1=tmpb[:], op0=ALU.mult,
             op1=ALU.mult, reads=[dt_all, tmpb], writes=[da_all])

        h = [P.sbuf([128, 6, 64], F32, "h%d" % g) for g in range(2)]
        hb = [P.sbuf([128, 6, 64], BF16, "hb%d" % g) for g in range(2)]
        for g in range(2):
            P.op("pool", "memset", h[g][:], 0.0, writes=[h[g]])
            P.op("pool", "memset", hb[g][:], 0.0, writes=[hb[g]])

        xr_ring = Ring([P.sbuf([128, 515], F32, "xr%d" % i) for i in range(4)])
        acc_ring = Ring([P.sbuf([128, 512], F32, "acc%d" % i) for i in range(2)])
        xc_sets = [[P.sbuf([128, 512], F32, "xc%d_%d" % (k, i)) for i in range(8)] for k in range(2)]
        bc_sets = [[P.sbuf([128, 512], BF16, "bc%d_%d" % (k, i)) for i in range(4)] for k in range(2)]
        xt_ring = Ring([P.sbuf([128, 12, 64], F32, "xt%d" % i) for i in range(2)])
        bt_ring = Ring([P.sbuf([128, 256], BF16, "bt%d" % i) for i in range(2)])
        E_ring = Ring([P.sbuf([128, 36], F32, "E%d" % i) for i in range(2)])
        s2_ring = Ring([P.sbuf([128, 12], F32, "s2%d" % i) for i in range(2)])
        xdt_ring = Ring([P.sbuf([128, 12, 64], BF16, "xdt%d" % i) for i in range(2)])
        xw_ring = Ring([P.sbuf([128, 12, 64], BF16, "xw%d" % i) for i in range(2)])
        xd_ring = Ring([P.sbuf([128, 12, 64], F32, "xd%d" % i) for i in range(2)])
        cbm_ring = Ring([P.sbuf([128, 128], F32, "cbm%d" % i) for i in range(2)])
        aj_ring = Ring([P.sbuf([128, 128], F32, "aj%d" % i) for i in range(3)])
        dec_ring = Ring([P.sbuf([128, 128], F32, "dec%d" % i) for i in range(3)])
        sc_ring = Ring([P.sbuf([128, 128], BF16, "sc%d" % i) for i in range(3)])
        y_ring = Ring([P.sbuf([128, 12, 64], F32, "y%d" % i) for i in range(2)])
        t1_ring = Ring([P.sbuf([128, 6, 64], F32, "t1%d" % i) for i in range(2)])
        z_ring = Ring([P.sbuf([128, 768], F32, "z%d" % i) for i in range(2)])
        sq_ring = Ring([P.sbuf([128, 768], F32, "sqq%d" % i) for i in range(2)])
        ss_ring = Ring([P.sbuf([128, 2], F32, "ss%d" % i) for i in range(2)])
        o_ring = Ring([P.sbuf([128, 768], F32, "o%d" % i) for i in range(2)])
        outs = []
        for tb in range(SEQ // 512):
            xc = xc_sets[tb % 2]
            bc = bc_sets[tb % 2]
            for ch in range(10):
                xr = xr_ring.get()
                P.dma("sp", xr[:, :], xbc[ch * 128:(ch + 1) * 128, tb * 512:tb * 512 + 515], writes=[xr])
                acc = acc_ring.get()
                P.op("dve", "tensor_scalar", acc[:, :], xr[:, 0:512], cw[:, ch, 0:1], cb[:, ch:ch + 1], ALU.mult, ALU.add,
                     reads=[xr, cw, cb], writes=[acc])
                for k in range(1, 4):
                    P.op("dve", "scalar_tensor_tensor", out=acc[:, :], in0=xr[:, k:k + 512], scalar=cw[:, ch, k:k + 1],
                         in1=acc[:, :], op0=ALU.mult, op1=ALU.add, reads=[xr, cw, acc], writes=[acc])
                if ch < 8:
                    P.op("act", "activation", out=xc[ch][:, :], in_=acc[:, :], func=AF.Silu, reads=[acc], writes=[xc[ch]])
                    if ch >= 6:
                        P.op("dve", "tensor_copy", bc[ch - 6][:, :], xc[ch][:, :], reads=[xc[ch]], writes=[bc[ch - 6]])
                else:
                    P.op("act", "activation", out=bc[ch - 6][:, :], in_=acc[:, :], func=AF.Silu, reads=[acc], writes=[bc[ch - 6]])
            for sc_i in range(4):
                c = tb * 4 + sc_i
                ts = slice(sc_i * 128, (sc_i + 1) * 128)
                xt = xt_ring.get(); bt = bt_ring.get()
                xtf = xt[:].rearrange("p j d -> p (j d)")
                for (lo, n) in ((0, 4), (4, 2)):
                    ps = pp.get()
                    for i in range(n):
                        P.op("pe", "transpose", ps[:, i * 128:(i + 1) * 128], xc[lo + i][:, ts], ident[:, :],
                             reads=[xc[lo + i], ident], writes=[ps])
                    P.op("dve", "tensor_copy", xtf[:, lo * 128:(lo + n) * 128], ps[:, 0:n * 128], reads=[ps], writes=[xt])
                ps = pp.get()
                for i in range(2):
                    P.op("pe", "transpose", ps[:, i * 128:(i + 1) * 128], xc[6 + i][:, ts], ident[:, :],
                         reads=[xc[6 + i], ident], writes=[ps])
                P.op("act", "activation", out=bt[:, :], in_=ps[:, 0:256], func=AF.Copy, reads=[ps], writes=[bt])
                dac = da_all[:, c * 12:(c + 1) * 12]
                dtc = dt_all[:, c * 12:(c + 1) * 12]
                ps = pp.get()
                P.op("pe", "matmul", ps[:, 0:12], tri[:, :], dac, start=True, stop=True, reads=[tri, da_all], writes=[ps])
                P.op("pe", "matmul", ps[:, 12:24], strict[:, :], dac, start=True, stop=True, reads=[strict, da_all], writes=[ps])
                P.op("pe", "matmul", ps[:, 24:36], onesf[:, :], dac, start=True, stop=True, reads=[onesf, da_all], writes=[ps])
                E = E_ring.get()
                P.op("act", "activation", out=E[:, :], in_=ps[:, 0:36], func=AF.Exp, reads=[ps], writes=[E])
                s2 = s2_ring.get()
                P.op("dve", "tensor_tensor", out=s2[:, :], in0=dtc, in1=E[:, 12:24], op=ALU.mult, reads=[dt_all, E], writes=[s2])
                xdt = xdt_ring.get(); xw = xw_ring.get(); xd = xd_ring.get()
                P.op("dve", "tensor_tensor", out=xdt[:], in0=xt[:], in1=dtc.unsqueeze(2).broadcast_to([128, 12, 64]),
                     op=ALU.mult, reads=[xt, dt_all], writes=[xdt])
                P.op("dve", "tensor_tensor", out=xw[:], in0=xt[:], in1=s2[:, :].unsqueeze(2).broadcast_to([128, 12, 64]),
                     op=ALU.mult, reads=[xt, s2], writes=[xw])
                P.op("pool", "tensor_tensor", out=xd[:], in0=xt[:], in1=dsk[:], op=ALU.mult, reads=[xt, dsk], writes=[xd])
                y = y_ring.get()
                for gi in range(2):
                    BT = bc[gi]; CT = bc[2 + gi]
                    ps_cb = pp.get()
                    P.op("pe", "matmul", ps_cb[:, 0:128], BT[:, ts], CT[:, ts], start=True, stop=True, reads=[BT, CT], writes=[ps_cb])
                    cbm = cbm_ring.get()
                    P.op("dve", "tensor_tensor", out=cbm[:, :], in0=ps_cb[:, 0:128], in1=tri[:, :], op=ALU.mult,
                         reads=[ps_cb, tri], writes=[cbm])
                    ps_yo = pp.get()
                    P.op("pe", "matmul", ps_yo[:, 0:384], CT[:, ts], hb[gi][:].rearrange("p j d -> p (j d)"),
                         start=True, stop=True, reads=[CT, hb[gi]], writes=[ps_yo])
                    ps_yd = pp.get()
                    for jj in range(6):
                        j = gi * 6 + jj
                        aj = aj_ring.get()
                        P.op("pool", "tensor_scalar", aj[:, :], strict[:, :], da_all[:, c * 12 + j:c * 12 + j + 1], None, ALU.mult,
                             reads=[strict, da_all], writes=[aj])
                        ps_seg = pp.get()
                        P.op("pe", "matmul", ps_seg[:, 0:128], aj[:, :], tri[:, :], start=True, stop=True, reads=[aj, tri], writes=[ps_seg])
                        dec = dec_ring.get()
                        P.op("act", "activation", out=dec[:, :], in_=ps_seg[:, 0:128], func=AF.Exp, reads=[ps_seg], writes=[dec])
                        scb = sc_ring.get()
                        P.op("dve", "tensor_tensor", out=scb[:, :], in0=dec[:, :], in1=cbm[:, :], op=ALU.mult,
                             reads=[dec, cbm], writes=[scb])
                        P.op("pe", "matmul", ps_yd[:, jj * 64:(jj + 1) * 64], scb[:, :], xdt[:, j, :], start=True, stop=True,
                             reads=[scb, xdt], writes=[ps_yd])
                    t1 = t1_ring.get()
                    P.op("dve", "tensor_tensor", out=t1[:], in0=ps_yo[:, 0:384].rearrange("p (j d) -> p j d", d=64),
                         in1=E[:, gi * 6:gi * 6 + 6].unsqueeze(2).broadcast_to([128, 6, 64]), op=ALU.mult,
                         reads=[ps_yo, E], writes=[t1])
                    P.op("dve", "tensor_tensor", out=t1[:], in0=ps_yd[:, 0:384].rearrange("p (j d) -> p j d", d=64),
                         in1=t1[:], op=ALU.add, reads=[ps_yd, t1], writes=[t1])
                    P.op("dve", "tensor_tensor", out=y[:, gi * 6:(gi + 1) * 6, :], in0=xd[:, gi * 6:(gi + 1) * 6, :], in1=t1[:],
                         op=ALU.add, reads=[xd, t1], writes=[y])
                    ps_st = pp.get()
                    P.op("pe", "matmul", ps_st[:, 0:384], bt[:, gi * 128:(gi + 1) * 128],
                         xw[:, gi * 6:(gi + 1) * 6, :].rearrange("p j d -> p (j d)"), start=True, stop=True,
                         reads=[bt, xw], writes=[ps_st])
                    P.op("dve", "tensor_tensor", out=h[gi][:], in0=h[gi][:],
                         in1=E[:, 24 + gi * 6:24 + gi * 6 + 6].unsqueeze(2).broadcast_to([128, 6, 64]), op=ALU.mult,
                         reads=[h[gi], E], writes=[h[gi]])
                    P.op("dve", "tensor_tensor", out=h[gi][:], in0=ps_st[:, 0:384].rearrange("p (j d) -> p j d", d=64),
                         in1=h[gi][:], op=ALU.add, reads=[ps_st, h[gi]], writes=[h[gi]])
                    P.op("act", "activation", out=hb[gi][:], in_=h[gi][:], func=AF.Copy, reads=[h[gi]], writes=[hb[gi]])
                zt = z_ring.get()
                P.dma("sp", zt[:, :], z_i[c * 128:(c + 1) * 128, :], writes=[zt])
                P.op("act", "activation", out=zt[:, :], in_=zt[:, :], func=AF.Silu, reads=[zt], writes=[zt])
                yf = y[:].rearrange("p j d -> p (j d)")
                P.op("dve", "tensor_tensor", out=yf, in0=yf, in1=zt[:, :], op=ALU.mult, reads=[y, zt], writes=[y])
                sq = sq_ring.get()
                P.op("act", "activation", out=sq[:, :], in_=yf, func=AF.Square, reads=[y], writes=[sq])
                ss = ss_ring.get()
                P.op("dve", "tensor_reduce", out=ss[:, :], in_=sq[:, :].rearrange("p (g f) -> p g f", g=2), axis=AX.X, op=ALU.add,
                     reads=[sq], writes=[ss])
                P.op("dve", "tensor_scalar", ss[:, :], ss[:, :], 1.0 / 384, EPS, ALU.mult, ALU.add, reads=[ss], writes=[ss])
                P.op("act", "activation", out=ss[:, :], in_=ss[:, :], func=AF.Sqrt, reads=[ss], writes=[ss])
                P.op("dve", "reciprocal", ss[:, :], ss[:, :], reads=[ss], writes=[ss])
                ot = o_ring.get()
                for gi in range(2):
                    P.op("dve", "scalar_tensor_tensor", out=ot[:, gi * 384:(gi + 1) * 384], in0=yf[:, gi * 384:(gi + 1) * 384],
                         scalar=ss[:, gi:gi + 1], in1=normw[:, gi * 384:(gi + 1) * 384], op0=ALU.mult, op1=ALU.mult,
                         reads=[y, ss, normw], writes=[ot])
                outs.append(P.dma("sp", yo[c * 128:(c + 1) * 128, :], ot[:, :], reads=[ot]))
        P.emit(final_wait_ops=outs)
    return nc


def _rep(v, n=128):
    return np.ascontiguousarray(np.broadcast_to(np.asarray(v, np.float32).reshape(1, -1), (n, np.asarray(v).size)))


def ssd_consts():
    k = np.arange(128)
    tri = (k[:, None] <= k[None, :]).astype(np.float32)
    strict = (k[:, None] > k[None, :]).astype(np.float32)
    return {"tri": tri, "strict": strict, "onesf": np.ones((128, 128), np.float32), "ident": np.eye(128, dtype=np.float32)}


def progB_inputs(xbcT_b, dt_b, z_b, Pm):
    maps = []
    cst = ssd_consts()
    for c in range(NCORES):
        b, gp = c // 4, c % 4
        g0 = 2 * gp
        chs = np.concatenate([np.arange(g0 * 384, (g0 + 2) * 384), 3072 + np.arange(g0 * 128, (g0 + 2) * 128),
                              4096 + np.arange(g0 * 128, (g0 + 2) * 128)])
        hs = np.arange(g0 * 6, g0 * 6 + 12)
        xp = np.zeros((1280, SEQ + 3), np.float32)
        xp[:, 3:] = xbcT_b[b][chs]
        m = {"xbcT": xp,
             "convw": np.ascontiguousarray(Pm["conv_w"][:, chs].T.reshape(10, 128, 4).transpose(1, 0, 2)),
             "convb": np.ascontiguousarray(Pm["conv_b"][chs].reshape(10, 128).T),
             "dtraw": np.ascontiguousarray(dt_b[b][:, hs]),
             "dtb": _rep(np.tile(Pm["dt_bias"][hs], NCH)), "alog": _rep(np.tile(Pm["a_log"][hs], NCH)),
             "dsk": _rep(np.repeat(Pm["d_skip"][hs], 64)), "normw": _rep(Pm["ssd_norm"][g0 * 384:(g0 + 2) * 384]),
             "z": np.ascontiguousarray(z_b[b][:, g0 * 384:(g0 + 2) * 384])}
        m.update(cst)
        maps.append(m)
    return maps


def progB_gather(ys):
    out = np.zeros((2, SEQ, 3072), np.float32)
    for c in range(NCORES):
        b, gp = c // 4, c % 4
        out[b][:, gp * 768:(gp + 1) * 768] = ys[c]
    return out


NBLK = SEQ // 256
BIGNEG = -30000.0


def build_progC1():
    nc = bass.Bass("TRN2", target_bir_lowering=False)
    di = lambda n, sh: nc.dram_tensor(n, sh, F32, kind="ExternalInput").ap()
    q_i = di("qT", [1024, NTOK]); k_i = di("kT_all", [1024, SEQ]); v_i = di("v_all", [SEQ, 1024])
    ko_i = di("kT_own", [1024, NTOK]); vo_i = di("v_own", [NTOK, 1024])
    tna_i = di("Tna", [128, 8 * 384]); tca_i = di("Tca", [128, 8 * 384]); ab_i = di("abias", [128, 512])
    gb_i = di("gbias", [128, 64]); pm_i = di("pastmask", [128, 64]); oh_i = di("onehot", [16, 16 * 128])
    id_i = di("ident", [128, 128])
    yo = nc.dram_tensor("yT", [1024, NTOK], F32, kind="ExternalOutput").ap()
    scale = 128.0 ** -0.5
    with contextlib.ExitStack() as stack:
        P = Prog(nc, stack)
        pp = PsumPool(P, 4)
        pacc = PsumPool(P, 4, pfx="pacc")
        tna = P.sbuf([128, 8, 384], F32, "tna"); tca = P.sbuf([128, 8, 384], F32, "tca")
        abias = P.sbuf([128, 512], F32, "abias"); gbias = P.sbuf([128, 64], F32, "gbias")
        pmask = P.sbuf([128, 64], F32, "pmask"); onehot = P.sbuf([16, 16, 128], BF16, "onehot")
        ident = P.sbuf([128, 128], F32, "ident"); ones_bf = P.sbuf([128, 128], BF16, "ones")
        P.dma("sp", tna[:], tna_i.rearrange("p (h u) -> p h u", h=8), writes=[tna])
        P.dma("sp", tca[:], tca_i.rearrange("p (h u) -> p h u", h=8), writes=[tca])
        for (b, a) in ((abias, ab_i), (gbias, gb_i), (pmask, pm_i), (ident, id_i)):
            P.dma("sp", b[:], a, writes=[b])
        P.dma("pool", onehot[:], oh_i.rearrange("k (n m) -> k n m", n=16), writes=[onehot])
        P.op("pool", "memset", ones_bf[:, :], 1.0, writes=[ones_bf])
        kf_ring = Ring([P.sbuf([128, SEQ], F32, "kf%d" % i) for i in range(2)])
        kb_ring = Ring([P.sbuf([128, SEQ], BF16, "kb%d" % i) for i in range(2)])
        qf_ring = Ring([P.sbuf([128, NTOK], F32, "qf%d" % i) for i in range(2)])
        qb_ring = Ring([P.sbuf([128, NTOK], BF16, "qb%d" % i) for i in range(2)])
        ko_ring = Ring([P.sbuf([128, NTOK], BF16, "ko%d" % i) for i in range(2)])
        vb_ring = Ring([P.sbuf([128, 32, 128], BF16, "vb%d" % i) for i in range(2)])
        vo_ring = Ring([P.sbuf([128, 8, 128], BF16, "vo%d" % i) for i in range(2)])
        km_ring = Ring([P.sbuf([128, 16], F32, "km%d" % i) for i in range(2)])
        gm_ring = Ring([P.sbuf([128, 16], F32, "gm%d" % i) for i in range(2)])
        t8_ring = Ring([P.sbuf([128, 8], F32, "t8%d" % i) for i in range(2)])
        sel_ring = Ring([P.sbuf([128, 16], F32, "sel%d" % i) for i in range(2)])
        ns_ring = Ring([P.sbuf([16, 256], BF16, "ns%d" % i) for i in range(2)])
        lg_ring = Ring([P.sbuf([128, 256], F32, "lg%d" % i) for i in range(3)])
        pT_ring = Ring([P.sbuf([128, 256], BF16, "pT%d" % i) for i in range(3)])
        rd_ring = Ring([P.sbuf([128, 256], F32, "rd%d" % i) for i in range(2)])
        o_ring = Ring([P.sbuf([128, 256], F32, "oo%d" % i) for i in range(2)])
        outs = []
        for h in range(8):
            hr = slice(h * 128, (h + 1) * 128)
            kf = kf_ring.get(); kb = kb_ring.get(); qf = qf_ring.get(); qb = qb_ring.get()
            ko = ko_ring.get(); vb = vb_ring.get(); vo = vo_ring.get(); km = km_ring.get()
            P.dma("sp", kf[:, :], k_i[hr, :], writes=[kf])
            P.dma("sp", qf[:, :], q_i[hr, :], writes=[qf])
            P.dma("pool", ko[:, :], ko_i[hr, :], writes=[ko])
            P.dma("pool", vb[:], v_i[:, hr].rearrange("(t p) d -> p t d", p=128), writes=[vb])
            P.dma("pool", vo[:], vo_i[:, hr].rearrange("(t p) d -> p t d", p=128), writes=[vo])
            P.op("act", "activation", out=kb[:, :], in_=kf[:, :], func=AF.Copy, reads=[kf], writes=[kb])
            P.op("act", "activation", out=qb[:, :], in_=qf[:, :], func=AF.Copy, reads=[qf], writes=[qb])
            P.op("dve", "tensor_reduce", out=km[:, :], in_=kf[:, :].rearrange("p (n s) -> p n s", s=256), axis=AX.X, op=ALU.add,
                 reads=[kf], writes=[km])
            P.op("dve", "tensor_scalar", km[:, :], km[:, :], 1.0 / 256, None, ALU.mult, reads=[km], writes=[km])
            for qi in range(4):
                qs_ = slice(qi * 256, (qi + 1) * 256)
                ns = ns_ring.get()
                for qs in range(2):
                    ps_g = pp.get()
                    P.op("pe", "matmul", ps_g[:, 0:16], qf[:, qi * 256 + qs * 128:qi * 256 + (qs + 1) * 128], km[:, :],
                         start=True, stop=True, reads=[qf, km], writes=[ps_g])
                    gm = gm_ring.get(); t8 = t8_ring.get(); sel = sel_ring.get()
                    P.op("dve", "tensor_tensor", out=gm[:, :], in0=ps_g[:, 0:16], in1=gbias[:, qi * 16:(qi + 1) * 16], op=ALU.add,
                         reads=[ps_g, gbias], writes=[gm])
                    P.op("dve", "max", out=t8[:, :], in_=gm[:, :], reads=[gm], writes=[t8])
                    P.op("dve", "tensor_scalar", sel[:, :], gm[:, :], t8[:, 2:3], None, ALU.is_ge, reads=[gm, t8], writes=[sel])
                    P.op("dve", "tensor_tensor", out=sel[:, :], in0=sel[:, :], in1=pmask[:, qi * 16:(qi + 1) * 16], op=ALU.mult,
                         reads=[sel, pmask], writes=[sel])
                    P.op("dve", "tensor_scalar", sel[:, :], sel[:, :], -1.0, -BIGNEG, ALU.add, ALU.mult, reads=[sel], writes=[sel])
                    ps_t = pp.get()
                    P.op("pe", "transpose", ps_t[0:16, 0:128], sel[:, :], ident[:, :], reads=[sel, ident], writes=[ps_t])
                    P.op("act", "activation", out=ns[:, qs * 128:(qs + 1) * 128], in_=ps_t[0:16, 0:128], func=AF.Copy,
                         reads=[ps_t], writes=[ns])
                ps_o = pacc.get(); ps_d = pacc.get()
                tiles = [(n, kt) for n in range(NBLK) for kt in range(2)] + [(-1, 0), (-1, 1)]
                for ti, (n, kt) in enumerate(tiles):
                    first, last = (ti == 0), (ti == len(tiles) - 1)
                    ps_s = pp.get()
                    lg = lg_ring.get(); pT = pT_ring.get()
                    tsl = slice(128, 384) if kt == 0 else slice(0, 256)
                    if n >= 0:
                        P.op("pe", "matmul", ps_s[:, 0:256], kb[:, n * 256 + kt * 128:n * 256 + (kt + 1) * 128], qb[:, qs_],
                             start=True, stop=False, reads=[kb, qb], writes=[ps_s])
                        P.op("pe", "matmul", ps_s[:, 0:256], onehot[:, n, :], ns[:, :], start=False, stop=True,
                             reads=[onehot, ns], writes=[ps_s])
                        P.op("dve", "scalar_tensor_tensor", out=lg[:, :], in0=ps_s[:, 0:256], scalar=scale, in1=tna[:, h, tsl],
                             op0=ALU.mult, op1=ALU.add, reads=[ps_s, tna], writes=[lg])
                        bi = (h * 4 + qi) * 16 + n
                        P.op("act", "activation", out=pT[:, :], in_=lg[:, :], func=AF.Exp, bias=abias[:, bi:bi + 1],
                             reads=[lg, abias], writes=[pT])
                        vl = vb[:, n * 2 + kt, :]
                        vbuf = vb
                    else:
                        P.op("pe", "matmul", ps_s[:, 0:256], ko[:, qi * 256 + kt * 128:qi * 256 + (kt + 1) * 128], qb[:, qs_],
                             start=True, stop=True, reads=[ko, qb], writes=[ps_s])
                        P.op("dve", "scalar_tensor_tensor", out=lg[:, :], in0=ps_s[:, 0:256], scalar=scale, in1=tca[:, h, tsl],
                             op0=ALU.mult, op1=ALU.add, reads=[ps_s, tca], writes=[lg])
                        P.op("act", "activation", out=pT[:, :], in_=lg[:, :], func=AF.Exp, reads=[lg], writes=[pT])
                        vl = vo[:, qi * 2 + kt, :]
                        vbuf = vo
                    P.op("pe", "matmul", ps_o[:, 0:256], vl, pT[:, :], start=first, stop=last, reads=[vbuf, pT], writes=[ps_o])
                    P.op("pe", "matmul", ps_d[:, 0:256], ones_bf[:, :], pT[:, :], start=first, stop=last,
                         reads=[ones_bf, pT], writes=[ps_d])
                rd = rd_ring.get(); ot = o_ring.get()
                P.op("dve", "reciprocal", rd[:, :], ps_d[:, 0:256], reads=[ps_d], writes=[rd])
                P.op("dve", "tensor_tensor", out=ot[:, :], in0=ps_o[:, 0:256], in1=rd[:, :], op=ALU.mult, reads=[ps_o, rd], writes=[ot])
                outs.append(P.dma("sp", yo[hr, qs_], ot[:, :], reads=[ot]))
        P.emit(final_wait_ops=outs)
    return nc


def attn_consts(seg):
    sl = np.arange(128)[:, None].astype(np.float64)
    u = np.arange(384)[None, :].astype(np.float64)
    dist = u - 128 - sl
    slopes = 2.0 ** (-(np.arange(8) + 1.0))
    tna = np.zeros((128, 8, 384), np.float32); tca = np.zeros((128, 8, 384), np.float32)
    for h in range(8):
        tna[:, h] = -slopes[h] * dist
        tca[:, h] = np.where(dist >= 0, -slopes[h] * dist, BIGNEG)
    abias = np.zeros((8, 4, 16), np.float32); gb = np.zeros((4, 16), np.float32); pm = np.zeros((4, 16), np.float32)
    for qi in range(4):
        G = 4 * seg + qi
        for n in range(16):
            if n < G:
                pm[qi, n] = 1.0
                abias[:, qi, n] = -slopes * 256.0 * (G - n)
            else:
                gb[qi, n] = -1e30
    oh = np.zeros((16, 16, 128), np.float32)
    for n in range(16):
        oh[n, n, :] = 1.0
    return {"Tna": tna.reshape(128, -1), "Tca": tca.reshape(128, -1), "abias": _rep(abias.reshape(-1)),
            "gbias": _rep(gb.reshape(-1)), "pastmask": _rep(pm.reshape(-1)), "onehot": oh.reshape(16, -1),
            "ident": np.eye(128, dtype=np.float32)}


def progC1_inputs(qT_c, kT_c, v_c):
    maps = []
    for c in range(NCORES):
        b, seg = c // 4, c % 4
        m = {"qT": qT_c[c], "kT_own": kT_c[c], "v_own": v_c[c],
             "kT_all": np.ascontiguousarray(np.concatenate([kT_c[b * 4 + s] for s in range(4)], axis=1)),
             "v_all": np.ascontiguousarray(np.concatenate([v_c[b * 4 + s] for s in range(4)], axis=0))}
        m.update(attn_consts(seg))
        maps.append(m)
    return maps


MEMLEN = 256


def build_progC2(T=NTOK):
    nc = bass.Bass("TRN2", target_bir_lowering=False)
    di = lambda n, sh: nc.dram_tensor(n, sh, F32, kind="ExternalInput").ap()
    xin = di("xT", [D, T]); yin = di("yT", [4096, T]); wout = di("w_out", [4096, D]); mem_i = di("memT", [D, MEMLEN])
    xmg_i = di("xmg", [128, 16]); mmg_i = di("mmg", [128, 16]); ffg_i = di("ffg", [128, 16])
    mqg_i = di("mqg", [128, 1]); mkg_i = di("mkg", [128, 1])
    wq = di("wq", [D, 512]); wk = di("wk", [D, 512]); wv = di("wv", [D, 512]); wo = di("wo", [512, D])
    wgu = di("w_gu", [D, 2 * DFF]); wdn = di("w_down", [DFF, D])
    xo = nc.dram_tensor("xoT", [D, T], F32, kind="ExternalOutput").ap()
    NKC = D // 128
    scale = 128.0 ** -0.5
    with contextlib.ExitStack() as stack:
        P = Prog(nc, stack)
        pp = PsumPool(P, 8)
        xT = [P.sbuf([128, T], F32, "xT%d" % c) for c in range(NKC)]
        hT = [P.sbuf([128, T], BF16, "hT%d" % c) for c in range(NKC)]
        aT = [P.sbuf([128, T], BF16, "aT%d" % c) for c in range(12)]
        g_xm = P.sbuf([128, NKC], F32, "g_xm"); g_mm = P.sbuf([128, NKC], F32, "g_mm"); g_ff = P.sbuf([128, NKC], F32, "g_ff")
        mqg = P.sbuf([128, 1], F32, "mqg"); mkg = P.sbuf([128, 1], F32, "mkg")
        ones_bf = P.sbuf([128, 128], BF16, "ones")
        rstd = P.sbuf([128, T], F32, "rstd")
        sq_ring = Ring([P.sbuf([128, 512], BF16, "sq%d" % i) for i in range(3)])
        sg_ring = Ring([P.sbuf([128, 512], F32, "sg%d" % i) for i in range(3)])
        wg_ring = Ring([P.sbuf([128, NKC, 256], BF16, "wg%d" % i) for i in range(2)])
        wu_ring = Ring([P.sbuf([128, NKC, 256], BF16, "wu%d" % i) for i in range(2)])
        wd_ring = Ring([P.sbuf([128, 12, 512], BF16, "wd%d" % i) for i in range(2)])
        w_ring = Ring(wg_ring.bufs + wu_ring.bufs)
        mr_ring = Ring([P.sbuf([128, MEMLEN], F32, "mr%d" % i) for i in range(3)])
        mhT = [P.sbuf([128, MEMLEN], BF16, "mhT%d" % c) for c in range(NKC)]
        kmT = [P.sbuf([128, MEMLEN], BF16, "kmT%d" % c) for c in range(4)]
        vm = [P.sbuf([128, 512], BF16, "vm%d" % c) for c in range(2)]
        pT_ring = sq_ring
        rd_ring = sg_ring

        xv = xin.rearrange("(c p) t -> c p t", p=128)
        yv = yin.rearrange("(c p) t -> c p t", p=128)
        mv = mem_i.rearrange("(c p) t -> c p t", p=128)
        P.op("pool", "memset", ones_bf[:, :], 1.0, writes=[ones_bf])
        for (b, a) in ((g_xm, xmg_i), (g_mm, mmg_i), (g_ff, ffg_i), (mqg, mqg_i), (mkg, mkg_i)):
            P.dma("sp", b[:], a, writes=[b])
        for c in range(NKC):
            P.dma("sp", xT[c][:, :], xv[c], writes=[xT[c]])

        def cons_add(fc, h0, w, ps):
            P.op("dve", "tensor_tensor", out=xT[fc][:, h0:h0 + w], in0=ps[:, 0:w], in1=xT[fc][:, h0:h0 + w], op=ALU.add,
                 reads=[ps, xT[fc]], writes=[xT[fc]])
        for kh in range(2):
            for c in range(NKC):
                P.dma("pool", hT[c][:, :], yv[kh * NKC + c], writes=[hT[c]])
            lin_fm(P, pp, hT, wout[kh * D:(kh + 1) * D, :], D, w_ring, T, cons_add)
        ps_m = pp.get()
        for c in range(NKC):
            mt = mr_ring.get()
            P.dma("sp", mt[:, :], mv[c], writes=[mt])
            sq = sq_ring.get()
            P.op("act", "activation", out=sq[:, 0:MEMLEN], in_=mt[:, :], func=AF.Square, reads=[mt], writes=[sq])
            P.op("pe", "matmul", ps_m[:, 0:MEMLEN], ones_bf[:, :], sq[:, 0:MEMLEN], start=(c == 0), stop=(c == NKC - 1),
                 reads=[sq, ones_bf], writes=[ps_m])
        P.op("dve", "tensor_scalar", rstd[:, 0:MEMLEN], ps_m[:, 0:MEMLEN], 1.0 / D, EPS, ALU.mult, ALU.add, reads=[ps_m], writes=[rstd])
        P.op("act", "activation", out=rstd[:, 0:MEMLEN], in_=rstd[:, 0:MEMLEN], func=AF.Sqrt, reads=[rstd], writes=[rstd])
        P.op("dve", "reciprocal", rstd[:, 0:MEMLEN], rstd[:, 0:MEMLEN], reads=[rstd], writes=[rstd])
        for c in range(NKC):
            mt = mr_ring.get()
            P.dma("sp", mt[:, :], mv[c], writes=[mt])
            P.op("dve", "scalar_tensor_tensor", out=mhT[c][:, :], in0=mt[:, :], scalar=g_mm[:, c:c + 1], in1=rstd[:, 0:MEMLEN],
                 op0=ALU.mult, op1=ALU.mult, reads=[mt, g_mm, rstd], writes=[mhT[c]])

        def dst_k(fc, h0, w):
            return kmT[fc][:, h0:h0 + w], kmT[fc]
        lin_fm(P, pp, mhT, wk, 512, w_ring, MEMLEN, headnorm_consume(P, pp, ones_bf, mkg, sq_ring, sg_ring, dst_k))

        def cons_v(tt, c0, cw, ps):
            P.op("act", "activation", out=vm[tt][:, c0:c0 + cw], in_=ps[:, 0:cw], func=AF.Copy, reads=[ps], writes=[vm[tt]])
        lin_tm(P, pp, mhT, wv, 512, w_ring, MEMLEN, cons_v)
        rmsnorm_fm(P, pp, xT, g_xm, hT, ones_bf, sq_ring, rstd, NKC, T, D)
        qmT = aT[0:4]
        oT = aT[4:8]

        def dst_q(fc, h0, w):
            return qmT[fc][:, h0:h0 + w], qmT[fc]
        lin_fm(P, pp, hT, wq, 512, w_ring, T, headnorm_consume(P, pp, ones_bf, mqg, sq_ring, sg_ring, dst_q))
        for mh in range(4):
            for h0 in range(0, T, 512):
                w = min(512, T - h0)
                ps_o = pp.get(); ps_d = pp.get()
                for kt in range(2):
                    ps_s = pp.get()
                    P.op("pe", "matmul", ps_s[:, 0:w], kmT[mh][:, kt * 128:(kt + 1) * 128], qmT[mh][:, h0:h0 + w],
                         start=True, stop=True, reads=[kmT[mh], qmT[mh]], writes=[ps_s])
                    pT = pT_ring.get()
                    P.op("act", "activation", out=pT[:, 0:w], in_=ps_s[:, 0:w], func=AF.Exp, scale=scale, reads=[ps_s], writes=[pT])
                    P.op("pe", "matmul", ps_o[:, 0:w], vm[kt][:, mh * 128:(mh + 1) * 128], pT[:, 0:w], start=(kt == 0),
                         stop=(kt == 1), reads=[vm[kt], pT], writes=[ps_o])
                    P.op("pe", "matmul", ps_d[:, 0:w], ones_bf[:, :], pT[:, 0:w], start=(kt == 0), stop=(kt == 1),
                         reads=[ones_bf, pT], writes=[ps_d])
                rd = rd_ring.get()
                P.op("dve", "reciprocal", rd[:, 0:w], ps_d[:, 0:w], reads=[ps_d], writes=[rd])
                P.op("dve", "tensor_tensor", out=oT[mh][:, h0:h0 + w], in0=ps_o[:, 0:w], in1=rd[:, 0:w], op=ALU.mult,
                     reads=[ps_o, rd], writes=[oT[mh]])
        lin_fm(P, pp, oT, wo, D, w_ring, T, cons_add, nkc=4)
        rmsnorm_fm(P, pp, xT, g_ff, hT, ones_bf, sq_ring, rstd, NKC, T, D)
        ffn_fm(P, pp, xT, hT, wgu, wdn, aT, wg_ring, wu_ring, wd_ring, sg_ring, T)
        xov = xo.rearrange("(c p) t -> c p t", p=128)
        outs = [P.dma("sp", xov[c], xT[c][:, :], reads=[xT[c]]) for c in range(NKC)]
        P.emit(final_wait_ops=outs)
    return nc


U32 = mybir.dt.uint32


def _xbc_row0(fc):
    if fc < 24:
        j, loc = fc // 6, fc % 6
    elif fc < 32:
        j, loc = (fc - 24) // 2, 6 + (fc - 24) % 2
    else:
        j, loc = (fc - 32) // 2, 8 + (fc - 32) % 2
    return j * 1280 + loc * 128


def build_fused(stop=None, skip_cc=()):
    T = NTOK
    NKC = D // 128
    nc = bass.Bass("TRN2", target_bir_lowering=False)
    di = lambda n, sh, dt=F32: nc.dram_tensor(n, sh, dt, kind="ExternalInput").ap()
    x_in = di("xT", [D, T]); mem_i = di("memT", [D, MEMLEN])
    W = {n: di(n, sh) for n, sh in (("w_gu1", [2, D, 2 * DFF]), ("w_down1", [2, DFF, D]), ("w_in", [2, D, N_IN]),
                                   ("w_out", [2, 4096, D]), ("wq", [2, D, 512]), ("wk", [2, D, 512]), ("wv", [2, D, 512]),
                                   ("wo", [2, 512, D]), ("w_gu2", [2, D, 2 * DFF]), ("w_down2", [2, DFF, D]))}
    G = {n: di(n, [2, 128, 16]) for n in ("ffg1", "mixg", "xmg", "mmg", "ffg2")}
    G.update({n: di(n, [2, 128, 1]) for n in ("qg", "kg", "mqg", "mkg")})
    S = {n: di(n, sh) for n, sh in (("convw", [2, 128, 10, 4]), ("convb", [2, 128, 10]), ("dtb", [2, 128, 384]),
                                   ("alog", [2, 128, 384]), ("dsk", [2, 128, 768]), ("normw", [2, 128, 768]),
                                   ("tri", [128, 128]), ("strict", [128, 128]), ("onesf", [128, 128]), ("ident", [128, 128]),
                                   ("Tna", [128, 8 * 384]), ("Tca", [128, 8 * 384]), ("abias", [128, 512]),
                                   ("gbias", [128, 64]), ("pastmask", [128, 64]), ("onehot", [16, 16 * 128]))}
    IDX = {n: di(n, [128, 1], U32) for n in ("idx_x", "idx_t", "idx_d", "idx_y")}
    out_ap = nc.dram_tensor("xoT", [D, T], F32, kind="ExternalOutput").ap()
    dr = lambda n, sh: Buf(nc.dram_tensor(n, sh, F32).ap(), n)
    xres = dr("xres", [D, T]); xres1 = dr("xres1", [D, T]); q_s = dr("q_s", [1024, T]); ya_s = dr("ya_s", [1024, T])
    kv_send = dr("kv_send", [2048, T]); kv_g = dr("kv_g", [4 * 2048, T])
    x_send = dr("x_send", [5120, T]); x_g = dr("x_g", [4 * 5120, T])
    z_send = dr("z_send", [4 * T, 768]); z_g = dr("z_g", [16 * T, 768])
    dt_send = dr("dt_send", [4 * T, 12]); dt_g = dr("dt_g", [16 * T, 12])
    y_send = dr("y_send", [3072, T]); y_g = dr("y_g", [4 * 3072, T])
    RG = [[0, 1, 2, 3], [4, 5, 6, 7]]
    scale = 128.0 ** -0.5

    with contextlib.ExitStack() as stack:
        P = Prog(nc, stack)
        P.begin_phases()

        def gather(out_ap_, out_buf, src, idx_t, row0, reads=()):
            width = src.t.shape[1]
            return P.add("pool", lambda e: e.indirect_dma_start(
                out=out_ap_, out_offset=None, in_=src.t, in_offset=bass.IndirectOffsetOnAxis(ap=idx_t[:, 0:1], axis=0),
                element_offset=row0 * width), reads=[src, idx_t] + list(reads), writes=[out_buf], is_dma=True)

        def finish():
            with P.phase(final=True):
                tb_ = [P.sbuf([128, T], F32, "fin%d" % i) for i in range(2)]
                sv = xres1.t.rearrange("(c p) t -> c p t", p=128)
                dv_ = out_ap.rearrange("(c p) t -> c p t", p=128)
                for c in range(NKC):
                    P.dma("sp", tb_[c % 2][:, :], sv[c], reads=[xres1], writes=[tb_[c % 2]])
                    P.dma("sp", dv_[c], tb_[c % 2][:, :], reads=[tb_[c % 2]])

        _coll = P.collective

        def coll(kind, groups, a, b, reads=(), writes=(), tag=None):
            if tag in skip_cc:
                return None
            rows = a.shape[0]
            if tag == "dt":
                return _coll(kind, groups, a, b, reads=reads, writes=writes)
            for k in range(rows // 256):
                _coll(kind, groups, a[k * 256:(k + 1) * 256, :], b[k * 1024:(k + 1) * 1024, :], reads=reads, writes=writes)

        for l in range(2):
            with P.phase():
                pp = PsumPool(P, 8)
                xT = [P.sbuf([128, T], F32, "xT%d" % c) for c in range(NKC)]
                hT = [P.sbuf([128, T], BF16, "hT%d" % c) for c in range(NKC)]
                aT = [P.sbuf([128, T], BF16, "aT%d" % c) for c in range(12)]
                g1 = P.sbuf([128, NKC], F32, "g1"); g2 = P.sbuf([128, NKC], F32, "g2")
                qg = P.sbuf([128, 1], F32, "qg"); kg = P.sbuf([128, 1], F32, "kg")
                ones_bf = P.sbuf([128, 128], BF16, "ones")
                rstd = P.sbuf([128, T], F32, "rstd")
                sq_ring = Ring([P.sbuf([128, 512], BF16, "sq%d" % i) for i in range(3)])
                sg_ring = Ring([P.sbuf([128, 512], F32, "sg%d" % i) for i in range(3)])
                wg_ring = Ring([P.sbuf([128, NKC, 256], BF16, "wg%d" % i) for i in range(2)])
                wu_ring = Ring([P.sbuf([128, NKC, 256], BF16, "wu%d" % i) for i in range(2)])
                wd_ring = Ring([P.sbuf([128, 12, 512], BF16, "wd%d" % i) for i in range(2)])
                w_ring = Ring(wg_ring.bufs + wu_ring.bufs)
                st_ring = Ring([P.sbuf([128, 512], F32, "st%d" % i) for i in range(4)])
                P.op("dve", "memset", ones_bf[:, :], 1.0, writes=[ones_bf])
                P.dma("sp", g1[:, :], G["ffg1"][l], writes=[g1]); P.dma("sp", g2[:, :], G["mixg"][l], writes=[g2])
                P.dma("sp", qg[:, :], G["qg"][l], writes=[qg]); P.dma("sp", kg[:, :], G["kg"][l], writes=[kg])
                src = x_in if l == 0 else xres.t
                xv = src.rearrange("(c p) t -> c p t", p=128)
                for c in range(NKC):
                    P.dma("sp", xT[c][:, :], xv[c], reads=([] if l == 0 else [xres]), writes=[xT[c]])
                rmsnorm_fm(P, pp, xT, g1, hT, ones_bf, sq_ring, rstd, NKC, T, D)
                ffn_fm(P, pp, xT, hT, W["w_gu1"][l], W["w_down1"][l], aT, wg_ring, wu_ring, wd_ring, sg_ring, T)
                x1v = xres1.t.rearrange("(c p) t -> c p t", p=128)
                for c in range(NKC):
                    P.dma("sp", x1v[c], xT[c][:, :], reads=[xT[c]], writes=[xres1])
                rmsnorm_fm(P, pp, xT, g2, hT, ones_bf, sq_ring, rstd, NKC, T, D)
                win = W["w_in"][l]
                for (dstb, rbase, col0, gbuf) in ((q_s, 0, OQ, qg), (kv_send, 0, OK_, kg)):
                    holder = {}

                    def dst_of(fc, h0, w, holder=holder):
                        st = st_ring.get()
                        holder["st"] = st
                        return st[:, 0:w], st
                    cons0 = headnorm_consume(P, pp, ones_bf, gbuf, sq_ring, sg_ring, dst_of)

                    def cons(fc, h0, w, ps, cons0=cons0, holder=holder, dstb=dstb, rbase=rbase):
                        cons0(fc, h0, w, ps)
                        st = holder["st"]
                        P.dma("sp", dstb.t[rbase + fc * 128:rbase + (fc + 1) * 128, h0:h0 + w], st[:, 0:w], reads=[st], writes=[dstb])
                    lin_fm(P, pp, hT, win[:, col0:col0 + 1024], 1024, w_ring, T, cons)

                def cons_xbc(fc, h0, w, ps):
                    st = st_ring.get()
                    P.op("act", "activation", out=st[:, 0:w], in_=ps[:, 0:w], func=AF.Copy, reads=[ps], writes=[st])
                    r0 = _xbc_row0(fc)
                    P.dma("sp", x_send.t[r0:r0 + 128, h0:h0 + w], st[:, 0:w], reads=[st], writes=[x_send])
                lin_fm(P, pp, hT, win[:, OXBC:OXBC + NXBC], NXBC, w_ring, T, cons_xbc)

                def cons_v(tt, c0, cw, ps):
                    st = st_ring.get()
                    P.op("dve", "tensor_copy", st[:, 0:cw], ps[:, 0:cw], reads=[ps], writes=[st])
                    P.dma("sp", kv_send.t[1024 + tt * 128:1024 + (tt + 1) * 128, c0:c0 + cw], st[:, 0:cw], reads=[st], writes=[kv_send])
                lin_tm(P, pp, hT, win[:, OV:OV + NV], NV, w_ring, T, cons_v)

                def cons_z(tt, c0, cw, ps):
                    st = st_ring.get()
                    P.op("dve", "tensor_copy", st[:, 0:cw], ps[:, 0:cw], reads=[ps], writes=[st])
                    j, lc0 = c0 // 768, c0 % 768
                    P.dma("sp", z_send.t[j * T + tt * 128:j * T + (tt + 1) * 128, lc0:lc0 + cw], st[:, 0:cw], reads=[st], writes=[z_send])
                lin_tm(P, pp, hT, win[:, OZ:OZ + NZ], NZ, w_ring, T, cons_z)

                def cons_dt(tt, c0, cw, ps):
                    st = st_ring.get()
                    P.op("dve", "tensor_copy", st[:, 0:cw], ps[:, 0:cw], reads=[ps], writes=[st])
                    for j in range(4):
                        P.dma("sp", dt_send.t[j * T + tt * 128:j * T + (tt + 1) * 128, :], st[:, 12 * j:12 * j + 12], reads=[st], writes=[dt_send])
                lin_tm(P, pp, hT, win[:, ODT:ODT + NDT], NDT, w_ring, T, cons_dt)
                coll("AllGather", RG, kv_send.t, kv_g.t, reads=[kv_send], writes=[kv_g], tag="kv")

            if stop == (l, "A"):
                finish()
                return nc
            with P.phase():
                coll("AllGather", RG, x_send.t, x_g.t, reads=[x_send], writes=[x_g], tag="x")
                coll("AllGather", RG, z_send.t, z_g.t, reads=[z_send], writes=[z_g], tag="z")
                coll("AllGather", RG, dt_send.t, dt_g.t, reads=[dt_send], writes=[dt_g], tag="dt")
                pp = PsumPool(P, 4)
                pacc = PsumPool(P, 4, pfx="pacc")
                tna = P.sbuf([128, 8, 384], F32, "tna"); tca = P.sbuf([128, 8, 384], F32, "tca")
                abias = P.sbuf([128, 512], F32, "abias"); gbias = P.sbuf([128, 64], F32, "gbias")
                pmask = P.sbuf([128, 64], F32, "pmask"); onehot = P.sbuf([16, 16, 128], BF16, "onehot")
                ident = P.sbuf([128, 128], F32, "ident"); ones_bf = P.sbuf([128, 128], BF16, "ones")
                P.dma("sp", tna[:], S["Tna"].rearrange("p (h u) -> p h u", h=8), writes=[tna])
                P.dma("sp", tca[:], S["Tca"].rearrange("p (h u) -> p h u", h=8), writes=[tca])
                for (b, a) in ((abias, S["abias"]), (gbias, S["gbias"]), (pmask, S["pastmask"]), (ident, S["ident"])):
                    P.dma("sp", b[:], a, writes=[b])
                ohs = P.sbuf([16, 16, 128], F32, "ohs")
                P.dma("sp", ohs[:], S["onehot"].rearrange("k (n m) -> k n m", n=16), writes=[ohs])
                P.op("dve", "tensor_copy", onehot[:], ohs[:], reads=[ohs], writes=[onehot])
                P.op("dve", "memset", ones_bf[:, :], 1.0, writes=[ones_bf])
                kf_ring = Ring([P.sbuf([128, SEQ], F32, "kf%d" % i) for i in range(2)])
                kb_ring = Ring([P.sbuf([128, SEQ], BF16, "kb%d" % i) for i in range(2)])
                qf_ring = Ring([P.sbuf([128, NTOK], F32, "qf%d" % i) for i in range(2)])
                qb_ring = Ring([P.sbuf([128, NTOK], BF16, "qb%d" % i) for i in range(2)])
                ko_ring = Ring([P.sbuf([128, NTOK], BF16, "ko%d" % i) for i in range(2)])
                vb_ring = Ring([P.sbuf([128, 32, 128], BF16, "vb%d" % i) for i in range(2)])
                vo_ring = Ring([P.sbuf([128, 8, 128], BF16, "vo%d" % i) for i in range(2)])
                kos_ring = Ring([P.sbuf([128, NTOK], F32, "kos%d" % i) for i in range(2)])
                vbs_ring = Ring([P.sbuf([128, 32, 128], F32, "vbs%d" % i) for i in range(2)])
                vos_ring = Ring([P.sbuf([128, 8, 128], F32, "vos%d" % i) for i in range(2)])
                km_ring = Ring([P.sbuf([128, 16], F32, "km%d" % i) for i in range(2)])
                gm_ring = Ring([P.sbuf([128, 16], F32, "gm%d" % i) for i in range(2)])
                t8_ring = Ring([P.sbuf([128, 8], F32, "t8%d" % i) for i in range(2)])
                sel_ring = Ring([P.sbuf([128, 16], F32, "sel%d" % i) for i in range(2)])
                ns_ring = Ring([P.sbuf([16, 256], BF16, "ns%d" % i) for i in range(2)])
                lg_ring = Ring([P.sbuf([128, 256], F32, "lg%d" % i) for i in range(4)])
                pT_ring = Ring([P.sbuf([128, 256], BF16, "pT%d" % i) for i in range(8)])
                rd_ring = Ring([P.sbuf([128, 256], F32, "rd%d" % i) for i in range(2)])
                o_ring = Ring([P.sbuf([128, 256], F32, "oo%d" % i) for i in range(2)])
                for h in range(8):
                    hr = slice(h * 128, (h + 1) * 128)
                    kf = kf_ring.get(); kb = kb_ring.get(); qf = qf_ring.get(); qb = qb_ring.get()
                    ko = ko_ring.get(); vb = vb_ring.get(); vo = vo_ring.get(); km = km_ring.get()
                    kos = kos_ring.get(); vbs = vbs_ring.get(); vos = vos_ring.get()
                    for sgm in range(4):
                        kr0 = (h // 2) * 1024 + sgm * 256 + (h % 2) * 128
                        P.dma("sp", kf[:, sgm * T:(sgm + 1) * T], kv_g.t[kr0:kr0 + 128, :], reads=[kv_g], writes=[kf])
                        for a_ in range(4):
                            vr0 = (4 + a_) * 1024 + sgm * 256
                            P.dma("sp", vbs[:, sgm * 8 + 2 * a_:sgm * 8 + 2 * a_ + 2, :],
                                  kv_g.t[vr0:vr0 + 256, hr].rearrange("(t p) d -> p t d", p=128), reads=[kv_g], writes=[vbs])
                    P.dma("sp", qf[:, :], q_s.t[hr, :], reads=[q_s], writes=[qf])
                    P.dma("sp", kos[:, :], kv_send.t[hr, :], reads=[kv_send], writes=[kos])
                    P.dma("sp", vos[:], kv_send.t[1024:2048, hr].rearrange("(t p) d -> p t d", p=128), reads=[kv_send], writes=[vos])
                    P.op("act", "activation", out=ko[:, :], in_=kos[:, :], func=AF.Copy, reads=[kos], writes=[ko])
                    P.op("dve", "tensor_copy", vb[:], vbs[:], reads=[vbs], writes=[vb])
                    P.op("dve", "tensor_copy", vo[:], vos[:], reads=[vos], writes=[vo])
                    P.op("act", "activation", out=kb[:, :], in_=kf[:, :], func=AF.Copy, reads=[kf], writes=[kb])
                    P.op("act", "activation", out=qb[:, :], in_=qf[:, :], func=AF.Copy, reads=[qf], writes=[qb])
                    P.op("dve", "tensor_reduce", out=km[:, :], in_=kf[:, :].rearrange("p (n s) -> p n s", s=256), axis=AX.X,
                         op=ALU.add, reads=[kf], writes=[km])
                    P.op("dve", "tensor_scalar", km[:, :], km[:, :], 1.0 / 256, None, ALU.mult, reads=[km], writes=[km])
                    for qi in range(4):
                        qs_ = slice(qi * 256, (qi + 1) * 256)
                        ns = ns_ring.get()
                        for qs in range(2):
                            ps_g = pp.get()
                            P.op("pe", "matmul", ps_g[:, 0:16], qf[:, qi * 256 + qs * 128:qi * 256 + (qs + 1) * 128], km[:, :],
                                 start=True, stop=True, reads=[qf, km], writes=[ps_g])
                            gm = gm_ring.get(); t8 = t8_ring.get(); sel = sel_ring.get()
                            P.op("dve", "tensor_tensor", out=gm[:, :], in0=ps_g[:, 0:16], in1=gbias[:, qi * 16:(qi + 1) * 16],
                                 op=ALU.add, reads=[ps_g, gbias], writes=[gm])
                            P.op("dve", "max", out=t8[:, :], in_=gm[:, :], reads=[gm], writes=[t8])
                            P.op("dve", "tensor_scalar", sel[:, :], gm[:, :], t8[:, 2:3], None, ALU.is_ge, reads=[gm, t8], writes=[sel])
                            P.op("dve", "tensor_tensor", out=sel[:, :], in0=sel[:, :], in1=pmask[:, qi * 16:(qi + 1) * 16],
                                 op=ALU.mult, reads=[sel, pmask], writes=[sel])
                            P.op("dve", "tensor_scalar", sel[:, :], sel[:, :], -1.0, -BIGNEG, ALU.add, ALU.mult, reads=[sel], writes=[sel])
                            ps_t = pp.get()
                            P.op("pe", "transpose", ps_t[0:16, 0:128], sel[:, :], ident[:, :], reads=[sel, ident], writes=[ps_t])
                            P.op("act", "activation", out=ns[:, qs * 128:(qs + 1) * 128], in_=ps_t[0:16, 0:128], func=AF.Copy,
                                 reads=[ps_t], writes=[ns])
                        ps_o = pacc.get(); ps_d = pacc.get()
                        tiles = [(n, kt) for n in range(NBLK) for kt in range(2)] + [(-1, 0), (-1, 1)]
                        LA = 3
                        pend = {}

                        def stage1(ti):
                            n, kt = tiles[ti]
                            ps_s = pp.get()
                            lg = lg_ring.get(); pT = pT_ring.get()
                            tsl = slice(128, 384) if kt == 0 else slice(0, 256)
                            if n >= 0:
                                P.op("pe", "matmul", ps_s[:, 0:256], kb[:, n * 256 + kt * 128:n * 256 + (kt + 1) * 128], qb[:, qs_],
                                     start=True, stop=False, reads=[kb, qb], writes=[ps_s])
                                P.op("pe", "matmul", ps_s[:, 0:256], onehot[:, n, :], ns[:, :], start=False, stop=True,
                                     reads=[onehot, ns], writes=[ps_s])
                                P.op("dve", "scalar_tensor_tensor", out=lg[:, :], in0=ps_s[:, 0:256], scalar=scale, in1=tna[:, h, tsl],
                                     op0=ALU.mult, op1=ALU.add, reads=[ps_s, tna], writes=[lg])
                                bi = (h * 4 + qi) * 16 + n
                                P.op("act", "activation", out=pT[:, :], in_=lg[:, :], func=AF.Exp, bias=abias[:, bi:bi + 1],
                                     reads=[lg, abias], writes=[pT])
                                pend[ti] = (vb[:, n * 2 + kt, :], vb, pT)
                            else:
                                P.op("pe", "matmul", ps_s[:, 0:256], ko[:, qi * 256 + kt * 128:qi * 256 + (kt + 1) * 128], qb[:, qs_],
                                     start=True, stop=True, reads=[ko, qb], writes=[ps_s])
                                P.op("dve", "scalar_tensor_tensor", out=lg[:, :], in0=ps_s[:, 0:256], scalar=scale, in1=tca[:, h, tsl],
                                     op0=ALU.mult, op1=ALU.add, reads=[ps_s, tca], writes=[lg])
                                P.op("act", "activation", out=pT[:, :], in_=lg[:, :], func=AF.Exp, reads=[lg], writes=[pT])
                                pend[ti] = (vo[:, qi * 2 + kt, :], vo, pT)

                        def stage2(ti):
                            vl, vbuf, pT = pend.pop(ti)
                            first, last = (ti == 0), (ti == len(tiles) - 1)
                            P.op("pe", "matmul", ps_o[:, 0:256], vl, pT[:, :], start=first, stop=last, reads=[vbuf, pT], writes=[ps_o])
                            P.op("pe", "matmul", ps_d[:, 0:256], ones_bf[:, :], pT[:, :], start=first, stop=last,
                                 reads=[ones_bf, pT], writes=[ps_d])
                        for ti in range(len(tiles) + LA):
                            if ti < len(tiles):
                                stage1(ti)
                            if ti - LA >= 0:
                                stage2(ti - LA)
                        rd = rd_ring.get(); ot = o_ring.get()
                        P.op("dve", "reciprocal", rd[:, :], ps_d[:, 0:256], reads=[ps_d], writes=[rd])
                        P.op("dve", "tensor_tensor", out=ot[:, :], in0=ps_o[:, 0:256], in1=rd[:, :], op=ALU.mult,
                             reads=[ps_o, rd], writes=[ot])
                        P.dma("sp", ya_s.t[hr, qs_], ot[:, :], reads=[ot], writes=[ya_s])

            if stop == (l, "C1"):
                finish()
                return nc
            with P.phase():
                pp = PsumPool(P, 8)
                cw = P.sbuf([128, 10, 4], F32, "cw"); cb = P.sbuf([128, 10], F32, "cb")
                dt_all = P.sbuf([128, 384], F32, "dt_all"); da_all = P.sbuf([128, 384], F32, "da_all")
                tmpa = P.sbuf([128, 384], F32, "tmpa"); tmpb = P.sbuf([128, 384], F32, "tmpb")
                dsk = P.sbuf([128, 12, 64], F32, "dsk"); normw = P.sbuf([128, 768], F32, "normw")
                tri = P.sbuf([128, 128], F32, "tri"); strict = P.sbuf([128, 128], F32, "strict")
                onesf = P.sbuf([128, 128], F32, "onesf"); ident = P.sbuf([128, 128], F32, "ident")
                idx_x = P.sbuf([128, 1], U32, "idx_x"); idx_t = P.sbuf([128, 1], U32, "idx_t"); idx_d = P.sbuf([128, 1], U32, "idx_d")
                for (b, a) in ((cw, S["convw"][l]), (cb, S["convb"][l]), (tmpa, S["dtb"][l]), (tmpb, S["alog"][l]),
                               (normw, S["normw"][l]), (tri, S["tri"]), (strict, S["strict"]), (onesf, S["onesf"]),
                               (ident, S["ident"]), (idx_x, IDX["idx_x"]), (idx_t, IDX["idx_t"]), (idx_d, IDX["idx_d"])):
                    P.dma("sp", b[:], a, writes=[b])
                P.dma("sp", dsk[:], S["dsk"][l].rearrange("p (j d) -> p j d", d=64), writes=[dsk])
                for c in range(NCH):
                    sgm, lc = c // 8, c % 8
                    gather(dt_all[:, c * 12:(c + 1) * 12], dt_all, dt_g, idx_d, sgm * 4 * T + lc * 128)
                P.op("dve", "tensor_tensor", out=dt_all[:], in0=dt_all[:], in1=tmpa[:], op=ALU.add, reads=[dt_all, tmpa], writes=[dt_all])
                P.op("act", "activation", out=dt_all[:], in_=dt_all[:], func=AF.Exp, reads=[dt_all], writes=[dt_all])
                P.op("dve", "tensor_scalar", dt_all[:], dt_all[:], 1.0, None, ALU.add, reads=[dt_all], writes=[dt_all])
                P.op("act", "activation", out=dt_all[:], in_=dt_all[:], func=AF.Ln, reads=[dt_all], writes=[dt_all])
                P.op("act", "activation", out=tmpb[:], in_=tmpb[:], func=AF.Exp, reads=[tmpb], writes=[tmpb])
                P.op("dve", "scalar_tensor_tensor", out=da_all[:], in0=dt_all[:], scalar=-1.0, in1=tmpb[:], op0=ALU.mult,
                     op1=ALU.mult, reads=[dt_all, tmpb], writes=[da_all])
                h = [P.sbuf([128, 6, 64], F32, "h%d" % g) for g in range(2)]
                hb = [P.sbuf([128, 6, 64], BF16, "hb%d" % g) for g in range(2)]
                for g in range(2):
                    P.op("pool", "memset", h[g][:], 0.0, writes=[h[g]])
                    P.op("pool", "memset", hb[g][:], 0.0, writes=[hb[g]])
                halo = [P.sbuf([128, 4], F32, "halo%d" % i) for i in range(10)]
                xr_ring = Ring([P.sbuf([128, T + 3], F32, "xr%d" % i) for i in range(3)])
                acc_ring = Ring([P.sbuf([128, 512], F32, "acc%d" % i) for i in range(2)])
                xc_sets = [[P.sbuf([128, T], F32, "xc%d_%d" % (k, i)) for i in range(8)] for k in range(2)]
                bc_sets = [[P.sbuf([128, T], BF16, "bc%d_%d" % (k, i)) for i in range(4)] for k in range(2)]
                xt_ring = Ring([P.sbuf([128, 12, 64], F32, "xt%d" % i) for i in range(2)])
                bt_ring = Ring([P.sbuf([128, 256], BF16, "bt%d" % i) for i in range(2)])
                E_ring = Ring([P.sbuf([128, 36], F32, "E%d" % i) for i in range(2)])
                s2_ring = Ring([P.sbuf([128, 12], F32, "s2%d" % i) for i in range(2)])
                xdt_ring = Ring([P.sbuf([128, 12, 64], BF16, "xdt%d" % i) for i in range(2)])
                xw_ring = Ring([P.sbuf([128, 12, 64], BF16, "xw%d" % i) for i in range(2)])
                xd_ring = Ring([P.sbuf([128, 12, 64], F32, "xd%d" % i) for i in range(2)])
                cbm_ring = Ring([P.sbuf([128, 128], F32, "cbm%d" % i) for i in range(2)])
                ajall_ring = Ring([P.sbuf([128, 12, 128], F32, "ajall%d" % i) for i in range(2)])
                scall_ring = Ring([P.sbuf([128, 12, 128], BF16, "scall%d" % i) for i in range(2)])
                dec_ring = Ring([P.sbuf([128, 512], F32, "dec%d" % i) for i in range(3)])
                y_ring = Ring([P.sbuf([128, 12, 64], F32, "y%d" % i) for i in range(2)])
                t1_ring = Ring([P.sbuf([128, 6, 64], F32, "t1%d" % i) for i in range(2)])
                z_ring = Ring([P.sbuf([128, 768], F32, "z%d" % i) for i in range(2)])
                sq_ring = Ring([P.sbuf([128, 768], F32, "sqq%d" % i) for i in range(2)])
                ss_ring = Ring([P.sbuf([128, 2], F32, "ss%d" % i) for i in range(2)])
                o_ring = Ring([P.sbuf([128, 768], F32, "o%d" % i) for i in range(2)])
                yT_ring = Ring([P.sbuf([128, 6, 128], F32, "yTt%d" % i) for i in range(2)])
                ysv = y_send.t.rearrange("(s f p) t -> s p f t", s=4, p=128)
                y_seg = [Buf(None, "y_seg%d" % i) for i in range(4)]
                for sgm in range(4):
                    xc = xc_sets[sgm % 2]
                    bc = bc_sets[sgm % 2]
                    for ch in range(10):
                        xr = xr_ring.get()
                        gather(xr[:, 3:T + 3], xr, x_g, idx_x, (ch // 2) * 1024 + (ch % 2) * 128 + sgm * 256)
                        if sgm == 0:
                            P.op("pool", "memset", xr[:, 0:3], 0.0, writes=[xr])
                        else:
                            P.op("act", "activation", out=xr[:, 0:3], in_=halo[ch][:, 0:3], func=AF.Copy, reads=[halo[ch]], writes=[xr])
                        P.op("act", "activation", out=halo[ch][:, 0:3], in_=xr[:, T:T + 3], func=AF.Copy, reads=[xr], writes=[halo[ch]])
                        for h0 in (0, 512):
                            acc = acc_ring.get()
                            P.op("dve", "tensor_scalar", acc[:, :], xr[:, h0:h0 + 512], cw[:, ch, 0:1], cb[:, ch:ch + 1], ALU.mult, ALU.add,
                                 reads=[xr, cw, cb], writes=[acc])
                            for k in range(1, 4):
                                P.op("dve", "scalar_tensor_tensor", out=acc[:, :], in0=xr[:, h0 + k:h0 + k + 512], scalar=cw[:, ch, k:k + 1],
                                     in1=acc[:, :], op0=ALU.mult, op1=ALU.add, reads=[xr, cw, acc], writes=[acc])
                            if ch < 8:
                                P.op("act", "activation", out=xc[ch][:, h0:h0 + 512], in_=acc[:, :], func=AF.Silu, reads=[acc], writes=[xc[ch]])
                                if ch >= 6:
                                    P.op("dve", "tensor_copy", bc[ch - 6][:, h0:h0 + 512], xc[ch][:, h0:h0 + 512], reads=[xc[ch]], writes=[bc[ch - 6]])
                            else:
                                P.op("act", "activation", out=bc[ch - 6][:, h0:h0 + 512], in_=acc[:, :], func=AF.Silu, reads=[acc], writes=[bc[ch - 6]])
                    for lc in range(8):
                        c = sgm * 8 + lc
                        ts = slice(lc * 128, (lc + 1) * 128)
                        xt = xt_ring.get(); bt = bt_ring.get()
                        xtf = xt[:].rearrange("p j d -> p (j d)")
                        for (lo, n) in ((0, 4), (4, 2)):
                            ps = pp.get()
                            for i in range(n):
                                P.op("pe", "transpose", ps[:, i * 128:(i + 1) * 128], xc[lo + i][:, ts], ident[:, :],
                                     reads=[xc[lo + i], ident], writes=[ps])
                            P.op("dve", "tensor_copy", xtf[:, lo * 128:(lo + n) * 128], ps[:, 0:n * 128], reads=[ps], writes=[xt])
                        ps = pp.get()
                        for i in range(2):
                            P.op("pe", "transpose", ps[:, i * 128:(i + 1) * 128], xc[6 + i][:, ts], ident[:, :],
                                 reads=[xc[6 + i], ident], writes=[ps])
                        P.op("act", "activation", out=bt[:, :], in_=ps[:, 0:256], func=AF.Copy, reads=[ps], writes=[bt])
                        dac = da_all[:, c * 12:(c + 1) * 12]
                        dtc = dt_all[:, c * 12:(c + 1) * 12]
                        ps = pp.get()
                        P.op("pe", "matmul", ps[:, 0:12], tri[:, :], dac, start=True, stop=True, reads=[tri, da_all], writes=[ps])
                        P.op("pe", "matmul", ps[:, 12:24], strict[:, :], dac, start=True, stop=True, reads=[strict, da_all], writes=[ps])
                        P.op("pe", "matmul", ps[:, 24:36], onesf[:, :], dac, start=True, stop=True, reads=[onesf, da_all], writes=[ps])
                        E = E_ring.get()
                        P.op("act", "activation", out=E[:, :], in_=ps[:, 0:36], func=AF.Exp, reads=[ps], writes=[E])
                        s2 = s2_ring.get()
                        P.op("dve", "tensor_tensor", out=s2[:, :], in0=dtc, in1=E[:, 12:24], op=ALU.mult, reads=[dt_all, E], writes=[s2])
                        xdt = xdt_ring.get(); xw = xw_ring.get(); xd = xd_ring.get()
                        P.op("dve", "tensor_tensor", out=xdt[:], in0=xt[:], in1=dtc.unsqueeze(2).broadcast_to([128, 12, 64]),
                             op=ALU.mult, reads=[xt, dt_all], writes=[xdt])
                        P.op("dve", "tensor_tensor", out=xw[:], in0=xt[:], in1=s2[:, :].unsqueeze(2).broadcast_to([128, 12, 64]),
                             op=ALU.mult, reads=[xt, s2], writes=[xw])
                        P.op("dve", "tensor_tensor", out=xd[:], in0=xt[:], in1=dsk[:], op=ALU.mult, reads=[xt, dsk], writes=[xd])
                        y = y_ring.get()
                        ajall = ajall_ring.get()
                        P.op("dve", "tensor_tensor", out=ajall[:], in0=strict[:, :].unsqueeze(1).broadcast_to([128, 12, 128]),
                             in1=dac.unsqueeze(2).broadcast_to([128, 12, 128]), op=ALU.mult, reads=[strict, da_all], writes=[ajall])
                        cbms = []; yos = []
                        for gi in range(2):
                            BT = bc[gi]; CT = bc[2 + gi]
                            ps_cb = pp.get()
                            P.op("pe", "matmul", ps_cb[:, 0:128], BT[:, ts], CT[:, ts], start=True, stop=True, reads=[BT, CT], writes=[ps_cb])
                            cbm = cbm_ring.get()
                            P.op("dve", "tensor_tensor", out=cbm[:, :], in0=ps_cb[:, 0:128], in1=tri[:, :], op=ALU.mult,
                                 reads=[ps_cb, tri], writes=[cbm])
                            ps_yo = pp.get()
                            P.op("pe", "matmul", ps_yo[:, 0:384], CT[:, ts], hb[gi][:].rearrange("p j d -> p (j d)"),
                                 start=True, stop=True, reads=[CT, hb[gi]], writes=[ps_yo])
                            cbms.append(cbm); yos.append(ps_yo)
                        scall = scall_ring.get()
                        for gi in range(2):
                            for (j0, nh) in ((0, 4), (4, 2)):
                                ps_seg = pp.get()
                                for i in range(nh):
                                    j = gi * 6 + j0 + i
                                    P.op("pe", "matmul", ps_seg[:, i * 128:(i + 1) * 128], ajall[:, j, :], tri[:, :], start=True, stop=True,
                                         reads=[ajall, tri], writes=[ps_seg])
                                dec = dec_ring.get()
                                P.op("act", "activation", out=dec[:, 0:nh * 128], in_=ps_seg[:, 0:nh * 128], func=AF.Exp, reads=[ps_seg], writes=[dec])
                                P.op("dve", "tensor_tensor", out=scall[:, gi * 6 + j0:gi * 6 + j0 + nh, :],
                                     in0=dec[:, 0:nh * 128].rearrange("p (j s) -> p j s", s=128),
                                     in1=cbms[gi][:, :].unsqueeze(1).broadcast_to([128, nh, 128]), op=ALU.mult,
                                     reads=[dec, cbms[gi]], writes=[scall])
                        for gi in range(2):
                            ps_yd = pp.get()
                            ps_yo = yos[gi]
                            for jj in range(6):
                                j = gi * 6 + jj
                                P.op("pe", "matmul", ps_yd[:, jj * 64:(jj + 1) * 64], scall[:, j, :], xdt[:, j, :], start=True, stop=True,
                                     reads=[scall, xdt], writes=[ps_yd])
                            t1 = t1_ring.get()
                            P.op("dve", "tensor_tensor", out=t1[:], in0=ps_yo[:, 0:384].rearrange("p (j d) -> p j d", d=64),
                                 in1=E[:, gi * 6:gi * 6 + 6].unsqueeze(2).broadcast_to([128, 6, 64]), op=ALU.mult,
                                 reads=[ps_yo, E], writes=[t1])
                            P.op("dve", "tensor_tensor", out=t1[:], in0=ps_yd[:, 0:384].rearrange("p (j d) -> p j d", d=64),
                                 in1=t1[:], op=ALU.add, reads=[ps_yd, t1], writes=[t1])
                            P.op("dve", "tensor_tensor", out=y[:, gi * 6:(gi + 1) * 6, :], in0=xd[:, gi * 6:(gi + 1) * 6, :], in1=t1[:],
                                 op=ALU.add, reads=[xd, t1], writes=[y])
                            ps_st = pp.get()
                            P.op("pe", "matmul", ps_st[:, 0:384], bt[:, gi * 128:(gi + 1) * 128],
                                 xw[:, gi * 6:(gi + 1) * 6, :].rearrange("p j d -> p (j d)"), start=True, stop=True,
                                 reads=[bt, xw], writes=[ps_st])
                            P.op("dve", "tensor_tensor", out=h[gi][:], in0=h[gi][:],
                                 in1=E[:, 24 + gi * 6:24 + gi * 6 + 6].unsqueeze(2).broadcast_to([128, 6, 64]), op=ALU.mult,
                                 reads=[h[gi], E], writes=[h[gi]])
                            P.op("dve", "tensor_tensor", out=h[gi][:], in0=ps_st[:, 0:384].rearrange("p (j d) -> p j d", d=64),
                                 in1=h[gi][:], op=ALU.add, reads=[ps_st, h[gi]], writes=[h[gi]])
                            P.op("act", "activation", out=hb[gi][:], in_=h[gi][:], func=AF.Copy, reads=[h[gi]], writes=[hb[gi]])
                        zt = z_ring.get()
                        gather(zt[:, :], zt, z_g, idx_t, (lc // 2) * 1024 + sgm * 256 + (lc % 2) * 128)
                        P.op("act", "activation", out=zt[:, :], in_=zt[:, :], func=AF.Silu, reads=[zt], writes=[zt])
                        yf = y[:].rearrange("p j d -> p (j d)")
                        P.op("dve", "tensor_tensor", out=yf, in0=yf, in1=zt[:, :], op=ALU.mult, reads=[y, zt], writes=[y])
                        sq = sq_ring.get()
                        P.op("act", "activation", out=sq[:, :], in_=yf, func=AF.Square, reads=[y], writes=[sq])
                        ss = ss_ring.get()
                        P.op("dve", "tensor_reduce", out=ss[:, :], in_=sq[:, :].rearrange("p (g f) -> p g f", g=2), axis=AX.X, op=ALU.add,
                             reads=[sq], writes=[ss])
                        P.op("dve", "tensor_scalar", ss[:, :], ss[:, :], 1.0 / 384, EPS, ALU.mult, ALU.add, reads=[ss], writes=[ss])
                        P.op("act", "activation", out=ss[:, :], in_=ss[:, :], func=AF.Sqrt, reads=[ss], writes=[ss])
                        P.op("dve", "reciprocal", ss[:, :], ss[:, :], reads=[ss], writes=[ss])
                        ot = o_ring.get()
                        for gi in range(2):
                            P.op("dve", "scalar_tensor_tensor", out=ot[:, gi * 384:(gi + 1) * 384], in0=yf[:, gi * 384:(gi + 1) * 384],
                                 scalar=ss[:, gi:gi + 1], in1=normw[:, gi * 384:(gi + 1) * 384], op0=ALU.mult, op1=ALU.mult,
                                 reads=[y, ss, normw], writes=[ot])
                        yTt = yT_ring.get()
                        yTf = yTt[:].rearrange("p f t -> p (f t)")
                        for (lo, n) in ((0, 4), (4, 2)):
                            ps = pp.get()
                            for i in range(n):
                                P.op("pe", "transpose", ps[:, i * 128:(i + 1) * 128], ot[:, (lo + i) * 128:(lo + i + 1) * 128], ident[:, :],
                                     reads=[ot, ident], writes=[ps])
                            P.op("act", "activation", out=yTf[:, lo * 128:(lo + n) * 128], in_=ps[:, 0:n * 128], func=AF.Copy,
                                 reads=[ps], writes=[yTt])
                        P.dma("sp", ysv[sgm][:, :, ts], yTt[:], reads=[yTt], writes=[y_seg[sgm]])
                    if "y" not in skip_cc:
                        for k in range(3 * sgm, 3 * sgm + 3):
                            _coll("AllGather", RG, y_send.t[k * 256:(k + 1) * 256, :], y_g.t[k * 1024:(k + 1) * 1024, :],
                                  reads=[y_seg[sgm]], writes=[y_g])

            if stop == (l, "B"):
                finish()
                return nc
            with P.phase(final=(l == 1)):
                pp = PsumPool(P, 8)
                xT = [P.sbuf([128, T], F32, "xT%d" % c) for c in range(NKC)]
                hT = [P.sbuf([128, T], BF16, "hT%d" % c) for c in range(NKC)]
                aT = [P.sbuf([128, T], BF16, "aT%d" % c) for c in range(12)]
                g_xm = P.sbuf([128, NKC], F32, "g_xm"); g_mm = P.sbuf([128, NKC], F32, "g_mm"); g_ff = P.sbuf([128, NKC], F32, "g_ff")
                mqg = P.sbuf([128, 1], F32, "mqg"); mkg = P.sbuf([128, 1], F32, "mkg")
                idx_y = P.sbuf([128, 1], U32, "idx_y")
                ones_bf = P.sbuf([128, 128], BF16, "ones")
                rstd = P.sbuf([128, T], F32, "rstd")
                sq_ring = Ring([P.sbuf([128, 512], BF16, "sq%d" % i) for i in range(3)])
                sg_ring = Ring([P.sbuf([128, 512], F32, "sg%d" % i) for i in range(3)])
                wg_ring = Ring([P.sbuf([128, NKC, 256], BF16, "wg%d" % i) for i in range(2)])
                wu_ring = Ring([P.sbuf([128, NKC, 256], BF16, "wu%d" % i) for i in range(2)])
                wd_ring = Ring([P.sbuf([128, 12, 512], BF16, "wd%d" % i) for i in range(2)])
                w_ring = Ring(wg_ring.bufs + wu_ring.bufs)
                mr_ring = Ring([P.sbuf([128, MEMLEN], F32, "mr%d" % i) for i in range(3)])
                mhT = [P.sbuf([128, MEMLEN], BF16, "mhT%d" % c) for c in range(NKC)]
                kmT = [P.sbuf([128, MEMLEN], BF16, "kmT%d" % c) for c in range(4)]
                vm = [P.sbuf([128, 512], BF16, "vm%d" % c) for c in range(2)]
                pT_ring = sq_ring
                rd_ring = sg_ring
                ystg = Ring([rstd])
                xv = xres1.t.rearrange("(c p) t -> c p t", p=128)
                yav = ya_s.t.rearrange("(c p) t -> c p t", p=128)
                mv = mem_i.rearrange("(c p) t -> c p t", p=128)
                P.op("dve", "memset", ones_bf[:, :], 1.0, writes=[ones_bf])
                for (b, a) in ((g_xm, G["xmg"][l]), (g_mm, G["mmg"][l]), (g_ff, G["ffg2"][l]), (mqg, G["mqg"][l]),
                               (mkg, G["mkg"][l]), (idx_y, IDX["idx_y"])):
                    P.dma("sp", b[:], a, writes=[b])
                for c in range(NKC):
                    P.dma("sp", xT[c][:, :], xv[c], reads=[xres1], writes=[xT[c]])

                def cons_add(fc, h0, w, ps):
                    P.op("dve", "tensor_tensor", out=xT[fc][:, h0:h0 + w], in0=ps[:, 0:w], in1=xT[fc][:, h0:h0 + w], op=ALU.add,
                         reads=[ps, xT[fc]], writes=[xT[fc]])
                for kh in range(2):
                    for c in range(NKC):
                        m = kh * NKC + c
                        if m < 8:
                            P.dma("pool", hT[c][:, :], yav[m], reads=[ya_s], writes=[hT[c]])
                        else:
                            ms = m - 8
                            r, fcx = ms // 6, ms % 6
                            stg = ystg.get()
                            gather(stg[:, :], stg, y_g, idx_y, (fcx // 2) * 1024 + r * 256 + (fcx % 2) * 128)
                            P.op("act", "activation", out=hT[c][:, :], in_=stg[:, :], func=AF.Copy, reads=[stg], writes=[hT[c]])
                    lin_fm(P, pp, hT, W["w_out"][l][kh * D:(kh + 1) * D, :], D, w_ring, T, cons_add)
                ps_m = pp.get()
                for c in range(NKC):
                    mt = mr_ring.get()
                    P.dma("sp", mt[:, :], mv[c], writes=[mt])
                    sq = sq_ring.get()
                    P.op("act", "activation", out=sq[:, 0:MEMLEN], in_=mt[:, :], func=AF.Square, reads=[mt], writes=[sq])
                    P.op("pe", "matmul", ps_m[:, 0:MEMLEN], ones_bf[:, :], sq[:, 0:MEMLEN], start=(c == 0), stop=(c == NKC - 1),
                         reads=[sq, ones_bf], writes=[ps_m])
                P.op("dve", "tensor_scalar", rstd[:, 0:MEMLEN], ps_m[:, 0:MEMLEN], 1.0 / D, EPS, ALU.mult, ALU.add, reads=[ps_m], writes=[rstd])
                P.op("act", "activation", out=rstd[:, 0:MEMLEN], in_=rstd[:, 0:MEMLEN], func=AF.Sqrt, reads=[rstd], writes=[rstd])
                P.op("dve", "reciprocal", rstd[:, 0:MEMLEN], rstd[:, 0:MEMLEN], reads=[rstd], writes=[rstd])
                for c in range(NKC):
                    mt = mr_ring.get()
                    P.dma("sp", mt[:, :], mv[c], writes=[mt])
                    P.op("dve", "scalar_tensor_tensor", out=mhT[c][:, :], in0=mt[:, :], scalar=g_mm[:, c:c + 1], in1=rstd[:, 0:MEMLEN],
                         op0=ALU.mult, op1=ALU.mult, reads=[mt, g_mm, rstd], writes=[mhT[c]])

                def dst_k(fc, h0, w):
                    return kmT[fc][:, h0:h0 + w], kmT[fc]
                lin_fm(P, pp, mhT, W["wk"][l], 512, w_ring, MEMLEN, headnorm_consume(P, pp, ones_bf, mkg, sq_ring, sg_ring, dst_k))

                def cons_vm(tt, c0, cw_, ps):
                    P.op("act", "activation", out=vm[tt][:, c0:c0 + cw_], in_=ps[:, 0:cw_], func=AF.Copy, reads=[ps], writes=[vm[tt]])
                lin_tm(P, pp, mhT, W["wv"][l], 512, w_ring, MEMLEN, cons_vm)
                rmsnorm_fm(P, pp, xT, g_xm, hT, ones_bf, sq_ring, rstd, NKC, T, D)
                qmT = aT[0:4]
                oT = aT[4:8]

                def dst_q(fc, h0, w):
                    return qmT[fc][:, h0:h0 + w], qmT[fc]
                lin_fm(P, pp, hT, W["wq"][l], 512, w_ring, T, headnorm_consume(P, pp, ones_bf, mqg, sq_ring, sg_ring, dst_q))
                for mh in range(4):
                    for h0 in range(0, T, 512):
                        w = min(512, T - h0)
                        ps_o = pp.get(); ps_d = pp.get()
                        for kt in range(2):
                            ps_s = pp.get()
                            P.op("pe", "matmul", ps_s[:, 0:w], kmT[mh][:, kt * 128:(kt + 1) * 128], qmT[mh][:, h0:h0 + w],
                                 start=True, stop=True, reads=[kmT[mh], qmT[mh]], writes=[ps_s])
                            pT = pT_ring.get()
                            P.op("act", "activation", out=pT[:, 0:w], in_=ps_s[:, 0:w], func=AF.Exp, scale=scale, reads=[ps_s], writes=[pT])
                            P.op("pe", "matmul", ps_o[:, 0:w], vm[kt][:, mh * 128:(mh + 1) * 128], pT[:, 0:w], start=(kt == 0),
                                 stop=(kt == 1), reads=[vm[kt], pT], writes=[ps_o])
                            P.op("pe", "matmul", ps_d[:, 0:w], ones_bf[:, :], pT[:, 0:w], start=(kt == 0), stop=(kt == 1),
                                 reads=[ones_bf, pT], writes=[ps_d])
                        rd = rd_ring.get()
                        P.op("dve", "reciprocal", rd[:, 0:w], ps_d[:, 0:w], reads=[ps_d], writes=[rd])
                        P.op("dve", "tensor_tensor", out=oT[mh][:, h0:h0 + w], in0=ps_o[:, 0:w], in1=rd[:, 0:w], op=ALU.mult,
                             reads=[ps_o, rd], writes=[oT[mh]])
                lin_fm(P, pp, oT, W["wo"][l], D, w_ring, T, cons_add, nkc=4)
                rmsnorm_fm(P, pp, xT, g_ff, hT, ones_bf, sq_ring, rstd, NKC, T, D)
                ffn_fm(P, pp, xT, hT, W["w_gu2"][l], W["w_down2"][l], aT, wg_ring, wu_ring, wd_ring, sg_ring, T)
                dst = xres if l == 0 else None
                dv = (xres.t if l == 0 else out_ap).rearrange("(c p) t -> c p t", p=128)
                for c in range(NKC):
                    P.dma("sp", dv[c], xT[c][:, :], reads=[xT[c]], writes=([xres] if l == 0 else []))
    return nc


def fused_inputs(I):
    T = NTOK
    xs = I["x"].astype(np.float32).reshape(NCORES, T, D)
    st2 = lambda k: np.ascontiguousarray(np.stack([_pg(I[k][l]) for l in range(2)]))
    shared = {"w_gu1": I["ff1_w_gu"], "w_down1": I["ff1_w_down"], "w_in": I["w_in"], "w_out": I["w_out"],
              "wq": I["mem_wq"], "wk": I["mem_wk"], "wv": I["mem_wv"], "wo": I["mem_wo"],
              "w_gu2": I["ff2_w_gu"], "w_down2": I["ff2_w_down"],
              "ffg1": st2("ff1_norm"), "mixg": st2("mix_norm"), "xmg": st2("xmem_norm"), "mmg": st2("mem_norm"),
              "ffg2": st2("ff2_norm"), "qg": st2("q_norm"), "kg": st2("k_norm"), "mqg": st2("mem_q_norm"), "mkg": st2("mem_k_norm")}
    shared = {k: np.ascontiguousarray(np.asarray(v, np.float32)) for k, v in shared.items()}
    shared.update(ssd_consts())
    p = np.arange(128, dtype=np.uint32).reshape(128, 1)
    maps = []
    for c in range(NCORES):
        b, seg = c // 4, c % 4
        gp = seg
        g0 = 2 * gp
        chs = np.concatenate([np.arange(g0 * 384, (g0 + 2) * 384), 3072 + np.arange(g0 * 128, (g0 + 2) * 128),
                              4096 + np.arange(g0 * 128, (g0 + 2) * 128)])
        hs = np.arange(g0 * 6, g0 * 6 + 12)
        m = dict(shared)
        m["xT"] = np.ascontiguousarray(xs[c].T)
        m["memT"] = np.ascontiguousarray(I["mem"][b].astype(np.float32).T)
        m["convw"] = np.ascontiguousarray(np.stack([I["conv_w"][l][:, chs].T.reshape(10, 128, 4).transpose(1, 0, 2) for l in range(2)])).astype(np.float32)
        m["convb"] = np.ascontiguousarray(np.stack([I["conv_b"][l][chs].reshape(10, 128).T for l in range(2)])).astype(np.float32)
        m["dtb"] = np.stack([_rep(np.tile(I["dt_bias"][l][hs], NCH)) for l in range(2)])
        m["alog"] = np.stack([_rep(np.tile(I["a_log"][l][hs], NCH)) for l in range(2)])
        m["dsk"] = np.stack([_rep(np.repeat(I["d_skip"][l][hs], 64)) for l in range(2)])
        m["normw"] = np.stack([_rep(I["ssd_norm"][l][g0 * 384:(g0 + 2) * 384]) for l in range(2)])
        ac = attn_consts(seg)
        m.update(ac)
        m["idx_x"] = (gp * 5120 + p).astype(np.uint32)
        m["idx_t"] = (gp * 4096 + p).astype(np.uint32)
        m["idx_d"] = (gp * T + p).astype(np.uint32)
        m["idx_y"] = (seg * 3072 + p).astype(np.uint32)
        maps.append(m)
    return maps


def kernel_fused(**inputs):
    I = {k: np.asarray(v) for k, v in inputs.items()}
    nc = _prog("fused", build_fused)
    res = _run(nc, fused_inputs(I))
    out = np.stack([res[c]["xoT"].T for c in range(NCORES)], axis=0).reshape(2, SEQ, D)
    return np.ascontiguousarray(out.astype(np.float32))


_PROGS = {}


def _prog(name, fn):
    if name not in _PROGS:
        _PROGS[name] = fn()
    return _PROGS[name]


def _pg(g):
    return np.ascontiguousarray(np.asarray(g, np.float32).reshape(-1, 128).T)


def _run(nc, in_maps):
    res = run_bass_kernel_spmd(nc, in_maps, core_ids=list(range(NCORES)))
    return res.results


def kernel_unfused(**inputs):
    I = {k: np.asarray(v) for k, v in inputs.items()}
    T = NTOK
    x = I["x"].astype(np.float32)
    xs = x.reshape(NCORES, T, D)
    xT = [np.ascontiguousarray(xs[c].T) for c in range(NCORES)]
    memT = [np.ascontiguousarray(I["mem"][b].T) for b in range(2)]
    ncA = _prog("A", build_progA)
    ncB = _prog("B", build_progB)
    ncC1 = _prog("C1", build_progC1)
    ncC2 = _prog("C2", build_progC2)
    for l in range(2):
        mapsA = [{"xT": xT[c], "ffg": _pg(I["ff1_norm"][l]), "mixg": _pg(I["mix_norm"][l]),
                  "w_gu": I["ff1_w_gu"][l], "w_down": I["ff1_w_down"][l], "w_in": I["w_in"][l],
                  "qg": _pg(I["q_norm"][l]), "kg": _pg(I["k_norm"][l])} for c in range(NCORES)]
        rA = _run(ncA, mapsA)
        xbcT_b = [np.concatenate([rA[b * 4 + s]["xbcT"] for s in range(4)], axis=1) for b in range(2)]
        dt_b = [np.concatenate([rA[b * 4 + s]["dt"] for s in range(4)], axis=0) for b in range(2)]
        z_b = [np.concatenate([rA[b * 4 + s]["z"] for s in range(4)], axis=0) for b in range(2)]
        Pm = {k: I[k][l] for k in ("conv_w", "conv_b", "dt_bias", "a_log", "d_skip", "ssd_norm")}
        rB = _run(ncB, progB_inputs(xbcT_b, dt_b, z_b, Pm))
        y_ssd = progB_gather([r["y"] for r in rB])
        rC1 = _run(ncC1, progC1_inputs([rA[c]["qT"] for c in range(NCORES)], [rA[c]["kT"] for c in range(NCORES)],
                                       [rA[c]["v"] for c in range(NCORES)]))
        mapsC2 = []
        for c in range(NCORES):
            b, seg = c // 4, c % 4
            yT = np.ascontiguousarray(np.concatenate([rC1[c]["yT"], y_ssd[b, seg * T:(seg + 1) * T].T], axis=0))
            mapsC2.append({"xT": rA[c]["x1T"], "yT": yT, "w_out": I["w_out"][l], "memT": memT[b],
                           "xmg": _pg(I["xmem_norm"][l]), "mmg": _pg(I["mem_norm"][l]), "ffg": _pg(I["ff2_norm"][l]),
                           "mqg": _pg(I["mem_q_norm"][l]), "mkg": _pg(I["mem_k_norm"][l]),
                           "wq": I["mem_wq"][l], "wk": I["mem_wk"][l], "wv": I["mem_wv"][l], "wo": I["mem_wo"][l],
                           "w_gu": I["ff2_w_gu"][l], "w_down": I["ff2_w_down"][l]})
        rC2 = _run(ncC2, mapsC2)
        xT = [rC2[c]["xoT"] for c in range(NCORES)]
    out = np.stack([xT[c].T for c in range(NCORES)], axis=0).reshape(2, SEQ, D)
    return np.ascontiguousarray(out.astype(np.float32))


def kernel(**inputs):
    return kernel_fused(**inputs)
```

```python
import numpy as np
import contextlib
import concourse.bass as bass
import concourse.mybir as mybir
from concourse.bass_utils import run_bass_kernel_spmd

F32 = mybir.dt.float32
BF16 = mybir.dt.bfloat16
AF = mybir.ActivationFunctionType
ALU = mybir.AluOpType
AX = mybir.AxisListType

D = 2048
DFF = 5632
NTOK = 1024
NCORES = 8
EPS = 1e-6


class Buf:
    def __init__(self, t, name=""):
        self.t = t
        self.name = name
        self.last_w = None
        self.readers = []

    def __getitem__(self, idx):
        return self.t[idx]


class Op:
    __slots__ = ("eng", "emit", "deps", "marked", "count", "dma_sem", "dma_val", "is_dma", "is_cc")

    def __init__(self, eng, emit, is_dma=False):
        self.eng = eng
        self.emit = emit
        self.deps = []
        self.marked = False
        self.count = None
        self.is_dma = is_dma
        self.dma_sem = None
        self.dma_val = None
        self.is_cc = False


class Prog:
    ENGS = ("pe", "dve", "act", "pool", "sp")
    NS = 8

    def __init__(self, nc, stack):
        self.nc = nc
        self.stack = stack
        self.ops = {e: [] for e in self.ENGS}
        self.sem = {e: stack.enter_context(nc.semaphore("prog_" + e)) for e in ("pe", "dve", "act", "pool")}
        self.dsem = {q: [stack.enter_context(nc.semaphore("dma_%s_%d" % (q, i))) for i in range(self.NS)]
                     for q in ("sp", "act", "pool")}
        self.ndma = {q: 0 for q in ("sp", "act", "pool")}
        self.nbuf = 0
        self.cc_sem = stack.enter_context(nc.semaphore("cc_sem"))
        self.ncc = 0

    def sbuf(self, shape, dtype, name=None):
        self.nbuf += 1
        name = "s_%s_%d" % (name or "sb", self.nbuf)
        t = self.stack.enter_context(self.nc.sbuf_tensor(name, list(shape), dtype))
        return Buf(t, name)

    def psum(self, shape, dtype=F32, name=None):
        self.nbuf += 1
        name = "%s_%d" % (name or "ps", self.nbuf)
        t = self.stack.enter_context(self.nc.psum_tensor(name, list(shape), dtype))
        return Buf(t, name)

    def add(self, eng, emit, reads=(), writes=(), is_dma=False):
        op = Op(eng, emit, is_dma)
        deps = []
        for b in reads:
            if b.last_w is not None:
                deps.append(b.last_w)
        for b in writes:
            if b.last_w is not None:
                deps.append(b.last_w)
            deps.extend(b.readers)
        seen = set()
        for d in deps:
            if d is op or id(d) in seen:
                continue
            seen.add(id(d))
            if d.eng == "pe" and eng == "pe" and not d.is_dma:
                continue
            op.deps.append(d)
            if not d.is_dma:
                d.marked = True
        for b in reads:
            b.readers.append(op)
        for b in writes:
            b.last_w = op
            b.readers = []
        if is_dma:
            q = eng
            i = self.ndma[q]
            self.ndma[q] += 1
            op.dma_sem = self.dsem[q][i % self.NS]
            op.dma_val = 16 * (i // self.NS + 1)
        self.ops[eng].append(op)
        return op

    def op(self, eng, method, *args, reads=(), writes=(), **kw):
        return self.add(eng, lambda e: getattr(e, method)(*args, **kw), reads, writes)

    def collective(self, kind, groups, src_ap, dst_ap, reads=(), writes=()):
        op = self.add("pool", lambda e: e.collective_compute(kind, ALU.bypass, replica_groups=groups, ins=[src_ap], outs=[dst_ap]),
                      reads, writes, is_dma=True)
        self.ndma["pool"] -= 1
        self.ncc += 1
        op.dma_sem = self.cc_sem
        op.dma_val = self.ncc
        op.is_cc = True
        return op

    def dma(self, q, out, in_, reads=(), writes=()):
        return self.add(q, lambda e: e.dma_start(out=out, in_=in_), reads, writes, is_dma=True)

    def begin_phases(self):
        self.cnt = {e: 0 for e in ("pe", "dve", "act", "pool")}
        self.waited = {e: {} for e in self.ENGS}
        self.barrier = []
        self.phase_id = 0
        self.last_dma = {}

    @contextlib.contextmanager
    def phase(self, final=False):
        outer = self.stack
        with contextlib.ExitStack() as st:
            self.stack = st
            try:
                yield
                self.emit_phase(final)
            finally:
                self.stack = outer

    def emit_phase(self, final=False):
        nc = self.nc
        prog = self
        for e in ("pe", "dve", "act", "pool"):
            for op in reversed(self.ops[e]):
                if not op.is_dma:
                    op.marked = True
                    break
        for e in ("pe", "dve", "act", "pool"):
            for op in self.ops[e]:
                if op.is_dma:
                    continue
                if op.marked:
                    self.cnt[e] += 1
                    op.count = self.cnt[e]
        barrier = list(self.barrier)

        def run(engname, engine):
            waited = prog.waited[engname]

            def wait(s, v):
                if waited.get(id(s), 0) >= v:
                    return
                engine.wait_ge(s, v)
                waited[id(s)] = v
            for (s, v) in barrier:
                wait(s, v)
            for op in prog.ops[engname]:
                if op.is_dma and not op.is_cc:
                    prev = op.dma_val - 16
                    if prev > 0:
                        wait(op.dma_sem, prev)
                for d in op.deps:
                    if d.is_dma:
                        wait(d.dma_sem, d.dma_val)
                    elif d.count is not None:
                        wait(prog.sem[d.eng], d.count)
                ins = op.emit(engine)
                if op.is_cc:
                    ins.then_inc(op.dma_sem, 1)
                elif op.is_dma:
                    ins.then_inc(op.dma_sem, 16)
                elif op.marked:
                    ins.then_inc(prog.sem[engname], 1)
            if final:
                for (s, v) in final_pairs:
                    wait(s, v)

        for e in self.ENGS:
            for op in self.ops[e]:
                if op.is_dma and not op.is_cc:
                    self.last_dma[id(op.dma_sem)] = (op.dma_sem, op.dma_val)
        nb = [(self.sem[e], self.cnt[e]) for e in ("pe", "dve", "act", "pool") if self.cnt[e] > 0]
        nb += list(self.last_dma.values())
        final_pairs = nb
        with nc.Block() as block:
            @block.tensor
            def _(eng):
                run("pe", eng)

            @block.vector
            def _(eng):
                run("dve", eng)

            @block.scalar
            def _(eng):
                run("act", eng)

            @block.gpsimd
            def _(eng):
                run("pool", eng)

            @block.sync
            def _(eng):
                run("sp", eng)
        self.barrier = nb
        self.phase_id += 1
        for e in self.ENGS:
            for op in self.ops[e]:
                op.emit = None
                if not op.is_dma and op.count is None:
                    op.count = -1
            self.ops[e] = []

    def emit(self, final_wait_ops=()):
        nc = self.nc
        for e in ("pe", "dve", "act", "pool"):
            c = 0
            for op in self.ops[e]:
                if op.is_dma:
                    continue
                if op.marked:
                    c += 1
                    op.count = c
        prog = self

        def run(engname, engine):
            waited = {}
            for op in prog.ops[engname]:
                if op.is_dma and not op.is_cc:
                    prev = op.dma_val - 16
                    if prev > 0 and waited.get(id(op.dma_sem), 0) < prev:
                        engine.wait_ge(op.dma_sem, prev)
                        waited[id(op.dma_sem)] = prev
                for d in op.deps:
                    if d.is_dma:
                        s, v = d.dma_sem, d.dma_val
                    else:
                        s, v = prog.sem[d.eng], d.count
                    if waited.get(id(s), 0) >= v:
                        continue
                    engine.wait_ge(s, v)
                    waited[id(s)] = v
                ins = op.emit(engine)
                if op.is_cc:
                    ins.then_inc(op.dma_sem, 1)
                elif op.is_dma:
                    ins.then_inc(op.dma_sem, 16)
                elif op.marked:
                    ins.then_inc(prog.sem[engname], 1)
            if engname == "sp":
                for d in final_wait_ops:
                    s, v = (d.dma_sem, d.dma_val) if d.is_dma else (prog.sem[d.eng], d.count)
                    engine.wait_ge(s, v)

        with nc.Block() as block:
            @block.tensor
            def _(eng):
                run("pe", eng)

            @block.vector
            def _(eng):
                run("dve", eng)

            @block.scalar
            def _(eng):
                run("act", eng)

            @block.gpsimd
            def _(eng):
                run("pool", eng)

            @block.sync
            def _(eng):
                run("sp", eng)


class PsumPool:
    def __init__(self, P, n=8, pfx="psb"):
        self.bufs = [P.psum([128, 512], F32, name="%s%d" % (pfx, i)) for i in range(n)]
        self.i = 0

    def get(self):
        b = self.bufs[self.i % len(self.bufs)]
        self.i += 1
        return b


class Ring:
    def __init__(self, bufs):
        self.bufs = bufs
        self.i = 0

    def get(self):
        b = self.bufs[self.i % len(self.bufs)]
        self.i += 1
        return b


def rmsnorm_fm(P, pp, xT, gain, hT, ones_bf, sq_ring, rstd, nchunk, T, dim):
    for h0 in range(0, T, 512):
        w = min(512, T - h0)
        ps = pp.get()
        for c in range(nchunk):
            sq = sq_ring.get()
            P.op("act", "activation", out=sq[:, 0:w], in_=xT[c][:, h0:h0 + w], func=AF.Square,
                 reads=[xT[c]], writes=[sq])
            P.op("pe", "matmul", ps[:, 0:w], ones_bf[:, :], sq[:, 0:w], start=(c == 0), stop=(c == nchunk - 1),
                 reads=[sq, ones_bf], writes=[ps])
        P.op("dve", "tensor_scalar", rstd[:, h0:h0 + w], ps[:, 0:w], 1.0 / dim, EPS, ALU.mult, ALU.add,
             reads=[ps], writes=[rstd])
        P.op("act", "activation", out=rstd[:, h0:h0 + w], in_=rstd[:, h0:h0 + w], func=AF.Sqrt,
             reads=[rstd], writes=[rstd])
        P.op("dve", "reciprocal", rstd[:, h0:h0 + w], rstd[:, h0:h0 + w], reads=[rstd], writes=[rstd])
        for c in range(nchunk):
            P.op("dve", "scalar_tensor_tensor", out=hT[c][:, h0:h0 + w], in0=xT[c][:, h0:h0 + w],
                 scalar=gain[:, c:c + 1], in1=rstd[:, h0:h0 + w], op0=ALU.mult, op1=ALU.mult,
                 reads=[xT[c], gain, rstd], writes=[hT[c]])


def ffn_fm(P, pp, xT, hT, w_gu, w_down, aT, wg_ring, wu_ring, wd_ring, sg_ring, T):
    NKC = D // 128
    groups = [(0, 6), (6, 12), (12, 17), (17, 22)]
    wgu_v = w_gu.rearrange("(kc p) f -> p kc f", p=128)
    wd_v = w_down.rearrange("(fc p) d -> p fc d", p=128)
    halves = [(h0, min(512, T - h0)) for h0 in range(0, T, 512)]
    for (p0, p1) in groups:
        nfc = 2 * (p1 - p0)
        for pr in range(p0, p1):
            wg = wg_ring.get()
            wu = wu_ring.get()
            P.dma("pool", wg[:, :, :], wgu_v[:, :, pr * 256:(pr + 1) * 256], writes=[wg])
            P.dma("pool", wu[:, :, :], wgu_v[:, :, DFF + pr * 256:DFF + (pr + 1) * 256], writes=[wu])
            for j in range(2):
                fl = 2 * (pr - p0) + j
                for (h0, w) in halves:
                    pg = pp.get()
                    pu = pp.get()
                    for kc in range(NKC):
                        P.op("pe", "matmul", pg[:, 0:w], wg[:, kc, j * 128:(j + 1) * 128], hT[kc][:, h0:h0 + w],
                             start=(kc == 0), stop=(kc == NKC - 1), reads=[wg, hT[kc]], writes=[pg])
                    for kc in range(NKC):
                        P.op("pe", "matmul", pu[:, 0:w], wu[:, kc, j * 128:(j + 1) * 128], hT[kc][:, h0:h0 + w],
                             start=(kc == 0), stop=(kc == NKC - 1), reads=[wu, hT[kc]], writes=[pu])
                    sg = sg_ring.get()
                    P.op("act", "activation", out=sg[:, 0:w], in_=pg[:, 0:w], func=AF.Silu, reads=[pg], writes=[sg])
                    P.op("dve", "tensor_tensor", out=aT[fl][:, h0:h0 + w], in0=pu[:, 0:w], in1=sg[:, 0:w], op=ALU.mult,
                         reads=[pu, sg], writes=[aT[fl]])
        for dq in range(D // 512):
            wd = wd_ring.get()
            P.dma("pool", wd[:, 0:nfc, :], wd_v[:, 2 * p0:2 * p1, dq * 512:(dq + 1) * 512], writes=[wd])
            for dj in range(4):
                dc = dq * 4 + dj
                for (h0, w) in halves:
                    po = pp.get()
                    for fl in range(nfc):
                        P.op("pe", "matmul", po[:, 0:w], wd[:, fl, dj * 128:(dj + 1) * 128], aT[fl][:, h0:h0 + w],
                             start=(fl == 0), stop=(fl == nfc - 1), reads=[wd, aT[fl]], writes=[po])
                    P.op("dve", "scalar_tensor_tensor", out=xT[dc][:, h0:h0 + w], in0=po[:, 0:w], scalar=0.5,
                         in1=xT[dc][:, h0:h0 + w], op0=ALU.mult, op1=ALU.add, reads=[po, xT[dc]], writes=[xT[dc]])


def build_ffn_prog(T=NTOK):
    nc = bass.Bass("TRN2", target_bir_lowering=False)
    xin = nc.dram_tensor("xT", [D, T], F32, kind="ExternalInput").ap()
    gin = nc.dram_tensor("gain", [128, D // 128], F32, kind="ExternalInput").ap()
    wgu = nc.dram_tensor("w_gu", [D, 2 * DFF], F32, kind="ExternalInput").ap()
    wdn = nc.dram_tensor("w_down", [DFF, D], F32, kind="ExternalInput").ap()
    yout = nc.dram_tensor("yT", [D, T], F32, kind="ExternalOutput").ap()
    NKC = D // 128
    with contextlib.ExitStack() as stack:
        P = Prog(nc, stack)
        pp = PsumPool(P, 8)
        xT = [P.sbuf([128, T], F32, "xT%d" % c) for c in range(NKC)]
        hT = [P.sbuf([128, T], BF16, "hT%d" % c) for c in range(NKC)]
        aT = [P.sbuf([128, T], BF16, "aT%d" % c) for c in range(12)]
        gain = P.sbuf([128, NKC], F32, "gain")
        ones_bf = P.sbuf([128, 128], BF16, "ones")
        rstd = P.sbuf([128, T], F32, "rstd")
        sq_ring = Ring([P.sbuf([128, 512], BF16, "sq%d" % i) for i in range(3)])
        sg_ring = Ring([P.sbuf([128, 512], F32, "sg%d" % i) for i in range(3)])
        wg_ring = Ring([P.sbuf([128, NKC, 256], BF16, "wg%d" % i) for i in range(2)])
        wu_ring = Ring([P.sbuf([128, NKC, 256], BF16, "wu%d" % i) for i in range(2)])
        wd_ring = Ring([P.sbuf([128, 12, 512], BF16, "wd%d" % i) for i in range(2)])

        xv = xin.rearrange("(c p) t -> c p t", p=128)
        yv = yout.rearrange("(c p) t -> c p t", p=128)
        P.op("pool", "memset", ones_bf[:, :], 1.0, writes=[ones_bf])
        P.dma("sp", gain[:, :], gin[:, :], writes=[gain])
        for c in range(NKC):
            P.dma("sp", xT[c][:, :], xv[c], writes=[xT[c]])
        rmsnorm_fm(P, pp, xT, gain, hT, ones_bf, sq_ring, rstd, NKC, T, D)
        ffn_fm(P, pp, xT, hT, wgu, wdn, aT, wg_ring, wu_ring, wd_ring, sg_ring, T)
        outs = []
        for c in range(NKC):
            outs.append(P.dma("sp", yv[c], xT[c][:, :], reads=[xT[c]]))
        P.emit(final_wait_ops=outs)
    return nc


def lin_fm(P, pp, hT, w_ap, ncols, w_ring, T, consume, nkc=D // 128):
    wv = w_ap.rearrange("(kc p) f -> p kc f", p=128)
    halves = [(h0, min(512, T - h0)) for h0 in range(0, T, 512)]
    for c0 in range(0, ncols, 256):
        cw = min(256, ncols - c0)
        wt = w_ring.get()
        P.dma("pool", wt[:, 0:nkc, 0:cw], wv[:, :, c0:c0 + cw], writes=[wt])
        for j in range(cw // 128):
            for (h0, w) in halves:
                ps = pp.get()
                for kc in range(nkc):
                    P.op("pe", "matmul", ps[:, 0:w], wt[:, kc, j * 128:(j + 1) * 128], hT[kc][:, h0:h0 + w],
                         start=(kc == 0), stop=(kc == nkc - 1), reads=[wt, hT[kc]], writes=[ps])
                consume(c0 // 128 + j, h0, w, ps)


def lin_tm(P, pp, hT, w_ap, ncols, w_ring, T, consume, nkc=D // 128):
    wv = w_ap.rearrange("(kc p) f -> p kc f", p=128)
    for c0 in range(0, ncols, 256):
        cw = min(256, ncols - c0)
        wt = w_ring.get()
        P.dma("pool", wt[:, 0:nkc, 0:cw], wv[:, :, c0:c0 + cw], writes=[wt])
        for tt in range(T // 128):
            ps = pp.get()
            for kc in range(nkc):
                P.op("pe", "matmul", ps[:, 0:cw], hT[kc][:, tt * 128:(tt + 1) * 128], wt[:, kc, 0:cw],
                     start=(kc == 0), stop=(kc == nkc - 1), reads=[wt, hT[kc]], writes=[ps])
            consume(tt, c0, cw, ps)


NQ, NK, NV, NZ, NXBC, NDT = 1024, 1024, 1024, 3072, 5120, 48
OQ, OK_, OV, OZ, OXBC, ODT = 0, 1024, 2048, 3072, 6144, 11264
N_IN = 11312


def headnorm_consume(P, pp, ones_bf, gain_hd, sq_ring, rs_ring, dst_of):
    def consume(fc, h0, w, ps):
        sq = sq_ring.get()
        P.op("act", "activation", out=sq[:, 0:w], in_=ps[:, 0:w], func=AF.Square, reads=[ps], writes=[sq])
        ps2 = pp.get()
        P.op("pe", "matmul", ps2[:, 0:w], ones_bf[:, :], sq[:, 0:w], start=True, stop=True,
             reads=[sq, ones_bf], writes=[ps2])
        rs = rs_ring.get()
        P.op("dve", "tensor_scalar", rs[:, 0:w], ps2[:, 0:w], 1.0 / 128, EPS, ALU.mult, ALU.add, reads=[ps2], writes=[rs])
        P.op("act", "activation", out=rs[:, 0:w], in_=rs[:, 0:w], func=AF.Sqrt, reads=[rs], writes=[rs])
        P.op("dve", "reciprocal", rs[:, 0:w], rs[:, 0:w], reads=[rs], writes=[rs])
        ap, buf = dst_of(fc, h0, w)
        P.op("dve", "scalar_tensor_tensor", out=ap, in0=ps[:, 0:w], scalar=gain_hd[:, 0:1], in1=rs[:, 0:w],
             op0=ALU.mult, op1=ALU.mult, reads=[ps, gain_hd, rs], writes=[buf])
    return consume


def build_progA(T=NTOK, do_ffn=True):
    nc = bass.Bass("TRN2", target_bir_lowering=False)
    di = lambda n, s: nc.dram_tensor(n, s, F32, kind="ExternalInput").ap()
    do = lambda n, s: nc.dram_tensor(n, s, F32, kind="ExternalOutput").ap()
    xin = di("xT", [D, T]); ffg = di("ffg", [128, 16]); mixg = di("mixg", [128, 16])
    wgu = di("w_gu", [D, 2 * DFF]); wdn = di("w_down", [DFF, D]); win = di("w_in", [D, N_IN])
    qg_in = di("qg", [128, 1]); kg_in = di("kg", [128, 1])
    x1o = do("x1T", [D, T]); qo = do("qT", [NQ, T]); ko = do("kT", [NK, T]); vo = do("v", [T, NV])
    zo = do("z", [T, NZ]); xbco = do("xbcT", [NXBC, T]); dto = do("dt", [T, NDT])
    NKC = D // 128
    with contextlib.ExitStack() as stack:
        P = Prog(nc, stack)
        pp = PsumPool(P, 8)
        xT = [P.sbuf([128, T], F32, "xT%d" % c) for c in range(NKC)]
        hT = [P.sbuf([128, T], BF16, "hT%d" % c) for c in range(NKC)]
        aT = [P.sbuf([128, T], BF16, "aT%d" % c) for c in range(12)]
        g1 = P.sbuf([128, NKC], F32, "g1"); g2 = P.sbuf([128, NKC], F32, "g2")
        qg = P.sbuf([128, 1], F32, "qg"); kg = P.sbuf([128, 1], F32, "kg")
        ones_bf = P.sbuf([128, 128], BF16, "ones")
        rstd = P.sbuf([128, T], F32, "rstd")
        sq_ring = Ring([P.sbuf([128, 512], BF16, "sq%d" % i) for i in range(3)])
        sg_ring = Ring([P.sbuf([128, 512], F32, "sg%d" % i) for i in range(3)])
        wg_ring = Ring([P.sbuf([128, NKC, 256], BF16, "wg%d" % i) for i in range(2)])
        wu_ring = Ring([P.sbuf([128, NKC, 256], BF16, "wu%d" % i) for i in range(2)])
        wd_ring = Ring([P.sbuf([128, 12, 512], BF16, "wd%d" % i) for i in range(2)])
        w_ring = Ring(wg_ring.bufs + wu_ring.bufs)
        st_ring = Ring([P.sbuf([128, 512], F32, "st%d" % i) for i in range(4)])

        xv = xin.rearrange("(c p) t -> c p t", p=128)
        P.op("pool", "memset", ones_bf[:, :], 1.0, writes=[ones_bf])
        P.dma("sp", g1[:, :], ffg[:, :], writes=[g1]); P.dma("sp", g2[:, :], mixg[:, :], writes=[g2])
        P.dma("sp", qg[:, :], qg_in[:, :], writes=[qg]); P.dma("sp", kg[:, :], kg_in[:, :], writes=[kg])
        for c in range(NKC):
            P.dma("sp", xT[c][:, :], xv[c], writes=[xT[c]])
        outs = []
        if do_ffn:
            rmsnorm_fm(P, pp, xT, g1, hT, ones_bf, sq_ring, rstd, NKC, T, D)
            ffn_fm(P, pp, xT, hT, wgu, wdn, aT, wg_ring, wu_ring, wd_ring, sg_ring, T)
        x1v = x1o.rearrange("(c p) t -> c p t", p=128)
        for c in range(NKC):
            outs.append(P.dma("sp", x1v[c], xT[c][:, :], reads=[xT[c]]))
        rmsnorm_fm(P, pp, xT, g2, hT, ones_bf, sq_ring, rstd, NKC, T, D)

        for (o_ap, col0, gbuf) in ((qo, OQ, qg), (ko, OK_, kg)):
            ov = o_ap.rearrange("(c p) t -> c p t", p=128)

            def dst_of(fc, h0, w, ov=ov):
                st = st_ring.get()
                dst_of.last = (st, fc, h0, w)
                return st[:, 0:w], st
            cons0 = headnorm_consume(P, pp, ones_bf, gbuf, sq_ring, sg_ring, dst_of)

            def cons(fc, h0, w, ps, cons0=cons0, ov=ov):
                cons0(fc, h0, w, ps)
                st, fc, h0, w = dst_of.last
                outs.append(P.dma("sp", ov[fc][:, h0:h0 + w], st[:, 0:w], reads=[st]))
            lin_fm(P, pp, hT, win[:, col0:col0 + 1024], 1024, w_ring, T, cons)

        xbv = xbco.rearrange("(c p) t -> c p t", p=128)

        def cons_xbc(fc, h0, w, ps):
            st = st_ring.get()
            P.op("act", "activation", out=st[:, 0:w], in_=ps[:, 0:w], func=AF.Copy, reads=[ps], writes=[st])
            outs.append(P.dma("sp", xbv[fc][:, h0:h0 + w], st[:, 0:w], reads=[st]))
        lin_fm(P, pp, hT, win[:, OXBC:OXBC + NXBC], NXBC, w_ring, T, cons_xbc)

        for (o_ap, col0, n) in ((vo, OV, NV), (zo, OZ, NZ), (dto, ODT, NDT)):
            def cons_tm(tt, c0, cw, ps, o_ap=o_ap):
                st = st_ring.get()
                P.op("dve", "tensor_copy", st[:, 0:cw], ps[:, 0:cw], reads=[ps], writes=[st])
                outs.append(P.dma("sp", o_ap[tt * 128:(tt + 1) * 128, c0:c0 + cw], st[:, 0:cw], reads=[st]))
            lin_tm(P, pp, hT, win[:, col0:col0 + n], n, w_ring, T, cons_tm)
        P.emit(final_wait_ops=outs)
    return nc


SEQ = 4096
NCH = SEQ // 128


def build_progB():
    nc = bass.Bass("TRN2", target_bir_lowering=False)
    di = lambda n, sh: nc.dram_tensor(n, sh, F32, kind="ExternalInput").ap()
    xbc = di("xbcT", [1280, SEQ + 3]); convw_i = di("convw", [128, 10, 4]); convb_i = di("convb", [128, 10])
    dtraw_i = di("dtraw", [SEQ, 12]); dtb_i = di("dtb", [128, 384]); alog_i = di("alog", [128, 384])
    dsk_i = di("dsk", [128, 768]); normw_i = di("normw", [128, 768]); z_i = di("z", [SEQ, 768])
    tri_i = di("tri", [128, 128]); strict_i = di("strict", [128, 128]); onesf_i = di("onesf", [128, 128])
    ident_i = di("ident", [128, 128])
    yo = nc.dram_tensor("y", [SEQ, 768], F32, kind="ExternalOutput").ap()
    with contextlib.ExitStack() as stack:
        P = Prog(nc, stack)
        pp = PsumPool(P, 8)
        cw = P.sbuf([128, 10, 4], F32, "cw"); cb = P.sbuf([128, 10], F32, "cb")
        dt_all = P.sbuf([128, 384], F32, "dt_all"); da_all = P.sbuf([128, 384], F32, "da_all")
        tmpa = P.sbuf([128, 384], F32, "tmpa"); tmpb = P.sbuf([128, 384], F32, "tmpb")
        dsk = P.sbuf([128, 12, 64], F32, "dsk"); normw = P.sbuf([128, 768], F32, "normw")
        tri = P.sbuf([128, 128], F32, "tri"); strict = P.sbuf([128, 128], F32, "strict")
        onesf = P.sbuf([128, 128], F32, "onesf"); ident = P.sbuf([128, 128], F32, "ident")
        for (b, a) in ((cw, convw_i), (cb, convb_i), (tmpa, dtb_i), (tmpb, alog_i),
                       (normw, normw_i), (tri, tri_i), (strict, strict_i), (onesf, onesf_i), (ident, ident_i)):
            P.dma("sp", b[:], a, writes=[b])
        P.dma("sp", dsk[:], dsk_i.rearrange("p (j d) -> p j d", d=64), writes=[dsk])
        P.dma("sp", dt_all[:].rearrange("p (c j) -> p c j", j=12), dtraw_i.rearrange("(c l) j -> l c j", l=128),
              writes=[dt_all])
        P.op("dve", "tensor_tensor", out=dt_all[:], in0=dt_all[:], in1=tmpa[:], op=ALU.add, reads=[dt_all, tmpa], writes=[dt_all])
        P.op("act", "activation", out=dt_all[:], in_=dt_all[:], func=AF.Exp, reads=[dt_all], writes=[dt_all])
        P.op("dve", "tensor_scalar", dt_all[:], dt_all[:], 1.0, None, ALU.add, reads=[dt_all], writes=[dt_all])
        P.op("act", "activation", out=dt_all[:], in_=dt_all[:], func=AF.Ln, reads=[dt_all], writes=[dt_all])
        P.op("act", "activation", out=tmpb[:], in_=tmpb[:], func=AF.Exp, reads=[tmpb], writes=[tmpb])
        P.op("dve", "scalar_tensor_tensor", out=da_all[:], in0=dt_all[:], scalar=-1.0, in1=tmpb[:], op0=ALU.mult,
             op1=ALU.mult, reads=[dt_all, tmpb], writes=[da_all])

        h = [P.sbuf([128, 6, 64], F32, "h%d" % g) for g in range(2)]
        hb = [P.sbuf([128, 6, 64], BF16, "hb%d" % g) for g in range(2)]
        for g in range(2):
            P.op("pool", "memset", h[g][:], 0.0, writes=[h[g]])
            P.op("pool", "memset", hb[g][:], 0.0, writes=[hb[g]])

        xr_ring = Ring([P.sbuf([128, 515], F32, "xr%d" % i) for i in range(4)])
        acc_ring = Ring([P.sbuf([128, 512], F32, "acc%d" % i) for i in range(2)])
        xc_sets = [[P.sbuf([128, 512], F32, "xc%d_%d" % (k, i)) for i in range(8)] for k in range(2)]
        bc_sets = [[P.sbuf([128, 512], BF16, "bc%d_%d" % (k, i)) for i in range(4)] for k in range(2)]
        xt_ring = Ring([P.sbuf([128, 12, 64], F32, "xt%d" % i) for i in range(2)])
        bt_ring = Ring([P.sbuf([128, 256], BF16, "bt%d" % i) for i in range(2)])
        E_ring = Ring([P.sbuf([128, 36], F32, "E%d" % i) for i in range(2)])
        s2_ring = Ring([P.sbuf([128, 12], F32, "s2%d" % i) for i in range(2)])
        xdt_ring = Ring([P.sbuf([128, 12, 64], BF16, "xdt%d" % i) for i in range(2)])
        xw_ring = Ring([P.sbuf([128, 12, 64], BF16, "xw%d" % i) for i in range(2)])
        xd_ring = Ring([P.sbuf([128, 12, 64], F32, "xd%d" % i) for i in range(2)])
        cbm_ring = Ring([P.sbuf([128, 128], F32, "cbm%d" % i) for i in range(2)])
        aj_ring = Ring([P.sbuf([128, 128], F32, "aj%d" % i) for i in range(3)])
        dec_ring = Ring([P.sbuf([128, 128], F32, "dec%d" % i) for i in range(3)])
        sc_ring = Ring([P.sbuf([128, 128], BF16, "sc%d" % i) for i in range(3)])
        y_ring = Ring([P.sbuf([128, 12, 64], F32, "y%d" % i) for i in range(2)])
        t1_ring = Ring([P.sbuf([128, 6, 64], F32, "t1%d" % i) for i in range(2)])
        z_ring = Ring([P.sbuf([128, 768], F32, "z%d" % i) for i in range(2)])
        sq_ring = Ring([P.sbuf([128, 768], F32, "sqq%d" % i) for i in range(2)])
        ss_ring = Ring([P.sbuf([128, 2], F32, "ss%d" % i) for i in range(2)])
        o_ring = Ring([P.sbuf([128, 768], F32, "o%d" % i) for i in range(2)])
        outs = []
        for tb in range(SEQ // 512):
            xc = xc_sets[tb % 2]
            bc = bc_sets[tb % 2]
            for ch in range(10):
                xr = xr_ring.get()
                P.dma("sp", xr[:, :], xbc[ch * 128:(ch + 1) * 128, tb * 512:tb * 512 + 515], writes=[xr])
                acc = acc_ring.get()
                P.op("dve", "tensor_scalar", acc[:, :], xr[:, 0:512], cw[:, ch, 0:1], cb[:, ch:ch + 1], ALU.mult, ALU.add,
                     reads=[xr, cw, cb], writes=[acc])
                for k in range(1, 4):
                    P.op("dve", "scalar_tensor_tensor", out=acc[:, :], in0=xr[:, k:k + 512], scalar=cw[:, ch, k:k + 1],
                         in1=acc[:, :], op0=ALU.mult, op1=ALU.add, reads=[xr, cw, acc], writes=[acc])
                if ch < 8:
                    P.op("act", "activation", out=xc[ch][:, :], in_=acc[:, :], func=AF.Silu, reads=[acc], writes=[xc[ch]])
                    if ch >= 6:
                        P.op("dve", "tensor_copy", bc[ch - 6][:, :], xc[ch][:, :], reads=[xc[ch]], writes=[bc[ch - 6]])
                else:
                    P.op("act", "activation", out=bc[ch - 6][:, :], in_=acc[:, :], func=AF.Silu, reads=[acc], writes=[bc[ch - 6]])
            for sc_i in range(4):
                c = tb * 4 + sc_i
                ts = slice(sc_i * 128, (sc_i + 1) * 128)
                xt = xt_ring.get(); bt = bt_ring.get()
                xtf = xt[:].rearrange("p j d -> p (j d)")
                for (lo, n) in ((0, 4), (4, 2)):
                    ps = pp.get()
                    for i in range(n):
                        P.op("pe", "transpose", ps[:, i * 128:(i + 1) * 128], xc[lo + i][:, ts], ident[:, :],
                             reads=[xc[lo + i], ident], writes=[ps])
                    P.op("dve", "tensor_copy", xtf[:, lo * 128:(lo + n) * 128], ps[:, 0:n * 128], reads=[ps], writes=[xt])
                ps = pp.get()
                for i in range(2):
                    P.op("pe", "transpose", ps[:, i * 128:(i + 1) * 128], xc[6 + i][:, ts], ident[:, :],
                         reads=[xc[6 + i], ident], writes=[ps])
                P.op("act", "activation", out=bt[:, :], in_=ps[:, 0:256], func=AF.Copy, reads=[ps], writes=[bt])
                dac = da_all[:, c * 12:(c + 1) * 12]
                dtc = dt_all[:, c * 12:(c + 1) * 12]
                ps = pp.get()
                P.op("pe", "matmul", ps[:, 0:12], tri[:, :], dac, start=True, stop=True, reads=[tri, da_all], writes=[ps])
                P.op("pe", "matmul", ps[:, 12:24], strict[:, :], dac, start=True, stop=True, reads=[strict, da_all], writes=[ps])
                P.op("pe", "matmul", ps[:, 24:36], onesf[:, :], dac, start=True, stop=True, reads=[onesf, da_all], writes=[ps])
                E = E_ring.get()
                P.op("act", "activation", out=E[:, :], in_=ps[:, 0:36], func=AF.Exp, reads=[ps], writes=[E])
                s2 = s2_ring.get()
                P.op("dve", "tensor_tensor", out=s2[:, :], in0=dtc, in1=E[:, 12:24], op=ALU.mult, reads=[dt_all, E], writes=[s2])
                xdt = xdt_ring.get(); xw = xw_ring.get(); xd = xd_ring.get()
                P.op("dve", "tensor_tensor", out=xdt[:], in0=xt[:], in1=dtc.unsqueeze(2).broadcast_to([128, 12, 64]),
                     op=ALU.mult, reads=[xt, dt_all], writes=[xdt])
                P.op("dve", "tensor_tensor", out=xw[:], in0=xt[:], in1=s2[:, :].unsqueeze(2).broadcast_to([128, 12, 64]),
                     op=ALU.mult, reads=[xt, s2], writes=[xw])
                P.op("pool", "tensor_tensor", out=xd[:], in0=xt[:], in1=dsk[:], op=ALU.mult, reads=[xt, dsk], writes=[xd])
                y = y_ring.get()
                for gi in range(2):
                    BT = bc[gi]; CT = bc[2 + gi]
                    ps_cb = pp.get()
                    P.op("pe", "matmul", ps_cb[:, 0:128], BT[:, ts], CT[:, ts], start=True, stop=True, reads=[BT, CT], writes=[ps_cb])
                    cbm = cbm_ring.get()
                    P.op("dve", "tensor_tensor", out=cbm[:, :], in0=ps_cb[:, 0:128], in1=tri[:, :], op=ALU.mult,
                         reads=[ps_cb, tri], writes=[cbm])
                    ps_yo = pp.get()
                    P.op("pe", "matmul", ps_yo[:, 0:384], CT[:, ts], hb[gi][:].rearrange("p j d -> p (j d)"),
                         start=True, stop=True, reads=[CT, hb[gi]], writes=[ps_yo])
                    ps_yd = pp.get()
                    for jj in range(6):
                        j = gi * 6 + jj
                        aj = aj_ring.get()
                        P.op("pool", "tensor_scalar", aj[:, :], strict[:, :], da_all[:, c * 12 + j:c * 12 + j + 1], None, ALU.mult,
                             reads=[strict, da_all], writes=[aj])
                        ps_seg = pp.get()
                        P.op("pe", "matmul", ps_seg[:, 0:128], aj[:, :], tri[:, :], start=True, stop=True, reads=[aj, tri], writes=[ps_seg])
                        dec = dec_ring.get()
                        P.op("act", "activation", out=dec[:, :], in_=ps_seg[:, 0:128], func=AF.Exp, reads=[ps_seg], writes=[dec])
                        scb = sc_ring.get()
                        P.op("dve", "tensor_tensor", out=scb[:, :], in0=dec[:, :], in1=cbm[:, :], op=ALU.mult,
                             reads=[dec, cbm], writes=[scb])
                        P.op("pe", "matmul", ps_yd[:, jj * 64:(jj + 1) * 64], scb[:, :], xdt[:, j, :], start=True, stop=True,
                             reads=[scb, xdt], writes=[ps_yd])
                    t1 = t1_ring.get()
                    P.op("dve", "tensor_tensor", out=t1[:], in0=ps_yo[:, 0:384].rearrange("p (j d) -> p j d", d=64),
                         in1=E[:, gi * 6:gi * 6 + 6].unsqueeze(2).broadcast_to([128, 6, 64]), op=ALU.mult,
                         reads=[ps_yo, E], writes=[t1])
                    P.op("dve", "tensor_tensor", out=t1[:], in0=ps_yd[:, 0:384].rearrange("p (j d) -> p j d", d=64),
                         in1=t1[:], op=ALU.add, reads=[ps_yd, t1], writes=[t1])
                    P.op("dve", "tensor_tensor", out=y[:, gi * 6:(gi + 1) * 6, :], in0=xd[:, gi * 6:(gi + 1) * 6, :], in1=t1[:],
                         op=ALU.add, reads=[xd, t1], writes=[y])
                    ps_st = pp.get()
                    P.op("pe", "matmul", ps_st[:, 0:384], bt[:, gi * 128:(gi + 1) * 128],
                         xw[:, gi * 6:(gi + 1) * 6, :].rearrange("p j d -> p (j d)"), start=True, stop=True,
                         reads=[bt, xw], writes=[ps_st])
                    P.op("dve", "tensor_tensor", out=h[gi][:], in0=h[gi][:],
                         in1=E[:, 24 + gi * 6:24 + gi * 6 + 6].unsqueeze(2).broadcast_to([128, 6, 64]), op=ALU.mult,
                         reads=[h[gi], E], writes=[h[gi]])
                    P.op("dve", "tensor_tensor", out=h[gi][:], in0=ps_st[:, 0:384].rearrange("p (j d) -> p j d", d=64),
                         in1=h[gi][:], op=ALU.add, reads=[ps_st, h[gi]], writes=[h[gi]])
                    P.op("act", "activation", out=hb[gi][:], in_=h[gi][:], func=AF.Copy, reads=[h[gi]], writes=[hb[gi]])
                zt = z_ring.get()
                P.dma("sp", zt[:, :], z_i[c * 128:(c + 1) * 128, :], writes=[zt])
                P.op("act", "activation", out=zt[:, :], in_=zt[:, :], func=AF.Silu, reads=[zt], writes=[zt])
                yf = y[:].rearrange("p j d -> p (j d)")
                P.op("dve", "tensor_tensor", out=yf, in0=yf, in1=zt[:, :], op=ALU.mult, reads=[y, zt], writes=[y])
                sq = sq_ring.get()
                P.op("act", "activation", out=sq[:, :], in_=yf, func=AF.Square, reads=[y], writes=[sq])
                ss = ss_ring.get()
                P.op("dve", "tensor_reduce", out=ss[:, :], in_=sq[:, :].rearrange("p (g f) -> p g f", g=2), axis=AX.X, op=ALU.add,
                     reads=[sq], writes=[ss])
                P.op("dve", "tensor_scalar", ss[:, :], ss[:, :], 1.0 / 384, EPS, ALU.mult, ALU.add, reads=[ss], writes=[ss])
                P.op("act", "activation", out=ss[:, :], in_=ss[:, :], func=AF.Sqrt, reads=[ss], writes=[ss])
                P.op("dve", "reciprocal", ss[:, :], ss[:, :], reads=[ss], writes=[ss])
                ot = o_ring.get()
                for gi in range(2):
                    P.op("dve", "scalar_tensor_tensor", out=ot[:, gi * 384:(gi + 1) * 384], in0=yf[:, gi * 384:(gi + 1) * 384],
                         scalar=ss[:, gi:gi + 1], in1=normw[:, gi * 384:(gi + 1) * 384], op0=ALU.mult, op1=ALU.mult,
                         reads=[y, ss, normw], writes=[ot])
                outs.append(P.dma("sp", yo[c * 128:(c + 1) * 128, :], ot[:, :], reads=[ot]))
        P.emit(final_wait_ops=outs)
    return nc


def _rep(v, n=128):
    return np.ascontiguousarray(np.broadcast_to(np.asarray(v, np.float32).reshape(1, -1), (n, np.asarray(v).size)))


def ssd_consts():
    k = np.arange(128)
    tri = (k[:, None] <= k[None, :]).astype(np.float32)
    strict = (k[:, None] > k[None, :]).astype(np.float32)
    return {"tri": tri, "strict": strict, "onesf": np.ones((128, 128), np.float32), "ident": np.eye(128, dtype=np.float32)}


def progB_inputs(xbcT_b, dt_b, z_b, Pm):
    maps = []
    cst = ssd_consts()
    for c in range(NCORES):
        b, gp = c // 4, c % 4
        g0 = 2 * gp
        chs = np.concatenate([np.arange(g0 * 384, (g0 + 2) * 384), 3072 + np.arange(g0 * 128, (g0 + 2) * 128),
                              4096 + np.arange(g0 * 128, (g0 + 2) * 128)])
        hs = np.arange(g0 * 6, g0 * 6 + 12)
        xp = np.zeros((1280, SEQ + 3), np.float32)
        xp[:, 3:] = xbcT_b[b][chs]
        m = {"xbcT": xp,
             "convw": np.ascontiguousarray(Pm["conv_w"][:, chs].T.reshape(10, 128, 4).transpose(1, 0, 2)),
             "convb": np.ascontiguousarray(Pm["conv_b"][chs].reshape(10, 128).T),
             "dtraw": np.ascontiguousarray(dt_b[b][:, hs]),
             "dtb": _rep(np.tile(Pm["dt_bias"][hs], NCH)), "alog": _rep(np.tile(Pm["a_log"][hs], NCH)),
             "dsk": _rep(np.repeat(Pm["d_skip"][hs], 64)), "normw": _rep(Pm["ssd_norm"][g0 * 384:(g0 + 2) * 384]),
             "z": np.ascontiguousarray(z_b[b][:, g0 * 384:(g0 + 2) * 384])}
        m.update(cst)
        maps.append(m)
    return maps


def progB_gather(ys):
    out = np.zeros((2, SEQ, 3072), np.float32)
    for c in range(NCORES):
        b, gp = c // 4, c % 4
        out[b][:, gp * 768:(gp + 1) * 768] = ys[c]
    return out


NBLK = SEQ // 256
BIGNEG = -30000.0


def build_progC1():
    nc = bass.Bass("TRN2", target_bir_lowering=False)
    di = lambda n, sh: nc.dram_tensor(n, sh, F32, kind="ExternalInput").ap()
    q_i = di("qT", [1024, NTOK]); k_i = di("kT_all", [1024, SEQ]); v_i = di("v_all", [SEQ, 1024])
    ko_i = di("kT_own", [1024, NTOK]); vo_i = di("v_own", [NTOK, 1024])
    tna_i = di("Tna", [128, 8 * 384]); tca_i = di("Tca", [128, 8 * 384]); ab_i = di("abias", [128, 512])
    gb_i = di("gbias", [128, 64]); pm_i = di("pastmask", [128, 64]); oh_i = di("onehot", [16, 16 * 128])
    id_i = di("ident", [128, 128])
    yo = nc.dram_tensor("yT", [1024, NTOK], F32, kind="ExternalOutput").ap()
    scale = 128.0 ** -0.5
    with contextlib.ExitStack() as stack:
        P = Prog(nc, stack)
        pp = PsumPool(P, 4)
        pacc = PsumPool(P, 4, pfx="pacc")
        tna = P.sbuf([128, 8, 384], F32, "tna"); tca = P.sbuf([128, 8, 384], F32, "tca")
        abias = P.sbuf([128, 512], F32, "abias"); gbias = P.sbuf([128, 64], F32, "gbias")
        pmask = P.sbuf([128, 64], F32, "pmask"); onehot = P.sbuf([16, 16, 128], BF16, "onehot")
        ident = P.sbuf([128, 128], F32, "ident"); ones_bf = P.sbuf([128, 128], BF16, "ones")
        P.dma("sp", tna[:], tna_i.rearrange("p (h u) -> p h u", h=8), writes=[tna])
        P.dma("sp", tca[:], tca_i.rearrange("p (h u) -> p h u", h=8), writes=[tca])
        for (b, a) in ((abias, ab_i), (gbias, gb_i), (pmask, pm_i), (ident, id_i)):
            P.dma("sp", b[:], a, writes=[b])
        P.dma("pool", onehot[:], oh_i.rearrange("k (n m) -> k n m", n=16), writes=[onehot])
        P.op("pool", "memset", ones_bf[:, :], 1.0, writes=[ones_bf])
        kf_ring = Ring([P.sbuf([128, SEQ], F32, "kf%d" % i) for i in range(2)])
        kb_ring = Ring([P.sbuf([128, SEQ], BF16, "kb%d" % i) for i in range(2)])
        qf_ring = Ring([P.sbuf([128, NTOK], F32, "qf%d" % i) for i in range(2)])
        qb_ring = Ring([P.sbuf([128, NTOK], BF16, "qb%d" % i) for i in range(2)])
        ko_ring = Ring([P.sbuf([128, NTOK], BF16, "ko%d" % i) for i in range(2)])
        vb_ring = Ring([P.sbuf([128, 32, 128], BF16, "vb%d" % i) for i in range(2)])
        vo_ring = Ring([P.sbuf([128, 8, 128], BF16, "vo%d" % i) for i in range(2)])
        km_ring = Ring([P.sbuf([128, 16], F32, "km%d" % i) for i in range(2)])
        gm_ring = Ring([P.sbuf([128, 16], F32, "gm%d" % i) for i in range(2)])
        t8_ring = Ring([P.sbuf([128, 8], F32, "t8%d" % i) for i in range(2)])
        sel_ring = Ring([P.sbuf([128, 16], F32, "sel%d" % i) for i in range(2)])
        ns_ring = Ring([P.sbuf([16, 256], BF16, "ns%d" % i) for i in range(2)])
        lg_ring = Ring([P.sbuf([128, 256], F32, "lg%d" % i) for i in range(3)])
        pT_ring = Ring([P.sbuf([128, 256], BF16, "pT%d" % i) for i in range(3)])
        rd_ring = Ring([P.sbuf([128, 256], F32, "rd%d" % i) for i in range(2)])
        o_ring = Ring([P.sbuf([128, 256], F32, "oo%d" % i) for i in range(2)])
        outs = []
        for h in range(8):
            hr = slice(h * 128, (h + 1) * 128)
            kf = kf_ring.get(); kb = kb_ring.get(); qf = qf_ring.get(); qb = qb_ring.get()
            ko = ko_ring.get(); vb = vb_ring.get(); vo = vo_ring.get(); km = km_ring.get()
            P.dma("sp", kf[:, :], k_i[hr, :], writes=[kf])
            P.dma("sp", qf[:, :], q_i[hr, :], writes=[qf])
            P.dma("pool", ko[:, :], ko_i[hr, :], writes=[ko])
            P.dma("pool", vb[:], v_i[:, hr].rearrange("(t p) d -> p t d", p=128), writes=[vb])
            P.dma("pool", vo[:], vo_i[:, hr].rearrange("(t p) d -> p t d", p=128), writes=[vo])
            P.op("act", "activation", out=kb[:, :], in_=kf[:, :], func=AF.Copy, reads=[kf], writes=[kb])
            P.op("act", "activation", out=qb[:, :], in_=qf[:, :], func=AF.Copy, reads=[qf], writes=[qb])
            P.op("dve", "tensor_reduce", out=km[:, :], in_=kf[:, :].rearrange("p (n s) -> p n s", s=256), axis=AX.X, op=ALU.add,
                 reads=[kf], writes=[km])
            P.op("dve", "tensor_scalar", km[:, :], km[:, :], 1.0 / 256, None, ALU.mult, reads=[km], writes=[km])
            for qi in range(4):
                qs_ = slice(qi * 256, (qi + 1) * 256)
                ns = ns_ring.get()
                for qs in range(2):
                    ps_g = pp.get()
                    P.op("pe", "matmul", ps_g[:, 0:16], qf[:, qi * 256 + qs * 128:qi * 256 + (qs + 1) * 128], km[:, :],
                         start=True, stop=True, reads=[qf, km], writes=[ps_g])
                    gm = gm_ring.get(); t8 = t8_ring.get(); sel = sel_ring.get()
                    P.op("dve", "tensor_tensor", out=gm[:, :], in0=ps_g[:, 0:16], in1=gbias[:, qi * 16:(qi + 1) * 16], op=ALU.add,
                         reads=[ps_g, gbias], writes=[gm])
                    P.op("dve", "max", out=t8[:, :], in_=gm[:, :], reads=[gm], writes=[t8])
                    P.op("dve", "tensor_scalar", sel[:, :], gm[:, :], t8[:, 2:3], None, ALU.is_ge, reads=[gm, t8], writes=[sel])
                    P.op("dve", "tensor_tensor", out=sel[:, :], in0=sel[:, :], in1=pmask[:, qi * 16:(qi + 1) * 16], op=ALU.mult,
                         reads=[sel, pmask], writes=[sel])
                    P.op("dve", "tensor_scalar", sel[:, :], sel[:, :], -1.0, -BIGNEG, ALU.add, ALU.mult, reads=[sel], writes=[sel])
                    ps_t = pp.get()
                    P.op("pe", "transpose", ps_t[0:16, 0:128], sel[:, :], ident[:, :], reads=[sel, ident], writes=[ps_t])
                    P.op("act", "activation", out=ns[:, qs * 128:(qs + 1) * 128], in_=ps_t[0:16, 0:128], func=AF.Copy,
                         reads=[ps_t], writes=[ns])
                ps_o = pacc.get(); ps_d = pacc.get()
                tiles = [(n, kt) for n in range(NBLK) for kt in range(2)] + [(-1, 0), (-1, 1)]
                for ti, (n, kt) in enumerate(tiles):
                    first, last = (ti == 0), (ti == len(tiles) - 1)
                    ps_s = pp.get()
                    lg = lg_ring.get(); pT = pT_ring.get()
                    tsl = slice(128, 384) if kt == 0 else slice(0, 256)
                    if n >= 0:
                        P.op("pe", "matmul", ps_s[:, 0:256], kb[:, n * 256 + kt * 128:n * 256 + (kt + 1) * 128], qb[:, qs_],
                             start=True, stop=False, reads=[kb, qb], writes=[ps_s])
                        P.op("pe", "matmul", ps_s[:, 0:256], onehot[:, n, :], ns[:, :], start=False, stop=True,
                             reads=[onehot, ns], writes=[ps_s])
                        P.op("dve", "scalar_tensor_tensor", out=lg[:, :], in0=ps_s[:, 0:256], scalar=scale, in1=tna[:, h, tsl],
                             op0=ALU.mult, op1=ALU.add, reads=[ps_s, tna], writes=[lg])
                        bi = (h * 4 + qi) * 16 + n
                        P.op("act", "activation", out=pT[:, :], in_=lg[:, :], func=AF.Exp, bias=abias[:, bi:bi + 1],
                             reads=[lg, abias], writes=[pT])
                        vl = vb[:, n * 2 + kt, :]
                        vbuf = vb
                    else:
                        P.op("pe", "matmul", ps_s[:, 0:256], ko[:, qi * 256 + kt * 128:qi * 256 + (kt + 1) * 128], qb[:, qs_],
                             start=True, stop=True, reads=[ko, qb], writes=[ps_s])
                        P.op("dve", "scalar_tensor_tensor", out=lg[:, :], in0=ps_s[:, 0:256], scalar=scale, in1=tca[:, h, tsl],
                             op0=ALU.mult, op1=ALU.add, reads=[ps_s, tca], writes=[lg])
                        P.op("act", "activation", out=pT[:, :], in_=lg[:, :], func=AF.Exp, reads=[lg], writes=[pT])
                        vl = vo[:, qi * 2 + kt, :]
                        vbuf = vo
                    P.op("pe", "matmul", ps_o[:, 0:256], vl, pT[:, :], start=first, stop=last, reads=[vbuf, pT], writes=[ps_o])
                    P.op("pe", "matmul", ps_d[:, 0:256], ones_bf[:, :], pT[:, :], start=first, stop=last,
                         reads=[ones_bf, pT], writes=[ps_d])
                rd = rd_ring.get(); ot = o_ring.get()
                P.op("dve", "reciprocal", rd[:, :], ps_d[:, 0:256], reads=[ps_d], writes=[rd])
                P.op("dve", "tensor_tensor", out=ot[:, :], in0=ps_o[:, 0:256], in1=rd[:, :], op=ALU.mult, reads=[ps_o, rd], writes=[ot])
                outs.append(P.dma("sp", yo[hr, qs_], ot[:, :], reads=[ot]))
        P.emit(final_wait_ops=outs)
    return nc


def attn_consts(seg):
    sl = np.arange(128)[:, None].astype(np.float64)
    u = np.arange(384)[None, :].astype(np.float64)
    dist = u - 128 - sl
    slopes = 2.0 ** (-(np.arange(8) + 1.0))
    tna = np.zeros((128, 8, 384), np.float32); tca = np.zeros((128, 8, 384), np.float32)
    for h in range(8):
        tna[:, h] = -slopes[h] * dist
        tca[:, h] = np.where(dist >= 0, -slopes[h] * dist, BIGNEG)
    abias = np.zeros((8, 4, 16), np.float32); gb = np.zeros((4, 16), np.float32); pm = np.zeros((4, 16), np.float32)
    for qi in range(4):
        G = 4 * seg + qi
        for n in range(16):
            if n < G:
                pm[qi, n] = 1.0
                abias[:, qi, n] = -slopes * 256.0 * (G - n)
            else:
                gb[qi, n] = -1e30
    oh = np.zeros((16, 16, 128), np.float32)
    for n in range(16):
        oh[n, n, :] = 1.0
    return {"Tna": tna.reshape(128, -1), "Tca": tca.reshape(128, -1), "abias": _rep(abias.reshape(-1)),
            "gbias": _rep(gb.reshape(-1)), "pastmask": _rep(pm.reshape(-1)), "onehot": oh.reshape(16, -1),
            "ident": np.eye(128, dtype=np.float32)}


def progC1_inputs(qT_c, kT_c, v_c):
    maps = []
    for c in range(NCORES):
        b, seg = c // 4, c % 4
        m = {"qT": qT_c[c], "kT_own": kT_c[c], "v_own": v_c[c],
             "kT_all": np.ascontiguousarray(np.concatenate([kT_c[b * 4 + s] for s in range(4)], axis=1)),
             "v_all": np.ascontiguousarray(np.concatenate([v_c[b * 4 + s] for s in range(4)], axis=0))}
        m.update(attn_consts(seg))
        maps.append(m)
    return maps


MEMLEN = 256


def build_progC2(T=NTOK):
    nc = bass.Bass("TRN2", target_bir_lowering=False)
    di = lambda n, sh: nc.dram_tensor(n, sh, F32, kind="ExternalInput").ap()
    xin = di("xT", [D, T]); yin = di("yT", [4096, T]); wout = di("w_out", [4096, D]); mem_i = di("memT", [D, MEMLEN])
    xmg_i = di("xmg", [128, 16]); mmg_i = di("mmg", [128, 16]); ffg_i = di("ffg", [128, 16])
    mqg_i = di("mqg", [128, 1]); mkg_i = di("mkg", [128, 1])
    wq = di("wq", [D, 512]); wk = di("wk", [D, 512]); wv = di("wv", [D, 512]); wo = di("wo", [512, D])
    wgu = di("w_gu", [D, 2 * DFF]); wdn = di("w_down", [DFF, D])
    xo = nc.dram_tensor("xoT", [D, T], F32, kind="ExternalOutput").ap()
    NKC = D // 128
    scale = 128.0 ** -0.5
    with contextlib.ExitStack() as stack:
        P = Prog(nc, stack)
        pp = PsumPool(P, 8)
        xT = [P.sbuf([128, T], F32, "xT%d" % c) for c in range(NKC)]
        hT = [P.sbuf([128, T], BF16, "hT%d" % c) for c in range(NKC)]
        aT = [P.sbuf([128, T], BF16, "aT%d" % c) for c in range(12)]
        g_xm = P.sbuf([128, NKC], F32, "g_xm"); g_mm = P.sbuf([128, NKC], F32, "g_mm"); g_ff = P.sbuf([128, NKC], F32, "g_ff")
        mqg = P.sbuf([128, 1], F32, "mqg"); mkg = P.sbuf([128, 1], F32, "mkg")
        ones_bf = P.sbuf([128, 128], BF16, "ones")
        rstd = P.sbuf([128, T], F32, "rstd")
        sq_ring = Ring([P.sbuf([128, 512], BF16, "sq%d" % i) for i in range(3)])
        sg_ring = Ring([P.sbuf([128, 512], F32, "sg%d" % i) for i in range(3)])
        wg_ring = Ring([P.sbuf([128, NKC, 256], BF16, "wg%d" % i) for i in range(2)])
        wu_ring = Ring([P.sbuf([128, NKC, 256], BF16, "wu%d" % i) for i in range(2)])
        wd_ring = Ring([P.sbuf([128, 12, 512], BF16, "wd%d" % i) for i in range(2)])
        w_ring = Ring(wg_ring.bufs + wu_ring.bufs)
        mr_ring = Ring([P.sbuf([128, MEMLEN], F32, "mr%d" % i) for i in range(3)])
        mhT = [P.sbuf([128, MEMLEN], BF16, "mhT%d" % c) for c in range(NKC)]
        kmT = [P.sbuf([128, MEMLEN], BF16, "kmT%d" % c) for c in range(4)]
        vm = [P.sbuf([128, 512], BF16, "vm%d" % c) for c in range(2)]
        pT_ring = sq_ring
        rd_ring = sg_ring

        xv = xin.rearrange("(c p) t -> c p t", p=128)
        yv = yin.rearrange("(c p) t -> c p t", p=128)
        mv = mem_i.rearrange("(c p) t -> c p t", p=128)
        P.op("pool", "memset", ones_bf[:, :], 1.0, writes=[ones_bf])
        for (b, a) in ((g_xm, xmg_i), (g_mm, mmg_i), (g_ff, ffg_i), (mqg, mqg_i), (mkg, mkg_i)):
            P.dma("sp", b[:], a, writes=[b])
        for c in range(NKC):
            P.dma("sp", xT[c][:, :], xv[c], writes=[xT[c]])

        def cons_add(fc, h0, w, ps):
            P.op("dve", "tensor_tensor", out=xT[fc][:, h0:h0 + w], in0=ps[:, 0:w], in1=xT[fc][:, h0:h0 + w], op=ALU.add,
                 reads=[ps, xT[fc]], writes=[xT[fc]])
        for kh in range(2):
            for c in range(NKC):
                P.dma("pool", hT[c][:, :], yv[kh * NKC + c], writes=[hT[c]])
            lin_fm(P, pp, hT, wout[kh * D:(kh + 1) * D, :], D, w_ring, T, cons_add)
        ps_m = pp.get()
        for c in range(NKC):
            mt = mr_ring.get()
            P.dma("sp", mt[:, :], mv[c], writes=[mt])
            sq = sq_ring.get()
            P.op("act", "activation", out=sq[:, 0:MEMLEN], in_=mt[:, :], func=AF.Square, reads=[mt], writes=[sq])
            P.op("pe", "matmul", ps_m[:, 0:MEMLEN], ones_bf[:, :], sq[:, 0:MEMLEN], start=(c == 0), stop=(c == NKC - 1),
                 reads=[sq, ones_bf], writes=[ps_m])
        P.op("dve", "tensor_scalar", rstd[:, 0:MEMLEN], ps_m[:, 0:MEMLEN], 1.0 / D, EPS, ALU.mult, ALU.add, reads=[ps_m], writes=[rstd])
        P.op("act", "activation", out=rstd[:, 0:MEMLEN], in_=rstd[:, 0:MEMLEN], func=AF.Sqrt, reads=[rstd], writes=[rstd])
        P.op("dve", "reciprocal", rstd[:, 0:MEMLEN], rstd[:, 0:MEMLEN], reads=[rstd], writes=[rstd])
        for c in range(NKC):
            mt = mr_ring.get()
            P.dma("sp", mt[:, :], mv[c], writes=[mt])
            P.op("dve", "scalar_tensor_tensor", out=mhT[c][:, :], in0=mt[:, :], scalar=g_mm[:, c:c + 1], in1=rstd[:, 0:MEMLEN],
                 op0=ALU.mult, op1=ALU.mult, reads=[mt, g_mm, rstd], writes=[mhT[c]])

        def dst_k(fc, h0, w):
            return kmT[fc][:, h0:h0 + w], kmT[fc]
        lin_fm(P, pp, mhT, wk, 512, w_ring, MEMLEN, headnorm_consume(P, pp, ones_bf, mkg, sq_ring, sg_ring, dst_k))

        def cons_v(tt, c0, cw, ps):
            P.op("act", "activation", out=vm[tt][:, c0:c0 + cw], in_=ps[:, 0:cw], func=AF.Copy, reads=[ps], writes=[vm[tt]])
        lin_tm(P, pp, mhT, wv, 512, w_ring, MEMLEN, cons_v)
        rmsnorm_fm(P, pp, xT, g_xm, hT, ones_bf, sq_ring, rstd, NKC, T, D)
        qmT = aT[0:4]
        oT = aT[4:8]

        def dst_q(fc, h0, w):
            return qmT[fc][:, h0:h0 + w], qmT[fc]
        lin_fm(P, pp, hT, wq, 512, w_ring, T, headnorm_consume(P, pp, ones_bf, mqg, sq_ring, sg_ring, dst_q))
        for mh in range(4):
            for h0 in range(0, T, 512):
                w = min(512, T - h0)
                ps_o = pp.get(); ps_d = pp.get()
                for kt in range(2):
                    ps_s = pp.get()
                    P.op("pe", "matmul", ps_s[:, 0:w], kmT[mh][:, kt * 128:(kt + 1) * 128], qmT[mh][:, h0:h0 + w],
                         start=True, stop=True, reads=[kmT[mh], qmT[mh]], writes=[ps_s])
                    pT = pT_ring.get()
                    P.op("act", "activation", out=pT[:, 0:w], in_=ps_s[:, 0:w], func=AF.Exp, scale=scale, reads=[ps_s], writes=[pT])
                    P.op("pe", "matmul", ps_o[:, 0:w], vm[kt][:, mh * 128:(mh + 1) * 128], pT[:, 0:w], start=(kt == 0),
                         stop=(kt == 1), reads=[vm[kt], pT], writes=[ps_o])
                    P.op("pe", "matmul", ps_d[:, 0:w], ones_bf[:, :], pT[:, 0:w], start=(kt == 0), stop=(kt == 1),
                         reads=[ones_bf, pT], writes=[ps_d])
                rd = rd_ring.get()
                P.op("dve", "reciprocal", rd[:, 0:w], ps_d[:, 0:w], reads=[ps_d], writes=[rd])
                P.op("dve", "tensor_tensor", out=oT[mh][:, h0:h0 + w], in0=ps_o[:, 0:w], in1=rd[:, 0:w], op=ALU.mult,
                     reads=[ps_o, rd], writes=[oT[mh]])
        lin_fm(P, pp, oT, wo, D, w_ring, T, cons_add, nkc=4)
        rmsnorm_fm(P, pp, xT, g_ff, hT, ones_bf, sq_ring, rstd, NKC, T, D)
        ffn_fm(P, pp, xT, hT, wgu, wdn, aT, wg_ring, wu_ring, wd_ring, sg_ring, T)
        xov = xo.rearrange("(c p) t -> c p t", p=128)
        outs = [P.dma("sp", xov[c], xT[c][:, :], reads=[xT[c]]) for c in range(NKC)]
        P.emit(final_wait_ops=outs)
    return nc


U32 = mybir.dt.uint32


def _xbc_row0(fc):
    if fc < 24:
        j, loc = fc // 6, fc % 6
    elif fc < 32:
        j, loc = (fc - 24) // 2, 6 + (fc - 24) % 2
    else:
        j, loc = (fc - 32) // 2, 8 + (fc - 32) % 2
    return j * 1280 + loc * 128


def build_fused(stop=None, skip_cc=()):
    T = NTOK
    NKC = D // 128
    nc = bass.Bass("TRN2", target_bir_lowering=False)
    di = lambda n, sh, dt=F32: nc.dram_tensor(n, sh, dt, kind="ExternalInput").ap()
    x_in = di("xT", [D, T]); mem_i = di("memT", [D, MEMLEN])
    W = {n: di(n, sh) for n, sh in (("w_gu1", [2, D, 2 * DFF]), ("w_down1", [2, DFF, D]), ("w_in", [2, D, N_IN]),
                                   ("w_out", [2, 4096, D]), ("wq", [2, D, 512]), ("wk", [2, D, 512]), ("wv", [2, D, 512]),
                                   ("wo", [2, 512, D]), ("w_gu2", [2, D, 2 * DFF]), ("w_down2", [2, DFF, D]))}
    G = {n: di(n, [2, 128, 16]) for n in ("ffg1", "mixg", "xmg", "mmg", "ffg2")}
    G.update({n: di(n, [2, 128, 1]) for n in ("qg", "kg", "mqg", "mkg")})
    S = {n: di(n, sh) for n, sh in (("convw", [2, 128, 10, 4]), ("convb", [2, 128, 10]), ("dtb", [2, 128, 384]),
                                   ("alog", [2, 128, 384]), ("dsk", [2, 128, 768]), ("normw", [2, 128, 768]),
                                   ("tri", [128, 128]), ("strict", [128, 128]), ("onesf", [128, 128]), ("ident", [128, 128]),
                                   ("Tna", [128, 8 * 384]), ("Tca", [128, 8 * 384]), ("abias", [128, 512]),
                                   ("gbias", [128, 64]), ("pastmask", [128, 64]), ("onehot", [16, 16 * 128]))}
    IDX = {n: di(n, [128, 1], U32) for n in ("idx_x", "idx_t", "idx_d", "idx_y")}
    out_ap = nc.dram_tensor("xoT", [D, T], F32, kind="ExternalOutput").ap()
    dr = lambda n, sh: Buf(nc.dram_tensor(n, sh, F32).ap(), n)
    xres = dr("xres", [D, T]); xres1 = dr("xres1", [D, T]); q_s = dr("q_s", [1024, T]); ya_s = dr("ya_s", [1024, T])
    kv_send = dr("kv_send", [2048, T]); kv_g = dr("kv_g", [4 * 2048, T])
    x_send = dr("x_send", [5120, T]); x_g = dr("x_g", [4 * 5120, T])
    z_send = dr("z_send", [4 * T, 768]); z_g = dr("z_g", [16 * T, 768])
    dt_send = dr("dt_send", [4 * T, 12]); dt_g = dr("dt_g", [16 * T, 12])
    y_send = dr("y_send", [3072, T]); y_g = dr("y_g", [4 * 3072, T])
    RG = [[0, 1, 2, 3], [4, 5, 6, 7]]
    scale = 128.0 ** -0.5

    with contextlib.ExitStack() as stack:
        P = Prog(nc, stack)
        P.begin_phases()

        def gather(out_ap_, out_buf, src, idx_t, row0, reads=()):
            width = src.t.shape[1]
            return P.add("pool", lambda e: e.indirect_dma_start(
                out=out_ap_, out_offset=None, in_=src.t, in_offset=bass.IndirectOffsetOnAxis(ap=idx_t[:, 0:1], axis=0),
                element_offset=row0 * width), reads=[src, idx_t] + list(reads), writes=[out_buf], is_dma=True)

        def finish():
            with P.phase(final=True):
                tb_ = [P.sbuf([128, T], F32, "fin%d" % i) for i in range(2)]
                sv = xres1.t.rearrange("(c p) t -> c p t", p=128)
                dv_ = out_ap.rearrange("(c p) t -> c p t", p=128)
                for c in range(NKC):
                    P.dma("sp", tb_[c % 2][:, :], sv[c], reads=[xres1], writes=[tb_[c % 2]])
                    P.dma("sp", dv_[c], tb_[c % 2][:, :], reads=[tb_[c % 2]])

        _coll = P.collective

        def coll(kind, groups, a, b, reads=(), writes=(), tag=None):
            if tag in skip_cc:
                return None
            rows = a.shape[0]
            if tag == "dt":
                return _coll(kind, groups, a, b, reads=reads, writes=writes)
            for k in range(rows // 256):
                _coll(kind, groups, a[k * 256:(k + 1) * 256, :], b[k * 1024:(k + 1) * 1024, :], reads=reads, writes=writes)

        for l in range(2):
            with P.phase():
                pp = PsumPool(P, 8)
                xT = [P.sbuf([128, T], F32, "xT%d" % c) for c in range(NKC)]
                hT = [P.sbuf([128, T], BF16, "hT%d" % c) for c in range(NKC)]
                aT = [P.sbuf([128, T], BF16, "aT%d" % c) for c in range(12)]
                g1 = P.sbuf([128, NKC], F32, "g1"); g2 = P.sbuf([128, NKC], F32, "g2")
                qg = P.sbuf([128, 1], F32, "qg"); kg = P.sbuf([128, 1], F32, "kg")
                ones_bf = P.sbuf([128, 128], BF16, "ones")
                rstd = P.sbuf([128, T], F32, "rstd")
                sq_ring = Ring([P.sbuf([128, 512], BF16, "sq%d" % i) for i in range(3)])
                sg_ring = Ring([P.sbuf([128, 512], F32, "sg%d" % i) for i in range(3)])
                wg_ring = Ring([P.sbuf([128, NKC, 256], BF16, "wg%d" % i) for i in range(2)])
                wu_ring = Ring([P.sbuf([128, NKC, 256], BF16, "wu%d" % i) for i in range(2)])
                wd_ring = Ring([P.sbuf([128, 12, 512], BF16, "wd%d" % i) for i in range(2)])
                w_ring = Ring(wg_ring.bufs + wu_ring.bufs)
                st_ring = Ring([P.sbuf([128, 512], F32, "st%d" % i) for i in range(4)])
                P.op("dve", "memset", ones_bf[:, :], 1.0, writes=[ones_bf])
                P.dma("sp", g1[:, :], G["ffg1"][l], writes=[g1]); P.dma("sp", g2[:, :], G["mixg"][l], writes=[g2])
                P.dma("sp", qg[:, :], G["qg"][l], writes=[qg]); P.dma("sp", kg[:, :], G["kg"][l], writes=[kg])
                src = x_in if l == 0 else xres.t
                xv = src.rearrange("(c p) t -> c p t", p=128)
                for c in range(NKC):
                    P.dma("sp", xT[c][:, :], xv[c], reads=([] if l == 0 else [xres]), writes=[xT[c]])
                rmsnorm_fm(P, pp, xT, g1, hT, ones_bf, sq_ring, rstd, NKC, T, D)
                ffn_fm(P, pp, xT, hT, W["w_gu1"][l], W["w_down1"][l], aT, wg_ring, wu_ring, wd_ring, sg_ring, T)
                x1v = xres1.t.rearrange("(c p) t -> c p t", p=128)
                for c in range(NKC):
                    P.dma("sp", x1v[c], xT[c][:, :], reads=[xT[c]], writes=[xres1])
                rmsnorm_fm(P, pp, xT, g2, hT, ones_bf, sq_ring, rstd, NKC, T, D)
                win = W["w_in"][l]
                for (dstb, rbase, col0, gbuf) in ((q_s, 0, OQ, qg), (kv_send, 0, OK_, kg)):
                    holder = {}

                    def dst_of(fc, h0, w, holder=holder):
                        st = st_ring.get()
                        holder["st"] = st
                        return st[:, 0:w], st
                    cons0 = headnorm_consume(P, pp, ones_bf, gbuf, sq_ring, sg_ring, dst_of)

                    def cons(fc, h0, w, ps, cons0=cons0, holder=holder, dstb=dstb, rbase=rbase):
                        cons0(fc, h0, w, ps)
                        st = holder["st"]
                        P.dma("sp", dstb.t[rbase + fc * 128:rbase + (fc + 1) * 128, h0:h0 + w], st[:, 0:w], reads=[st], writes=[dstb])
                    lin_fm(P, pp, hT, win[:, col0:col0 + 1024], 1024, w_ring, T, cons)

                def cons_xbc(fc, h0, w, ps):
                    st = st_ring.get()
                    P.op("act", "activation", out=st[:, 0:w], in_=ps[:, 0:w], func=AF.Copy, reads=[ps], writes=[st])
                    r0 = _xbc_row0(fc)
                    P.dma("sp", x_send.t[r0:r0 + 128, h0:h0 + w], st[:, 0:w], reads=[st], writes=[x_send])
                lin_fm(P, pp, hT, win[:, OXBC:OXBC + NXBC], NXBC, w_ring, T, cons_xbc)

                def cons_v(tt, c0, cw, ps):
                    st = st_ring.get()
                    P.op("dve", "tensor_copy", st[:, 0:cw], ps[:, 0:cw], reads=[ps], writes=[st])
                    P.dma("sp", kv_send.t[1024 + tt * 128:1024 + (tt + 1) * 128, c0:c0 + cw], st[:, 0:cw], reads=[st], writes=[kv_send])
                lin_tm(P, pp, hT, win[:, OV:OV + NV], NV, w_ring, T, cons_v)

                def cons_z(tt, c0, cw, ps):
                    st = st_ring.get()
                    P.op("dve", "tensor_copy", st[:, 0:cw], ps[:, 0:cw], reads=[ps], writes=[st])
                    j, lc0 = c0 // 768, c0 % 768
                    P.dma("sp", z_send.t[j * T + tt * 128:j * T + (tt + 1) * 128, lc0:lc0 + cw], st[:, 0:cw], reads=[st], writes=[z_send])
                lin_tm(P, pp, hT, win[:, OZ:OZ + NZ], NZ, w_ring, T, cons_z)

                def cons_dt(tt, c0, cw, ps):
                    st = st_ring.get()
                    P.op("dve", "tensor_copy", st[:, 0:cw], ps[:, 0:cw], reads=[ps], writes=[st])
                    for j in range(4):
                        P.dma("sp", dt_send.t[j * T + tt * 128:j * T + (tt + 1) * 128, :], st[:, 12 * j:12 * j + 12], reads=[st], writes=[dt_send])
                lin_tm(P, pp, hT, win[:, ODT:ODT + NDT], NDT, w_ring, T, cons_dt)
                coll("AllGather", RG, kv_send.t, kv_g.t, reads=[kv_send], writes=[kv_g], tag="kv")

            if stop == (l, "A"):
                finish()
                return nc
            with P.phase():
                coll("AllGather", RG, x_send.t, x_g.t, reads=[x_send], writes=[x_g], tag="x")
                coll("AllGather", RG, z_send.t, z_g.t, reads=[z_send], writes=[z_g], tag="z")
                coll("AllGather", RG, dt_send.t, dt_g.t, reads=[dt_send], writes=[dt_g], tag="dt")
                pp = PsumPool(P, 4)
                pacc = PsumPool(P, 4, pfx="pacc")
                tna = P.sbuf([128, 8, 384], F32, "tna"); tca = P.sbuf([128, 8, 384], F32, "tca")
                abias = P.sbuf([128, 512], F32, "abias"); gbias = P.sbuf([128, 64], F32, "gbias")
                pmask = P.sbuf([128, 64], F32, "pmask"); onehot = P.sbuf([16, 16, 128], BF16, "onehot")
                ident = P.sbuf([128, 128], F32, "ident"); ones_bf = P.sbuf([128, 128], BF16, "ones")
                P.dma("sp", tna[:], S["Tna"].rearrange("p (h u) -> p h u", h=8), writes=[tna])
                P.dma("sp", tca[:], S["Tca"].rearrange("p (h u) -> p h u", h=8), writes=[tca])
                for (b, a) in ((abias, S["abias"]), (gbias, S["gbias"]), (pmask, S["pastmask"]), (ident, S["ident"])):
                    P.dma("sp", b[:], a, writes=[b])
                ohs = P.sbuf([16, 16, 128], F32, "ohs")
                P.dma("sp", ohs[:], S["onehot"].rearrange("k (n m) -> k n m", n=16), writes=[ohs])
                P.op("dve", "tensor_copy", onehot[:], ohs[:], reads=[ohs], writes=[onehot])
                P.op("dve", "memset", ones_bf[:, :], 1.0, writes=[ones_bf])
                kf_ring = Ring([P.sbuf([128, SEQ], F32, "kf%d" % i) for i in range(2)])
                kb_ring = Ring([P.sbuf([128, SEQ], BF16, "kb%d" % i) for i in range(2)])
                qf_ring = Ring([P.sbuf([128, NTOK], F32, "qf%d" % i) for i in range(2)])
                qb_ring = Ring([P.sbuf([128, NTOK], BF16, "qb%d" % i) for i in range(2)])
                ko_ring = Ring([P.sbuf([128, NTOK], BF16, "ko%d" % i) for i in range(2)])
                vb_ring = Ring([P.sbuf([128, 32, 128], BF16, "vb%d" % i) for i in range(2)])
                vo_ring = Ring([P.sbuf([128, 8, 128], BF16, "vo%d" % i) for i in range(2)])
                kos_ring = Ring([P.sbuf([128, NTOK], F32, "kos%d" % i) for i in range(2)])
                vbs_ring = Ring([P.sbuf([128, 32, 128], F32, "vbs%d" % i) for i in range(2)])
                vos_ring = Ring([P.sbuf([128, 8, 128], F32, "vos%d" % i) for i in range(2)])
                km_ring = Ring([P.sbuf([128, 16], F32, "km%d" % i) for i in range(2)])
                gm_ring = Ring([P.sbuf([128, 16], F32, "gm%d" % i) for i in range(2)])
                t8_ring = Ring([P.sbuf([128, 8], F32, "t8%d" % i) for i in range(2)])
                sel_ring = Ring([P.sbuf([128, 16], F32, "sel%d" % i) for i in range(2)])
                ns_ring = Ring([P.sbuf([16, 256], BF16, "ns%d" % i) for i in range(2)])
                lg_ring = Ring([P.sbuf([128, 256], F32, "lg%d" % i) for i in range(4)])
                pT_ring = Ring([P.sbuf([128, 256], BF16, "pT%d" % i) for i in range(8)])
                rd_ring = Ring([P.sbuf([128, 256], F32, "rd%d" % i) for i in range(2)])
                o_ring = Ring([P.sbuf([128, 256], F32, "oo%d" % i) for i in range(2)])
                for h in range(8):
                    hr = slice(h * 128, (h + 1) * 128)
                    kf = kf_ring.get(); kb = kb_ring.get(); qf = qf_ring.get(); qb = qb_ring.get()
                    ko = ko_ring.get(); vb = vb_ring.get(); vo = vo_ring.get(); km = km_ring.get()
                    kos = kos_ring.get(); vbs = vbs_ring.get(); vos = vos_ring.get()
                    for sgm in range(4):
                        kr0 = (h // 2) * 1024 + sgm * 256 + (h % 2) * 128
                        P.dma("sp", kf[:, sgm * T:(sgm + 1) * T], kv_g.t[kr0:kr0 + 128, :], reads=[kv_g], writes=[kf])
                        for a_ in range(4):
                            vr0 = (4 + a_) * 1024 + sgm * 256
                            P.dma("sp", vbs[:, sgm * 8 + 2 * a_:sgm * 8 + 2 * a_ + 2, :],
                                  kv_g.t[vr0:vr0 + 256, hr].rearrange("(t p) d -> p t d", p=128), reads=[kv_g], writes=[vbs])
                    P.dma("sp", qf[:, :], q_s.t[hr, :], reads=[q_s], writes=[qf])
                    P.dma("sp", kos[:, :], kv_send.t[hr, :], reads=[kv_send], writes=[kos])
                    P.dma("sp", vos[:], kv_send.t[1024:2048, hr].rearrange("(t p) d -> p t d", p=128), reads=[kv_send], writes=[vos])
                    P.op("act", "activation", out=ko[:, :], in_=kos[:, :], func=AF.Copy, reads=[kos], writes=[ko])
                    P.op("dve", "tensor_copy", vb[:], vbs[:], reads=[vbs], writes=[vb])
                    P.op("dve", "tensor_copy", vo[:], vos[:], reads=[vos], writes=[vo])
                    P.op("act", "activation", out=kb[:, :], in_=kf[:, :], func=AF.Copy, reads=[kf], writes=[kb])
                    P.op("act", "activation", out=qb[:, :], in_=qf[:, :], func=AF.Copy, reads=[qf], writes=[qb])
                    P.op("dve", "tensor_reduce", out=km[:, :], in_=kf[:, :].rearrange("p (n s) -> p n s", s=256), axis=AX.X,
                         op=ALU.add, reads=[kf], writes=[km])
                    P.op("dve", "tensor_scalar", km[:, :], km[:, :], 1.0 / 256, None, ALU.mult, reads=[km], writes=[km])
                    for qi in range(4):
                        qs_ = slice(qi * 256, (qi + 1) * 256)
                        ns = ns_ring.get()
                        for qs in range(2):
                            ps_g = pp.get()
                            P.op("pe", "matmul", ps_g[:, 0:16], qf[:, qi * 256 + qs * 128:qi * 256 + (qs + 1) * 128], km[:, :],
                                 start=True, stop=True, reads=[qf, km], writes=[ps_g])
                            gm = gm_ring.get(); t8 = t8_ring.get(); sel = sel_ring.get()
                            P.op("dve", "tensor_tensor", out=gm[:, :], in0=ps_g[:, 0:16], in1=gbias[:, qi * 16:(qi + 1) * 16],
                                 op=ALU.add, reads=[ps_g, gbias], writes=[gm])
                            P.op("dve", "max", out=t8[:, :], in_=gm[:, :], reads=[gm], writes=[t8])
                            P.op("dve", "tensor_scalar", sel[:, :], gm[:, :], t8[:, 2:3], None, ALU.is_ge, reads=[gm, t8], writes=[sel])
                            P.op("dve", "tensor_tensor", out=sel[:, :], in0=sel[:, :], in1=pmask[:, qi * 16:(qi + 1) * 16],
                                 op=ALU.mult, reads=[sel, pmask], writes=[sel])
                            P.op("dve", "tensor_scalar", sel[:, :], sel[:, :], -1.0, -BIGNEG, ALU.add, ALU.mult, reads=[sel], writes=[sel])
                            ps_t = pp.get()
                            P.op("pe", "transpose", ps_t[0:16, 0:128], sel[:, :], ident[:, :], reads=[sel, ident], writes=[ps_t])
                            P.op("act", "activation", out=ns[:, qs * 128:(qs + 1) * 128], in_=ps_t[0:16, 0:128], func=AF.Copy,
                                 reads=[ps_t], writes=[ns])
                        ps_o = pacc.get(); ps_d = pacc.get()
                        tiles = [(n, kt) for n in range(NBLK) for kt in range(2)] + [(-1, 0), (-1, 1)]
                        LA = 3
                        pend = {}

                        def stage1(ti):
                            n, kt = tiles[ti]
                            ps_s = pp.get()
                            lg = lg_ring.get(); pT = pT_ring.get()
                            tsl = slice(128, 384) if kt == 0 else slice(0, 256)
                            if n >= 0:
                                P.op("pe", "matmul", ps_s[:, 0:256], kb[:, n * 256 + kt * 128:n * 256 + (kt + 1) * 128], qb[:, qs_],
                                     start=True, stop=False, reads=[kb, qb], writes=[ps_s])
                                P.op("pe", "matmul", ps_s[:, 0:256], onehot[:, n, :], ns[:, :], start=False, stop=True,
                                     reads=[onehot, ns], writes=[ps_s])
                                P.op("dve", "scalar_tensor_tensor", out=lg[:, :], in0=ps_s[:, 0:256], scalar=scale, in1=tna[:, h, tsl],
                                     op0=ALU.mult, op1=ALU.add, reads=[ps_s, tna], writes=[lg])
                                bi = (h * 4 + qi) * 16 + n
                                P.op("act", "activation", out=pT[:, :], in_=lg[:, :], func=AF.Exp, bias=abias[:, bi:bi + 1],
                                     reads=[lg, abias], writes=[pT])
                                pend[ti] = (vb[:, n * 2 + kt, :], vb, pT)
                            else:
                                P.op("pe", "matmul", ps_s[:, 0:256], ko[:, qi * 256 + kt * 128:qi * 256 + (kt + 1) * 128], qb[:, qs_],
                                     start=True, stop=True, reads=[ko, qb], writes=[ps_s])
                                P.op("dve", "scalar_tensor_tensor", out=lg[:, :], in0=ps_s[:, 0:256], scalar=scale, in1=tca[:, h, tsl],
                                     op0=ALU.mult, op1=ALU.add, reads=[ps_s, tca], writes=[lg])
                                P.op("act", "activation", out=pT[:, :], in_=lg[:, :], func=AF.Exp, reads=[lg], writes=[pT])
                                pend[ti] = (vo[:, qi * 2 + kt, :], vo, pT)

                        def stage2(ti):
                            vl, vbuf, pT = pend.pop(ti)
                            first, last = (ti == 0), (ti == len(tiles) - 1)
                            P.op("pe", "matmul", ps_o[:, 0:256], vl, pT[:, :], start=first, stop=last, reads=[vbuf, pT], writes=[ps_o])
                            P.op("pe", "matmul", ps_d[:, 0:256], ones_bf[:, :], pT[:, :], start=first, stop=last,
                                 reads=[ones_bf, pT], writes=[ps_d])
                        for ti in range(len(tiles) + LA):
                            if ti < len(tiles):
                                stage1(ti)
                            if ti - LA >= 0:
                                stage2(ti - LA)
                        rd = rd_ring.get(); ot = o_ring.get()
                        P.op("dve", "reciprocal", rd[:, :], ps_d[:, 0:256], reads=[ps_d], writes=[rd])
                        P.op("dve", "tensor_tensor", out=ot[:, :], in0=ps_o[:, 0:256], in1=rd[:, :], op=ALU.mult,
                             reads=[ps_o, rd], writes=[ot])
                        P.dma("sp", ya_s.t[hr, qs_], ot[:, :], reads=[ot], writes=[ya_s])

            if stop == (l, "C1"):
                finish()
                return nc
            with P.phase():
                pp = PsumPool(P, 8)
                cw = P.sbuf([128, 10, 4], F32, "cw"); cb = P.sbuf([128, 10], F32, "cb")
                dt_all = P.sbuf([128, 384], F32, "dt_all"); da_all = P.sbuf([128, 384], F32, "da_all")
                tmpa = P.sbuf([128, 384], F32, "tmpa"); tmpb = P.sbuf([128, 384], F32, "tmpb")
                dsk = P.sbuf([128, 12, 64], F32, "dsk"); normw = P.sbuf([128, 768], F32, "normw")
                tri = P.sbuf([128, 128], F32, "tri"); strict = P.sbuf([128, 128], F32, "strict")
                onesf = P.sbuf([128, 128], F32, "onesf"); ident = P.sbuf([128, 128], F32, "ident")
                idx_x = P.sbuf([128, 1], U32, "idx_x"); idx_t = P.sbuf([128, 1], U32, "idx_t"); idx_d = P.sbuf([128, 1], U32, "idx_d")
                for (b, a) in ((cw, S["convw"][l]), (cb, S["convb"][l]), (tmpa, S["dtb"][l]), (tmpb, S["alog"][l]),
                               (normw, S["normw"][l]), (tri, S["tri"]), (strict, S["strict"]), (onesf, S["onesf"]),
                               (ident, S["ident"]), (idx_x, IDX["idx_x"]), (idx_t, IDX["idx_t"]), (idx_d, IDX["idx_d"])):
                    P.dma("sp", b[:], a, writes=[b])
                P.dma("sp", dsk[:], S["dsk"][l].rearrange("p (j d) -> p j d", d=64), writes=[dsk])
                for c in range(NCH):
                    sgm, lc = c // 8, c % 8
                    gather(dt_all[:, c * 12:(c + 1) * 12], dt_all, dt_g, idx_d, sgm * 4 * T + lc * 128)
                P.op("dve", "tensor_tensor", out=dt_all[:], in0=dt_all[:], in1=tmpa[:], op=ALU.add, reads=[dt_all, tmpa], writes=[dt_all])
                P.op("act", "activation", out=dt_all[:], in_=dt_all[:], func=AF.Exp, reads=[dt_all], writes=[dt_all])
                P.op("dve", "tensor_scalar", dt_all[:], dt_all[:], 1.0, None, ALU.add, reads=[dt_all], writes=[dt_all])
                P.op("act", "activation", out=dt_all[:], in_=dt_all[:], func=AF.Ln, reads=[dt_all], writes=[dt_all])
                P.op("act", "activation", out=tmpb[:], in_=tmpb[:], func=AF.Exp, reads=[tmpb], writes=[tmpb])
                P.op("dve", "scalar_tensor_tensor", out=da_all[:], in0=dt_all[:], scalar=-1.0, in1=tmpb[:], op0=ALU.mult,
                     op1=ALU.mult, reads=[dt_all, tmpb], writes=[da_all])
                h = [P.sbuf([128, 6, 64], F32, "h%d" % g) for g in range(2)]
                hb = [P.sbuf([128, 6, 64], BF16, "hb%d" % g) for g in range(2)]
                for g in range(2):
                    P.op("pool", "memset", h[g][:], 0.0, writes=[h[g]])
                    P.op("pool", "memset", hb[g][:], 0.0, writes=[hb[g]])
                halo = [P.sbuf([128, 4], F32, "halo%d" % i) for i in range(10)]
                xr_ring = Ring([P.sbuf([128, T + 3], F32, "xr%d" % i) for i in range(3)])
                acc_ring = Ring([P.sbuf([128, 512], F32, "acc%d" % i) for i in range(2)])
                xc_sets = [[P.sbuf([128, T], F32, "xc%d_%d" % (k, i)) for i in range(8)] for k in range(2)]
                bc_sets = [[P.sbuf([128, T], BF16, "bc%d_%d" % (k, i)) for i in range(4)] for k in range(2)]
                xt_ring = Ring([P.sbuf([128, 12, 64], F32, "xt%d" % i) for i in range(2)])
                bt_ring = Ring([P.sbuf([128, 256], BF16, "bt%d" % i) for i in range(2)])
                E_ring = Ring([P.sbuf([128, 36], F32, "E%d" % i) for i in range(2)])
                s2_ring = Ring([P.sbuf([128, 12], F32, "s2%d" % i) for i in range(2)])
                xdt_ring = Ring([P.sbuf([128, 12, 64], BF16, "xdt%d" % i) for i in range(2)])
                xw_ring = Ring([P.sbuf([128, 12, 64], BF16, "xw%d" % i) for i in range(2)])
                xd_ring = Ring([P.sbuf([128, 12, 64], F32, "xd%d" % i) for i in range(2)])
                cbm_ring = Ring([P.sbuf([128, 128], F32, "cbm%d" % i) for i in range(2)])
                ajall_ring = Ring([P.sbuf([128, 12, 128], F32, "ajall%d" % i) for i in range(2)])
                scall_ring = Ring([P.sbuf([128, 12, 128], BF16, "scall%d" % i) for i in range(2)])
                dec_ring = Ring([P.sbuf([128, 512], F32, "dec%d" % i) for i in range(3)])
                y_ring = Ring([P.sbuf([128, 12, 64], F32, "y%d" % i) for i in range(2)])
                t1_ring = Ring([P.sbuf([128, 6, 64], F32, "t1%d" % i) for i in range(2)])
                z_ring = Ring([P.sbuf([128, 768], F32, "z%d" % i) for i in range(2)])
                sq_ring = Ring([P.sbuf([128, 768], F32, "sqq%d" % i) for i in range(2)])
                ss_ring = Ring([P.sbuf([128, 2], F32, "ss%d" % i) for i in range(2)])
                o_ring = Ring([P.sbuf([128, 768], F32, "o%d" % i) for i in range(2)])
                yT_ring = Ring([P.sbuf([128, 6, 128], F32, "yTt%d" % i) for i in range(2)])
                ysv = y_send.t.rearrange("(s f p) t -> s p f t", s=4, p=128)
                y_seg = [Buf(None, "y_seg%d" % i) for i in range(4)]
                def conv(sgm):
                    xc = xc_sets[sgm % 2]
                    bc = bc_sets[sgm % 2]
                    for ch in range(10):
                        xr = xr_ring.get()
                        gather(xr[:, 3:T + 3], xr, x_g, idx_x, (ch // 2) * 1024 + (ch % 2) * 128 + sgm * 256)
                        if sgm == 0:
                            P.op("pool", "memset", xr[:, 0:3], 0.0, writes=[xr])
                        else:
                            P.op("act", "activation", out=xr[:, 0:3], in_=halo[ch][:, 0:3], func=AF.Copy, reads=[halo[ch]], writes=[xr])
                        P.op("act", "activation", out=halo[ch][:, 0:3], in_=xr[:, T:T + 3], func=AF.Copy, reads=[xr], writes=[halo[ch]])
                        for h0 in (0, 512):
                            acc = acc_ring.get()
                            P.op("dve", "tensor_scalar", acc[:, :], xr[:, h0:h0 + 512], cw[:, ch, 0:1], cb[:, ch:ch + 1], ALU.mult, ALU.add,
                                 reads=[xr, cw, cb], writes=[acc])
                            for k in range(1, 4):
                                P.op("dve", "scalar_tensor_tensor", out=acc[:, :], in0=xr[:, h0 + k:h0 + k + 512], scalar=cw[:, ch, k:k + 1],
                                     in1=acc[:, :], op0=ALU.mult, op1=ALU.add, reads=[xr, cw, acc], writes=[acc])
                            if ch < 8:
                                P.op("act", "activation", out=xc[ch][:, h0:h0 + 512], in_=acc[:, :], func=AF.Silu, reads=[acc], writes=[xc[ch]])
                                if ch >= 6:
                                    P.op("dve", "tensor_copy", bc[ch - 6][:, h0:h0 + 512], xc[ch][:, h0:h0 + 512], reads=[xc[ch]], writes=[bc[ch - 6]])
                            else:
                                P.op("act", "activation", out=bc[ch - 6][:, h0:h0 + 512], in_=acc[:, :], func=AF.Silu, reads=[acc], writes=[bc[ch - 6]])

                def S1(c, sgm, lc, xc, bc):
                    ts = slice(lc * 128, (lc + 1) * 128)
                    xt = xt_ring.get(); bt = bt_ring.get()
                    xtf = xt[:].rearrange("p j d -> p (j d)")
                    for (lo, n) in ((0, 4), (4, 2)):
                        ps = pp.get()
                        for i in range(n):
                            P.op("pe", "transpose", ps[:, i * 128:(i + 1) * 128], xc[lo + i][:, ts], ident[:, :],
                                 reads=[xc[lo + i], ident], writes=[ps])
                        P.op("dve", "tensor_copy", xtf[:, lo * 128:(lo + n) * 128], ps[:, 0:n * 128], reads=[ps], writes=[xt])
                    ps = pp.get()
                    for i in range(2):
                        P.op("pe", "transpose", ps[:, i * 128:(i + 1) * 128], xc[6 + i][:, ts], ident[:, :],
                             reads=[xc[6 + i], ident], writes=[ps])
                    P.op("act", "activation", out=bt[:, :], in_=ps[:, 0:256], func=AF.Copy, reads=[ps], writes=[bt])
                    dac = da_all[:, c * 12:(c + 1) * 12]
                    dtc = dt_all[:, c * 12:(c + 1) * 12]
                    ps = pp.get()
                    P.op("pe", "matmul", ps[:, 0:12], tri[:, :], dac, start=True, stop=True, reads=[tri, da_all], writes=[ps])
                    P.op("pe", "matmul", ps[:, 12:24], strict[:, :], dac, start=True, stop=True, reads=[strict, da_all], writes=[ps])
                    P.op("pe", "matmul", ps[:, 24:36], onesf[:, :], dac, start=True, stop=True, reads=[onesf, da_all], writes=[ps])
                    E = E_ring.get()
                    P.op("act", "activation", out=E[:, :], in_=ps[:, 0:36], func=AF.Exp, reads=[ps], writes=[E])
                    s2 = s2_ring.get()
                    P.op("dve", "tensor_tensor", out=s2[:, :], in0=dtc, in1=E[:, 12:24], op=ALU.mult, reads=[dt_all, E], writes=[s2])
                    xdt = xdt_ring.get(); xw = xw_ring.get(); xd = xd_ring.get()
                    P.op("dve", "tensor_tensor", out=xdt[:], in0=xt[:], in1=dtc.unsqueeze(2).broadcast_to([128, 12, 64]),
                         op=ALU.mult, reads=[xt, dt_all], writes=[xdt])
                    P.op("dve", "tensor_tensor", out=xw[:], in0=xt[:], in1=s2[:, :].unsqueeze(2).broadcast_to([128, 12, 64]),
                         op=ALU.mult, reads=[xt, s2], writes=[xw])
                    P.op("dve", "tensor_tensor", out=xd[:], in0=xt[:], in1=dsk[:], op=ALU.mult, reads=[xt, dsk], writes=[xd])
                    return dict(c=c, sgm=sgm, lc=lc, xc=xc, bc=bc, ts=ts, xt=xt, bt=bt, dac=dac, dtc=dtc, E=E, s2=s2, xdt=xdt, xw=xw, xd=xd)

                def S2(v):
                    c = v['c']; sgm = v['sgm']; lc = v['lc']; bc = v['bc']; ts = v['ts']; bt = v['bt']; dac = v['dac']; E = v['E']
                    xdt = v['xdt']; xw = v['xw']; xd = v['xd']
                    y = y_ring.get()
                    ajall = ajall_ring.get()
                    P.op("dve", "tensor_tensor", out=ajall[:], in0=strict[:, :].unsqueeze(1).broadcast_to([128, 12, 128]),
                         in1=dac.unsqueeze(2).broadcast_to([128, 12, 128]), op=ALU.mult, reads=[strict, da_all], writes=[ajall])
                    cbms = []; yos = []
                    for gi in range(2):
                        BT = bc[gi]; CT = bc[2 + gi]
                        ps_cb = pp.get()
                        P.op("pe", "matmul", ps_cb[:, 0:128], BT[:, ts], CT[:, ts], start=True, stop=True, reads=[BT, CT], writes=[ps_cb])
                        cbm = cbm_ring.get()
                        P.op("dve", "tensor_tensor", out=cbm[:, :], in0=ps_cb[:, 0:128], in1=tri[:, :], op=ALU.mult,
                             reads=[ps_cb, tri], writes=[cbm])
                        ps_yo = pp.get()
                        P.op("pe", "matmul", ps_yo[:, 0:384], CT[:, ts], hb[gi][:].rearrange("p j d -> p (j d)"),
                             start=True, stop=True, reads=[CT, hb[gi]], writes=[ps_yo])
                        cbms.append(cbm); yos.append(ps_yo)
                    scall = scall_ring.get()
                    for gi in range(2):
                        for (j0, nh) in ((0, 4), (4, 2)):
                            ps_seg = pp.get()
                            for i in range(nh):
                                j = gi * 6 + j0 + i
                                P.op("pe", "matmul", ps_seg[:, i * 128:(i + 1) * 128], ajall[:, j, :], tri[:, :], start=True, stop=True,
                                     reads=[ajall, tri], writes=[ps_seg])
                            dec = dec_ring.get()
                            P.op("act", "activation", out=dec[:, 0:nh * 128], in_=ps_seg[:, 0:nh * 128], func=AF.Exp, reads=[ps_seg], writes=[dec])
                            P.op("dve", "tensor_tensor", out=scall[:, gi * 6 + j0:gi * 6 + j0 + nh, :],
                                 in0=dec[:, 0:nh * 128].rearrange("p (j s) -> p j s", s=128),
                                 in1=cbms[gi][:, :].unsqueeze(1).broadcast_to([128, nh, 128]), op=ALU.mult,
                                 reads=[dec, cbms[gi]], writes=[scall])
                    for gi in range(2):
                        ps_yd = pp.get()
                        ps_yo = yos[gi]
                        for jj in range(6):
                            j = gi * 6 + jj
                            P.op("pe", "matmul", ps_yd[:, jj * 64:(jj + 1) * 64], scall[:, j, :], xdt[:, j, :], start=True, stop=True,
                                 reads=[scall, xdt], writes=[ps_yd])
                        t1 = t1_ring.get()
                        P.op("dve", "tensor_tensor", out=t1[:], in0=ps_yo[:, 0:384].rearrange("p (j d) -> p j d", d=64),
                             in1=E[:, gi * 6:gi * 6 + 6].unsqueeze(2).broadcast_to([128, 6, 64]), op=ALU.mult,
                             reads=[ps_yo, E], writes=[t1])
                        P.op("dve", "tensor_tensor", out=t1[:], in0=ps_yd[:, 0:384].rearrange("p (j d) -> p j d", d=64),
                             in1=t1[:], op=ALU.add, reads=[ps_yd, t1], writes=[t1])
                        P.op("dve", "tensor_tensor", out=y[:, gi * 6:(gi + 1) * 6, :], in0=xd[:, gi * 6:(gi + 1) * 6, :], in1=t1[:],
                             op=ALU.add, reads=[xd, t1], writes=[y])
                        ps_st = pp.get()
                        P.op("pe", "matmul", ps_st[:, 0:384], bt[:, gi * 128:(gi + 1) * 128],
                             xw[:, gi * 6:(gi + 1) * 6, :].rearrange("p j d -> p (j d)"), start=True, stop=True,
                             reads=[bt, xw], writes=[ps_st])
                        P.op("dve", "tensor_tensor", out=h[gi][:], in0=h[gi][:],
                             in1=E[:, 24 + gi * 6:24 + gi * 6 + 6].unsqueeze(2).broadcast_to([128, 6, 64]), op=ALU.mult,
                             reads=[h[gi], E], writes=[h[gi]])
                        P.op("dve", "tensor_tensor", out=h[gi][:], in0=ps_st[:, 0:384].rearrange("p (j d) -> p j d", d=64),
                             in1=h[gi][:], op=ALU.add, reads=[ps_st, h[gi]], writes=[h[gi]])
                        P.op("act", "activation", out=hb[gi][:], in_=h[gi][:], func=AF.Copy, reads=[h[gi]], writes=[hb[gi]])
                    v['y'] = y

                def S3(v):
                    c = v['c']; sgm = v['sgm']; lc = v['lc']; ts = v['ts']; y = v['y']
                    zt = z_ring.get()
                    gather(zt[:, :], zt, z_g, idx_t, (lc // 2) * 1024 + sgm * 256 + (lc % 2) * 128)
                    P.op("act", "activation", out=zt[:, :], in_=zt[:, :], func=AF.Silu, reads=[zt], writes=[zt])
                    yf = y[:].rearrange("p j d -> p (j d)")
                    P.op("dve", "tensor_tensor", out=yf, in0=yf, in1=zt[:, :], op=ALU.mult, reads=[y, zt], writes=[y])
                    sq = sq_ring.get()
                    P.op("act", "activation", out=sq[:, :], in_=yf, func=AF.Square, reads=[y], writes=[sq])
                    ss = ss_ring.get()
                    P.op("dve", "tensor_reduce", out=ss[:, :], in_=sq[:, :].rearrange("p (g f) -> p g f", g=2), axis=AX.X, op=ALU.add,
                         reads=[sq], writes=[ss])
                    P.op("dve", "tensor_scalar", ss[:, :], ss[:, :], 1.0 / 384, EPS, ALU.mult, ALU.add, reads=[ss], writes=[ss])
                    P.op("act", "activation", out=ss[:, :], in_=ss[:, :], func=AF.Sqrt, reads=[ss], writes=[ss])
                    P.op("dve", "reciprocal", ss[:, :], ss[:, :], reads=[ss], writes=[ss])
                    ot = o_ring.get()
                    for gi in range(2):
                        P.op("dve", "scalar_tensor_tensor", out=ot[:, gi * 384:(gi + 1) * 384], in0=yf[:, gi * 384:(gi + 1) * 384],
                             scalar=ss[:, gi:gi + 1], in1=normw[:, gi * 384:(gi + 1) * 384], op0=ALU.mult, op1=ALU.mult,
                             reads=[y, ss, normw], writes=[ot])
                    yTt = yT_ring.get()
                    yTf = yTt[:].rearrange("p f t -> p (f t)")
                    for (lo, n) in ((0, 4), (4, 2)):
                        ps = pp.get()
                        for i in range(n):
                            P.op("pe", "transpose", ps[:, i * 128:(i + 1) * 128], ot[:, (lo + i) * 128:(lo + i + 1) * 128], ident[:, :],
                                 reads=[ot, ident], writes=[ps])
                        P.op("act", "activation", out=yTf[:, lo * 128:(lo + n) * 128], in_=ps[:, 0:n * 128], func=AF.Copy,
                             reads=[ps], writes=[yTt])
                    P.dma("sp", ysv[sgm][:, :, ts], yTt[:], reads=[yTt], writes=[y_seg[sgm]])

                def ycoll(sgm):
                    if "y" not in skip_cc:
                        for k in range(3 * sgm, 3 * sgm + 3):
                            _coll("AllGather", RG, y_send.t[k * 256:(k + 1) * 256, :], y_g.t[k * 1024:(k + 1) * 1024, :],
                                  reads=[y_seg[sgm]], writes=[y_g])


                ctxs = {}
                for i in range(NCH + 2):
                    if i < NCH:
                        if i % 8 == 0:
                            conv(i // 8)
                        ctxs[i] = S1(i, i // 8, i % 8, xc_sets[(i // 8) % 2], bc_sets[(i // 8) % 2])
                    if 0 <= i - 1 < NCH:
                        S2(ctxs[i - 1])
                    if 0 <= i - 2 < NCH:
                        S3(ctxs.pop(i - 2))
                        if (i - 2) % 8 == 7:
                            ycoll((i - 2) // 8)
            if stop == (l, "B"):
                finish()
                return nc
            with P.phase(final=(l == 1)):
                pp = PsumPool(P, 8)
                xT = [P.sbuf([128, T], F32, "xT%d" % c) for c in range(NKC)]
                hT = [P.sbuf([128, T], BF16, "hT%d" % c) for c in range(NKC)]
                aT = [P.sbuf([128, T], BF16, "aT%d" % c) for c in range(12)]
                g_xm = P.sbuf([128, NKC], F32, "g_xm"); g_mm = P.sbuf([128, NKC], F32, "g_mm"); g_ff = P.sbuf([128, NKC], F32, "g_ff")
                mqg = P.sbuf([128, 1], F32, "mqg"); mkg = P.sbuf([128, 1], F32, "mkg")
                idx_y = P.sbuf([128, 1], U32, "idx_y")
                ones_bf = P.sbuf([128, 128], BF16, "ones")
                rstd = P.sbuf([128, T], F32, "rstd")
                sq_ring = Ring([P.sbuf([128, 512], BF16, "sq%d" % i) for i in range(3)])
                sg_ring = Ring([P.sbuf([128, 512], F32, "sg%d" % i) for i in range(3)])
                wg_ring = Ring([P.sbuf([128, NKC, 256], BF16, "wg%d" % i) for i in range(2)])
                wu_ring = Ring([P.sbuf([128, NKC, 256], BF16, "wu%d" % i) for i in range(2)])
                wd_ring = Ring([P.sbuf([128, 12, 512], BF16, "wd%d" % i) for i in range(2)])
                w_ring = Ring(wg_ring.bufs + wu_ring.bufs)
                mr_ring = Ring([P.sbuf([128, MEMLEN], F32, "mr%d" % i) for i in range(3)])
                mhT = [P.sbuf([128, MEMLEN], BF16, "mhT%d" % c) for c in range(NKC)]
                kmT = [P.sbuf([128, MEMLEN], BF16, "kmT%d" % c) for c in range(4)]
                vm = [P.sbuf([128, 512], BF16, "vm%d" % c) for c in range(2)]
                pT_ring = sq_ring
                rd_ring = sg_ring
                ystg = Ring([rstd])
                xv = xres1.t.rearrange("(c p) t -> c p t", p=128)
                yav = ya_s.t.rearrange("(c p) t -> c p t", p=128)
                mv = mem_i.rearrange("(c p) t -> c p t", p=128)
                P.op("dve", "memset", ones_bf[:, :], 1.0, writes=[ones_bf])
                for (b, a) in ((g_xm, G["xmg"][l]), (g_mm, G["mmg"][l]), (g_ff, G["ffg2"][l]), (mqg, G["mqg"][l]),
                               (mkg, G["mkg"][l]), (idx_y, IDX["idx_y"])):
                    P.dma("sp", b[:], a, writes=[b])
                for c in range(NKC):
                    P.dma("sp", xT[c][:, :], xv[c], reads=[xres1], writes=[xT[c]])

                def cons_add(fc, h0, w, ps):
                    P.op("dve", "tensor_tensor", out=xT[fc][:, h0:h0 + w], in0=ps[:, 0:w], in1=xT[fc][:, h0:h0 + w], op=ALU.add,
                         reads=[ps, xT[fc]], writes=[xT[fc]])
                for kh in range(2):
                    for c in range(NKC):
                        m = kh * NKC + c
                        if m < 8:
                            P.dma("pool", hT[c][:, :], yav[m], reads=[ya_s], writes=[hT[c]])
                        else:
                            ms = m - 8
                            r, fcx = ms // 6, ms % 6
                            stg = ystg.get()
                            gather(stg[:, :], stg, y_g, idx_y, (fcx // 2) * 1024 + r * 256 + (fcx % 2) * 128)
                            P.op("act", "activation", out=hT[c][:, :], in_=stg[:, :], func=AF.Copy, reads=[stg], writes=[hT[c]])
                    lin_fm(P, pp, hT, W["w_out"][l][kh * D:(kh + 1) * D, :], D, w_ring, T, cons_add)
                ps_m = pp.get()
                for c in range(NKC):
                    mt = mr_ring.get()
                    P.dma("sp", mt[:, :], mv[c], writes=[mt])
                    sq = sq_ring.get()
                    P.op("act", "activation", out=sq[:, 0:MEMLEN], in_=mt[:, :], func=AF.Square, reads=[mt], writes=[sq])
                    P.op("pe", "matmul", ps_m[:, 0:MEMLEN], ones_bf[:, :], sq[:, 0:MEMLEN], start=(c == 0), stop=(c == NKC - 1),
                         reads=[sq, ones_bf], writes=[ps_m])
                P.op("dve", "tensor_scalar", rstd[:, 0:MEMLEN], ps_m[:, 0:MEMLEN], 1.0 / D, EPS, ALU.mult, ALU.add, reads=[ps_m], writes=[rstd])
                P.op("act", "activation", out=rstd[:, 0:MEMLEN], in_=rstd[:, 0:MEMLEN], func=AF.Sqrt, reads=[rstd], writes=[rstd])
                P.op("dve", "reciprocal", rstd[:, 0:MEMLEN], rstd[:, 0:MEMLEN], reads=[rstd], writes=[rstd])
                for c in range(NKC):
                    mt = mr_ring.get()
                    P.dma("sp", mt[:, :], mv[c], writes=[mt])
                    P.op("dve", "scalar_tensor_tensor", out=mhT[c][:, :], in0=mt[:, :], scalar=g_mm[:, c:c + 1], in1=rstd[:, 0:MEMLEN],
                         op0=ALU.mult, op1=ALU.mult, reads=[mt, g_mm, rstd], writes=[mhT[c]])

                def dst_k(fc, h0, w):
                    return kmT[fc][:, h0:h0 + w], kmT[fc]
                lin_fm(P, pp, mhT, W["wk"][l], 512, w_ring, MEMLEN, headnorm_consume(P, pp, ones_bf, mkg, sq_ring, sg_ring, dst_k))

                def cons_vm(tt, c0, cw_, ps):
                    P.op("act", "activation", out=vm[tt][:, c0:c0 + cw_], in_=ps[:, 0:cw_], func=AF.Copy, reads=[ps], writes=[vm[tt]])
                lin_tm(P, pp, mhT, W["wv"][l], 512, w_ring, MEMLEN, cons_vm)
                rmsnorm_fm(P, pp, xT, g_xm, hT, ones_bf, sq_ring, rstd, NKC, T, D)
                qmT = aT[0:4]
                oT = aT[4:8]

                def dst_q(fc, h0, w):
                    return qmT[fc][:, h0:h0 + w], qmT[fc]
                lin_fm(P, pp, hT, W["wq"][l], 512, w_ring, T, headnorm_consume(P, pp, ones_bf, mqg, sq_ring, sg_ring, dst_q))
                for mh in range(4):
                    for h0 in range(0, T, 512):
                        w = min(512, T - h0)
                        ps_o = pp.get(); ps_d = pp.get()
                        for kt in range(2):
                            ps_s = pp.get()
                            P.op("pe", "matmul", ps_s[:, 0:w], kmT[mh][:, kt * 128:(kt + 1) * 128], qmT[mh][:, h0:h0 + w],
                                 start=True, stop=True, reads=[kmT[mh], qmT[mh]], writes=[ps_s])
                            pT = pT_ring.get()
                            P.op("act", "activation", out=pT[:, 0:w], in_=ps_s[:, 0:w], func=AF.Exp, scale=scale, reads=[ps_s], writes=[pT])
                            P.op("pe", "matmul", ps_o[:, 0:w], vm[kt][:, mh * 128:(mh + 1) * 128], pT[:, 0:w], start=(kt == 0),
                                 stop=(kt == 1), reads=[vm[kt], pT], writes=[ps_o])
                            P.op("pe", "matmul", ps_d[:, 0:w], ones_bf[:, :], pT[:, 0:w], start=(kt == 0), stop=(kt == 1),
                                 reads=[ones_bf, pT], writes=[ps_d])
                        rd = rd_ring.get()
                        P.op("dve", "reciprocal", rd[:, 0:w], ps_d[:, 0:w], reads=[ps_d], writes=[rd])
                        P.op("dve", "tensor_tensor", out=oT[mh][:, h0:h0 + w], in0=ps_o[:, 0:w], in1=rd[:, 0:w], op=ALU.mult,
                             reads=[ps_o, rd], writes=[oT[mh]])
                lin_fm(P, pp, oT, W["wo"][l], D, w_ring, T, cons_add, nkc=4)
                rmsnorm_fm(P, pp, xT, g_ff, hT, ones_bf, sq_ring, rstd, NKC, T, D)
                ffn_fm(P, pp, xT, hT, W["w_gu2"][l], W["w_down2"][l], aT, wg_ring, wu_ring, wd_ring, sg_ring, T)
                dst = xres if l == 0 else None
                dv = (xres.t if l == 0 else out_ap).rearrange("(c p) t -> c p t", p=128)
                for c in range(NKC):
                    P.dma("sp", dv[c], xT[c][:, :], reads=[xT[c]], writes=([xres] if l == 0 else []))
    return nc


def fused_inputs(I):
    T = NTOK
    xs = I["x"].astype(np.float32).reshape(NCORES, T, D)
    st2 = lambda k: np.ascontiguousarray(np.stack([_pg(I[k][l]) for l in range(2)]))
    shared = {"w_gu1": I["ff1_w_gu"], "w_down1": I["ff1_w_down"], "w_in": I["w_in"], "w_out": I["w_out"],
              "wq": I["mem_wq"], "wk": I["mem_wk"], "wv": I["mem_wv"], "wo": I["mem_wo"],
              "w_gu2": I["ff2_w_gu"], "w_down2": I["ff2_w_down"],
              "ffg1": st2("ff1_norm"), "mixg": st2("mix_norm"), "xmg": st2("xmem_norm"), "mmg": st2("mem_norm"),
              "ffg2": st2("ff2_norm"), "qg": st2("q_norm"), "kg": st2("k_norm"), "mqg": st2("mem_q_norm"), "mkg": st2("mem_k_norm")}
    shared = {k: np.ascontiguousarray(np.asarray(v, np.float32)) for k, v in shared.items()}
    shared.update(ssd_consts())
    p = np.arange(128, dtype=np.uint32).reshape(128, 1)
    maps = []
    for c in range(NCORES):
        b, seg = c // 4, c % 4
        gp = seg
        g0 = 2 * gp
        chs = np.concatenate([np.arange(g0 * 384, (g0 + 2) * 384), 3072 + np.arange(g0 * 128, (g0 + 2) * 128),
                              4096 + np.arange(g0 * 128, (g0 + 2) * 128)])
        hs = np.arange(g0 * 6, g0 * 6 + 12)
        m = dict(shared)
        m["xT"] = np.ascontiguousarray(xs[c].T)
        m["memT"] = np.ascontiguousarray(I["mem"][b].astype(np.float32).T)
        m["convw"] = np.ascontiguousarray(np.stack([I["conv_w"][l][:, chs].T.reshape(10, 128, 4).transpose(1, 0, 2) for l in range(2)])).astype(np.float32)
        m["convb"] = np.ascontiguousarray(np.stack([I["conv_b"][l][chs].reshape(10, 128).T for l in range(2)])).astype(np.float32)
        m["dtb"] = np.stack([_rep(np.tile(I["dt_bias"][l][hs], NCH)) for l in range(2)])
        m["alog"] = np.stack([_rep(np.tile(I["a_log"][l][hs], NCH)) for l in range(2)])
        m["dsk"] = np.stack([_rep(np.repeat(I["d_skip"][l][hs], 64)) for l in range(2)])
        m["normw"] = np.stack([_rep(I["ssd_norm"][l][g0 * 384:(g0 + 2) * 384]) for l in range(2)])
        ac = attn_consts(seg)
        m.update(ac)
        m["idx_x"] = (gp * 5120 + p).astype(np.uint32)
        m["idx_t"] = (gp * 4096 + p).astype(np.uint32)
        m["idx_d"] = (gp * T + p).astype(np.uint32)
        m["idx_y"] = (seg * 3072 + p).astype(np.uint32)
        maps.append(m)
    return maps


def kernel_fused(**inputs):
    I = {k: np.asarray(v) for k, v in inputs.items()}
    nc = _prog("fused", build_fused)
    res = _run(nc, fused_inputs(I))
    out = np.stack([res[c]["xoT"].T for c in range(NCORES)], axis=0).reshape(2, SEQ, D)
    return np.ascontiguousarray(out.astype(np.float32))


_PROGS = {}


def _prog(name, fn):
    if name not in _PROGS:
        _PROGS[name] = fn()
    return _PROGS[name]


def _pg(g):
    return np.ascontiguousarray(np.asarray(g, np.float32).reshape(-1, 128).T)


def _run(nc, in_maps):
    res = run_bass_kernel_spmd(nc, in_maps, core_ids=list(range(NCORES)))
    return res.results


def kernel_unfused(**inputs):
    I = {k: np.asarray(v) for k, v in inputs.items()}
    T = NTOK
    x = I["x"].astype(np.float32)
    xs = x.reshape(NCORES, T, D)
    xT = [np.ascontiguousarray(xs[c].T) for c in range(NCORES)]
    memT = [np.ascontiguousarray(I["mem"][b].T) for b in range(2)]
    ncA = _prog("A", build_progA)
    ncB = _prog("B", build_progB)
    ncC1 = _prog("C1", build_progC1)
    ncC2 = _prog("C2", build_progC2)
    for l in range(2):
        mapsA = [{"xT": xT[c], "ffg": _pg(I["ff1_norm"][l]), "mixg": _pg(I["mix_norm"][l]),
                  "w_gu": I["ff1_w_gu"][l], "w_down": I["ff1_w_down"][l], "w_in": I["w_in"][l],
                  "qg": _pg(I["q_norm"][l]), "kg": _pg(I["k_norm"][l])} for c in range(NCORES)]
        rA = _run(ncA, mapsA)
        xbcT_b = [np.concatenate([rA[b * 4 + s]["xbcT"] for s in range(4)], axis=1) for b in range(2)]
        dt_b = [np.concatenate([rA[b * 4 + s]["dt"] for s in range(4)], axis=0) for b in range(2)]
        z_b = [np.concatenate([rA[b * 4 + s]["z"] for s in range(4)], axis=0) for b in range(2)]
        Pm = {k: I[k][l] for k in ("conv_w", "conv_b", "dt_bias", "a_log", "d_skip", "ssd_norm")}
        rB = _run(ncB, progB_inputs(xbcT_b, dt_b, z_b, Pm))
        y_ssd = progB_gather([r["y"] for r in rB])
        rC1 = _run(ncC1, progC1_inputs([rA[c]["qT"] for c in range(NCORES)], [rA[c]["kT"] for c in range(NCORES)],
                                       [rA[c]["v"] for c in range(NCORES)]))
        mapsC2 = []
        for c in range(NCORES):
            b, seg = c // 4, c % 4
            yT = np.ascontiguousarray(np.concatenate([rC1[c]["yT"], y_ssd[b, seg * T:(seg + 1) * T].T], axis=0))
            mapsC2.append({"xT": rA[c]["x1T"], "yT": yT, "w_out": I["w_out"][l], "memT": memT[b],
                           "xmg": _pg(I["xmem_norm"][l]), "mmg": _pg(I["mem_norm"][l]), "ffg": _pg(I["ff2_norm"][l]),
                           "mqg": _pg(I["mem_q_norm"][l]), "mkg": _pg(I["mem_k_norm"][l]),
                           "wq": I["mem_wq"][l], "wk": I["mem_wk"][l], "wv": I["mem_wv"][l], "wo": I["mem_wo"][l],
                           "w_gu": I["ff2_w_gu"][l], "w_down": I["ff2_w_down"][l]})
        rC2 = _run(ncC2, mapsC2)
        xT = [rC2[c]["xoT"] for c in range(NCORES)]
    out = np.stack([xT[c].T for c in range(NCORES)], axis=0).reshape(2, SEQ, D)
    return np.ascontiguousarray(out.astype(np.float32))


def kernel(**inputs):
    return kernel_fused(**inputs)
```

```python
import numpy as np
import contextlib
import concourse.bass as bass
import concourse.mybir as mybir
from concourse.bass_utils import run_bass_kernel_spmd

F32 = mybir.dt.float32
BF16 = mybir.dt.bfloat16
AF = mybir.ActivationFunctionType
ALU = mybir.AluOpType
AX = mybir.AxisListType

D = 2048
DFF = 5632
NTOK = 1024
NCORES = 8
EPS = 1e-6


class Buf:
    def __init__(self, t, name=""):
        self.t = t
        self.name = name
        self.last_w = None
        self.readers = []

    def __getitem__(self, idx):
        return self.t[idx]


class Op:
    __slots__ = ("eng", "emit", "deps", "marked", "count", "dma_sem", "dma_val", "is_dma", "is_cc")

    def __init__(self, eng, emit, is_dma=False):
        self.eng = eng
        self.emit = emit
        self.deps = []
        self.marked = False
        self.count = None
        self.is_dma = is_dma
        self.dma_sem = None
        self.dma_val = None
        self.is_cc = False


class Prog:
    ENGS = ("pe", "dve", "act", "pool", "sp")
    NS = 8

    def __init__(self, nc, stack):
        self.nc = nc
        self.stack = stack
        self.ops = {e: [] for e in self.ENGS}
        self.sem = {e: stack.enter_context(nc.semaphore("prog_" + e)) for e in ("pe", "dve", "act", "pool")}
        self.dsem = {q: [stack.enter_context(nc.semaphore("dma_%s_%d" % (q, i))) for i in range(self.NS)]
                     for q in ("sp", "act", "pool")}
        self.ndma = {q: 0 for q in ("sp", "act", "pool")}
        self.nbuf = 0
        self.cc_sem = stack.enter_context(nc.semaphore("cc_sem"))
        self.ncc = 0

    def sbuf(self, shape, dtype, name=None):
        self.nbuf += 1
        name = "s_%s_%d" % (name or "sb", self.nbuf)
        t = self.stack.enter_context(self.nc.sbuf_tensor(name, list(shape), dtype))
        return Buf(t, name)

    def psum(self, shape, dtype=F32, name=None):
        self.nbuf += 1
        name = "%s_%d" % (name or "ps", self.nbuf)
        t = self.stack.enter_context(self.nc.psum_tensor(name, list(shape), dtype))
        return Buf(t, name)

    def add(self, eng, emit, reads=(), writes=(), is_dma=False):
        op = Op(eng, emit, is_dma)
        deps = []
        for b in reads:
            if b.last_w is not None:
                deps.append(b.last_w)
        for b in writes:
            if b.last_w is not None:
                deps.append(b.last_w)
            deps.extend(b.readers)
        seen = set()
        for d in deps:
            if d is op or id(d) in seen:
                continue
            seen.add(id(d))
            if d.eng == "pe" and eng == "pe" and not d.is_dma:
                continue
            op.deps.append(d)
            if not d.is_dma:
                d.marked = True
        for b in reads:
            b.readers.append(op)
        for b in writes:
            b.last_w = op
            b.readers = []
        if is_dma:
            q = eng
            i = self.ndma[q]
            self.ndma[q] += 1
            op.dma_sem = self.dsem[q][i % self.NS]
            op.dma_val = 16 * (i // self.NS + 1)
        self.ops[eng].append(op)
        return op

    def op(self, eng, method, *args, reads=(), writes=(), **kw):
        return self.add(eng, lambda e: getattr(e, method)(*args, **kw), reads, writes)

    def collective(self, kind, groups, src_ap, dst_ap, reads=(), writes=()):
        op = self.add("pool", lambda e: e.collective_compute(kind, ALU.bypass, replica_groups=groups, ins=[src_ap], outs=[dst_ap]),
                      reads, writes, is_dma=True)
        self.ndma["pool"] -= 1
        self.ncc += 1
        op.dma_sem = self.cc_sem
        op.dma_val = self.ncc
        op.is_cc = True
        return op

    def dma(self, q, out, in_, reads=(), writes=()):
        return self.add(q, lambda e: e.dma_start(out=out, in_=in_), reads, writes, is_dma=True)

    def begin_phases(self):
        self.cnt = {e: 0 for e in ("pe", "dve", "act", "pool")}
        self.waited = {e: {} for e in self.ENGS}
        self.barrier = []
        self.phase_id = 0
        self.last_dma = {}

    @contextlib.contextmanager
    def phase(self, final=False):
        outer = self.stack
        with contextlib.ExitStack() as st:
            self.stack = st
            try:
                yield
                self.emit_phase(final)
            finally:
                self.stack = outer

    def emit_phase(self, final=False):
        nc = self.nc
        prog = self
        for e in ("pe", "dve", "act", "pool"):
            for op in reversed(self.ops[e]):
                if not op.is_dma:
                    op.marked = True
                    break
        for e in ("pe", "dve", "act", "pool"):
            for op in self.ops[e]:
                if op.is_dma:
                    continue
                if op.marked:
                    self.cnt[e] += 1
                    op.count = self.cnt[e]
        barrier = list(self.barrier)

        def run(engname, engine):
            waited = prog.waited[engname]

            def wait(s, v):
                if waited.get(id(s), 0) >= v:
                    return
                engine.wait_ge(s, v)
                waited[id(s)] = v
            for (s, v) in barrier:
                wait(s, v)
            for op in prog.ops[engname]:
                if op.is_dma and not op.is_cc:
                    prev = op.dma_val - 16
                    if prev > 0:
                        wait(op.dma_sem, prev)
                for d in op.deps:
                    if d.is_dma:
                        wait(d.dma_sem, d.dma_val)
                    elif d.count is not None:
                        wait(prog.sem[d.eng], d.count)
                ins = op.emit(engine)
                if op.is_cc:
                    ins.then_inc(op.dma_sem, 1)
                elif op.is_dma:
                    ins.then_inc(op.dma_sem, 16)
                elif op.marked:
                    ins.then_inc(prog.sem[engname], 1)
            if final:
                for (s, v) in final_pairs:
                    wait(s, v)

        for e in self.ENGS:
            for op in self.ops[e]:
                if op.is_dma and not op.is_cc:
                    self.last_dma[id(op.dma_sem)] = (op.dma_sem, op.dma_val)
        nb = [(self.sem[e], self.cnt[e]) for e in ("pe", "dve", "act", "pool") if self.cnt[e] > 0]
        nb += list(self.last_dma.values())
        final_pairs = nb
        with nc.Block() as block:
            @block.tensor
            def _(eng):
                run("pe", eng)

            @block.vector
            def _(eng):
                run("dve", eng)

            @block.scalar
            def _(eng):
                run("act", eng)

            @block.gpsimd
            def _(eng):
                run("pool", eng)

            @block.sync
            def _(eng):
                run("sp", eng)
        self.barrier = nb
        self.phase_id += 1
        for e in self.ENGS:
            for op in self.ops[e]:
                op.emit = None
                if not op.is_dma and op.count is None:
                    op.count = -1
            self.ops[e] = []

    def emit(self, final_wait_ops=()):
        nc = self.nc
        for e in ("pe", "dve", "act", "pool"):
            c = 0
            for op in self.ops[e]:
                if op.is_dma:
                    continue
                if op.marked:
                    c += 1
                    op.count = c
        prog = self

        def run(engname, engine):
            waited = {}
            for op in prog.ops[engname]:
                if op.is_dma and not op.is_cc:
                    prev = op.dma_val - 16
                    if prev > 0 and waited.get(id(op.dma_sem), 0) < prev:
                        engine.wait_ge(op.dma_sem, prev)
                        waited[id(op.dma_sem)] = prev
                for d in op.deps:
                    if d.is_dma:
                        s, v = d.dma_sem, d.dma_val
                    else:
                        s, v = prog.sem[d.eng], d.count
                    if waited.get(id(s), 0) >= v:
                        continue
                    engine.wait_ge(s, v)
                    waited[id(s)] = v
                ins = op.emit(engine)
                if op.is_cc:
                    ins.then_inc(op.dma_sem, 1)
                elif op.is_dma:
                    ins.then_inc(op.dma_sem, 16)
                elif op.marked:
                    ins.then_inc(prog.sem[engname], 1)
            if engname == "sp":
                for d in final_wait_ops:
                    s, v = (d.dma_sem, d.dma_val) if d.is_dma else (prog.sem[d.eng], d.count)
                    engine.wait_ge(s, v)

        with nc.Block() as block:
            @block.tensor
            def _(eng):
                run("pe", eng)

            @block.vector
            def _(eng):
                run("dve", eng)

            @block.scalar
            def _(eng):
                run("act", eng)

            @block.gpsimd
            def _(eng):
                run("pool", eng)

            @block.sync
            def _(eng):
                run("sp", eng)


class PsumPool:
    def __init__(self, P, n=8, pfx="psb"):
        self.bufs = [P.psum([128, 512], F32, name="%s%d" % (pfx, i)) for i in range(n)]
        self.i = 0

    def get(self):
        b = self.bufs[self.i % len(self.bufs)]
        self.i += 1
        return b


class Ring:
    def __init__(self, bufs):
        self.bufs = bufs
        self.i = 0

    def get(self):
        b = self.bufs[self.i % len(self.bufs)]
        self.i += 1
        return b


def rmsnorm_fm(P, pp, xT, gain, hT, ones_bf, sq_ring, rstd, nchunk, T, dim):
    for h0 in range(0, T, 512):
        w = min(512, T - h0)
        ps = pp.get()
        for c in range(nchunk):
            sq = sq_ring.get()
            P.op("act", "activation", out=sq[:, 0:w], in_=xT[c][:, h0:h0 + w], func=AF.Square,
                 reads=[xT[c]], writes=[sq])
            P.op("pe", "matmul", ps[:, 0:w], ones_bf[:, :], sq[:, 0:w], start=(c == 0), stop=(c == nchunk - 1),
                 reads=[sq, ones_bf], writes=[ps])
        P.op("dve", "tensor_scalar", rstd[:, h0:h0 + w], ps[:, 0:w], 1.0 / dim, EPS, ALU.mult, ALU.add,
             reads=[ps], writes=[rstd])
        P.op("act", "activation", out=rstd[:, h0:h0 + w], in_=rstd[:, h0:h0 + w], func=AF.Sqrt,
             reads=[rstd], writes=[rstd])
        P.op("dve", "reciprocal", rstd[:, h0:h0 + w], rstd[:, h0:h0 + w], reads=[rstd], writes=[rstd])
        for c in range(nchunk):
            P.op("dve", "scalar_tensor_tensor", out=hT[c][:, h0:h0 + w], in0=xT[c][:, h0:h0 + w],
                 scalar=gain[:, c:c + 1], in1=rstd[:, h0:h0 + w], op0=ALU.mult, op1=ALU.mult,
                 reads=[xT[c], gain, rstd], writes=[hT[c]])


def ffn_fm(P, pp, xT, hT, w_gu, w_down, aT, wg_ring, wu_ring, wd_ring, sg_ring, T):
    NKC = D // 128
    groups = [(0, 6), (6, 12), (12, 17), (17, 22)]
    wgu_v = w_gu.rearrange("(kc p) f -> p kc f", p=128)
    wd_v = w_down.rearrange("(fc p) d -> p fc d", p=128)
    halves = [(h0, min(512, T - h0)) for h0 in range(0, T, 512)]
    for (p0, p1) in groups:
        nfc = 2 * (p1 - p0)
        for pr in range(p0, p1):
            wg = wg_ring.get()
            wu = wu_ring.get()
            P.dma("pool", wg[:, :, :], wgu_v[:, :, pr * 256:(pr + 1) * 256], writes=[wg])
            P.dma("pool", wu[:, :, :], wgu_v[:, :, DFF + pr * 256:DFF + (pr + 1) * 256], writes=[wu])
            for j in range(2):
                fl = 2 * (pr - p0) + j
                for (h0, w) in halves:
                    pg = pp.get()
                    pu = pp.get()
                    for kc in range(NKC):
                        P.op("pe", "matmul", pg[:, 0:w], wg[:, kc, j * 128:(j + 1) * 128], hT[kc][:, h0:h0 + w],
                             start=(kc == 0), stop=(kc == NKC - 1), reads=[wg, hT[kc]], writes=[pg])
                    for kc in range(NKC):
                        P.op("pe", "matmul", pu[:, 0:w], wu[:, kc, j * 128:(j + 1) * 128], hT[kc][:, h0:h0 + w],
                             start=(kc == 0), stop=(kc == NKC - 1), reads=[wu, hT[kc]], writes=[pu])
                    sg = sg_ring.get()
                    P.op("act", "activation", out=sg[:, 0:w], in_=pg[:, 0:w], func=AF.Silu, reads=[pg], writes=[sg])
                    P.op("dve", "tensor_tensor", out=aT[fl][:, h0:h0 + w], in0=pu[:, 0:w], in1=sg[:, 0:w], op=ALU.mult,
                         reads=[pu, sg], writes=[aT[fl]])
        for dq in range(D // 512):
            wd = wd_ring.get()
            P.dma("pool", wd[:, 0:nfc, :], wd_v[:, 2 * p0:2 * p1, dq * 512:(dq + 1) * 512], writes=[wd])
            for dj in range(4):
                dc = dq * 4 + dj
                for (h0, w) in halves:
                    po = pp.get()
                    for fl in range(nfc):
                        P.op("pe", "matmul", po[:, 0:w], wd[:, fl, dj * 128:(dj + 1) * 128], aT[fl][:, h0:h0 + w],
                             start=(fl == 0), stop=(fl == nfc - 1), reads=[wd, aT[fl]], writes=[po])
                    P.op("dve", "scalar_tensor_tensor", out=xT[dc][:, h0:h0 + w], in0=po[:, 0:w], scalar=0.5,
                         in1=xT[dc][:, h0:h0 + w], op0=ALU.mult, op1=ALU.add, reads=[po, xT[dc]], writes=[xT[dc]])


def build_ffn_prog(T=NTOK):
    nc = bass.Bass("TRN2", target_bir_lowering=False)
    xin = nc.dram_tensor("xT", [D, T], F32, kind="ExternalInput").ap()
    gin = nc.dram_tensor("gain", [128, D // 128], F32, kind="ExternalInput").ap()
    wgu = nc.dram_tensor("w_gu", [D, 2 * DFF], F32, kind="ExternalInput").ap()
    wdn = nc.dram_tensor("w_down", [DFF, D], F32, kind="ExternalInput").ap()
    yout = nc.dram_tensor("yT", [D, T], F32, kind="ExternalOutput").ap()
    NKC = D // 128
    with contextlib.ExitStack() as stack:
        P = Prog(nc, stack)
        pp = PsumPool(P, 8)
        xT = [P.sbuf([128, T], F32, "xT%d" % c) for c in range(NKC)]
        hT = [P.sbuf([128, T], BF16, "hT%d" % c) for c in range(NKC)]
        aT = [P.sbuf([128, T], BF16, "aT%d" % c) for c in range(12)]
        gain = P.sbuf([128, NKC], F32, "gain")
        ones_bf = P.sbuf([128, 128], BF16, "ones")
        rstd = P.sbuf([128, T], F32, "rstd")
        sq_ring = Ring([P.sbuf([128, 512], BF16, "sq%d" % i) for i in range(3)])
        sg_ring = Ring([P.sbuf([128, 512], F32, "sg%d" % i) for i in range(3)])
        wg_ring = Ring([P.sbuf([128, NKC, 256], BF16, "wg%d" % i) for i in range(2)])
        wu_ring = Ring([P.sbuf([128, NKC, 256], BF16, "wu%d" % i) for i in range(2)])
        wd_ring = Ring([P.sbuf([128, 12, 512], BF16, "wd%d" % i) for i in range(2)])

        xv = xin.rearrange("(c p) t -> c p t", p=128)
        yv = yout.rearrange("(c p) t -> c p t", p=128)
        P.op("pool", "memset", ones_bf[:, :], 1.0, writes=[ones_bf])
        P.dma("sp", gain[:, :], gin[:, :], writes=[gain])
        for c in range(NKC):
            P.dma("sp", xT[c][:, :], xv[c], writes=[xT[c]])
        rmsnorm_fm(P, pp, xT, gain, hT, ones_bf, sq_ring, rstd, NKC, T, D)
        ffn_fm(P, pp, xT, hT, wgu, wdn, aT, wg_ring, wu_ring, wd_ring, sg_ring, T)
        outs = []
        for c in range(NKC):
            outs.append(P.dma("sp", yv[c], xT[c][:, :], reads=[xT[c]]))
        P.emit(final_wait_ops=outs)
    return nc


def lin_fm(P, pp, hT, w_ap, ncols, w_ring, T, consume, nkc=D // 128):
    wv = w_ap.rearrange("(kc p) f -> p kc f", p=128)
    halves = [(h0, min(512, T - h0)) for h0 in range(0, T, 512)]
    for c0 in range(0, ncols, 256):
        cw = min(256, ncols - c0)
        wt = w_ring.get()
        P.dma("pool", wt[:, 0:nkc, 0:cw], wv[:, :, c0:c0 + cw], writes=[wt])
        for j in range(cw // 128):
            for (h0, w) in halves:
                ps = pp.get()
                for kc in range(nkc):
                    P.op("pe", "matmul", ps[:, 0:w], wt[:, kc, j * 128:(j + 1) * 128], hT[kc][:, h0:h0 + w],
                         start=(kc == 0), stop=(kc == nkc - 1), reads=[wt, hT[kc]], writes=[ps])
                consume(c0 // 128 + j, h0, w, ps)


def lin_tm(P, pp, hT, w_ap, ncols, w_ring, T, consume, nkc=D // 128):
    wv = w_ap.rearrange("(kc p) f -> p kc f", p=128)
    for c0 in range(0, ncols, 256):
        cw = min(256, ncols - c0)
        wt = w_ring.get()
        P.dma("pool", wt[:, 0:nkc, 0:cw], wv[:, :, c0:c0 + cw], writes=[wt])
        for tt in range(T // 128):
            ps = pp.get()
            for kc in range(nkc):
                P.op("pe", "matmul", ps[:, 0:cw], hT[kc][:, tt * 128:(tt + 1) * 128], wt[:, kc, 0:cw],
                     start=(kc == 0), stop=(kc == nkc - 1), reads=[wt, hT[kc]], writes=[ps])
            consume(tt, c0, cw, ps)


NQ, NK, NV, NZ, NXBC, NDT = 1024, 1024, 1024, 3072, 5120, 48
OQ, OK_, OV, OZ, OXBC, ODT = 0, 1024, 2048, 3072, 6144, 11264
N_IN = 11312


def headnorm_consume(P, pp, ones_bf, gain_hd, sq_ring, rs_ring, dst_of):
    def consume(fc, h0, w, ps):
        sq = sq_ring.get()
        P.op("act", "activation", out=sq[:, 0:w], in_=ps[:, 0:w], func=AF.Square, reads=[ps], writes=[sq])
        ps2 = pp.get()
        P.op("pe", "matmul", ps2[:, 0:w], ones_bf[:, :], sq[:, 0:w], start=True, stop=True,
             reads=[sq, ones_bf], writes=[ps2])
        rs = rs_ring.get()
        P.op("dve", "tensor_scalar", rs[:, 0:w], ps2[:, 0:w], 1.0 / 128, EPS, ALU.mult, ALU.add, reads=[ps2], writes=[rs])
        P.op("act", "activation", out=rs[:, 0:w], in_=rs[:, 0:w], func=AF.Sqrt, reads=[rs], writes=[rs])
        P.op("dve", "reciprocal", rs[:, 0:w], rs[:, 0:w], reads=[rs], writes=[rs])
        ap, buf = dst_of(fc, h0, w)
        P.op("dve", "scalar_tensor_tensor", out=ap, in0=ps[:, 0:w], scalar=gain_hd[:, 0:1], in1=rs[:, 0:w],
             op0=ALU.mult, op1=ALU.mult, reads=[ps, gain_hd, rs], writes=[buf])
    return consume


def build_progA(T=NTOK, do_ffn=True):
    nc = bass.Bass("TRN2", target_bir_lowering=False)
    di = lambda n, s: nc.dram_tensor(n, s, F32, kind="ExternalInput").ap()
    do = lambda n, s: nc.dram_tensor(n, s, F32, kind="ExternalOutput").ap()
    xin = di("xT", [D, T]); ffg = di("ffg", [128, 16]); mixg = di("mixg", [128, 16])
    wgu = di("w_gu", [D, 2 * DFF]); wdn = di("w_down", [DFF, D]); win = di("w_in", [D, N_IN])
    qg_in = di("qg", [128, 1]); kg_in = di("kg", [128, 1])
    x1o = do("x1T", [D, T]); qo = do("qT", [NQ, T]); ko = do("kT", [NK, T]); vo = do("v", [T, NV])
    zo = do("z", [T, NZ]); xbco = do("xbcT", [NXBC, T]); dto = do("dt", [T, NDT])
    NKC = D // 128
    with contextlib.ExitStack() as stack:
        P = Prog(nc, stack)
        pp = PsumPool(P, 8)
        xT = [P.sbuf([128, T], F32, "xT%d" % c) for c in range(NKC)]
        hT = [P.sbuf([128, T], BF16, "hT%d" % c) for c in range(NKC)]
        aT = [P.sbuf([128, T], BF16, "aT%d" % c) for c in range(12)]
        g1 = P.sbuf([128, NKC], F32, "g1"); g2 = P.sbuf([128, NKC], F32, "g2")
        qg = P.sbuf([128, 1], F32, "qg"); kg = P.sbuf([128, 1], F32, "kg")
        ones_bf = P.sbuf([128, 128], BF16, "ones")
        rstd = P.sbuf([128, T], F32, "rstd")
        sq_ring = Ring([P.sbuf([128, 512], BF16, "sq%d" % i) for i in range(3)])
        sg_ring = Ring([P.sbuf([128, 512], F32, "sg%d" % i) for i in range(3)])
        wg_ring = Ring([P.sbuf([128, NKC, 256], BF16, "wg%d" % i) for i in range(2)])
        wu_ring = Ring([P.sbuf([128, NKC, 256], BF16, "wu%d" % i) for i in range(2)])
        wd_ring = Ring([P.sbuf([128, 12, 512], BF16, "wd%d" % i) for i in range(2)])
        w_ring = Ring(wg_ring.bufs + wu_ring.bufs)
        st_ring = Ring([P.sbuf([128, 512], F32, "st%d" % i) for i in range(4)])

        xv = xin.rearrange("(c p) t -> c p t", p=128)
        P.op("pool", "memset", ones_bf[:, :], 1.0, writes=[ones_bf])
        P.dma("sp", g1[:, :], ffg[:, :], writes=[g1]); P.dma("sp", g2[:, :], mixg[:, :], writes=[g2])
        P.dma("sp", qg[:, :], qg_in[:, :], writes=[qg]); P.dma("sp", kg[:, :], kg_in[:, :], writes=[kg])
        for c in range(NKC):
            P.dma("sp", xT[c][:, :], xv[c], writes=[xT[c]])
        outs = []
        if do_ffn:
            rmsnorm_fm(P, pp, xT, g1, hT, ones_bf, sq_ring, rstd, NKC, T, D)
            ffn_fm(P, pp, xT, hT, wgu, wdn, aT, wg_ring, wu_ring, wd_ring, sg_ring, T)
        x1v = x1o.rearrange("(c p) t -> c p t", p=128)
        for c in range(NKC):
            outs.append(P.dma("sp", x1v[c], xT[c][:, :], reads=[xT[c]]))
        rmsnorm_fm(P, pp, xT, g2, hT, ones_bf, sq_ring, rstd, NKC, T, D)

        for (o_ap, col0, gbuf) in ((qo, OQ, qg), (ko, OK_, kg)):
            ov = o_ap.rearrange("(c p) t -> c p t", p=128)

            def dst_of(fc, h0, w, ov=ov):
                st = st_ring.get()
                dst_of.last = (st, fc, h0, w)
                return st[:, 0:w], st
            cons0 = headnorm_consume(P, pp, ones_bf, gbuf, sq_ring, sg_ring, dst_of)

            def cons(fc, h0, w, ps, cons0=cons0, ov=ov):
                cons0(fc, h0, w, ps)
                st, fc, h0, w = dst_of.last
                outs.append(P.dma("sp", ov[fc][:, h0:h0 + w], st[:, 0:w], reads=[st]))
            lin_fm(P, pp, hT, win[:, col0:col0 + 1024], 1024, w_ring, T, cons)

        xbv = xbco.rearrange("(c p) t -> c p t", p=128)

        def cons_xbc(fc, h0, w, ps):
            st = st_ring.get()
            P.op("act", "activation", out=st[:, 0:w], in_=ps[:, 0:w], func=AF.Copy, reads=[ps], writes=[st])
            outs.append(P.dma("sp", xbv[fc][:, h0:h0 + w], st[:, 0:w], reads=[st]))
        lin_fm(P, pp, hT, win[:, OXBC:OXBC + NXBC], NXBC, w_ring, T, cons_xbc)

        for (o_ap, col0, n) in ((vo, OV, NV), (zo, OZ, NZ), (dto, ODT, NDT)):
            def cons_tm(tt, c0, cw, ps, o_ap=o_ap):
                st = st_ring.get()
                P.op("dve", "tensor_copy", st[:, 0:cw], ps[:, 0:cw], reads=[ps], writes=[st])
                outs.append(P.dma("sp", o_ap[tt * 128:(tt + 1) * 128, c0:c0 + cw], st[:, 0:cw], reads=[st]))
            lin_tm(P, pp, hT, win[:, col0:col0 + n], n, w_ring, T, cons_tm)
        P.emit(final_wait_ops=outs)
    return nc


SEQ = 4096
NCH = SEQ // 128


def build_progB():
    nc = bass.Bass("TRN2", target_bir_lowering=False)
    di = lambda n, sh: nc.dram_tensor(n, sh, F32, kind="ExternalInput").ap()
    xbc = di("xbcT", [1280, SEQ + 3]); convw_i = di("convw", [128, 10, 4]); convb_i = di("convb", [128, 10])
    dtraw_i = di("dtraw", [SEQ, 12]); dtb_i = di("dtb", [128, 384]); alog_i = di("alog", [128, 384])
    dsk_i = di("dsk", [128, 768]); normw_i = di("normw", [128, 768]); z_i = di("z", [SEQ, 768])
    tri_i = di("tri", [128, 128]); strict_i = di("strict", [128, 128]); onesf_i = di("onesf", [128, 128])
    ident_i = di("ident", [128, 128])
    yo = nc.dram_tensor("y", [SEQ, 768], F32, kind="ExternalOutput").ap()
    with contextlib.ExitStack() as stack:
        P = Prog(nc, stack)
        pp = PsumPool(P, 8)
        cw = P.sbuf([128, 10, 4], F32, "cw"); cb = P.sbuf([128, 10], F32, "cb")
        dt_all = P.sbuf([128, 384], F32, "dt_all"); da_all = P.sbuf([128, 384], F32, "da_all")
        tmpa = P.sbuf([128, 384], F32, "tmpa"); tmpb = P.sbuf([128, 384], F32, "tmpb")
        dsk = P.sbuf([128, 12, 64], F32, "dsk"); normw = P.sbuf([128, 768], F32, "normw")
        tri = P.sbuf([128, 128], F32, "tri"); strict = P.sbuf([128, 128], F32, "strict")
        onesf = P.sbuf([128, 128], F32, "onesf"); ident = P.sbuf([128, 128], F32, "ident")
        for (b, a) in ((cw, convw_i), (cb, convb_i), (tmpa, dtb_i), (tmpb, alog_i),
                       (normw, normw_i), (tri, tri_i), (strict, strict_i), (onesf, onesf_i), (ident, ident_i)):
            P.dma("sp", b[:], a, writes=[b])
        P.dma("sp", dsk[:], dsk_i.rearrange("p (j d) -> p j d", d=64), writes=[dsk])
        P.dma("sp", dt_all[:].rearrange("p (c j) -> p c j", j=12), dtraw_i.rearrange("(c l) j -> l c j", l=128),
              writes=[dt_all])
        P.op("dve", "tensor_tensor", out=dt_all[:], in0=dt_all[:], in1=tmpa[:], op=ALU.add, reads=[dt_all, tmpa], writes=[dt_all])
        P.op("act", "activation", out=dt_all[:], in_=dt_all[:], func=AF.Exp, reads=[dt_all], writes=[dt_all])
        P.op("dve", "tensor_scalar", dt_all[:], dt_all[:], 1.0, None, ALU.add, reads=[dt_all], writes=[dt_all])
        P.op("act", "activation", out=dt_all[:], in_=dt_all[:], func=AF.Ln, reads=[dt_all], writes=[dt_all])
        P.op("act", "activation", out=tmpb[:], in_=tmpb[:], func=AF.Exp, reads=[tmpb], writes=[tmpb])
        P.op("dve", "scalar_tensor_tensor", out=da_all[:], in0=dt_all[:], scalar=-1.0, in1=tmpb[:], op0=ALU.mult,
             op1=ALU.mult, reads=[dt_all, tmpb], writes=[da_all])

        h = [P.sbuf([128, 6, 64], F32, "h%d" % g) for g in range(2)]
        hb = [P.sbuf([128, 6, 64], BF16, "hb%d" % g) for g in range(2)]
        for g in range(2):
            P.op("pool", "memset", h[g][:], 0.0, writes=[h[g]])
            P.op("pool", "memset", hb[g][:], 0.0, writes=[hb[g]])

        xr_ring = Ring([P.sbuf([128, 515], F32, "xr%d" % i) for i in range(4)])
        acc_ring = Ring([P.sbuf([128, 512], F32, "acc%d" % i) for i in range(2)])
        xc_sets = [[P.sbuf([128, 512], F32, "xc%d_%d" % (k, i)) for i in range(8)] for k in range(2)]
        bc_sets = [[P.sbuf([128, 512], BF16, "bc%d_%d" % (k, i)) for i in range(4)] for k in range(2)]
        xt_ring = Ring([P.sbuf([128, 12, 64], F32, "xt%d" % i) for i in range(2)])
        bt_ring = Ring([P.sbuf([128, 256], BF16, "bt%d" % i) for i in range(2)])
        E_ring = Ring([P.sbuf([128, 36], F32, "E%d" % i) for i in range(2)])
        s2_ring = Ring([P.sbuf([128, 12], F32, "s2%d" % i) for i in range(2)])
        xdt_ring = Ring([P.sbuf([128, 12, 64], BF16, "xdt%d" % i) for i in range(2)])
        xw_ring = Ring([P.sbuf([128, 12, 64], BF16, "xw%d" % i) for i in range(2)])
        xd_ring = Ring([P.sbuf([128, 12, 64], F32, "xd%d" % i) for i in range(2)])
        cbm_ring = Ring([P.sbuf([128, 128], F32, "cbm%d" % i) for i in range(2)])
        aj_ring = Ring([P.sbuf([128, 128], F32, "aj%d" % i) for i in range(3)])
        dec_ring = Ring([P.sbuf([128, 128], F32, "dec%d" % i) for i in range(3)])
        sc_ring = Ring([P.sbuf([128, 128], BF16, "sc%d" % i) for i in range(3)])
        y_ring = Ring([P.sbuf([128, 12, 64], F32, "y%d" % i) for i in range(2)])
        t1_ring = Ring([P.sbuf([128, 6, 64], F32, "t1%d" % i) for i in range(2)])
        z_ring = Ring([P.sbuf([128, 768], F32, "z%d" % i) for i in range(2)])
        sq_ring = Ring([P.sbuf([128, 768], F32, "sqq%d" % i) for i in range(2)])
        ss_ring = Ring([P.sbuf([128, 2], F32, "ss%d" % i) for i in range(2)])
        o_ring = Ring([P.sbuf([128, 768], F32, "o%d" % i) for i in range(2)])
        outs = []
        for tb in range(SEQ // 512):
            xc = xc_sets[tb % 2]
            bc = bc_sets[tb % 2]
            for ch in range(10):
                xr = xr_ring.get()
                P.dma("sp", xr[:, :], xbc[ch * 128:(ch + 1) * 128, tb * 512:tb * 512 + 515], writes=[xr])
                acc = acc_ring.get()
                P.op("dve", "tensor_scalar", acc[:, :], xr[:, 0:512], cw[:, ch, 0:1], cb[:, ch:ch + 1], ALU.mult, ALU.add,
                     reads=[xr, cw, cb], writes=[acc])
                for k in range(1, 4):
                    P.op("dve", "scalar_tensor_tensor", out=acc[:, :], in0=xr[:, k:k + 512], scalar=cw[:, ch, k:k + 1],
                         in1=acc[:, :], op0=ALU.mult, op1=ALU.add, reads=[xr, cw, acc], writes=[acc])
                if ch < 8:
                    P.op("act", "activation", out=xc[ch][:, :], in_=acc[:, :], func=AF.Silu, reads=[acc], writes=[xc[ch]])
                    if ch >= 6:
                        P.op("dve", "tensor_copy", bc[ch - 6][:, :], xc[ch][:, :], reads=[xc[ch]], writes=[bc[ch - 6]])
                else:
                    P.op("act", "activation", out=bc[ch - 6][:, :], in_=acc[:, :], func=AF.Silu, reads=[acc], writes=[bc[ch - 6]])
            for sc_i in range(4):
                c = tb * 4 + sc_i
                ts = slice(sc_i * 128, (sc_i + 1) * 128)
                xt = xt_ring.get(); bt = bt_ring.get()
                xtf = xt[:].rearrange("p j d -> p (j d)")
                for (lo, n) in ((0, 4), (4, 2)):
                    ps = pp.get()
                    for i in range(n):
                        P.op("pe", "transpose", ps[:, i * 128:(i + 1) * 128], xc[lo + i][:, ts], ident[:, :],
                             reads=[xc[lo + i], ident], writes=[ps])
                    P.op("dve", "tensor_copy", xtf[:, lo * 128:(lo + n) * 128], ps[:, 0:n * 128], reads=[ps], writes=[xt])
                ps = pp.get()
                for i in range(2):
                    P.op("pe", "transpose", ps[:, i * 128:(i + 1) * 128], xc[6 + i][:, ts], ident[:, :],
                         reads=[xc[6 + i], ident], writes=[ps])
                P.op("act", "activation", out=bt[:, :], in_=ps[:, 0:256], func=AF.Copy, reads=[ps], writes=[bt])
                dac = da_all[:, c * 12:(c + 1) * 12]
                dtc = dt_all[:, c * 12:(c + 1) * 12]
                ps = pp.get()
                P.op("pe", "matmul", ps[:, 0:12], tri[:, :], dac, start=True, stop=True, reads=[tri, da_all], writes=[ps])
                P.op("pe", "matmul", ps[:, 12:24], strict[:, :], dac, start=True, stop=True, reads=[strict, da_all], writes=[ps])
                P.op("pe", "matmul", ps[:, 24:36], onesf[:, :], dac, start=True, stop=True, reads=[onesf, da_all], writes=[ps])
                E = E_ring.get()
                P.op("act", "activation", out=E[:, :], in_=ps[:, 0:36], func=AF.Exp, reads=[ps], writes=[E])
                s2 = s2_ring.get()
                P.op("dve", "tensor_tensor", out=s2[:, :], in0=dtc, in1=E[:, 12:24], op=ALU.mult, reads=[dt_all, E], writes=[s2])
                xdt = xdt_ring.get(); xw = xw_ring.get(); xd = xd_ring.get()
                P.op("dve", "tensor_tensor", out=xdt[:], in0=xt[:], in1=dtc.unsqueeze(2).broadcast_to([128, 12, 64]),
                     op=ALU.mult, reads=[xt, dt_all], writes=[xdt])
                P.op("dve", "tensor_tensor", out=xw[:], in0=xt[:], in1=s2[:, :].unsqueeze(2).broadcast_to([128, 12, 64]),
                     op=ALU.mult, reads=[xt, s2], writes=[xw])
                P.op("pool", "tensor_tensor", out=xd[:], in0=xt[:], in1=dsk[:], op=ALU.mult, reads=[xt, dsk], writes=[xd])
                y = y_ring.get()
                for gi in range(2):
                    BT = bc[gi]; CT = bc[2 + gi]
                    ps_cb = pp.get()
                    P.op("pe", "matmul", ps_cb[:, 0:128], BT[:, ts], CT[:, ts], start=True, stop=True, reads=[BT, CT], writes=[ps_cb])
                    cbm = cbm_ring.get()
                    P.op("dve", "tensor_tensor", out=cbm[:, :], in0=ps_cb[:, 0:128], in1=tri[:, :], op=ALU.mult,
                         reads=[ps_cb, tri], writes=[cbm])
                    ps_yo = pp.get()
                    P.op("pe", "matmul", ps_yo[:, 0:384], CT[:, ts], hb[gi][:].rearrange("p j d -> p (j d)"),
                         start=True, stop=True, reads=[CT, hb[gi]], writes=[ps_yo])
                    ps_yd = pp.get()
                    for jj in range(6):
                        j = gi * 6 + jj
                        aj = aj_ring.get()
                        P.op("pool", "tensor_scalar", aj[:, :], strict[:, :], da_all[:, c * 12 + j:c * 12 + j + 1], None, ALU.mult,
                             reads=[strict, da_all], writes=[aj])
                        ps_seg = pp.get()
                        P.op("pe", "matmul", ps_seg[:, 0:128], aj[:, :], tri[:, :], start=True, stop=True, reads=[aj, tri], writes=[ps_seg])
                        dec = dec_ring.get()
                        P.op("act", "activation", out=dec[:, :], in_=ps_seg[:, 0:128], func=AF.Exp, reads=[ps_seg], writes=[dec])
                        scb = sc_ring.get()
                        P.op("dve", "tensor_tensor", out=scb[:, :], in0=dec[:, :], in1=cbm[:, :], op=ALU.mult,
                             reads=[dec, cbm], writes=[scb])
                        P.op("pe", "matmul", ps_yd[:, jj * 64:(jj + 1) * 64], scb[:, :], xdt[:, j, :], start=True, stop=True,
                             reads=[scb, xdt], writes=[ps_yd])
                    t1 = t1_ring.get()
                    P.op("dve", "tensor_tensor", out=t1[:], in0=ps_yo[:, 0:384].rearrange("p (j d) -> p j d", d=64),
                         in1=E[:, gi * 6:gi * 6 + 6].unsqueeze(2).broadcast_to([128, 6, 64]), op=ALU.mult,
                         reads=[ps_yo, E], writes=[t1])
                    P.op("dve", "tensor_tensor", out=t1[:], in0=ps_yd[:, 0:384].rearrange("p (j d) -> p j d", d=64),
                         in1=t1[:], op=ALU.add, reads=[ps_yd, t1], writes=[t1])
                    P.op("dve", "tensor_tensor", out=y[:, gi * 6:(gi + 1) * 6, :], in0=xd[:, gi * 6:(gi + 1) * 6, :], in1=t1[:],
                         op=ALU.add, reads=[xd, t1], writes=[y])
                    ps_st = pp.get()
                    P.op("pe", "matmul", ps_st[:, 0:384], bt[:, gi * 128:(gi + 1) * 128],
                         xw[:, gi * 6:(gi + 1) * 6, :].rearrange("p j d -> p (j d)"), start=True, stop=True,
                         reads=[bt, xw], writes=[ps_st])
                    P.op("dve", "tensor_tensor", out=h[gi][:], in0=h[gi][:],
                         in1=E[:, 24 + gi * 6:24 + gi * 6 + 6].unsqueeze(2).broadcast_to([128, 6, 64]), op=ALU.mult,
                         reads=[h[gi], E], writes=[h[gi]])
                    P.op("dve", "tensor_tensor", out=h[gi][:], in0=ps_st[:, 0:384].rearrange("p (j d) -> p j d", d=64),
                         in1=h[gi][:], op=ALU.add, reads=[ps_st, h[gi]], writes=[h[gi]])
                    P.op("act", "activation", out=hb[gi][:], in_=h[gi][:], func=AF.Copy, reads=[h[gi]], writes=[hb[gi]])
                zt = z_ring.get()
                P.dma("sp", zt[:, :], z_i[c * 128:(c + 1) * 128, :], writes=[zt])
                P.op("act", "activation", out=zt[:, :], in_=zt[:, :], func=AF.Silu, reads=[zt], writes=[zt])
                yf = y[:].rearrange("p j d -> p (j d)")
                P.op("dve", "tensor_tensor", out=yf, in0=yf, in1=zt[:, :], op=ALU.mult, reads=[y, zt], writes=[y])
                sq = sq_ring.get()
                P.op("act", "activation", out=sq[:, :], in_=yf, func=AF.Square, reads=[y], writes=[sq])
                ss = ss_ring.get()
                P.op("dve", "tensor_reduce", out=ss[:, :], in_=sq[:, :].rearrange("p (g f) -> p g f", g=2), axis=AX.X, op=ALU.add,
                     reads=[sq], writes=[ss])
                P.op("dve", "tensor_scalar", ss[:, :], ss[:, :], 1.0 / 384, EPS, ALU.mult, ALU.add, reads=[ss], writes=[ss])
                P.op("act", "activation", out=ss[:, :], in_=ss[:, :], func=AF.Sqrt, reads=[ss], writes=[ss])
                P.op("dve", "reciprocal", ss[:, :], ss[:, :], reads=[ss], writes=[ss])
                ot = o_ring.get()
                for gi in range(2):
                    P.op("dve", "scalar_tensor_tensor", out=ot[:, gi * 384:(gi + 1) * 384], in0=yf[:, gi * 384:(gi + 1) * 384],
                         scalar=ss[:, gi:gi + 1], in1=normw[:, gi * 384:(gi + 1) * 384], op0=ALU.mult, op1=ALU.mult,
                         reads=[y, ss, normw], writes=[ot])
                outs.append(P.dma("sp", yo[c * 128:(c + 1) * 128, :], ot[:, :], reads=[ot]))
        P.emit(final_wait_ops=outs)
    return nc


def _rep(v, n=128):
    return np.ascontiguousarray(np.broadcast_to(np.asarray(v, np.float32).reshape(1, -1), (n, np.asarray(v).size)))


def ssd_consts():
    k = np.arange(128)
    tri = (k[:, None] <= k[None, :]).astype(np.float32)
    strict = (k[:, None] > k[None, :]).astype(np.float32)
    return {"tri": tri, "strict": strict, "onesf": np.ones((128, 128), np.float32), "ident": np.eye(128, dtype=np.float32)}


def progB_inputs(xbcT_b, dt_b, z_b, Pm):
    maps = []
    cst = ssd_consts()
    for c in range(NCORES):
        b, gp = c // 4, c % 4
        g0 = 2 * gp
        chs = np.concatenate([np.arange(g0 * 384, (g0 + 2) * 384), 3072 + np.arange(g0 * 128, (g0 + 2) * 128),
                              4096 + np.arange(g0 * 128, (g0 + 2) * 128)])
        hs = np.arange(g0 * 6, g0 * 6 + 12)
        xp = np.zeros((1280, SEQ + 3), np.float32)
        xp[:, 3:] = xbcT_b[b][chs]
        m = {"xbcT": xp,
             "convw": np.ascontiguousarray(Pm["conv_w"][:, chs].T.reshape(10, 128, 4).transpose(1, 0, 2)),
             "convb": np.ascontiguousarray(Pm["conv_b"][chs].reshape(10, 128).T),
             "dtraw": np.ascontiguousarray(dt_b[b][:, hs]),
             "dtb": _rep(np.tile(Pm["dt_bias"][hs], NCH)), "alog": _rep(np.tile(Pm["a_log"][hs], NCH)),
             "dsk": _rep(np.repeat(Pm["d_skip"][hs], 64)), "normw": _rep(Pm["ssd_norm"][g0 * 384:(g0 + 2) * 384]),
             "z": np.ascontiguousarray(z_b[b][:, g0 * 384:(g0 + 2) * 384])}
        m.update(cst)
        maps.append(m)
    return maps


def progB_gather(ys):
    out = np.zeros((2, SEQ, 3072), np.float32)
    for c in range(NCORES):
        b, gp = c // 4, c % 4
        out[b][:, gp * 768:(gp + 1) * 768] = ys[c]
    return out


NBLK = SEQ // 256
BIGNEG = -30000.0


def build_progC1():
    nc = bass.Bass("TRN2", target_bir_lowering=False)
    di = lambda n, sh: nc.dram_tensor(n, sh, F32, kind="ExternalInput").ap()
    q_i = di("qT", [1024, NTOK]); k_i = di("kT_all", [1024, SEQ]); v_i = di("v_all", [SEQ, 1024])
    ko_i = di("kT_own", [1024, NTOK]); vo_i = di("v_own", [NTOK, 1024])
    tna_i = di("Tna", [128, 8 * 384]); tca_i = di("Tca", [128, 8 * 384]); ab_i = di("abias", [128, 512])
    gb_i = di("gbias", [128, 64]); pm_i = di("pastmask", [128, 64]); oh_i = di("onehot", [16, 16 * 128])
    id_i = di("ident", [128, 128])
    yo = nc.dram_tensor("yT", [1024, NTOK], F32, kind="ExternalOutput").ap()
    scale = 128.0 ** -0.5
    with contextlib.ExitStack() as stack:
        P = Prog(nc, stack)
        pp = PsumPool(P, 4)
        pacc = PsumPool(P, 4, pfx="pacc")
        tna = P.sbuf([128, 8, 384], F32, "tna"); tca = P.sbuf([128, 8, 384], F32, "tca")
        abias = P.sbuf([128, 512], F32, "abias"); gbias = P.sbuf([128, 64], F32, "gbias")
        pmask = P.sbuf([128, 64], F32, "pmask"); onehot = P.sbuf([16, 16, 128], BF16, "onehot")
        ident = P.sbuf([128, 128], F32, "ident"); ones_bf = P.sbuf([128, 128], BF16, "ones")
        P.dma("sp", tna[:], tna_i.rearrange("p (h u) -> p h u", h=8), writes=[tna])
        P.dma("sp", tca[:], tca_i.rearrange("p (h u) -> p h u", h=8), writes=[tca])
        for (b, a) in ((abias, ab_i), (gbias, gb_i), (pmask, pm_i), (ident, id_i)):
            P.dma("sp", b[:], a, writes=[b])
        P.dma("pool", onehot[:], oh_i.rearrange("k (n m) -> k n m", n=16), writes=[onehot])
        P.op("pool", "memset", ones_bf[:, :], 1.0, writes=[ones_bf])
        kf_ring = Ring([P.sbuf([128, SEQ], F32, "kf%d" % i) for i in range(2)])
        kb_ring = Ring([P.sbuf([128, SEQ], BF16, "kb%d" % i) for i in range(2)])
        qf_ring = Ring([P.sbuf([128, NTOK], F32, "qf%d" % i) for i in range(2)])
        qb_ring = Ring([P.sbuf([128, NTOK], BF16, "qb%d" % i) for i in range(2)])
        ko_ring = Ring([P.sbuf([128, NTOK], BF16, "ko%d" % i) for i in range(2)])
        vb_ring = Ring([P.sbuf([128, 32, 128], BF16, "vb%d" % i) for i in range(2)])
        vo_ring = Ring([P.sbuf([128, 8, 128], BF16, "vo%d" % i) for i in range(2)])
        km_ring = Ring([P.sbuf([128, 16], F32, "km%d" % i) for i in range(2)])
        gm_ring = Ring([P.sbuf([128, 16], F32, "gm%d" % i) for i in range(2)])
        t8_ring = Ring([P.sbuf([128, 8], F32, "t8%d" % i) for i in range(2)])
        sel_ring = Ring([P.sbuf([128, 16], F32, "sel%d" % i) for i in range(2)])
        ns_ring = Ring([P.sbuf([16, 256], BF16, "ns%d" % i) for i in range(2)])
        lg_ring = Ring([P.sbuf([128, 256], F32, "lg%d" % i) for i in range(3)])
        pT_ring = Ring([P.sbuf([128, 256], BF16, "pT%d" % i) for i in range(3)])
        rd_ring = Ring([P.sbuf([128, 256], F32, "rd%d" % i) for i in range(2)])
        o_ring = Ring([P.sbuf([128, 256], F32, "oo%d" % i) for i in range(2)])
        outs = []
        for h in range(8):
            hr = slice(h * 128, (h + 1) * 128)
            kf = kf_ring.get(); kb = kb_ring.get(); qf = qf_ring.get(); qb = qb_ring.get()
            ko = ko_ring.get(); vb = vb_ring.get(); vo = vo_ring.get(); km = km_ring.get()
            P.dma("sp", kf[:, :], k_i[hr, :], writes=[kf])
            P.dma("sp", qf[:, :], q_i[hr, :], writes=[qf])
            P.dma("pool", ko[:, :], ko_i[hr, :], writes=[ko])
            P.dma("pool", vb[:], v_i[:, hr].rearrange("(t p) d -> p t d", p=128), writes=[vb])
            P.dma("pool", vo[:], vo_i[:, hr].rearrange("(t p) d -> p t d", p=128), writes=[vo])
            P.op("act", "activation", out=kb[:, :], in_=kf[:, :], func=AF.Copy, reads=[kf], writes=[kb])
            P.op("act", "activation", out=qb[:, :], in_=qf[:, :], func=AF.Copy, reads=[qf], writes=[qb])
            P.op("dve", "tensor_reduce", out=km[:, :], in_=kf[:, :].rearrange("p (n s) -> p n s", s=256), axis=AX.X, op=ALU.add,
                 reads=[kf], writes=[km])
            P.op("dve", "tensor_scalar", km[:, :], km[:, :], 1.0 / 256, None, ALU.mult, reads=[km], writes=[km])
            for qi in range(4):
                qs_ = slice(qi * 256, (qi + 1) * 256)
                ns = ns_ring.get()
                for qs in range(2):
                    ps_g = pp.get()
                    P.op("pe", "matmul", ps_g[:, 0:16], qf[:, qi * 256 + qs * 128:qi * 256 + (qs + 1) * 128], km[:, :],
                         start=True, stop=True, reads=[qf, km], writes=[ps_g])
                    gm = gm_ring.get(); t8 = t8_ring.get(); sel = sel_ring.get()
                    P.op("dve", "tensor_tensor", out=gm[:, :], in0=ps_g[:, 0:16], in1=gbias[:, qi * 16:(qi + 1) * 16], op=ALU.add,
                         reads=[ps_g, gbias], writes=[gm])
                    P.op("dve", "max", out=t8[:, :], in_=gm[:, :], reads=[gm], writes=[t8])
                    P.op("dve", "tensor_scalar", sel[:, :], gm[:, :], t8[:, 2:3], None, ALU.is_ge, reads=[gm, t8], writes=[sel])
                    P.op("dve", "tensor_tensor", out=sel[:, :], in0=sel[:, :], in1=pmask[:, qi * 16:(qi + 1) * 16], op=ALU.mult,
                         reads=[sel, pmask], writes=[sel])
                    P.op("dve", "tensor_scalar", sel[:, :], sel[:, :], -1.0, -BIGNEG, ALU.add, ALU.mult, reads=[sel], writes=[sel])
                    ps_t = pp.get()
                    P.op("pe", "transpose", ps_t[0:16, 0:128], sel[:, :], ident[:, :], reads=[sel, ident], writes=[ps_t])
                    P.op("act", "activation", out=ns[:, qs * 128:(qs + 1) * 128], in_=ps_t[0:16, 0:128], func=AF.Copy,
                         reads=[ps_t], writes=[ns])
                ps_o = pacc.get(); ps_d = pacc.get()
                tiles = [(n, kt) for n in range(NBLK) for kt in range(2)] + [(-1, 0), (-1, 1)]
                for ti, (n, kt) in enumerate(tiles):
                    first, last = (ti == 0), (ti == len(tiles) - 1)
                    ps_s = pp.get()
                    lg = lg_ring.get(); pT = pT_ring.get()
                    tsl = slice(128, 384) if kt == 0 else slice(0, 256)
                    if n >= 0:
                        P.op("pe", "matmul", ps_s[:, 0:256], kb[:, n * 256 + kt * 128:n * 256 + (kt + 1) * 128], qb[:, qs_],
                             start=True, stop=False, reads=[kb, qb], writes=[ps_s])
                        P.op("pe", "matmul", ps_s[:, 0:256], onehot[:, n, :], ns[:, :], start=False, stop=True,
                             reads=[onehot, ns], writes=[ps_s])
                        P.op("dve", "scalar_tensor_tensor", out=lg[:, :], in0=ps_s[:, 0:256], scalar=scale, in1=tna[:, h, tsl],
                             op0=ALU.mult, op1=ALU.add, reads=[ps_s, tna], writes=[lg])
                        bi = (h * 4 + qi) * 16 + n
                        P.op("act", "activation", out=pT[:, :], in_=lg[:, :], func=AF.Exp, bias=abias[:, bi:bi + 1],
                             reads=[lg, abias], writes=[pT])
                        vl = vb[:, n * 2 + kt, :]
                        vbuf = vb
                    else:
                        P.op("pe", "matmul", ps_s[:, 0:256], ko[:, qi * 256 + kt * 128:qi * 256 + (kt + 1) * 128], qb[:, qs_],
                             start=True, stop=True, reads=[ko, qb], writes=[ps_s])
                        P.op("dve", "scalar_tensor_tensor", out=lg[:, :], in0=ps_s[:, 0:256], scalar=scale, in1=tca[:, h, tsl],
                             op0=ALU.mult, op1=ALU.add, reads=[ps_s, tca], writes=[lg])
                        P.op("act", "activation", out=pT[:, :], in_=lg[:, :], func=AF.Exp, reads=[lg], writes=[pT])
                        vl = vo[:, qi * 2 + kt, :]
                        vbuf = vo
                    P.op("pe", "matmul", ps_o[:, 0:256], vl, pT[:, :], start=first, stop=last, reads=[vbuf, pT], writes=[ps_o])
                    P.op("pe", "matmul", ps_d[:, 0:256], ones_bf[:, :], pT[:, :], start=first, stop=last,
                         reads=[ones_bf, pT], writes=[ps_d])
                rd = rd_ring.get(); ot = o_ring.get()
                P.op("dve", "reciprocal", rd[:, :], ps_d[:, 0:256], reads=[ps_d], writes=[rd])
                P.op("dve", "tensor_tensor", out=ot[:, :], in0=ps_o[:, 0:256], in1=rd[:, :], op=ALU.mult, reads=[ps_o, rd], writes=[ot])
                outs.append(P.dma("sp", yo[hr, qs_], ot[:, :], reads=[ot]))
        P.emit(final_wait_ops=outs)
    return nc


def attn_consts(seg):
    sl = np.arange(128)[:, None].astype(np.float64)
    u = np.arange(384)[None, :].astype(np.float64)
    dist = u - 128 - sl
    slopes = 2.0 ** (-(np.arange(8) + 1.0))
    tna = np.zeros((128, 8, 384), np.float32); tca = np.zeros((128, 8, 384), np.float32)
    for h in range(8):
        tna[:, h] = -slopes[h] * dist
        tca[:, h] = np.where(dist >= 0, -slopes[h] * dist, BIGNEG)
    abias = np.zeros((8, 4, 16), np.float32); gb = np.zeros((4, 16), np.float32); pm = np.zeros((4, 16), np.float32)
    for qi in range(4):
        G = 4 * seg + qi
        for n in range(16):
            if n < G:
                pm[qi, n] = 1.0
                abias[:, qi, n] = -slopes * 256.0 * (G - n)
            else:
                gb[qi, n] = -1e30
    oh = np.zeros((16, 16, 128), np.float32)
    for n in range(16):
        oh[n, n, :] = 1.0
    return {"Tna": tna.reshape(128, -1), "Tca": tca.reshape(128, -1), "abias": _rep(abias.reshape(-1)),
            "gbias": _rep(gb.reshape(-1)), "pastmask": _rep(pm.reshape(-1)), "onehot": oh.reshape(16, -1),
            "ident": np.eye(128, dtype=np.float32)}


def progC1_inputs(qT_c, kT_c, v_c):
    maps = []
    for c in range(NCORES):
        b, seg = c // 4, c % 4
        m = {"qT": qT_c[c], "kT_own": kT_c[c], "v_own": v_c[c],
             "kT_all": np.ascontiguousarray(np.concatenate([kT_c[b * 4 + s] for s in range(4)], axis=1)),
             "v_all": np.ascontiguousarray(np.concatenate([v_c[b * 4 + s] for s in range(4)], axis=0))}
        m.update(attn_consts(seg))
        maps.append(m)
    return maps


MEMLEN = 256


def build_progC2(T=NTOK):
    nc = bass.Bass("TRN2", target_bir_lowering=False)
    di = lambda n, sh: nc.dram_tensor(n, sh, F32, kind="ExternalInput").ap()
    xin = di("xT", [D, T]); yin = di("yT", [4096, T]); wout = di("w_out", [4096, D]); mem_i = di("memT", [D, MEMLEN])
    xmg_i = di("xmg", [128, 16]); mmg_i = di("mmg", [128, 16]); ffg_i = di("ffg", [128, 16])
    mqg_i = di("mqg", [128, 1]); mkg_i = di("mkg", [128, 1])
    wq = di("wq", [D, 512]); wk = di("wk", [D, 512]); wv = di("wv", [D, 512]); wo = di("wo", [512, D])
    wgu = di("w_gu", [D, 2 * DFF]); wdn = di("w_down", [DFF, D])
    xo = nc.dram_tensor("xoT", [D, T], F32, kind="ExternalOutput").ap()
    NKC = D // 128
    scale = 128.0 ** -0.5
    with contextlib.ExitStack() as stack:
        P = Prog(nc, stack)
        pp = PsumPool(P, 8)
        xT = [P.sbuf([128, T], F32, "xT%d" % c) for c in range(NKC)]
        hT = [P.sbuf([128, T], BF16, "hT%d" % c) for c in range(NKC)]
        aT = [P.sbuf([128, T], BF16, "aT%d" % c) for c in range(12)]
        g_xm = P.sbuf([128, NKC], F32, "g_xm"); g_mm = P.sbuf([128, NKC], F32, "g_mm"); g_ff = P.sbuf([128, NKC], F32, "g_ff")
        mqg = P.sbuf([128, 1], F32, "mqg"); mkg = P.sbuf([128, 1], F32, "mkg")
        ones_bf = P.sbuf([128, 128], BF16, "ones")
        rstd = P.sbuf([128, T], F32, "rstd")
        sq_ring = Ring([P.sbuf([128, 512], BF16, "sq%d" % i) for i in range(3)])
        sg_ring = Ring([P.sbuf([128, 512], F32, "sg%d" % i) for i in range(3)])
        wg_ring = Ring([P.sbuf([128, NKC, 256], BF16, "wg%d" % i) for i in range(2)])
        wu_ring = Ring([P.sbuf([128, NKC, 256], BF16, "wu%d" % i) for i in range(2)])
        wd_ring = Ring([P.sbuf([128, 12, 512], BF16, "wd%d" % i) for i in range(2)])
        w_ring = Ring(wg_ring.bufs + wu_ring.bufs)
        mr_ring = Ring([P.sbuf([128, MEMLEN], F32, "mr%d" % i) for i in range(3)])
        mhT = [P.sbuf([128, MEMLEN], BF16, "mhT%d" % c) for c in range(NKC)]
        kmT = [P.sbuf([128, MEMLEN], BF16, "kmT%d" % c) for c in range(4)]
        vm = [P.sbuf([128, 512], BF16, "vm%d" % c) for c in range(2)]
        pT_ring = sq_ring
        rd_ring = sg_ring

        xv = xin.rearrange("(c p) t -> c p t", p=128)
        yv = yin.rearrange("(c p) t -> c p t", p=128)
        mv = mem_i.rearrange("(c p) t -> c p t", p=128)
        P.op("pool", "memset", ones_bf[:, :], 1.0, writes=[ones_bf])
        for (b, a) in ((g_xm, xmg_i), (g_mm, mmg_i), (g_ff, ffg_i), (mqg, mqg_i), (mkg, mkg_i)):
            P.dma("sp", b[:], a, writes=[b])
        for c in range(NKC):
            P.dma("sp", xT[c][:, :], xv[c], writes=[xT[c]])

        def cons_add(fc, h0, w, ps):
            P.op("dve", "tensor_tensor", out=xT[fc][:, h0:h0 + w], in0=ps[:, 0:w], in1=xT[fc][:, h0:h0 + w], op=ALU.add,
                 reads=[ps, xT[fc]], writes=[xT[fc]])
        for kh in range(2):
            for c in range(NKC):
                P.dma("pool", hT[c][:, :], yv[kh * NKC + c], writes=[hT[c]])
            lin_fm(P, pp, hT, wout[kh * D:(kh + 1) * D, :], D, w_ring, T, cons_add)
        ps_m = pp.get()
        for c in range(NKC):
            mt = mr_ring.get()
            P.dma("sp", mt[:, :], mv[c], writes=[mt])
            sq = sq_ring.get()
            P.op("act", "activation", out=sq[:, 0:MEMLEN], in_=mt[:, :], func=AF.Square, reads=[mt], writes=[sq])
            P.op("pe", "matmul", ps_m[:, 0:MEMLEN], ones_bf[:, :], sq[:, 0:MEMLEN], start=(c == 0), stop=(c == NKC - 1),
                 reads=[sq, ones_bf], writes=[ps_m])
        P.op("dve", "tensor_scalar", rstd[:, 0:MEMLEN], ps_m[:, 0:MEMLEN], 1.0 / D, EPS, ALU.mult, ALU.add, reads=[ps_m], writes=[rstd])
        P.op("act", "activation", out=rstd[:, 0:MEMLEN], in_=rstd[:, 0:MEMLEN], func=AF.Sqrt, reads=[rstd], writes=[rstd])
        P.op("dve", "reciprocal", rstd[:, 0:MEMLEN], rstd[:, 0:MEMLEN], reads=[rstd], writes=[rstd])
        for c in range(NKC):
            mt = mr_ring.get()
            P.dma("sp", mt[:, :], mv[c], writes=[mt])
            P.op("dve", "scalar_tensor_tensor", out=mhT[c][:, :], in0=mt[:, :], scalar=g_mm[:, c:c + 1], in1=rstd[:, 0:MEMLEN],
                 op0=ALU.mult, op1=ALU.mult, reads=[mt, g_mm, rstd], writes=[mhT[c]])

        def dst_k(fc, h0, w):
            return kmT[fc][:, h0:h0 + w], kmT[fc]
        lin_fm(P, pp, mhT, wk, 512, w_ring, MEMLEN, headnorm_consume(P, pp, ones_bf, mkg, sq_ring, sg_ring, dst_k))

        def cons_v(tt, c0, cw, ps):
            P.op("act", "activation", out=vm[tt][:, c0:c0 + cw], in_=ps[:, 0:cw], func=AF.Copy, reads=[ps], writes=[vm[tt]])
        lin_tm(P, pp, mhT, wv, 512, w_ring, MEMLEN, cons_v)
        rmsnorm_fm(P, pp, xT, g_xm, hT, ones_bf, sq_ring, rstd, NKC, T, D)
        qmT = aT[0:4]
        oT = aT[4:8]

        def dst_q(fc, h0, w):
            return qmT[fc][:, h0:h0 + w], qmT[fc]
        lin_fm(P, pp, hT, wq, 512, w_ring, T, headnorm_consume(P, pp, ones_bf, mqg, sq_ring, sg_ring, dst_q))
        for mh in range(4):
            for h0 in range(0, T, 512):
                w = min(512, T - h0)
                ps_o = pp.get(); ps_d = pp.get()
                for kt in range(2):
                    ps_s = pp.get()
                    P.op("pe", "matmul", ps_s[:, 0:w], kmT[mh][:, kt * 128:(kt + 1) * 128], qmT[mh][:, h0:h0 + w],
                         start=True, stop=True, reads=[kmT[mh], qmT[mh]], writes=[ps_s])
                    pT = pT_ring.get()
                    P.op("act", "activation", out=pT[:, 0:w], in_=ps_s[:, 0:w], func=AF.Exp, scale=scale, reads=[ps_s], writes=[pT])
                    P.op("pe", "matmul", ps_o[:, 0:w], vm[kt][:, mh * 128:(mh + 1) * 128], pT[:, 0:w], start=(kt == 0),
                         stop=(kt == 1), reads=[vm[kt], pT], writes=[ps_o])
                    P.op("pe", "matmul", ps_d[:, 0:w], ones_bf[:, :], pT[:, 0:w], start=(kt == 0), stop=(kt == 1),
                         reads=[ones_bf, pT], writes=[ps_d])
                rd = rd_ring.get()
                P.op("dve", "reciprocal", rd[:, 0:w], ps_d[:, 0:w], reads=[ps_d], writes=[rd])
                P.op("dve", "tensor_tensor", out=oT[mh][:, h0:h0 + w], in0=ps_o[:, 0:w], in1=rd[:, 0:w], op=ALU.mult,
                     reads=[ps_o, rd], writes=[oT[mh]])
        lin_fm(P, pp, oT, wo, D, w_ring, T, cons_add, nkc=4)
        rmsnorm_fm(P, pp, xT, g_ff, hT, ones_bf, sq_ring, rstd, NKC, T, D)
        ffn_fm(P, pp, xT, hT, wgu, wdn, aT, wg_ring, wu_ring, wd_ring, sg_ring, T)
        xov = xo.rearrange("(c p) t -> c p t", p=128)
        outs = [P.dma("sp", xov[c], xT[c][:, :], reads=[xT[c]]) for c in range(NKC)]
        P.emit(final_wait_ops=outs)
    return nc


U32 = mybir.dt.uint32


def _xbc_row0(fc):
    if fc < 24:
        j, loc = fc // 6, fc % 6
    elif fc < 32:
        j, loc = (fc - 24) // 2, 6 + (fc - 24) % 2
    else:
        j, loc = (fc - 32) // 2, 8 + (fc - 32) % 2
    return j * 1280 + loc * 128


def build_fused(stop=None, skip_cc=()):
    T = NTOK
    NKC = D // 128
    nc = bass.Bass("TRN2", target_bir_lowering=False)
    di = lambda n, sh, dt=F32: nc.dram_tensor(n, sh, dt, kind="ExternalInput").ap()
    x_in = di("xT", [D, T]); mem_i = di("memT", [D, MEMLEN])
    W = {n: di(n, sh) for n, sh in (("w_gu1", [2, D, 2 * DFF]), ("w_down1", [2, DFF, D]), ("w_in", [2, D, N_IN]),
                                   ("w_out", [2, 4096, D]), ("wq", [2, D, 512]), ("wk", [2, D, 512]), ("wv", [2, D, 512]),
                                   ("wo", [2, 512, D]), ("w_gu2", [2, D, 2 * DFF]), ("w_down2", [2, DFF, D]))}
    G = {n: di(n, [2, 128, 16]) for n in ("ffg1", "mixg", "xmg", "mmg", "ffg2")}
    G.update({n: di(n, [2, 128, 1]) for n in ("qg", "kg", "mqg", "mkg")})
    S = {n: di(n, sh) for n, sh in (("convw", [2, 128, 10, 4]), ("convb", [2, 128, 10]), ("dtb", [2, 128, 384]),
                                   ("alog", [2, 128, 384]), ("dsk", [2, 128, 768]), ("normw", [2, 128, 768]),
                                   ("tri", [128, 128]), ("strict", [128, 128]), ("onesf", [128, 128]), ("ident", [128, 128]),
                                   ("Tna", [128, 8 * 384]), ("Tca", [128, 8 * 384]), ("abias", [128, 512]),
                                   ("gbias", [128, 64]), ("pastmask", [128, 64]), ("onehot", [16, 16 * 128]))}
    IDX = {n: di(n, [128, 1], U32) for n in ("idx_x", "idx_t", "idx_d", "idx_y")}
    out_ap = nc.dram_tensor("xoT", [D, T], F32, kind="ExternalOutput").ap()
    dr = lambda n, sh: Buf(nc.dram_tensor(n, sh, F32).ap(), n)
    xres = dr("xres", [D, T]); xres1 = dr("xres1", [D, T]); q_s = dr("q_s", [1024, T]); ya_s = dr("ya_s", [1024, T])
    kv_send = dr("kv_send", [2048, T]); kv_g = dr("kv_g", [4 * 2048, T])
    x_send = dr("x_send", [5120, T]); x_g = dr("x_g", [4 * 5120, T])
    z_send = dr("z_send", [4 * T, 768]); z_g = dr("z_g", [16 * T, 768])
    dt_send = dr("dt_send", [4 * T, 12]); dt_g = dr("dt_g", [16 * T, 12])
    y_send = dr("y_send", [3072, T]); y_g = dr("y_g", [4 * 3072, T])
    RG = [[0, 1, 2, 3], [4, 5, 6, 7]]
    scale = 128.0 ** -0.5

    with contextlib.ExitStack() as stack:
        P = Prog(nc, stack)
        P.begin_phases()

        def gather(out_ap_, out_buf, src, idx_t, row0, reads=()):
            width = src.t.shape[1]
            return P.add("pool", lambda e: e.indirect_dma_start(
                out=out_ap_, out_offset=None, in_=src.t, in_offset=bass.IndirectOffsetOnAxis(ap=idx_t[:, 0:1], axis=0),
                element_offset=row0 * width), reads=[src, idx_t] + list(reads), writes=[out_buf], is_dma=True)

        def finish():
            with P.phase(final=True):
                tb_ = [P.sbuf([128, T], F32, "fin%d" % i) for i in range(2)]
                sv = xres1.t.rearrange("(c p) t -> c p t", p=128)
                dv_ = out_ap.rearrange("(c p) t -> c p t", p=128)
                for c in range(NKC):
                    P.dma("sp", tb_[c % 2][:, :], sv[c], reads=[xres1], writes=[tb_[c % 2]])
                    P.dma("sp", dv_[c], tb_[c % 2][:, :], reads=[tb_[c % 2]])

        _coll = P.collective

        def coll(kind, groups, a, b, reads=(), writes=(), tag=None):
            if tag in skip_cc:
                return None
            rows = a.shape[0]
            if tag == "dt":
                return _coll(kind, groups, a, b, reads=reads, writes=writes)
            for k in range(rows // 256):
                _coll(kind, groups, a[k * 256:(k + 1) * 256, :], b[k * 1024:(k + 1) * 1024, :], reads=reads, writes=writes)

        for l in range(2):
            with P.phase():
                pp = PsumPool(P, 8)
                xT = [P.sbuf([128, T], F32, "xT%d" % c) for c in range(NKC)]
                hT = [P.sbuf([128, T], BF16, "hT%d" % c) for c in range(NKC)]
                aT = [P.sbuf([128, T], BF16, "aT%d" % c) for c in range(12)]
                g1 = P.sbuf([128, NKC], F32, "g1"); g2 = P.sbuf([128, NKC], F32, "g2")
                qg = P.sbuf([128, 1], F32, "qg"); kg = P.sbuf([128, 1], F32, "kg")
                ones_bf = P.sbuf([128, 128], BF16, "ones")
                rstd = P.sbuf([128, T], F32, "rstd")
                sq_ring = Ring([P.sbuf([128, 512], BF16, "sq%d" % i) for i in range(3)])
                sg_ring = Ring([P.sbuf([128, 512], F32, "sg%d" % i) for i in range(3)])
                wg_ring = Ring([P.sbuf([128, NKC, 256], BF16, "wg%d" % i) for i in range(2)])
                wu_ring = Ring([P.sbuf([128, NKC, 256], BF16, "wu%d" % i) for i in range(2)])
                wd_ring = Ring([P.sbuf([128, 12, 512], BF16, "wd%d" % i) for i in range(2)])
                w_ring = Ring(wg_ring.bufs + wu_ring.bufs)
                st_ring = Ring([P.sbuf([128, 512], F32, "st%d" % i) for i in range(4)])
                P.op("dve", "memset", ones_bf[:, :], 1.0, writes=[ones_bf])
                P.dma("sp", g1[:, :], G["ffg1"][l], writes=[g1]); P.dma("sp", g2[:, :], G["mixg"][l], writes=[g2])
                P.dma("sp", qg[:, :], G["qg"][l], writes=[qg]); P.dma("sp", kg[:, :], G["kg"][l], writes=[kg])
                src = x_in if l == 0 else xres.t
                xv = src.rearrange("(c p) t -> c p t", p=128)
                for c in range(NKC):
                    P.dma("sp", xT[c][:, :], xv[c], reads=([] if l == 0 else [xres]), writes=[xT[c]])
                rmsnorm_fm(P, pp, xT, g1, hT, ones_bf, sq_ring, rstd, NKC, T, D)
                ffn_fm(P, pp, xT, hT, W["w_gu1"][l], W["w_down1"][l], aT, wg_ring, wu_ring, wd_ring, sg_ring, T)
                x1v = xres1.t.rearrange("(c p) t -> c p t", p=128)
                for c in range(NKC):
                    P.dma("sp", x1v[c], xT[c][:, :], reads=[xT[c]], writes=[xres1])
                rmsnorm_fm(P, pp, xT, g2, hT, ones_bf, sq_ring, rstd, NKC, T, D)
                win = W["w_in"][l]
                for (dstb, rbase, col0, gbuf) in ((q_s, 0, OQ, qg), (kv_send, 0, OK_, kg)):
                    holder = {}

                    def dst_of(fc, h0, w, holder=holder):
                        st = st_ring.get()
                        holder["st"] = st
                        return st[:, 0:w], st
                    cons0 = headnorm_consume(P, pp, ones_bf, gbuf, sq_ring, sg_ring, dst_of)

                    def cons(fc, h0, w, ps, cons0=cons0, holder=holder, dstb=dstb, rbase=rbase):
                        cons0(fc, h0, w, ps)
                        st = holder["st"]
                        P.dma("sp", dstb.t[rbase + fc * 128:rbase + (fc + 1) * 128, h0:h0 + w], st[:, 0:w], reads=[st], writes=[dstb])
                    lin_fm(P, pp, hT, win[:, col0:col0 + 1024], 1024, w_ring, T, cons)

                def cons_xbc(fc, h0, w, ps):
                    st = st_ring.get()
                    P.op("act", "activation", out=st[:, 0:w], in_=ps[:, 0:w], func=AF.Copy, reads=[ps], writes=[st])
                    r0 = _xbc_row0(fc)
                    P.dma("sp", x_send.t[r0:r0 + 128, h0:h0 + w], st[:, 0:w], reads=[st], writes=[x_send])
                lin_fm(P, pp, hT, win[:, OXBC:OXBC + NXBC], NXBC, w_ring, T, cons_xbc)

                def cons_v(tt, c0, cw, ps):
                    st = st_ring.get()
                    P.op("dve", "tensor_copy", st[:, 0:cw], ps[:, 0:cw], reads=[ps], writes=[st])
                    P.dma("sp", kv_send.t[1024 + tt * 128:1024 + (tt + 1) * 128, c0:c0 + cw], st[:, 0:cw], reads=[st], writes=[kv_send])
                lin_tm(P, pp, hT, win[:, OV:OV + NV], NV, w_ring, T, cons_v)

                def cons_z(tt, c0, cw, ps):
                    st = st_ring.get()
                    P.op("dve", "tensor_copy", st[:, 0:cw], ps[:, 0:cw], reads=[ps], writes=[st])
                    j, lc0 = c0 // 768, c0 % 768
                    P.dma("sp", z_send.t[j * T + tt * 128:j * T + (tt + 1) * 128, lc0:lc0 + cw], st[:, 0:cw], reads=[st], writes=[z_send])
                lin_tm(P, pp, hT, win[:, OZ:OZ + NZ], NZ, w_ring, T, cons_z)

                def cons_dt(tt, c0, cw, ps):
                    st = st_ring.get()
                    P.op("dve", "tensor_copy", st[:, 0:cw], ps[:, 0:cw], reads=[ps], writes=[st])
                    for j in range(4):
                        P.dma("sp", dt_send.t[j * T + tt * 128:j * T + (tt + 1) * 128, :], st[:, 12 * j:12 * j + 12], reads=[st], writes=[dt_send])
                lin_tm(P, pp, hT, win[:, ODT:ODT + NDT], NDT, w_ring, T, cons_dt)
                coll("AllGather", RG, kv_send.t, kv_g.t, reads=[kv_send], writes=[kv_g], tag="kv")

            if stop == (l, "A"):
                finish()
                return nc
            with P.phase():
                coll("AllGather", RG, x_send.t, x_g.t, reads=[x_send], writes=[x_g], tag="x")
                coll("AllGather", RG, z_send.t, z_g.t, reads=[z_send], writes=[z_g], tag="z")
                coll("AllGather", RG, dt_send.t, dt_g.t, reads=[dt_send], writes=[dt_g], tag="dt")
                pp = PsumPool(P, 4)
                pacc = PsumPool(P, 4, pfx="pacc")
                tna = P.sbuf([128, 8, 384], F32, "tna"); tca = P.sbuf([128, 8, 384], F32, "tca")
                abias = P.sbuf([128, 512], F32, "abias"); gbias = P.sbuf([128, 64], F32, "gbias")
                pmask = P.sbuf([128, 64], F32, "pmask"); onehot = P.sbuf([16, 16, 128], BF16, "onehot")
                ident = P.sbuf([128, 128], F32, "ident"); ones_bf = P.sbuf([128, 128], BF16, "ones")
                P.dma("sp", tna[:], S["Tna"].rearrange("p (h u) -> p h u", h=8), writes=[tna])
                P.dma("sp", tca[:], S["Tca"].rearrange("p (h u) -> p h u", h=8), writes=[tca])
                for (b, a) in ((abias, S["abias"]), (gbias, S["gbias"]), (pmask, S["pastmask"]), (ident, S["ident"])):
                    P.dma("sp", b[:], a, writes=[b])
                ohs = P.sbuf([16, 16, 128], F32, "ohs")
                P.dma("sp", ohs[:], S["onehot"].rearrange("k (n m) -> k n m", n=16), writes=[ohs])
                P.op("dve", "tensor_copy", onehot[:], ohs[:], reads=[ohs], writes=[onehot])
                P.op("dve", "memset", ones_bf[:, :], 1.0, writes=[ones_bf])
                kf_ring = Ring([P.sbuf([128, SEQ], F32, "kf%d" % i) for i in range(2)])
                kb_ring = Ring([P.sbuf([128, SEQ], BF16, "kb%d" % i) for i in range(2)])
                qf_ring = Ring([P.sbuf([128, NTOK], F32, "qf%d" % i) for i in range(2)])
                qb_ring = Ring([P.sbuf([128, NTOK], BF16, "qb%d" % i) for i in range(2)])
                ko_ring = Ring([P.sbuf([128, NTOK], BF16, "ko%d" % i) for i in range(2)])
                vb_ring = Ring([P.sbuf([128, 32, 128], BF16, "vb%d" % i) for i in range(2)])
                vo_ring = Ring([P.sbuf([128, 8, 128], BF16, "vo%d" % i) for i in range(2)])
                kos_ring = Ring([P.sbuf([128, NTOK], F32, "kos%d" % i) for i in range(2)])
                vbs_ring = Ring([P.sbuf([128, 32, 128], F32, "vbs%d" % i) for i in range(2)])
                vos_ring = Ring([P.sbuf([128, 8, 128], F32, "vos%d" % i) for i in range(2)])
                km_ring = Ring([P.sbuf([128, 16], F32, "km%d" % i) for i in range(2)])
                gm_ring = Ring([P.sbuf([128, 16], F32, "gm%d" % i) for i in range(2)])
                t8_ring = Ring([P.sbuf([128, 8], F32, "t8%d" % i) for i in range(2)])
                sel_ring = Ring([P.sbuf([128, 16], F32, "sel%d" % i) for i in range(2)])
                ns_ring = Ring([P.sbuf([16, 256], BF16, "ns%d" % i) for i in range(2)])
                lg_ring = Ring([P.sbuf([128, 256], F32, "lg%d" % i) for i in range(4)])
                pT_ring = Ring([P.sbuf([128, 256], BF16, "pT%d" % i) for i in range(8)])
                rd_ring = Ring([P.sbuf([128, 256], F32, "rd%d" % i) for i in range(2)])
                o_ring = Ring([P.sbuf([128, 256], F32, "oo%d" % i) for i in range(2)])
                for h in range(8):
                    hr = slice(h * 128, (h + 1) * 128)
                    kf = kf_ring.get(); kb = kb_ring.get(); qf = qf_ring.get(); qb = qb_ring.get()
                    ko = ko_ring.get(); vb = vb_ring.get(); vo = vo_ring.get(); km = km_ring.get()
                    kos = kos_ring.get(); vbs = vbs_ring.get(); vos = vos_ring.get()
                    for sgm in range(4):
                        kr0 = (h // 2) * 1024 + sgm * 256 + (h % 2) * 128
                        P.dma("sp", kf[:, sgm * T:(sgm + 1) * T], kv_g.t[kr0:kr0 + 128, :], reads=[kv_g], writes=[kf])
                        for a_ in range(4):
                            vr0 = (4 + a_) * 1024 + sgm * 256
                            P.dma("sp", vbs[:, sgm * 8 + 2 * a_:sgm * 8 + 2 * a_ + 2, :],
                                  kv_g.t[vr0:vr0 + 256, hr].rearrange("(t p) d -> p t d", p=128), reads=[kv_g], writes=[vbs])
                    P.dma("sp", qf[:, :], q_s.t[hr, :], reads=[q_s], writes=[qf])
                    P.dma("sp", kos[:, :], kv_send.t[hr, :], reads=[kv_send], writes=[kos])
                    P.dma("sp", vos[:], kv_send.t[1024:2048, hr].rearrange("(t p) d -> p t d", p=128), reads=[kv_send], writes=[vos])
                    P.op("act", "activation", out=ko[:, :], in_=kos[:, :], func=AF.Copy, reads=[kos], writes=[ko])
                    P.op("dve", "tensor_copy", vb[:], vbs[:], reads=[vbs], writes=[vb])
                    P.op("dve", "tensor_copy", vo[:], vos[:], reads=[vos], writes=[vo])
                    P.op("act", "activation", out=kb[:, :], in_=kf[:, :], func=AF.Copy, reads=[kf], writes=[kb])
                    P.op("act", "activation", out=qb[:, :], in_=qf[:, :], func=AF.Copy, reads=[qf], writes=[qb])
                    P.op("dve", "tensor_reduce", out=km[:, :], in_=kf[:, :].rearrange("p (n s) -> p n s", s=256), axis=AX.X,
                         op=ALU.add, reads=[kf], writes=[km])
                    P.op("dve", "tensor_scalar", km[:, :], km[:, :], 1.0 / 256, None, ALU.mult, reads=[km], writes=[km])
                    for qi in range(4):
                        qs_ = slice(qi * 256, (qi + 1) * 256)
                        ns = ns_ring.get()
                        for qs in range(2):
                            ps_g = pp.get()
                            P.op("pe", "matmul", ps_g[:, 0:16], qf[:, qi * 256 + qs * 128:qi * 256 + (qs + 1) * 128], km[:, :],
                                 start=True, stop=True, reads=[qf, km], writes=[ps_g])
                            gm = gm_ring.get(); t8 = t8_ring.get(); sel = sel_ring.get()
                            P.op("dve", "tensor_tensor", out=gm[:, :], in0=ps_g[:, 0:16], in1=gbias[:, qi * 16:(qi + 1) * 16],
                                 op=ALU.add, reads=[ps_g, gbias], writes=[gm])
                            P.op("dve", "max", out=t8[:, :], in_=gm[:, :], reads=[gm], writes=[t8])
                            P.op("dve", "tensor_scalar", sel[:, :], gm[:, :], t8[:, 2:3], None, ALU.is_ge, reads=[gm, t8], writes=[sel])
                            P.op("dve", "tensor_tensor", out=sel[:, :], in0=sel[:, :], in1=pmask[:, qi * 16:(qi + 1) * 16],
                                 op=ALU.mult, reads=[sel, pmask], writes=[sel])
                            P.op("dve", "tensor_scalar", sel[:, :], sel[:, :], -1.0, -BIGNEG, ALU.add, ALU.mult, reads=[sel], writes=[sel])
                            ps_t = pp.get()
                            P.op("pe", "transpose", ps_t[0:16, 0:128], sel[:, :], ident[:, :], reads=[sel, ident], writes=[ps_t])
                            P.op("act", "activation", out=ns[:, qs * 128:(qs + 1) * 128], in_=ps_t[0:16, 0:128], func=AF.Copy,
                                 reads=[ps_t], writes=[ns])
                        ps_o = pacc.get(); ps_d = pacc.get()
                        tiles = [(n, kt) for n in range(NBLK) for kt in range(2)] + [(-1, 0), (-1, 1)]
                        LA = 3
                        pend = {}

                        def stage1(ti):
                            n, kt = tiles[ti]
                            ps_s = pp.get()
                            lg = lg_ring.get(); pT = pT_ring.get()
                            tsl = slice(128, 384) if kt == 0 else slice(0, 256)
                            if n >= 0:
                                P.op("pe", "matmul", ps_s[:, 0:256], kb[:, n * 256 + kt * 128:n * 256 + (kt + 1) * 128], qb[:, qs_],
                                     start=True, stop=False, reads=[kb, qb], writes=[ps_s])
                                P.op("pe", "matmul", ps_s[:, 0:256], onehot[:, n, :], ns[:, :], start=False, stop=True,
                                     reads=[onehot, ns], writes=[ps_s])
                                P.op("dve", "scalar_tensor_tensor", out=lg[:, :], in0=ps_s[:, 0:256], scalar=scale, in1=tna[:, h, tsl],
                                     op0=ALU.mult, op1=ALU.add, reads=[ps_s, tna], writes=[lg])
                                bi = (h * 4 + qi) * 16 + n
                                P.op("act", "activation", out=pT[:, :], in_=lg[:, :], func=AF.Exp, bias=abias[:, bi:bi + 1],
                                     reads=[lg, abias], writes=[pT])
                                pend[ti] = (vb[:, n * 2 + kt, :], vb, pT)
                            else:
                                P.op("pe", "matmul", ps_s[:, 0:256], ko[:, qi * 256 + kt * 128:qi * 256 + (kt + 1) * 128], qb[:, qs_],
                                     start=True, stop=True, reads=[ko, qb], writes=[ps_s])
                                P.op("dve", "scalar_tensor_tensor", out=lg[:, :], in0=ps_s[:, 0:256], scalar=scale, in1=tca[:, h, tsl],
                                     op0=ALU.mult, op1=ALU.add, reads=[ps_s, tca], writes=[lg])
                                P.op("act", "activation", out=pT[:, :], in_=lg[:, :], func=AF.Exp, reads=[lg], writes=[pT])
                                pend[ti] = (vo[:, qi * 2 + kt, :], vo, pT)

                        def stage2(ti):
                            vl, vbuf, pT = pend.pop(ti)
                            first, last = (ti == 0), (ti == len(tiles) - 1)
                            P.op("pe", "matmul", ps_o[:, 0:256], vl, pT[:, :], start=first, stop=last, reads=[vbuf, pT], writes=[ps_o])
                            P.op("pe", "matmul", ps_d[:, 0:256], ones_bf[:, :], pT[:, :], start=first, stop=last,
                                 reads=[ones_bf, pT], writes=[ps_d])
                        for ti in range(len(tiles) + LA):
                            if ti < len(tiles):
                                stage1(ti)
                            if ti - LA >= 0:
                                stage2(ti - LA)
                        rd = rd_ring.get(); ot = o_ring.get()
                        P.op("dve", "reciprocal", rd[:, :], ps_d[:, 0:256], reads=[ps_d], writes=[rd])
                        P.op("dve", "tensor_tensor", out=ot[:, :], in0=ps_o[:, 0:256], in1=rd[:, :], op=ALU.mult,
                             reads=[ps_o, rd], writes=[ot])
                        P.dma("sp", ya_s.t[hr, qs_], ot[:, :], reads=[ot], writes=[ya_s])

            if stop == (l, "C1"):
                finish()
                return nc
            with P.phase():
                pp = PsumPool(P, 6)
                pyo = PsumPool(P, 2, pfx="pyo")
                cw = P.sbuf([128, 10, 4], F32, "cw"); cb = P.sbuf([128, 10], F32, "cb")
                dt_all = P.sbuf([128, 384], F32, "dt_all"); da_all = P.sbuf([128, 384], F32, "da_all")
                tmpa = P.sbuf([128, 384], F32, "tmpa"); tmpb = P.sbuf([128, 384], F32, "tmpb")
                dsk = P.sbuf([128, 12, 64], F32, "dsk"); normw = P.sbuf([128, 768], F32, "normw")
                tri = P.sbuf([128, 128], F32, "tri"); strict = P.sbuf([128, 128], F32, "strict")
                onesf = P.sbuf([128, 128], F32, "onesf"); ident = P.sbuf([128, 128], F32, "ident")
                idx_x = P.sbuf([128, 1], U32, "idx_x"); idx_t = P.sbuf([128, 1], U32, "idx_t"); idx_d = P.sbuf([128, 1], U32, "idx_d")
                for (b, a) in ((cw, S["convw"][l]), (cb, S["convb"][l]), (tmpa, S["dtb"][l]), (tmpb, S["alog"][l]),
                               (normw, S["normw"][l]), (tri, S["tri"]), (strict, S["strict"]), (onesf, S["onesf"]),
                               (ident, S["ident"]), (idx_x, IDX["idx_x"]), (idx_t, IDX["idx_t"]), (idx_d, IDX["idx_d"])):
                    P.dma("sp", b[:], a, writes=[b])
                P.dma("sp", dsk[:], S["dsk"][l].rearrange("p (j d) -> p j d", d=64), writes=[dsk])
                for c in range(NCH):
                    sgm, lc = c // 8, c % 8
                    gather(dt_all[:, c * 12:(c + 1) * 12], dt_all, dt_g, idx_d, sgm * 4 * T + lc * 128)
                P.op("dve", "tensor_tensor", out=dt_all[:], in0=dt_all[:], in1=tmpa[:], op=ALU.add, reads=[dt_all, tmpa], writes=[dt_all])
                P.op("act", "activation", out=dt_all[:], in_=dt_all[:], func=AF.Exp, reads=[dt_all], writes=[dt_all])
                P.op("dve", "tensor_scalar", dt_all[:], dt_all[:], 1.0, None, ALU.add, reads=[dt_all], writes=[dt_all])
                P.op("act", "activation", out=dt_all[:], in_=dt_all[:], func=AF.Ln, reads=[dt_all], writes=[dt_all])
                P.op("act", "activation", out=tmpb[:], in_=tmpb[:], func=AF.Exp, reads=[tmpb], writes=[tmpb])
                P.op("dve", "scalar_tensor_tensor", out=da_all[:], in0=dt_all[:], scalar=-1.0, in1=tmpb[:], op0=ALU.mult,
                     op1=ALU.mult, reads=[dt_all, tmpb], writes=[da_all])
                h = [P.sbuf([128, 6, 64], F32, "h%d" % g) for g in range(2)]
                hb = [P.sbuf([128, 6, 64], BF16, "hb%d" % g) for g in range(2)]
                for g in range(2):
                    P.op("pool", "memset", h[g][:], 0.0, writes=[h[g]])
                    P.op("pool", "memset", hb[g][:], 0.0, writes=[hb[g]])
                halo = [P.sbuf([128, 4], F32, "halo%d" % i) for i in range(10)]
                xr_ring = Ring([P.sbuf([128, T + 3], F32, "xr%d" % i) for i in range(3)])
                acc_ring = Ring([P.sbuf([128, 512], F32, "acc%d" % i) for i in range(2)])
                xc_sets = [[P.sbuf([128, T], F32, "xc%d_%d" % (k, i)) for i in range(8)] for k in range(2)]
                bc_sets = [[P.sbuf([128, T], BF16, "bc%d_%d" % (k, i)) for i in range(4)] for k in range(2)]
                xt_ring = Ring([P.sbuf([128, 12, 64], F32, "xt%d" % i) for i in range(2)])
                bt_ring = Ring([P.sbuf([128, 256], BF16, "bt%d" % i) for i in range(2)])
                E_ring = Ring([P.sbuf([128, 36], F32, "E%d" % i) for i in range(2)])
                s2_ring = Ring([P.sbuf([128, 12], F32, "s2%d" % i) for i in range(2)])
                xdt_ring = Ring([P.sbuf([128, 12, 64], BF16, "xdt%d" % i) for i in range(2)])
                xw_ring = Ring([P.sbuf([128, 12, 64], BF16, "xw%d" % i) for i in range(2)])
                xd_ring = Ring([P.sbuf([128, 12, 64], F32, "xd%d" % i) for i in range(2)])
                cbm_ring = Ring([P.sbuf([128, 128], F32, "cbm%d" % i) for i in range(2)])
                ajall_ring = Ring([P.sbuf([128, 12, 128], F32, "ajall%d" % i) for i in range(2)])
                scall_ring = Ring([P.sbuf([128, 12, 128], BF16, "scall%d" % i) for i in range(2)])
                dec_ring = Ring([P.sbuf([128, 512], F32, "dec%d" % i) for i in range(3)])
                y_ring = Ring([P.sbuf([128, 12, 64], F32, "y%d" % i) for i in range(2)])
                t1_ring = Ring([P.sbuf([128, 6, 64], F32, "t1%d" % i) for i in range(2)])
                z_ring = Ring([P.sbuf([128, 768], F32, "z%d" % i) for i in range(2)])
                sq_ring = Ring([P.sbuf([128, 768], F32, "sqq%d" % i) for i in range(2)])
                ss_ring = Ring([P.sbuf([128, 2], F32, "ss%d" % i) for i in range(2)])
                o_ring = Ring([P.sbuf([128, 768], F32, "o%d" % i) for i in range(2)])
                yT_ring = Ring([P.sbuf([128, 6, 128], F32, "yTt%d" % i) for i in range(2)])
                ysv = y_send.t.rearrange("(s f p) t -> s p f t", s=4, p=128)
                y_seg = [Buf(None, "y_seg%d" % i) for i in range(4)]
                def conv(sgm):
                    xc = xc_sets[sgm % 2]
                    bc = bc_sets[sgm % 2]
                    for ch in range(10):
                        xr = xr_ring.get()
                        gather(xr[:, 3:T + 3], xr, x_g, idx_x, (ch // 2) * 1024 + (ch % 2) * 128 + sgm * 256)
                        if sgm == 0:
                            P.op("pool", "memset", xr[:, 0:3], 0.0, writes=[xr])
                        else:
                            P.op("act", "activation", out=xr[:, 0:3], in_=halo[ch][:, 0:3], func=AF.Copy, reads=[halo[ch]], writes=[xr])
                        P.op("act", "activation", out=halo[ch][:, 0:3], in_=xr[:, T:T + 3], func=AF.Copy, reads=[xr], writes=[halo[ch]])
                        for h0 in (0, 512):
                            acc = acc_ring.get()
                            P.op("dve", "tensor_scalar", acc[:, :], xr[:, h0:h0 + 512], cw[:, ch, 0:1], cb[:, ch:ch + 1], ALU.mult, ALU.add,
                                 reads=[xr, cw, cb], writes=[acc])
                            for k in range(1, 4):
                                P.op("dve", "scalar_tensor_tensor", out=acc[:, :], in0=xr[:, h0 + k:h0 + k + 512], scalar=cw[:, ch, k:k + 1],
                                     in1=acc[:, :], op0=ALU.mult, op1=ALU.add, reads=[xr, cw, acc], writes=[acc])
                            if ch < 8:
                                P.op("act", "activation", out=xc[ch][:, h0:h0 + 512], in_=acc[:, :], func=AF.Silu, reads=[acc], writes=[xc[ch]])
                                if ch >= 6:
                                    P.op("dve", "tensor_copy", bc[ch - 6][:, h0:h0 + 512], xc[ch][:, h0:h0 + 512], reads=[xc[ch]], writes=[bc[ch - 6]])
                            else:
                                P.op("act", "activation", out=bc[ch - 6][:, h0:h0 + 512], in_=acc[:, :], func=AF.Silu, reads=[acc], writes=[bc[ch - 6]])

                def S1(c, sgm, lc, xc, bc):
                    ts = slice(lc * 128, (lc + 1) * 128)
                    xt = xt_ring.get(); bt = bt_ring.get()
                    xtf = xt[:].rearrange("p j d -> p (j d)")
                    for (lo, n) in ((0, 4), (4, 2)):
                        ps = pp.get()
                        for i in range(n):
                            P.op("pe", "transpose", ps[:, i * 128:(i + 1) * 128], xc[lo + i][:, ts], ident[:, :],
                                 reads=[xc[lo + i], ident], writes=[ps])
                        P.op("dve", "tensor_copy", xtf[:, lo * 128:(lo + n) * 128], ps[:, 0:n * 128], reads=[ps], writes=[xt])
                    ps = pp.get()
                    for i in range(2):
                        P.op("pe", "transpose", ps[:, i * 128:(i + 1) * 128], xc[6 + i][:, ts], ident[:, :],
                             reads=[xc[6 + i], ident], writes=[ps])
                    P.op("act", "activation", out=bt[:, :], in_=ps[:, 0:256], func=AF.Copy, reads=[ps], writes=[bt])
                    dac = da_all[:, c * 12:(c + 1) * 12]
                    dtc = dt_all[:, c * 12:(c + 1) * 12]
                    ps = pp.get()
                    P.op("pe", "matmul", ps[:, 0:12], tri[:, :], dac, start=True, stop=True, reads=[tri, da_all], writes=[ps])
                    P.op("pe", "matmul", ps[:, 12:24], strict[:, :], dac, start=True, stop=True, reads=[strict, da_all], writes=[ps])
                    P.op("pe", "matmul", ps[:, 24:36], onesf[:, :], dac, start=True, stop=True, reads=[onesf, da_all], writes=[ps])
                    E = E_ring.get()
                    P.op("act", "activation", out=E[:, :], in_=ps[:, 0:36], func=AF.Exp, reads=[ps], writes=[E])
                    s2 = s2_ring.get()
                    P.op("dve", "tensor_tensor", out=s2[:, :], in0=dtc, in1=E[:, 12:24], op=ALU.mult, reads=[dt_all, E], writes=[s2])
                    xdt = xdt_ring.get(); xw = xw_ring.get(); xd = xd_ring.get()
                    P.op("dve", "tensor_tensor", out=xdt[:], in0=xt[:], in1=dtc.unsqueeze(2).broadcast_to([128, 12, 64]),
                         op=ALU.mult, reads=[xt, dt_all], writes=[xdt])
                    P.op("dve", "tensor_tensor", out=xw[:], in0=xt[:], in1=s2[:, :].unsqueeze(2).broadcast_to([128, 12, 64]),
                         op=ALU.mult, reads=[xt, s2], writes=[xw])
                    P.op("dve", "tensor_tensor", out=xd[:], in0=xt[:], in1=dsk[:], op=ALU.mult, reads=[xt, dsk], writes=[xd])
                    return dict(c=c, sgm=sgm, lc=lc, xc=xc, bc=bc, ts=ts, xt=xt, bt=bt, dac=dac, dtc=dtc, E=E, s2=s2, xdt=xdt, xw=xw, xd=xd)

                def S2(v):
                    c = v['c']; sgm = v['sgm']; lc = v['lc']; bc = v['bc']; ts = v['ts']; bt = v['bt']; dac = v['dac']; E = v['E']
                    xdt = v['xdt']; xw = v['xw']; xd = v['xd']
                    y = y_ring.get()
                    yos = []
                    for gi in range(2):
                        CT = bc[2 + gi]
                        ps_yo = pyo.get()
                        P.op("pe", "matmul", ps_yo[:, 0:384], CT[:, ts], hb[gi][:].rearrange("p j d -> p (j d)"),
                             start=True, stop=True, reads=[CT, hb[gi]], writes=[ps_yo])
                        yos.append(ps_yo)
                    for gi in range(2):
                        ps_st = pp.get()
                        P.op("pe", "matmul", ps_st[:, 0:384], bt[:, gi * 128:(gi + 1) * 128],
                             xw[:, gi * 6:(gi + 1) * 6, :].rearrange("p j d -> p (j d)"), start=True, stop=True,
                             reads=[bt, xw], writes=[ps_st])
                        P.op("dve", "tensor_tensor", out=h[gi][:], in0=h[gi][:],
                             in1=E[:, 24 + gi * 6:24 + gi * 6 + 6].unsqueeze(2).broadcast_to([128, 6, 64]), op=ALU.mult,
                             reads=[h[gi], E], writes=[h[gi]])
                        P.op("dve", "tensor_tensor", out=h[gi][:], in0=ps_st[:, 0:384].rearrange("p (j d) -> p j d", d=64),
                             in1=h[gi][:], op=ALU.add, reads=[ps_st, h[gi]], writes=[h[gi]])
                        P.op("act", "activation", out=hb[gi][:], in_=h[gi][:], func=AF.Copy, reads=[h[gi]], writes=[hb[gi]])
                    ajall = ajall_ring.get()
                    P.op("dve", "tensor_tensor", out=ajall[:], in0=strict[:, :].unsqueeze(1).broadcast_to([128, 12, 128]),
                         in1=dac.unsqueeze(2).broadcast_to([128, 12, 128]), op=ALU.mult, reads=[strict, da_all], writes=[ajall])
                    cbms = []
                    for gi in range(2):
                        BT = bc[gi]; CT = bc[2 + gi]
                        ps_cb = pp.get()
                        P.op("pe", "matmul", ps_cb[:, 0:128], BT[:, ts], CT[:, ts], start=True, stop=True, reads=[BT, CT], writes=[ps_cb])
                        cbm = cbm_ring.get()
                        P.op("dve", "tensor_tensor", out=cbm[:, :], in0=ps_cb[:, 0:128], in1=tri[:, :], op=ALU.mult,
                             reads=[ps_cb, tri], writes=[cbm])
                        cbms.append(cbm)
                    scall = scall_ring.get()
                    for gi in range(2):
                        for (j0, nh) in ((0, 4), (4, 2)):
                            ps_seg = pp.get()
                            for i in range(nh):
                                j = gi * 6 + j0 + i
                                P.op("pe", "matmul", ps_seg[:, i * 128:(i + 1) * 128], ajall[:, j, :], tri[:, :], start=True, stop=True,
                                     reads=[ajall, tri], writes=[ps_seg])
                            dec = dec_ring.get()
                            P.op("act", "activation", out=dec[:, 0:nh * 128], in_=ps_seg[:, 0:nh * 128], func=AF.Exp, reads=[ps_seg], writes=[dec])
                            P.op("dve", "tensor_tensor", out=scall[:, gi * 6 + j0:gi * 6 + j0 + nh, :],
                                 in0=dec[:, 0:nh * 128].rearrange("p (j s) -> p j s", s=128),
                                 in1=cbms[gi][:, :].unsqueeze(1).broadcast_to([128, nh, 128]), op=ALU.mult,
                                 reads=[dec, cbms[gi]], writes=[scall])
                    for gi in range(2):
                        ps_yd = pp.get()
                        ps_yo = yos[gi]
                        for jj in range(6):
                            j = gi * 6 + jj
                            P.op("pe", "matmul", ps_yd[:, jj * 64:(jj + 1) * 64], scall[:, j, :], xdt[:, j, :], start=True, stop=True,
                                 reads=[scall, xdt], writes=[ps_yd])
                        t1 = t1_ring.get()
                        P.op("dve", "tensor_tensor", out=t1[:], in0=ps_yo[:, 0:384].rearrange("p (j d) -> p j d", d=64),
                             in1=E[:, gi * 6:gi * 6 + 6].unsqueeze(2).broadcast_to([128, 6, 64]), op=ALU.mult,
                             reads=[ps_yo, E], writes=[t1])
                        P.op("dve", "tensor_tensor", out=t1[:], in0=ps_yd[:, 0:384].rearrange("p (j d) -> p j d", d=64),
                             in1=t1[:], op=ALU.add, reads=[ps_yd, t1], writes=[t1])
                        P.op("dve", "tensor_tensor", out=y[:, gi * 6:(gi + 1) * 6, :], in0=xd[:, gi * 6:(gi + 1) * 6, :], in1=t1[:],
                             op=ALU.add, reads=[xd, t1], writes=[y])
                    v['y'] = y

                def S3(v):
                    c = v['c']; sgm = v['sgm']; lc = v['lc']; ts = v['ts']; y = v['y']
                    zt = z_ring.get()
                    gather(zt[:, :], zt, z_g, idx_t, (lc // 2) * 1024 + sgm * 256 + (lc % 2) * 128)
                    P.op("act", "activation", out=zt[:, :], in_=zt[:, :], func=AF.Silu, reads=[zt], writes=[zt])
                    yf = y[:].rearrange("p j d -> p (j d)")
                    P.op("dve", "tensor_tensor", out=yf, in0=yf, in1=zt[:, :], op=ALU.mult, reads=[y, zt], writes=[y])
                    sq = sq_ring.get()
                    P.op("act", "activation", out=sq[:, :], in_=yf, func=AF.Square, reads=[y], writes=[sq])
                    ss = ss_ring.get()
                    P.op("dve", "tensor_reduce", out=ss[:, :], in_=sq[:, :].rearrange("p (g f) -> p g f", g=2), axis=AX.X, op=ALU.add,
                         reads=[sq], writes=[ss])
                    P.op("dve", "tensor_scalar", ss[:, :], ss[:, :], 1.0 / 384, EPS, ALU.mult, ALU.add, reads=[ss], writes=[ss])
                    P.op("act", "activation", out=ss[:, :], in_=ss[:, :], func=AF.Sqrt, reads=[ss], writes=[ss])
                    P.op("dve", "reciprocal", ss[:, :], ss[:, :], reads=[ss], writes=[ss])
                    ot = o_ring.get()
                    for gi in range(2):
                        P.op("dve", "scalar_tensor_tensor", out=ot[:, gi * 384:(gi + 1) * 384], in0=yf[:, gi * 384:(gi + 1) * 384],
                             scalar=ss[:, gi:gi + 1], in1=normw[:, gi * 384:(gi + 1) * 384], op0=ALU.mult, op1=ALU.mult,
                             reads=[y, ss, normw], writes=[ot])
                    yTt = yT_ring.get()
                    yTf = yTt[:].rearrange("p f t -> p (f t)")
                    for (lo, n) in ((0, 4), (4, 2)):
                        ps = pp.get()
                        for i in range(n):
                            P.op("pe", "transpose", ps[:, i * 128:(i + 1) * 128], ot[:, (lo + i) * 128:(lo + i + 1) * 128], ident[:, :],
                                 reads=[ot, ident], writes=[ps])
                        P.op("act", "activation", out=yTf[:, lo * 128:(lo + n) * 128], in_=ps[:, 0:n * 128], func=AF.Copy,
                             reads=[ps], writes=[yTt])
                    P.dma("sp", ysv[sgm][:, :, ts], yTt[:], reads=[yTt], writes=[y_seg[sgm]])

                def ycoll(sgm):
                    if "y" not in skip_cc:
                        for k in range(3 * sgm, 3 * sgm + 3):
                            _coll("AllGather", RG, y_send.t[k * 256:(k + 1) * 256, :], y_g.t[k * 1024:(k + 1) * 1024, :],
                                  reads=[y_seg[sgm]], writes=[y_g])


                ctxs = {}
                for i in range(NCH + 2):
                    if i < NCH:
                        if i % 8 == 0:
                            conv(i // 8)
                        ctxs[i] = S1(i, i // 8, i % 8, xc_sets[(i // 8) % 2], bc_sets[(i // 8) % 2])
                    if 0 <= i - 1 < NCH:
                        S2(ctxs[i - 1])
                    if 0 <= i - 2 < NCH:
                        S3(ctxs.pop(i - 2))
                        if (i - 2) % 8 == 7:
                            ycoll((i - 2) // 8)
            if stop == (l, "B"):
                finish()
                return nc
            with P.phase(final=(l == 1)):
                pp = PsumPool(P, 8)
                xT = [P.sbuf([128, T], F32, "xT%d" % c) for c in range(NKC)]
                hT = [P.sbuf([128, T], BF16, "hT%d" % c) for c in range(NKC)]
                aT = [P.sbuf([128, T], BF16, "aT%d" % c) for c in range(12)]
                g_xm = P.sbuf([128, NKC], F32, "g_xm"); g_mm = P.sbuf([128, NKC], F32, "g_mm"); g_ff = P.sbuf([128, NKC], F32, "g_ff")
                mqg = P.sbuf([128, 1], F32, "mqg"); mkg = P.sbuf([128, 1], F32, "mkg")
                idx_y = P.sbuf([128, 1], U32, "idx_y")
                ones_bf = P.sbuf([128, 128], BF16, "ones")
                rstd = P.sbuf([128, T], F32, "rstd")
                sq_ring = Ring([P.sbuf([128, 512], BF16, "sq%d" % i) for i in range(3)])
                sg_ring = Ring([P.sbuf([128, 512], F32, "sg%d" % i) for i in range(3)])
                wg_ring = Ring([P.sbuf([128, NKC, 256], BF16, "wg%d" % i) for i in range(2)])
                wu_ring = Ring([P.sbuf([128, NKC, 256], BF16, "wu%d" % i) for i in range(2)])
                wd_ring = Ring([P.sbuf([128, 12, 512], BF16, "wd%d" % i) for i in range(2)])
                w_ring = Ring(wg_ring.bufs + wu_ring.bufs)
                mr_ring = Ring([P.sbuf([128, MEMLEN], F32, "mr%d" % i) for i in range(3)])
                mhT = [P.sbuf([128, MEMLEN], BF16, "mhT%d" % c) for c in range(NKC)]
                kmT = [P.sbuf([128, MEMLEN], BF16, "kmT%d" % c) for c in range(4)]
                vm = [P.sbuf([128, 512], BF16, "vm%d" % c) for c in range(2)]
                pT_ring = sq_ring
                rd_ring = sg_ring
                ystg = Ring([rstd])
                xv = xres1.t.rearrange("(c p) t -> c p t", p=128)
                yav = ya_s.t.rearrange("(c p) t -> c p t", p=128)
                mv = mem_i.rearrange("(c p) t -> c p t", p=128)
                P.op("dve", "memset", ones_bf[:, :], 1.0, writes=[ones_bf])
                for (b, a) in ((g_xm, G["xmg"][l]), (g_mm, G["mmg"][l]), (g_ff, G["ffg2"][l]), (mqg, G["mqg"][l]),
                               (mkg, G["mkg"][l]), (idx_y, IDX["idx_y"])):
                    P.dma("sp", b[:], a, writes=[b])
                for c in range(NKC):
                    P.dma("sp", xT[c][:, :], xv[c], reads=[xres1], writes=[xT[c]])

                def cons_add(fc, h0, w, ps):
                    P.op("dve", "tensor_tensor", out=xT[fc][:, h0:h0 + w], in0=ps[:, 0:w], in1=xT[fc][:, h0:h0 + w], op=ALU.add,
                         reads=[ps, xT[fc]], writes=[xT[fc]])
                for kh in range(2):
                    for c in range(NKC):
                        m = kh * NKC + c
                        if m < 8:
                            P.dma("pool", hT[c][:, :], yav[m], reads=[ya_s], writes=[hT[c]])
                        else:
                            ms = m - 8
                            r, fcx = ms // 6, ms % 6
                            stg = ystg.get()
                            gather(stg[:, :], stg, y_g, idx_y, (fcx // 2) * 1024 + r * 256 + (fcx % 2) * 128)
                            P.op("act", "activation", out=hT[c][:, :], in_=stg[:, :], func=AF.Copy, reads=[stg], writes=[hT[c]])
                    lin_fm(P, pp, hT, W["w_out"][l][kh * D:(kh + 1) * D, :], D, w_ring, T, cons_add)
                ps_m = pp.get()
                for c in range(NKC):
                    mt = mr_ring.get()
                    P.dma("sp", mt[:, :], mv[c], writes=[mt])
                    sq = sq_ring.get()
                    P.op("act", "activation", out=sq[:, 0:MEMLEN], in_=mt[:, :], func=AF.Square, reads=[mt], writes=[sq])
                    P.op("pe", "matmul", ps_m[:, 0:MEMLEN], ones_bf[:, :], sq[:, 0:MEMLEN], start=(c == 0), stop=(c == NKC - 1),
                         reads=[sq, ones_bf], writes=[ps_m])
                P.op("dve", "tensor_scalar", rstd[:, 0:MEMLEN], ps_m[:, 0:MEMLEN], 1.0 / D, EPS, ALU.mult, ALU.add, reads=[ps_m], writes=[rstd])
                P.op("act", "activation", out=rstd[:, 0:MEMLEN], in_=rstd[:, 0:MEMLEN], func=AF.Sqrt, reads=[rstd], writes=[rstd])
                P.op("dve", "reciprocal", rstd[:, 0:MEMLEN], rstd[:, 0:MEMLEN], reads=[rstd], writes=[rstd])
                for c in range(NKC):
                    mt = mr_ring.get()
                    P.dma("sp", mt[:, :], mv[c], writes=[mt])
                    P.op("dve", "scalar_tensor_tensor", out=mhT[c][:, :], in0=mt[:, :], scalar=g_mm[:, c:c + 1], in1=rstd[:, 0:MEMLEN],
                         op0=ALU.mult, op1=ALU.mult, reads=[mt, g_mm, rstd], writes=[mhT[c]])

                def dst_k(fc, h0, w):
                    return kmT[fc][:, h0:h0 + w], kmT[fc]
                lin_fm(P, pp, mhT, W["wk"][l], 512, w_ring, MEMLEN, headnorm_consume(P, pp, ones_bf, mkg, sq_ring, sg_ring, dst_k))

                def cons_vm(tt, c0, cw_, ps):
                    P.op("act", "activation", out=vm[tt][:, c0:c0 + cw_], in_=ps[:, 0:cw_], func=AF.Copy, reads=[ps], writes=[vm[tt]])
                lin_tm(P, pp, mhT, W["wv"][l], 512, w_ring, MEMLEN, cons_vm)
                rmsnorm_fm(P, pp, xT, g_xm, hT, ones_bf, sq_ring, rstd, NKC, T, D)
                qmT = aT[0:4]
                oT = aT[4:8]

                def dst_q(fc, h0, w):
                    return qmT[fc][:, h0:h0 + w], qmT[fc]
                lin_fm(P, pp, hT, W["wq"][l], 512, w_ring, T, headnorm_consume(P, pp, ones_bf, mqg, sq_ring, sg_ring, dst_q))
                for mh in range(4):
                    for h0 in range(0, T, 512):
                        w = min(512, T - h0)
                        ps_o = pp.get(); ps_d = pp.get()
                        for kt in range(2):
                            ps_s = pp.get()
                            P.op("pe", "matmul", ps_s[:, 0:w], kmT[mh][:, kt * 128:(kt + 1) * 128], qmT[mh][:, h0:h0 + w],
                                 start=True, stop=True, reads=[kmT[mh], qmT[mh]], writes=[ps_s])
                            pT = pT_ring.get()
                            P.op("act", "activation", out=pT[:, 0:w], in_=ps_s[:, 0:w], func=AF.Exp, scale=scale, reads=[ps_s], writes=[pT])
                            P.op("pe", "matmul", ps_o[:, 0:w], vm[kt][:, mh * 128:(mh + 1) * 128], pT[:, 0:w], start=(kt == 0),
                                 stop=(kt == 1), reads=[vm[kt], pT], writes=[ps_o])
                            P.op("pe", "matmul", ps_d[:, 0:w], ones_bf[:, :], pT[:, 0:w], start=(kt == 0), stop=(kt == 1),
                                 reads=[ones_bf, pT], writes=[ps_d])
                        rd = rd_ring.get()
                        P.op("dve", "reciprocal", rd[:, 0:w], ps_d[:, 0:w], reads=[ps_d], writes=[rd])
                        P.op("dve", "tensor_tensor", out=oT[mh][:, h0:h0 + w], in0=ps_o[:, 0:w], in1=rd[:, 0:w], op=ALU.mult,
                             reads=[ps_o, rd], writes=[oT[mh]])
                lin_fm(P, pp, oT, W["wo"][l], D, w_ring, T, cons_add, nkc=4)
                rmsnorm_fm(P, pp, xT, g_ff, hT, ones_bf, sq_ring, rstd, NKC, T, D)
                ffn_fm(P, pp, xT, hT, W["w_gu2"][l], W["w_down2"][l], aT, wg_ring, wu_ring, wd_ring, sg_ring, T)
                dst = xres if l == 0 else None
                dv = (xres.t if l == 0 else out_ap).rearrange("(c p) t -> c p t", p=128)
                for c in range(NKC):
                    P.dma("sp", dv[c], xT[c][:, :], reads=[xT[c]], writes=([xres] if l == 0 else []))
    return nc


def fused_inputs(I):
    T = NTOK
    xs = I["x"].astype(np.float32).reshape(NCORES, T, D)
    st2 = lambda k: np.ascontiguousarray(np.stack([_pg(I[k][l]) for l in range(2)]))
    shared = {"w_gu1": I["ff1_w_gu"], "w_down1": I["ff1_w_down"], "w_in": I["w_in"], "w_out": I["w_out"],
              "wq": I["mem_wq"], "wk": I["mem_wk"], "wv": I["mem_wv"], "wo": I["mem_wo"],
              "w_gu2": I["ff2_w_gu"], "w_down2": I["ff2_w_down"],
              "ffg1": st2("ff1_norm"), "mixg": st2("mix_norm"), "xmg": st2("xmem_norm"), "mmg": st2("mem_norm"),
              "ffg2": st2("ff2_norm"), "qg": st2("q_norm"), "kg": st2("k_norm"), "mqg": st2("mem_q_norm"), "mkg": st2("mem_k_norm")}
    shared = {k: np.ascontiguousarray(np.asarray(v, np.float32)) for k, v in shared.items()}
    shared.update(ssd_consts())
    p = np.arange(128, dtype=np.uint32).reshape(128, 1)
    maps = []
    for c in range(NCORES):
        b, seg = c // 4, c % 4
        gp = seg
        g0 = 2 * gp
        chs = np.concatenate([np.arange(g0 * 384, (g0 + 2) * 384), 3072 + np.arange(g0 * 128, (g0 + 2) * 128),
                              4096 + np.arange(g0 * 128, (g0 + 2) * 128)])
        hs = np.arange(g0 * 6, g0 * 6 + 12)
        m = dict(shared)
        m["xT"] = np.ascontiguousarray(xs[c].T)
        m["memT"] = np.ascontiguousarray(I["mem"][b].astype(np.float32).T)
        m["convw"] = np.ascontiguousarray(np.stack([I["conv_w"][l][:, chs].T.reshape(10, 128, 4).transpose(1, 0, 2) for l in range(2)])).astype(np.float32)
        m["convb"] = np.ascontiguousarray(np.stack([I["conv_b"][l][chs].reshape(10, 128).T for l in range(2)])).astype(np.float32)
        m["dtb"] = np.stack([_rep(np.tile(I["dt_bias"][l][hs], NCH)) for l in range(2)])
        m["alog"] = np.stack([_rep(np.tile(I["a_log"][l][hs], NCH)) for l in range(2)])
        m["dsk"] = np.stack([_rep(np.repeat(I["d_skip"][l][hs], 64)) for l in range(2)])
        m["normw"] = np.stack([_rep(I["ssd_norm"][l][g0 * 384:(g0 + 2) * 384]) for l in range(2)])
        ac = attn_consts(seg)
        m.update(ac)
        m["idx_x"] = (gp * 5120 + p).astype(np.uint32)
        m["idx_t"] = (gp * 4096 + p).astype(np.uint32)
        m["idx_d"] = (gp * T + p).astype(np.uint32)
        m["idx_y"] = (seg * 3072 + p).astype(np.uint32)
        maps.append(m)
    return maps


def kernel_fused(**inputs):
    I = {k: np.asarray(v) for k, v in inputs.items()}
    nc = _prog("fused", build_fused)
    res = _run(nc, fused_inputs(I))
    out = np.stack([res[c]["xoT"].T for c in range(NCORES)], axis=0).reshape(2, SEQ, D)
    return np.ascontiguousarray(out.astype(np.float32))


_PROGS = {}


def _prog(name, fn):
    if name not in _PROGS:
        _PROGS[name] = fn()
    return _PROGS[name]


def _pg(g):
    return np.ascontiguousarray(np.asarray(g, np.float32).reshape(-1, 128).T)


def _run(nc, in_maps):
    res = run_bass_kernel_spmd(nc, in_maps, core_ids=list(range(NCORES)))
    return res.results


def kernel_unfused(**inputs):
    I = {k: np.asarray(v) for k, v in inputs.items()}
    T = NTOK
    x = I["x"].astype(np.float32)
    xs = x.reshape(NCORES, T, D)
    xT = [np.ascontiguousarray(xs[c].T) for c in range(NCORES)]
    memT = [np.ascontiguousarray(I["mem"][b].T) for b in range(2)]
    ncA = _prog("A", build_progA)
    ncB = _prog("B", build_progB)
    ncC1 = _prog("C1", build_progC1)
    ncC2 = _prog("C2", build_progC2)
    for l in range(2):
        mapsA = [{"xT": xT[c], "ffg": _pg(I["ff1_norm"][l]), "mixg": _pg(I["mix_norm"][l]),
                  "w_gu": I["ff1_w_gu"][l], "w_down": I["ff1_w_down"][l], "w_in": I["w_in"][l],
                  "qg": _pg(I["q_norm"][l]), "kg": _pg(I["k_norm"][l])} for c in range(NCORES)]
        rA = _run(ncA, mapsA)
        xbcT_b = [np.concatenate([rA[b * 4 + s]["xbcT"] for s in range(4)], axis=1) for b in range(2)]
        dt_b = [np.concatenate([rA[b * 4 + s]["dt"] for s in range(4)], axis=0) for b in range(2)]
        z_b = [np.concatenate([rA[b * 4 + s]["z"] for s in range(4)], axis=0) for b in range(2)]
        Pm = {k: I[k][l] for k in ("conv_w", "conv_b", "dt_bias", "a_log", "d_skip", "ssd_norm")}
        rB = _run(ncB, progB_inputs(xbcT_b, dt_b, z_b, Pm))
        y_ssd = progB_gather([r["y"] for r in rB])
        rC1 = _run(ncC1, progC1_inputs([rA[c]["qT"] for c in range(NCORES)], [rA[c]["kT"] for c in range(NCORES)],
                                       [rA[c]["v"] for c in range(NCORES)]))
        mapsC2 = []
        for c in range(NCORES):
            b, seg = c // 4, c % 4
            yT = np.ascontiguousarray(np.concatenate([rC1[c]["yT"], y_ssd[b, seg * T:(seg + 1) * T].T], axis=0))
            mapsC2.append({"xT": rA[c]["x1T"], "yT": yT, "w_out": I["w_out"][l], "memT": memT[b],
                           "xmg": _pg(I["xmem_norm"][l]), "mmg": _pg(I["mem_norm"][l]), "ffg": _pg(I["ff2_norm"][l]),
                           "mqg": _pg(I["mem_q_norm"][l]), "mkg": _pg(I["mem_k_norm"][l]),
                           "wq": I["mem_wq"][l], "wk": I["mem_wk"][l], "wv": I["mem_wv"][l], "wo": I["mem_wo"][l],
                           "w_gu": I["ff2_w_gu"][l], "w_down": I["ff2_w_down"][l]})
        rC2 = _run(ncC2, mapsC2)
        xT = [rC2[c]["xoT"] for c in range(NCORES)]
    out = np.stack([xT[c].T for c in range(NCORES)], axis=0).reshape(2, SEQ, D)
    return np.ascontiguousarray(out.astype(np.float32))


def kernel(**inputs):
    return kernel_fused(**inputs)
```
